# Optimizing a Trainium2 kernel written in Bass

```python
import jax, jax.numpy as jnp
from jax import lax
import numpy as np

D_MODEL = 1024
BATCH = 4
SEQ = 4096
DEPTH = 1

CHUNK = 64
HEAD_DIM = 64
RW_WIDTH = 512
FX_WIDTH = 512
RW_HEADS = RW_WIDTH // HEAD_DIM
FX_HEADS = FX_WIDTH // HEAD_DIM
MIX_WIDTH = RW_WIDTH + FX_WIDTH
RW_DECAY_LORA = 32
RW_AAA_LORA = 32
RW_GATE_LORA = 96
RW_COLS = 3 * RW_WIDTH + RW_DECAY_LORA + RW_AAA_LORA + RW_GATE_LORA
FX_COLS = 4 * FX_WIDTH + FX_HEADS
IN_COLS = RW_COLS + FX_COLS
RW_SPLITS = [RW_WIDTH, 2 * RW_WIDTH, 3 * RW_WIDTH, 3 * RW_WIDTH + RW_DECAY_LORA,
             3 * RW_WIDTH + RW_DECAY_LORA + RW_AAA_LORA]
FX_SPLITS = [FX_WIDTH, 2 * FX_WIDTH, 3 * FX_WIDTH, 4 * FX_WIDTH]
RW_GN_EPS = 64e-5
QK_EPS = 1e-6
LN_EPS = 1e-5
Q_BLOCK = 128
N_EXPERTS = 32
TOP_K = 4
EXPERT_FF = D_MODEL
SWIGLU_ALPHA = 1.702
SWIGLU_LIMIT = 7.0
MOE_BLOCK = 128
DEEPNORM_ALPHA = (2 * DEPTH) ** 0.25
DEEPNORM_BETA = (8 * DEPTH) ** -0.25

kernel_name = 'hybrid_rwkv7_fox_moe_encoder'


def _layer_norm(x, w, b):
    xf = x.astype(jnp.float32)
    mu = jnp.mean(xf, axis=-1, keepdims=True)
    var = jnp.mean(jnp.square(xf - mu), axis=-1, keepdims=True)
    return ((xf - mu) * lax.rsqrt(var + LN_EPS) * w + b).astype(x.dtype)


def _rms_norm(x, w):
    xf = x.astype(jnp.float32)
    return (xf * lax.rsqrt(jnp.mean(jnp.square(xf), axis=-1, keepdims=True) + QK_EPS) * w).astype(x.dtype)


def _rwkv7_mixer(p, mu, w0, w2, a0, a2, g2, k_k, k_a, r_k, gn_w, gn_b):
    B, S, _ = p.shape
    H, N = RW_HEADS, HEAD_DIM
    prev = jnp.pad(p, ((0, 0), (1, 0), (0, 0)))[:, :-1]
    p = p + mu * (prev - p)
    r, k, v, wd, ad, gd = jnp.split(p, RW_SPLITS, axis=-1)
    w = -jax.nn.softplus(-(w0 + jnp.tanh(wd) @ w2)) - 0.5
    decay = jnp.exp(-jnp.exp(w.astype(jnp.float32)))
    a = jax.nn.sigmoid(a0 + ad @ a2)
    g = jax.nn.sigmoid(gd) @ g2
    kk = (k * k_k).reshape(B, S, H, N).astype(jnp.float32)
    kk = kk / jnp.maximum(jnp.linalg.norm(kk, axis=-1, keepdims=True), 1e-12)
    k = k * (1 + (a - 1) * k_a)
    r_h, k_h, v_h, a_h = (t.reshape(B, S, H, N) for t in (r, k, v, a))
    w_h = decay.reshape(B, S, H, N)

    def to_chunks(t):
        return t.astype(jnp.float32).transpose(1, 0, 2, 3).reshape(S // CHUNK, CHUNK, B, H, N)

    xs = tuple(to_chunks(t) for t in (r_h, w_h, k_h, v_h, -kk, kk * a_h))

    def step(state, inp):
        rt, wt, kt, vt, at, bt = inp
        sa = jnp.einsum('bhvk,bhk->bhv', state, at)
        state = state * wt[:, :, None, :] + sa[..., None] * bt[:, :, None, :] + vt[..., None] * kt[:, :, None, :]
        return state, jnp.einsum('bhvk,bhk->bhv', state, rt)

    def chunk_step(state, chunk):
        return lax.scan(step, state, chunk)

    s0 = jnp.zeros((B, H, N, N), jnp.float32)
    _, ys = lax.scan(chunk_step, s0, xs)
    y = ys.reshape(S, B, H, N).transpose(1, 0, 2, 3)
    ym = jnp.mean(y, axis=-1, keepdims=True)
    yv = jnp.mean(jnp.square(y - ym), axis=-1, keepdims=True)
    yn = ((y - ym) * lax.rsqrt(yv + RW_GN_EPS)).reshape(B, S, RW_WIDTH) * gn_w + gn_b
    bonus = (jnp.sum(r_h * k_h * r_k, axis=-1, keepdims=True) * v_h).reshape(B, S, RW_WIDTH)
    return (yn.astype(p.dtype) + bonus) * g


def _fox_mixer(p, b_f, q_norm, k_norm):
    B, S, _ = p.shape
    H, N = FX_HEADS, HEAD_DIM
    q, k, v, og, fz = jnp.split(p, FX_SPLITS, axis=-1)
    q = _rms_norm(q.reshape(B, S, H, N), q_norm).transpose(0, 2, 1, 3)
    k = _rms_norm(k.reshape(B, S, H, N), k_norm).transpose(0, 2, 1, 3)
    v = v.reshape(B, S, H, N).transpose(0, 2, 1, 3)
    log_f = jax.nn.log_sigmoid(fz.astype(jnp.float32) + b_f.astype(jnp.float32))
    c = jnp.cumsum(log_f, axis=1).transpose(0, 2, 1)
    scale = HEAD_DIM ** -0.5
    outs = []
    for i in range(S // Q_BLOCK):
        q0 = i * Q_BLOCK
        L = q0 + Q_BLOCK
        s = jnp.einsum('bhqd,bhkd->bhqk', q[:, :, q0:L], k[:, :, :L]).astype(jnp.float32) * scale
        s = s + c[:, :, q0:L, None] - c[:, :, None, :L]
        causal = jnp.arange(L)[None, :] <= (q0 + jnp.arange(Q_BLOCK))[:, None]
        s = jnp.where(causal, s, -jnp.inf)
        pr = jax.nn.softmax(s, axis=-1).astype(v.dtype)
        outs.append(jnp.einsum('bhqk,bhkd->bhqd', pr, v[:, :, :L]))
    o = jnp.concatenate(outs, axis=2).transpose(0, 2, 1, 3).reshape(B, S, FX_WIDTH)
    return o * jax.nn.sigmoid(og)


def _moe(h, router_w, router_b, w1, b1, w2, b2):
    B, S, D = h.shape
    T = B * S
    xt = h.reshape(T, D)
    logits = (xt @ router_w + router_b).astype(jnp.float32)
    top_val, top_idx = lax.top_k(logits, TOP_K)
    gates = jax.nn.softmax(top_val, axis=-1)
    e_flat = top_idx.reshape(-1).astype(jnp.int32)
    g_flat = gates.reshape(-1)
    tok_flat = jnp.repeat(jnp.arange(T, dtype=jnp.int32), TOP_K)
    n_assign = T * TOP_K
    n_rows = -(-n_assign // MOE_BLOCK) * MOE_BLOCK + N_EXPERTS * MOE_BLOCK
    n_blocks = n_rows // MOE_BLOCK
    order = jnp.argsort(e_flat, stable=True)
    e_sorted = e_flat[order]
    counts = jnp.bincount(e_flat, length=N_EXPERTS)
    starts = jnp.cumsum(counts) - counts
    padded = (counts + MOE_BLOCK - 1) // MOE_BLOCK * MOE_BLOCK
    pends = jnp.cumsum(padded)
    pstarts = pends - padded
    dest = pstarts[e_sorted] + (jnp.arange(n_assign, dtype=jnp.int32) - starts[e_sorted])
    row_tok = jnp.full((n_rows,), T, jnp.int32).at[dest].set(tok_flat[order])
    row_gate = jnp.zeros((n_rows,), jnp.float32).at[dest].set(g_flat[order])
    block_exp = jnp.minimum(jnp.searchsorted(pends, jnp.arange(n_blocks) * MOE_BLOCK, side='right'),
                            N_EXPERTS - 1).astype(jnp.int32)
    xs = jnp.concatenate([xt, jnp.zeros((1, D), xt.dtype)], axis=0)[row_tok].reshape(n_blocks, MOE_BLOCK, D)

    def expert_block(args):
        xb, e = args
        hid = xb @ w1[e] + b1[e]
        x_glu = jnp.minimum(hid[..., ::2], SWIGLU_LIMIT)
        x_lin = jnp.clip(hid[..., 1::2], -SWIGLU_LIMIT, SWIGLU_LIMIT)
        act = x_glu * jax.nn.sigmoid(SWIGLU_ALPHA * x_glu) * (x_lin + 1)
        return act @ w2[e] + b2[e]

    out = lax.map(expert_block, (xs, block_exp)).reshape(n_rows, D)
    out = out * row_gate[:, None].astype(out.dtype)
    y = jax.ops.segment_sum(out, row_tok, num_segments=T + 1)[:T]
    return y.reshape(B, S, D)


def setup_inputs(seed: int = 0) -> dict:
    key = jax.random.key(seed)
    ks = jax.random.split(key, 32)
    L = DEPTH
    nrm = lambda k, shape, s: jax.random.normal(k, shape, jnp.float32) * s
    uni = lambda k, shape, lo, hi: jax.random.uniform(k, shape, jnp.float32, lo, hi)
    return {
        'x': nrm(ks[0], (BATCH, SEQ, D_MODEL), 1.0),
        'ln_in_w': 1.0 + nrm(ks[1], (D_MODEL,), 0.02),
        'ln_in_b': nrm(ks[2], (D_MODEL,), 0.02),
        'w_in': nrm(ks[3], (L, D_MODEL, IN_COLS), D_MODEL ** -0.5),
        'rw_mu': uni(ks[4], (L, RW_COLS), 0.0, 1.0),
        'rw_w0': uni(ks[5], (L, RW_WIDTH), -6.0, 1.0),
        'rw_w2': nrm(ks[6], (L, RW_DECAY_LORA, RW_WIDTH), 0.1),
        'rw_a0': nrm(ks[7], (L, RW_WIDTH), 0.5),
        'rw_a2': nrm(ks[8], (L, RW_AAA_LORA, RW_WIDTH), 0.1),
        'rw_g2': nrm(ks[9], (L, RW_GATE_LORA, RW_WIDTH), RW_GATE_LORA ** -0.5),
        'rw_k_k': 0.85 + nrm(ks[10], (L, RW_WIDTH), 0.05),
        'rw_k_a': 1.0 + nrm(ks[11], (L, RW_WIDTH), 0.05),
        'rw_r_k': nrm(ks[12], (L, RW_HEADS, HEAD_DIM), 0.1),
        'rw_gn_w': 1.0 + nrm(ks[13], (L, RW_WIDTH), 0.02),
        'rw_gn_b': nrm(ks[14], (L, RW_WIDTH), 0.02),
        'fx_b_f': uni(ks[15], (L, FX_HEADS), 1.0, 5.0),
        'fx_q_norm': 1.0 + nrm(ks[16], (L, HEAD_DIM), 0.02),
        'fx_k_norm': 1.0 + nrm(ks[17], (L, HEAD_DIM), 0.02),
        'w_o': nrm(ks[18], (L, MIX_WIDTH, D_MODEL), MIX_WIDTH ** -0.5 * DEEPNORM_BETA),
        'ln1_w': 1.0 + nrm(ks[19], (L, D_MODEL), 0.02),
        'ln1_b': nrm(ks[20], (L, D_MODEL), 0.02),
        'router_w': nrm(ks[21], (L, D_MODEL, N_EXPERTS), D_MODEL ** -0.5),
        'router_b': nrm(ks[22], (L, N_EXPERTS), 0.01),
        'exp_w1': nrm(ks[23], (L, N_EXPERTS, D_MODEL, 2 * EXPERT_FF), D_MODEL ** -0.5),
        'exp_b1': nrm(ks[24], (L, N_EXPERTS, 2 * EXPERT_FF), 0.01),
        'exp_w2': nrm(ks[25], (L, N_EXPERTS, EXPERT_FF, D_MODEL), EXPERT_FF ** -0.5 * DEEPNORM_BETA),
        'exp_b2': nrm(ks[26], (L, N_EXPERTS, D_MODEL), 0.01),
        'ln2_w': 1.0 + nrm(ks[27], (L, D_MODEL), 0.02),
        'ln2_b': nrm(ks[28], (L, D_MODEL), 0.02),
    }


def reference(x, ln_in_w, ln_in_b, w_in, rw_mu, rw_w0, rw_w2, rw_a0, rw_a2, rw_g2, rw_k_k, rw_k_a,
              rw_r_k, rw_gn_w, rw_gn_b, fx_b_f, fx_q_norm, fx_k_norm, w_o, ln1_w, ln1_b,
              router_w, router_b, exp_w1, exp_b1, exp_w2, exp_b2, ln2_w, ln2_b):
    h = _layer_norm(x, ln_in_w, ln_in_b)
    for l in range(DEPTH):
        proj = h @ w_in[l]
        p_rw, p_fx = proj[..., :RW_COLS], proj[..., RW_COLS:]
        y_rw = _rwkv7_mixer(p_rw, rw_mu[l], rw_w0[l], rw_w2[l], rw_a0[l], rw_a2[l], rw_g2[l],
                            rw_k_k[l], rw_k_a[l], rw_r_k[l], rw_gn_w[l], rw_gn_b[l])
        y_fx = _fox_mixer(p_fx, fx_b_f[l], fx_q_norm[l], fx_k_norm[l])
        mix = jnp.concatenate([y_rw, y_fx], axis=-1) @ w_o[l]
        h = _layer_norm(DEEPNORM_ALPHA * h + mix, ln1_w[l], ln1_b[l])
        ffn = _moe(h, router_w[l], router_b[l], exp_w1[l], exp_b1[l], exp_w2[l], exp_b2[l])
        h = _layer_norm(DEEPNORM_ALPHA * h + ffn, ln2_w[l], ln2_b[l])
    return h
```

```python
import ml_dtypes
import contextlib
import numpy as np
import concourse.bass as bass
import concourse.mybir as mybir
from concourse.bass_utils import run_bass_kernel_spmd

F32 = mybir.dt.float32
BF16 = mybir.dt.bfloat16
I32 = mybir.dt.int32
U32 = mybir.dt.uint32
AF = mybir.ActivationFunctionType
ALU = mybir.AluOpType
AX = mybir.AxisListType

ENGS = ("pe", "act", "dve", "pool", "sp")


class Buf:
    __slots__ = ("name", "w", "rs")

    def __init__(self, name):
        self.name = name
        self.w = None
        self.rs = {}


class _Op:
    __slots__ = ("fn", "waits", "ev", "dma", "marked")

    def __init__(self, fn, waits, ev, dma):
        self.fn = fn
        self.waits = waits
        self.ev = ev
        self.dma = dma
        self.marked = False


class KB:
    def __init__(self, nc, n_dma_sems=20):
        self.nc = nc
        self.es = contextlib.ExitStack()
        self.q = {e: [] for e in ENGS}
        self.n_dma_sems = n_dma_sems
        self.dma_sems = {}
        self.dma_tgt = {}
        self.dma_rr = {}
        self.eng_sem = {}
        self.nbuf = 0
        self.allbufs = []
        self.base = {e: 0 for e in ENGS}
        self.barrier = []
        self.ps_stack = None
        for e in ENGS:
            self.eng_sem[e] = self.es.enter_context(nc.semaphore("es_" + e))
        for e in ("sp", "pool", "act"):
            self.dma_sems[e] = [self.es.enter_context(nc.semaphore(f"ds_{e}_{i}")) for i in range(n_dma_sems)]
            self.dma_tgt[e] = [0] * n_dma_sems
            self.dma_rr[e] = 0

    def begin_phase(self):
        self.ps_stack = contextlib.ExitStack()

    def end_phase(self):
        self.replay()
        self.ps_stack.close()
        self.ps_stack = None

    def sbuf(self, name, shape, dtype):
        st = self.ps_stack if self.ps_stack is not None else self.es
        return st.enter_context(self.nc.sbuf_tensor(name, list(shape), dtype))

    def psum(self, name, shape, dtype):
        st = self.ps_stack if self.ps_stack is not None else self.es
        return st.enter_context(self.nc.psum_tensor(name, list(shape), dtype))

    def buf(self, name=None):
        self.nbuf += 1
        b = Buf(name or f"b{self.nbuf}")
        self.allbufs.append(b)
        return b

    def bufs(self, n, name="b"):
        return [self.buf(f"{name}{i}") for i in range(n)]

    def op(self, eng, fn, r=(), w=(), dma=False):
        waits = []
        if not self.q[eng] and self.barrier:
            waits.extend(self.barrier)
        for b in r:
            if b.w is not None:
                waits.append(b.w)
        for b in w:
            if b.w is not None:
                waits.append(b.w)
            waits.extend(b.rs.values())
        if dma:
            i = self.dma_rr[eng]
            self.dma_rr[eng] = (i + 1) % self.n_dma_sems
            prev = self.dma_tgt[eng][i]
            if prev > 0:
                waits.append(("d", eng, i, prev))
            tgt = prev + 16
            self.dma_tgt[eng][i] = tgt
            ev = ("d", eng, i, tgt)
        else:
            ev = ("c", eng, len(self.q[eng]))
        self.q[eng].append(_Op(fn, waits, ev, dma))
        key = ev[:3] if ev[0] == "d" else ev[:2]
        for b in r:
            b.rs[key] = ev
        for b in w:
            b.w = ev
            b.rs = {}
        return ev

    def replay(self):
        nc = self.nc
        for e in ENGS:
            if self.q[e]:
                self.q[e][-1].marked = True
            for o in self.q[e]:
                for ev in o.waits:
                    if ev[0] == "c":
                        if ev[1] == "pe" and e == "pe":
                            continue
                        self.q[ev[1]][ev[2]].marked = True
        cnt = {}
        for e in ENGS:
            c = self.base[e]
            arr = []
            for o in self.q[e]:
                if o.marked and not o.dma:
                    c += 1
                arr.append(c)
            cnt[e] = arr

        def resolve(ev):
            if ev[0] == "c":
                return ("c", ev[1]), self.eng_sem[ev[1]], cnt[ev[1]][ev[2]]
            if ev[0] == "a":
                return ("c", ev[1]), self.eng_sem[ev[1]], ev[2]
            return ("d", ev[1], ev[2]), self.dma_sems[ev[1]][ev[2]], ev[3]

        if not hasattr(self, "seen"):
            self.seen = {e: {} for e in ENGS}

        def run_engine(ename, eobj):
            seen = self.seen[ename]
            for o in self.q[ename]:
                need = {}
                for ev in o.waits:
                    if ev[0] in ("c", "a") and ev[1] == "pe" and ename == "pe":
                        continue
                    k, sem, val = resolve(ev)
                    if seen.get(k, 0) >= val:
                        continue
                    if k not in need or need[k][1] < val:
                        need[k] = (sem, val)
                for k, (sem, val) in need.items():
                    eobj.wait_ge(sem, val)
                    seen[k] = val
                ins = o.fn(eobj)
                if o.dma:
                    ins.then_inc(self.dma_sems[o.ev[1]][o.ev[2]], 16)
                elif o.marked:
                    ins.then_inc(self.eng_sem[ename], 1)

        with nc.Block() as block:
            @block.tensor
            def _(t):
                run_engine("pe", t)

            @block.scalar
            def _(s):
                run_engine("act", s)

            @block.vector
            def _(v):
                run_engine("dve", v)

            @block.gpsimd
            def _(g):
                run_engine("pool", g)

            @block.sync
            def _(sy):
                run_engine("sp", sy)

        def absolutize(ev):
            if ev[0] == "c":
                return ("a", ev[1], cnt[ev[1]][ev[2]] if self.q[ev[1]][ev[2]].marked else cnt[ev[1]][-1])
            return ev
        for b in self.allbufs:
            if b.w is not None:
                b.w = absolutize(b.w)
            b.rs = {k: absolutize(v) for k, v in b.rs.items()}
        for e in ENGS:
            if self.q[e]:
                self.base[e] = cnt[e][-1]
            self.q[e] = []
        bar = [("a", e, self.base[e]) for e in ENGS if self.base[e] > 0]
        for e in ("sp", "pool", "act"):
            for i, t in enumerate(self.dma_tgt[e]):
                if t > 0:
                    bar.append(("d", e, i, t))
        self.barrier = bar

    def finish(self):
        nc = self.nc
        bar = list(self.barrier)
        with nc.Block() as block:
            @block.sync
            def _(sy):
                for ev in bar:
                    if ev[0] == "a":
                        sy.wait_ge(self.eng_sem[ev[1]], ev[2])
                    else:
                        sy.wait_ge(self.dma_sems[ev[1]][ev[2]], ev[3])

    def close(self):
        self.es.close()


D = 1024
KC = 8
RW_COLS = 1696
FX0 = RW_COLS
IN_COLS = 3752
LN_EPS = 1e-5
ALPHA = 2 ** 0.25


def col_tiles():
    t = []
    for nm, base in (("r", 0), ("k", 512), ("v", 1024)):
        for h in range(8):
            t.append((f"rw_{nm}{h}", base + 64 * h, 64))
    t.append(("rw_wa", 1536, 64))
    t.append(("rw_g", 1600, 96))
    for nm, base in (("q", 0), ("k", 512), ("og", 1536)):
        for h in range(8):
            t.append((f"fx_{nm}{h}", FX0 + base + 64 * h, 64))
    t.append(("fx_fz", FX0 + 2048, 8))
    return t


def consts_np():
    c = {}
    c["ident_f"] = np.eye(128, dtype=np.float32)
    c["ident_b"] = np.eye(128).astype(ml_dtypes.bfloat16)
    return c


class Ctx:
    pass


def setup(nc, T, ext_out=()):
    g = Ctx()
    g.nc = nc
    g.T = T
    g.NT = T // 128
    g.NB = T // 512
    ei = lambda n, s, d=F32: nc.dram_tensor(n, list(s), d, kind="ExternalInput").ap()
    g.x = ei("x", [T, D])
    g.ln_in_w = ei("ln_in_w", [1, D])
    g.ln_in_b = ei("ln_in_b", [1, D])
    g.w_in = ei("w_in", [D, IN_COLS])
    g.ident_f = ei("ident_f", [128, 128])
    g.ident_b = ei("ident_b", [128, 128], BF16)

    def scratch(n, s, d=F32):
        kind = "ExternalOutput" if n in ext_out else "Internal"
        return nc.dram_tensor(n, list(s), d, kind=kind).ap()

    g.scratch = scratch
    g.h0f = scratch("h0f", [T, D])
    g.pT = scratch("pT", [IN_COLS, T])
    g.vtok = scratch("vtok", [T, 512], BF16)
    return g


def phase01(kb, g):
    nc, T, NT, NB = g.nc, g.T, g.NT, g.NB
    h0T = kb.sbuf("h0T", [128, KC, T], BF16)
    h0T_b = kb.bufs(NT, "h0T")
    wbf = kb.sbuf("wbf", [128, KC, IN_COLS], BF16)
    wbf_b = kb.bufs(KC, "wbf")
    gam = kb.sbuf("gam", [128, D], F32)
    bet = kb.sbuf("bet", [128, D], F32)
    gb_b = kb.buf("gb")
    identf = kb.sbuf("identf", [128, 128], F32)
    id_b = kb.buf("id")
    epsc = kb.sbuf("epsc", [128, 1], F32)
    eps_b = kb.buf("eps")
    xt = [kb.sbuf(f"xt{i}", [128, D], F32) for i in range(2)]
    xt_b = kb.bufs(2, "xt")
    hn = [kb.sbuf(f"hn{i}", [128, D], F32) for i in range(2)]
    hn_b = kb.bufs(2, "hn")
    st = [kb.sbuf(f"st{i}", [128, 2, 6], F32) for i in range(2)]
    mv = [kb.sbuf(f"mv{i}", [128, 2], F32) for i in range(2)]
    rs = [kb.sbuf(f"rs{i}", [128, 1], F32) for i in range(2)]
    st_b = kb.bufs(2, "st")
    ps = [kb.psum(f"ps{i}", [128, 512], F32) for i in range(4)]
    ps_b = kb.bufs(4, "ps")
    stage = [kb.sbuf(f"stage{i}", [128, 512], F32) for i in range(3)]
    stage_b = kb.bufs(3, "stage")
    vst = [kb.sbuf(f"vst{i}", [128, 512], BF16) for i in range(2)]
    vst_b = kb.bufs(2, "vst")

    kb.op("sp", lambda e: e.dma_start(out=gam[:], in_=g.ln_in_w.broadcast_to([128, D])), w=[gb_b], dma=True)
    kb.op("sp", lambda e: e.dma_start(out=bet[:], in_=g.ln_in_b.broadcast_to([128, D])), w=[gb_b], dma=True)
    kb.op("sp", lambda e: e.dma_start(out=identf[:], in_=g.ident_f), w=[id_b], dma=True)
    kb.op("pool", lambda e: e.memset(epsc[:], LN_EPS), w=[eps_b])
    half = IN_COLS // 2
    for kc in range(KC):
        for hf in range(2):
            kb.op("pool", lambda e, kc=kc, hf=hf: e.dma_start(
                out=wbf[:, kc, hf * half:(hf + 1) * half],
                in_=g.w_in[kc * 128:(kc + 1) * 128, hf * half:(hf + 1) * half]),
                w=[wbf_b[kc]], dma=True)

    for i in range(NT):
        s = i % 2
        kb.op("sp", lambda e, i=i, s=s: e.dma_start(out=xt[s][:], in_=g.x[i * 128:(i + 1) * 128, :]), w=[xt_b[s]], dma=True)
        for hf in range(2):
            kb.op("dve", lambda e, s=s, hf=hf: e.bn_stats(out=st[s][:, hf, :], in_=xt[s][:, hf * 512:(hf + 1) * 512]),
                  r=[xt_b[s]], w=[st_b[s]])
        kb.op("dve", lambda e, s=s: e.bn_aggr(out=mv[s][:], in_=st[s][:].rearrange("p a b -> p (a b)")), r=[st_b[s]], w=[st_b[s]])
        kb.op("act", lambda e, s=s: e.activation(out=rs[s][:], in_=mv[s][:, 1:2], func=AF.Sqrt, bias=epsc[:], scale=1.0),
              r=[st_b[s], eps_b], w=[st_b[s]])
        kb.op("dve", lambda e, s=s: e.reciprocal(out=rs[s][:], in_=rs[s][:]), r=[st_b[s]], w=[st_b[s]])
        kb.op("dve", lambda e, s=s: e.tensor_scalar(out=hn[s][:], in0=xt[s][:], scalar1=mv[s][:, 0:1], scalar2=rs[s][:],
                                                    op0=ALU.subtract, op1=ALU.mult),
              r=[xt_b[s], st_b[s]], w=[hn_b[s]])
        kb.op("dve", lambda e, s=s: e.tensor_tensor(out=hn[s][:], in0=hn[s][:], in1=gam[:], op=ALU.mult), r=[hn_b[s], gb_b], w=[hn_b[s]])
        kb.op("pool", lambda e, s=s: e.tensor_tensor(out=hn[s][:], in0=hn[s][:], in1=bet[:], op=ALU.add), r=[hn_b[s], gb_b], w=[hn_b[s]])
        kb.op("sp", lambda e, i=i, s=s: e.dma_start(out=g.h0f[i * 128:(i + 1) * 128, :], in_=hn[s][:]), r=[hn_b[s]], dma=True)
        for hf in range(2):
            p = hf
            for c in range(4):
                kc = hf * 4 + c
                kb.op("pe", lambda e, s=s, p=p, c=c, kc=kc: e.transpose(out=ps[p][:, c * 128:(c + 1) * 128],
                                                                       in_=hn[s][:, kc * 128:(kc + 1) * 128], identity=identf[:]),
                      r=[hn_b[s], id_b], w=[ps_b[p]])
            eng = "act" if hf == 0 else "dve"
            if eng == "act":
                kb.op("act", lambda e, i=i, p=p, hf=hf: e.copy(out=h0T[:, hf * 4:(hf + 1) * 4, i * 128:(i + 1) * 128],
                                                               in_=ps[p][:].rearrange("p (c t) -> p c t", c=4)),
                      r=[ps_b[p]], w=[h0T_b[i]])
            else:
                kb.op("dve", lambda e, i=i, p=p, hf=hf: e.tensor_copy(out=h0T[:, hf * 4:(hf + 1) * 4, i * 128:(i + 1) * 128],
                                                                      in_=ps[p][:].rearrange("p (c t) -> p c t", c=4)),
                      r=[ps_b[p]], w=[h0T_b[i]])

    n = 0
    for (nm, c0, ncol) in col_tiles():
        for tb in range(NB):
            p = 2 + (n % 2)
            sg = n % 3
            for kc in range(KC):
                kb.op("pe", lambda e, p=p, kc=kc, c0=c0, ncol=ncol, tb=tb: e.matmul(
                    ps[p][0:ncol, :], lhsT=wbf[:, kc, c0:c0 + ncol], rhs=h0T[:, kc, tb * 512:(tb + 1) * 512],
                    start=(kc == 0), stop=(kc == KC - 1)),
                    r=[wbf_b[kc]] + h0T_b[tb * 4:(tb + 1) * 4], w=[ps_b[p]])
            if n % 2 == 0:
                kb.op("act", lambda e, p=p, sg=sg, ncol=ncol: e.copy(out=stage[sg][0:ncol, :], in_=ps[p][0:ncol, :]),
                      r=[ps_b[p]], w=[stage_b[sg]])
            else:
                kb.op("dve", lambda e, p=p, sg=sg, ncol=ncol: e.tensor_copy(out=stage[sg][0:ncol, :], in_=ps[p][0:ncol, :]),
                      r=[ps_b[p]], w=[stage_b[sg]])
            kb.op("sp", lambda e, sg=sg, c0=c0, ncol=ncol, tb=tb: e.dma_start(
                out=g.pT[c0:c0 + ncol, tb * 512:(tb + 1) * 512], in_=stage[sg][0:ncol, :]),
                r=[stage_b[sg]], dma=True)
            n += 1
    vc0 = FX0 + 1024
    for i in range(NT):
        p = 2 + (i % 2)
        s = i % 2
        for kc in range(KC):
            kb.op("pe", lambda e, p=p, kc=kc, i=i: e.matmul(
                ps[p][:, :], lhsT=h0T[:, kc, i * 128:(i + 1) * 128], rhs=wbf[:, kc, vc0:vc0 + 512],
                start=(kc == 0), stop=(kc == KC - 1)),
                r=[wbf_b[kc], h0T_b[i]], w=[ps_b[p]])
        kb.op("act", lambda e, p=p, s=s: e.copy(out=vst[s][:], in_=ps[p][:]), r=[ps_b[p]], w=[vst_b[s]])
        kb.op("sp", lambda e, s=s, i=i: e.dma_start(out=g.vtok[i * 128:(i + 1) * 128, :], in_=vst[s][:]), r=[vst_b[s]], dma=True)


def setup2(nc, g):
    ei = lambda n, s, d=F32: nc.dram_tensor(n, list(s), d, kind="ExternalInput").ap()
    g.fx_b_f = ei("fx_b_f", [8, 1])
    g.fx_q_norm = ei("fx_q_norm", [64, 1])
    g.fx_k_norm = ei("fx_k_norm", [64, 1])
    g.tri = ei("tri", [128, 128], BF16)
    g.yT = g.scratch("yT", [1024, g.T], BF16)


def consts2_np():
    c = {}
    k = np.arange(128)[:, None]
    q = np.arange(128)[None, :]
    c["tri"] = (k <= q).astype(np.float32).astype(ml_dtypes.bfloat16)
    return c


def phase2(kb, g):
    nc, T, NT, NB = g.nc, g.T, g.NT, g.NB
    onesf = kb.sbuf("onesf", [128, 128], F32)
    identf = kb.sbuf("identf2", [128, 128], F32)
    tri = kb.sbuf("tri_sb", [128, 128], BF16)
    cst_b = kb.buf("cst2")
    epsq = kb.sbuf("epsq", [128, 1], F32)
    negb = kb.sbuf("negb", [8, 1], F32)
    qw = kb.sbuf("qw", [64, 1], F32)
    kw = kb.sbuf("kw", [64, 1], F32)
    fz = kb.sbuf("fz", [8, T], F32)
    cp = kb.sbuf("cp", [8, T], F32)
    ones8 = kb.sbuf("ones8", [8, T], F32)
    cq8 = kb.sbuf("cq8", [8, T], F32)
    fz_b = kb.buf("fz")
    cq8_b = kb.buf("cq8")
    negc = kb.sbuf("negc", [128, NT, 8], F32)
    negc_b = kb.buf("negc")
    qrow = kb.sbuf("qrow", [65, T], F32)
    qrow_b = kb.buf("qrow")
    qraw = kb.sbuf("qraw", [64, T], F32)
    kraw = kb.sbuf("kraw", [64, T], F32)
    sq = kb.sbuf("sq", [64, T], F32)
    raw_b = {"q": kb.buf("qraw"), "k": kb.buf("kraw")}
    sq_b = kb.buf("sq")
    ograw = kb.sbuf("ograw", [64, T], F32)
    og_b = kb.buf("ograw")
    sg = kb.sbuf("sg", [64, T], BF16)
    sg_b = kb.buf("sg")
    Qa = kb.sbuf("Qa", [65, T], BF16)
    Ka = kb.sbuf("Ka", [65, T], BF16)
    Qa_b = kb.buf("Qa")
    Ka_b = kb.buf("Ka")
    Va = kb.sbuf("Va", [128, NT, 65], BF16)
    Va_b = kb.buf("Va")
    rst = [kb.sbuf(f"rst{i}", [64, 512], F32) for i in range(2)]
    rst_b = kb.bufs(2, "rst")
    PT = [kb.sbuf(f"PT{i}", [128, 512], BF16) for i in range(3)]
    PT_b = kb.bufs(3, "PT")
    dn = kb.sbuf("dn", [65, 512], F32)
    dn_b = kb.buf("dn")
    bcs = kb.sbuf("bcs", [64, 512], F32)
    bcs_b = kb.buf("bcs")
    o1 = kb.sbuf("o1", [64, 512], F32)
    o1_b = kb.buf("o1")
    yfx = [kb.sbuf(f"yfx{i}", [64, 512], BF16) for i in range(2)]
    yfx_b = kb.bufs(2, "yfx")
    psS = [kb.psum(f"psS{i}", [128, 512], F32) for i in range(2)]
    psS_b = kb.bufs(2, "psS")
    psO = [kb.psum(f"psO{i}", [128, 512], F32) for i in range(2)]
    psO_b = kb.bufs(2, "psO")
    psB = kb.psum("psB", [128, 512], F32)
    psB_b = kb.buf("psB")
    psN = [kb.psum(f"psN{i}", [128, 512], F32) for i in range(2)]
    psN_b = kb.bufs(2, "psN")

    kb.op("pool", lambda e: e.memset(onesf[:], 1.0), w=[cst_b])
    kb.op("pool", lambda e: e.memset(ones8[:], 1.0), w=[cst_b])
    kb.op("pool", lambda e: e.memset(epsq[:], 1e-6), w=[cst_b])
    kb.op("pool", lambda e: e.memset(Ka[64:65, :], 1.0), w=[Ka_b])
    kb.op("pool", lambda e: e.memset(Va[:, :, 64:65], 1.0), w=[Va_b])
    kb.op("sp", lambda e: e.dma_start(out=identf[:], in_=g.ident_f), w=[cst_b], dma=True)
    kb.op("sp", lambda e: e.dma_start(out=tri[:], in_=g.tri), w=[cst_b], dma=True)
    kb.op("sp", lambda e: e.dma_start(out=negb[:], in_=g.fx_b_f), w=[cst_b], dma=True)
    kb.op("sp", lambda e: e.dma_start(out=qw[:], in_=g.fx_q_norm), w=[cst_b], dma=True)
    kb.op("sp", lambda e: e.dma_start(out=kw[:], in_=g.fx_k_norm), w=[cst_b], dma=True)
    kb.op("dve", lambda e: e.tensor_scalar(out=negb[:], in0=negb[:], scalar1=-1.0, scalar2=None, op0=ALU.mult), r=[cst_b], w=[cst_b])
    fzr = FX0 + 2048
    kb.op("sp", lambda e: e.dma_start(out=fz[:], in_=g.pT[fzr:fzr + 8, :]), w=[fz_b], dma=True)
    kb.op("act", lambda e: e.activation(out=fz[:], in_=fz[:], func=AF.Exp, bias=negb[:], scale=-1.0), r=[fz_b, cst_b], w=[fz_b])
    kb.op("act", lambda e: e.activation(out=fz[:], in_=fz[:], func=AF.Ln, bias=1.0, scale=1.0), r=[fz_b], w=[fz_b])
    kb.op("dve", lambda e: e.tensor_tensor_scan(out=cp[:], data0=ones8[:], data1=fz[:], initial=0.0, op0=ALU.mult, op1=ALU.add),
          r=[fz_b, cst_b], w=[cq8_b])
    kb.op("dve", lambda e: e.tensor_scalar(out=cq8[:], in0=cp[:], scalar1=-8.0, scalar2=None, op0=ALU.mult), r=[cq8_b], w=[cq8_b])
    for i in range(NT):
        kb.op("pe", lambda e, i=i: e.transpose(out=psB[:, i * 8:(i + 1) * 8], in_=cp[:, i * 128:(i + 1) * 128], identity=identf[0:8, 0:8]),
              r=[cq8_b, cst_b], w=[psB_b])
    kb.op("act", lambda e: e.copy(out=negc[:].rearrange("p a b -> p (a b)"), in_=psB[:, 0:NT * 8]), r=[psB_b], w=[negc_b])

    for h in range(8):
        kb.op("sp", lambda e, h=h: e.dma_start(out=qraw[:], in_=g.pT[FX0 + 64 * h:FX0 + 64 * h + 64, :]), w=[raw_b["q"]], dma=True)
        kb.op("sp", lambda e, h=h: e.dma_start(out=kraw[:], in_=g.pT[FX0 + 512 + 64 * h:FX0 + 512 + 64 * h + 64, :]), w=[raw_b["k"]], dma=True)
        kb.op("sp", lambda e, h=h: e.dma_start(out=ograw[:], in_=g.pT[FX0 + 1536 + 64 * h:FX0 + 1536 + 64 * h + 64, :]), w=[og_b], dma=True)
        kb.op("sp", lambda e, h=h: e.dma_start(out=Va[:, :, 0:64], in_=g.vtok[:, 64 * h:64 * h + 64].rearrange("(j p) d -> p j d", p=128)),
              w=[Va_b], dma=True)
        kb.op("sp", lambda e, h=h: e.dma_start(out=qrow[64:65, :], in_=cq8[h:h + 1, :]), r=[cq8_b], w=[qrow_b], dma=True)
        kb.op("act", lambda e: e.copy(out=Qa[64:65, :], in_=qrow[64:65, :]), r=[qrow_b], w=[Qa_b])
        kb.op("act", lambda e: e.activation(out=sg[:], in_=ograw[:], func=AF.Sigmoid), r=[og_b], w=[sg_b])
        n = 0
        for nm, raw, wcol, dst, dst_b in (("q", qraw, qw, Qa, Qa_b), ("k", kraw, kw, Ka, Ka_b)):
            kb.op("act", lambda e, raw=raw: e.activation(out=sq[:], in_=raw[:], func=AF.Square), r=[raw_b[nm]], w=[sq_b])
            for tb in range(NB):
                p = n % 2
                n += 1
                sl = slice(tb * 512, (tb + 1) * 512)
                kb.op("pe", lambda e, p=p, sl=sl: e.matmul(psN[p][0:64, :], lhsT=onesf[0:64, 0:64], rhs=sq[:, sl], start=True, stop=True),
                      r=[sq_b, cst_b], w=[psN_b[p]])
                kb.op("act", lambda e, p=p: e.activation(out=rst[p][:], in_=psN[p][0:64, :], func=AF.Sqrt, bias=epsq[0:64, :], scale=1.0 / 64),
                      r=[psN_b[p], cst_b], w=[rst_b[p]])
                kb.op("dve", lambda e, p=p: e.reciprocal(out=rst[p][:], in_=rst[p][:]), r=[rst_b[p]], w=[rst_b[p]])
                kb.op("dve", lambda e, p=p, sl=sl, raw=raw, wcol=wcol, dst=dst: e.scalar_tensor_tensor(
                    out=dst[0:64, sl], in0=raw[:, sl], scalar=wcol[:, 0:1], in1=rst[p][:], op0=ALU.mult, op1=ALU.mult),
                    r=[raw_b[nm], rst_b[p], cst_b], w=[dst_b])
        it = 0
        for gq in range(NB):
            po = gq % 2
            jmax = 4 * gq + 3
            for j in range(jmax + 1):
                col0 = max(0, j - 4 * gq) * 128
                ps_i = it % 2
                pt_i = it % 3
                it += 1
                kb.op("pe", lambda e, ps_i=ps_i, j=j, gq=gq, col0=col0: e.matmul(
                    psS[ps_i][:, col0:512], lhsT=Ka[0:65, j * 128:(j + 1) * 128], rhs=Qa[0:65, gq * 512 + col0:(gq + 1) * 512],
                    start=True, stop=True), r=[Ka_b, Qa_b], w=[psS_b[ps_i]])
                kb.op("act", lambda e, ps_i=ps_i, pt_i=pt_i, j=j, h=h, col0=col0: e.activation(
                    out=PT[pt_i][:, col0:512], in_=psS[ps_i][:, col0:512], func=AF.Exp, bias=negc[:, j, h:h + 1], scale=0.125),
                    r=[psS_b[ps_i], negc_b], w=[PT_b[pt_i]])
                if j >= 4 * gq:
                    kb.op("pool", lambda e, pt_i=pt_i, col0=col0: e.tensor_tensor(
                        out=PT[pt_i][:, col0:col0 + 128], in0=PT[pt_i][:, col0:col0 + 128], in1=tri[:], op=ALU.mult),
                        r=[PT_b[pt_i], cst_b], w=[PT_b[pt_i]])
                kb.op("pe", lambda e, po=po, pt_i=pt_i, j=j, col0=col0, jmax=jmax: e.matmul(
                    psO[po][0:65, col0:512], lhsT=Va[:, j, 0:65], rhs=PT[pt_i][:, col0:512],
                    start=(j == 0), stop=(j == jmax), skip_group_check=True), r=[Va_b, PT_b[pt_i]], w=[psO_b[po]])
            kb.op("act", lambda e, po=po: e.copy(out=dn[64:65, :], in_=psO[po][64:65, :]), r=[psO_b[po]], w=[dn_b])
            kb.op("dve", lambda e: e.reciprocal(out=dn[64:65, :], in_=dn[64:65, :]), r=[dn_b], w=[dn_b])
            kb.op("pe", lambda e: e.matmul(psB[0:64, :], lhsT=onesf[64:65, 0:64], rhs=dn[64:65, :], start=True, stop=True),
                  r=[dn_b, cst_b], w=[psB_b])
            kb.op("act", lambda e: e.copy(out=bcs[:], in_=psB[0:64, :]), r=[psB_b], w=[bcs_b])
            kb.op("dve", lambda e, po=po: e.tensor_tensor(out=o1[:], in0=psO[po][0:64, :], in1=bcs[:], op=ALU.mult),
                  r=[psO_b[po], bcs_b], w=[o1_b])
            yi = gq % 2
            kb.op("pool", lambda e, yi=yi, gq=gq: e.tensor_tensor(out=yfx[yi][:], in0=o1[:], in1=sg[:, gq * 512:(gq + 1) * 512], op=ALU.mult),
                  r=[o1_b, sg_b], w=[yfx_b[yi]])
            kb.op("sp", lambda e, yi=yi, gq=gq, h=h: e.dma_start(out=g.yT[512 + 64 * h:512 + 64 * h + 64, gq * 512:(gq + 1) * 512], in_=yfx[yi][:]),
                  r=[yfx_b[yi]], dma=True)


CW = 0.6065306597126334


def setup3(nc, g):
    ei = lambda n, s, d=F32: nc.dram_tensor(n, list(s), d, kind="ExternalInput").ap()
    g.rw_mu = ei("rw_mu", [RW_COLS, 1])
    g.rw_w2a2 = ei("rw_w2a2", [64, 512])
    g.rw_g2 = ei("rw_g2", [96, 512])
    g.rw_cols = ei("rw_cols", [64, 8, 8])
    g.maskG = ei("maskG", [64, 320])
    g.resetm = ei("resetm", [64, 512])


def consts3_np(d):
    c = {}
    i = np.arange(64)[:, None]
    t = np.arange(64)[None, :]
    SU = (i < t).astype(np.float32)
    U = (i <= t).astype(np.float32)
    SL = (i > t).astype(np.float32)
    c["maskG"] = np.concatenate([SU, U, SU, U, SL], axis=1)
    rm = np.ones((64, 512), np.float32)
    rm[:, ::64] = 0.0
    c["resetm"] = rm
    c["rw_mu"] = d["rw_mu"][0][:, None]
    c["rw_w2a2"] = np.concatenate([d["rw_w2"][0], d["rw_a2"][0]], axis=0)
    c["rw_g2"] = d["rw_g2"][0]
    cols = np.zeros((64, 8, 8), np.float32)
    for j, nm in enumerate(["rw_w0", "rw_a0", "rw_k_k", "rw_k_a", "rw_r_k", "rw_gn_w", "rw_gn_b"]):
        cols[:, :, j] = d[nm][0].reshape(8, 64).T
    c["rw_cols"] = cols
    return c


def phase3(kb, g):
    nc, T, NT, NB = g.nc, g.T, g.NT, g.NB
    A = lambda n, s, d=F32: kb.sbuf("r3_" + n, s, d)
    onesf = A("onesf", [64, 64]); identb = A("identb", [64, 64], BF16); identf = A("identf", [64, 64])
    maskG = A("maskG", [64, 320]); resetm = A("resetm", [64, 512])
    w2a2 = A("w2a2", [32, 512]); a2t = A("a2t", [32, 512]); g2 = A("g2", [96, 512]); cols = A("cols", [64, 8, 8])
    mu_rkv = A("mu_rkv", [64, 24]); mu_wa = A("mu_wa", [32, 1]); mu_ad = A("mu_ad", [32, 1]); mu_g = A("mu_g", [96, 1])
    eps12 = A("eps12", [64, 1]); epsgn = A("epsgn", [64, 1])
    cst = kb.buf("cst3")
    kb.op("pool", lambda e: e.memset(onesf[:], 1.0), w=[cst])
    kb.op("pool", lambda e: e.memset(eps12[:], 0.0), w=[cst])
    kb.op("pool", lambda e: e.memset(epsgn[:], 64e-5), w=[cst])
    kb.op("sp", lambda e: e.dma_start(out=identb[:], in_=g.ident_b[0:64, 0:64]), w=[cst], dma=True)
    kb.op("sp", lambda e: e.dma_start(out=identf[:], in_=g.ident_f[0:64, 0:64]), w=[cst], dma=True)
    kb.op("sp", lambda e: e.dma_start(out=maskG[:], in_=g.maskG), w=[cst], dma=True)
    kb.op("sp", lambda e: e.dma_start(out=resetm[:], in_=g.resetm), w=[cst], dma=True)
    kb.op("sp", lambda e: e.dma_start(out=w2a2[:], in_=g.rw_w2a2[0:32, :]), w=[cst], dma=True)
    kb.op("sp", lambda e: e.dma_start(out=a2t[:], in_=g.rw_w2a2[32:64, :]), w=[cst], dma=True)
    kb.op("sp", lambda e: e.dma_start(out=g2[:], in_=g.rw_g2), w=[cst], dma=True)
    kb.op("sp", lambda e: e.dma_start(out=cols[:], in_=g.rw_cols), w=[cst], dma=True)
    for j in range(24):
        kb.op("sp", lambda e, j=j: e.dma_start(out=mu_rkv[:, j:j + 1], in_=g.rw_mu[64 * j:64 * j + 64, :]), w=[cst], dma=True)
    kb.op("sp", lambda e: e.dma_start(out=mu_wa[:], in_=g.rw_mu[1536:1568, :]), w=[cst], dma=True)
    kb.op("sp", lambda e: e.dma_start(out=mu_ad[:], in_=g.rw_mu[1568:1600, :]), w=[cst], dma=True)
    kb.op("sp", lambda e: e.dma_start(out=mu_g[:], in_=g.rw_mu[1600:1696, :]), w=[cst], dma=True)

    NS = 2
    cur = [A(f"cur{i}", [96, 512]) for i in range(3)]; prv = [A(f"prv{i}", [96, 512]) for i in range(3)]
    ld_b = kb.bufs(3, "ld")
    ldn = [0]
    wam = A("wam", [32, 512]); adm = A("adm", [32, 512]); gdm = A("gdm", [96, 512]); wam_b = kb.buf("wam"); adm_b = kb.buf("adm"); gdm_b = kb.buf("gdm")
    rm = A("rm", [64, 512]); km = A("km", [64, 512]); vm = A("vm", [64, 512])
    rm_b = kb.buf("rm"); km_b = kb.buf("km"); vm_b = kb.buf("vm")
    sgw = A("sgw", [64, 512]); asig = A("asig", [64, 512]); gg = A("gg", [64, 512])
    sgw_b = kb.buf("sgw"); asig_b = kb.buf("asig"); gg_b = kb.buf("gg")
    kk = A("kk", [64, 512]); t1 = A("t1", [64, 512]); t2 = A("t2", [64, 512]); kmod = A("kmod", [64, 512]); bb = A("bb", [64, 512])
    kk_b = kb.buf("kk"); t1_b = kb.buf("t1"); t2_b = kb.buf("t2"); kmod_b = kb.buf("kmod"); bb_b = kb.buf("bb")
    cumS = A("cumS", [64, 512]); gincl = A("gincl", [64, 512]); ginv = A("ginv", [64, 512]); gexcl = A("gexcl", [64, 512])
    cum_b = kb.buf("cum"); gincl_b = kb.buf("gincl"); ginv_b = kb.buf("ginv"); gexcl_b = kb.buf("gexcl")
    bon = A("bon", [64, 512]); bon_b = kb.buf("bon")
    AR = [A(f"AR{i}", [64, 8, 128], BF16) for i in range(2)]; BK = [A(f"BK{i}", [64, 8, 128], BF16) for i in range(2)]
    vb = [A(f"vb{i}", [64, 512], BF16) for i in range(2)]
    AR_b = kb.bufs(2, "AR"); BK_b = kb.bufs(2, "BK"); vb_b = kb.bufs(2, "vb")
    TM = [A(f"TM{i}", [64, 320], BF16) for i in range(NS)]; TM_b = kb.bufs(NS, "TM"); TMx_b = kb.bufs(NS, "TMx")
    NM = [A(f"NM{i}", [64, 320], BF16) for i in range(NS)]; NM_b = kb.bufs(NS, "NM")
    DB = [[A(f"DB{i}_{j}", [64, 192], BF16) for j in range(2)] for i in range(NS)]
    DB_b = [kb.bufs(2, f"DB{i}") for i in range(NS)]
    AW = [A(f"AW{i}", [64, 128], BF16) for i in range(NS)]; AW_b = kb.bufs(NS, "AW")
    G1 = [A(f"G1{i}", [64, 64]) for i in range(NS)]; G1_b = kb.bufs(NS, "G1")
    Hg = [A(f"Hg{i}", [64, 64]) for i in range(NS)]; Hg_b = kb.bufs(NS, "Hg")
    Ry = [A(f"Ry{i}", [64, 64]) for i in range(NS)]; Ry_b = kb.bufs(NS, "Ry")
    ST = [[A(f"ST{h}_{j}", [64, 64]) for j in range(2)] for h in range(8)]
    ST_b = [kb.bufs(2, f"ST{h}") for h in range(8)]
    ysb = A("ysb", [64, 512]); ysq = A("ysq", [64, 512]); ymean = A("ymean", [64, 512]); yvar = A("yvar", [64, 512])
    ysb_b = kb.buf("ysb"); ysq_b = kb.buf("ysq"); ymean_b = kb.buf("ymean"); yvar_b = kb.buf("yvar")
    yout = [A(f"yout{i}", [64, 512], BF16) for i in range(2)]; yout_b = kb.bufs(2, "yout")
    tw = A("tw", [32, 512]); sgd = A("sgd", [96, 512]); tw_b = kb.buf("tw"); sgd_b = kb.buf("sgd")
    psT = kb.psum("r3psT", [128, 1024], BF16); psT_b = kb.buf("psT")
    psG = kb.psum("r3psG", [128, 512], F32); psG_b = kb.buf("psG")
    psD = [kb.psum(f"r3psD{i}", [128, 512], F32) for i in range(2)]; psD_b = kb.bufs(2, "psD")
    psA = kb.psum("r3psA", [128, 512], F32)
    psX_b = kb.buf("psA"); psAW_b = psX_b; psGp_b = psX_b; psH_b = psX_b; psR_b = psX_b
    psY = kb.psum("r3psY", [128, 512], F32); psY_b = kb.buf("psY")
    psS = kb.psum("r3psS", [128, 512], F32); psS_b = kb.buf("psS")
    psL = kb.psum("r3psL", [128, 512], F32); psL_b = kb.buf("psL")

    for h in range(8):
        kb.op("pool", lambda e, h=h: e.memset(ST[h][0][:], 0.0), w=[ST_b[h][0]])

    def load_mixed(rows0, nrows, tb, mucol, dst, dst_b):
        i = ldn[0] % 3
        ldn[0] += 1
        t0 = tb * 512
        kb.op("sp", lambda e: e.dma_start(out=cur[i][0:nrows, :], in_=g.pT[rows0:rows0 + nrows, t0:t0 + 512]), w=[ld_b[i]], dma=True)
        if tb == 0:
            kb.op("pool", lambda e: e.memset(prv[i][0:nrows, 0:1], 0.0), w=[ld_b[i]])
            kb.op("sp", lambda e: e.dma_start(out=prv[i][0:nrows, 1:512], in_=g.pT[rows0:rows0 + nrows, 0:511]), w=[ld_b[i]], dma=True)
        else:
            kb.op("sp", lambda e: e.dma_start(out=prv[i][0:nrows, :], in_=g.pT[rows0:rows0 + nrows, t0 - 1:t0 + 511]), w=[ld_b[i]], dma=True)
        kb.op("pool", lambda e: e.tensor_tensor(out=prv[i][0:nrows, :], in0=prv[i][0:nrows, :], in1=cur[i][0:nrows, :], op=ALU.subtract),
              r=[ld_b[i]], w=[ld_b[i]])
        kb.op("dve", lambda e: e.scalar_tensor_tensor(out=dst[0:nrows, :], in0=prv[i][0:nrows, :], scalar=mucol, in1=cur[i][0:nrows, :],
                                                       op0=ALU.mult, op1=ALU.add), r=[ld_b[i], cst], w=[dst_b])

    un = 0
    for tb in range(NB):
        load_mixed(1536, 32, tb, mu_wa[:, 0:1], wam, wam_b)
        load_mixed(1568, 32, tb, mu_ad[:, 0:1], adm, adm_b)
        load_mixed(1600, 96, tb, mu_g[:, 0:1], gdm, gdm_b)
        kb.op("act", lambda e: e.activation(out=tw[0:32, :], in_=wam[0:32, :], func=AF.Tanh), r=[wam_b], w=[tw_b])
        kb.op("act", lambda e: e.activation(out=sgd[:], in_=gdm[:], func=AF.Sigmoid), r=[gdm_b], w=[sgd_b])
        for h in range(8):
            hc = slice(64 * h, 64 * h + 64)
            pz = tb * 8 + h
            z = pz % 2
            C = lambda j, h=h: cols[:, h, j:j + 1]
            load_mixed(64 * h, 64, tb, mu_rkv[:, h:h + 1], rm, rm_b)
            load_mixed(512 + 64 * h, 64, tb, mu_rkv[:, 8 + h:9 + h], km, km_b)
            load_mixed(1024 + 64 * h, 64, tb, mu_rkv[:, 16 + h:17 + h], vm, vm_b)
            kb.op("pe", lambda e, hc=hc: e.matmul(psL[0:64, :], lhsT=w2a2[0:32, hc], rhs=tw[0:32, :], start=True, stop=True),
                  r=[tw_b, cst], w=[psL_b])
            kb.op("act", lambda e, C=C: e.activation(out=sgw[:], in_=psL[0:64, :], func=AF.Sigmoid, bias=C(0), scale=1.0),
                  r=[psL_b, cst], w=[sgw_b])
            kb.op("pe", lambda e, hc=hc: e.matmul(psL[0:64, :], lhsT=a2t[0:32, hc], rhs=adm[0:32, :], start=True, stop=True),
                  r=[adm_b, cst], w=[psL_b])
            kb.op("act", lambda e, C=C: e.activation(out=asig[:], in_=psL[0:64, :], func=AF.Sigmoid, bias=C(1), scale=1.0),
                  r=[psL_b, cst], w=[asig_b])
            kb.op("pe", lambda e, hc=hc: e.matmul(psL[0:64, :], lhsT=g2[0:96, hc], rhs=sgd[0:96, :], start=True, stop=True),
                  r=[sgd_b, cst], w=[psL_b])
            kb.op("act", lambda e: e.copy(out=gg[:], in_=psL[0:64, :]), r=[psL_b], w=[gg_b])
            kb.op("dve", lambda e, C=C: e.tensor_scalar(out=kk[:], in0=km[:], scalar1=C(2), scalar2=None, op0=ALU.mult), r=[km_b, cst], w=[kk_b])
            kb.op("pool", lambda e: e.tensor_tensor(out=t1[:], in0=kk[:], in1=kk[:], op=ALU.mult), r=[kk_b], w=[t1_b])
            kb.op("pe", lambda e: e.matmul(psL[0:64, :], lhsT=onesf[:, :], rhs=t1[:], start=True, stop=True), r=[t1_b, cst], w=[psL_b])
            kb.op("act", lambda e: e.activation(out=t2[:], in_=psL[0:64, :], func=AF.Sqrt, bias=eps12[:], scale=1.0), r=[psL_b, cst], w=[t2_b])
            kb.op("dve", lambda e: e.tensor_scalar(out=t2[:], in0=t2[:], scalar1=1e-12, scalar2=None, op0=ALU.max), r=[t2_b], w=[t2_b])
            kb.op("dve", lambda e: e.reciprocal(out=t2[:], in_=t2[:]), r=[t2_b], w=[t2_b])
            kb.op("dve", lambda e: e.tensor_tensor(out=kk[:], in0=kk[:], in1=t2[:], op=ALU.mult), r=[kk_b, t2_b], w=[kk_b])
            kb.op("dve", lambda e, C=C: e.tensor_scalar(out=t1[:], in0=asig[:], scalar1=-1.0, scalar2=C(3), op0=ALU.add, op1=ALU.mult),
                  r=[asig_b, cst], w=[t1_b])
            kb.op("dve", lambda e: e.scalar_tensor_tensor(out=kmod[:], in0=t1[:], scalar=1.0, in1=km[:], op0=ALU.add, op1=ALU.mult),
                  r=[t1_b, km_b], w=[kmod_b])
            kb.op("pool", lambda e: e.tensor_tensor(out=bb[:], in0=kk[:], in1=asig[:], op=ALU.mult), r=[kk_b, asig_b], w=[bb_b])
            kb.op("dve", lambda e: e.tensor_tensor_scan(out=cumS[:], data0=resetm[:], data1=sgw[:], initial=0.0, op0=ALU.mult, op1=ALU.add),
                  r=[sgw_b, cst], w=[cum_b])
            kb.op("act", lambda e: e.activation(out=gincl[:], in_=cumS[:], func=AF.Exp, scale=-CW), r=[cum_b], w=[gincl_b])
            kb.op("act", lambda e: e.activation(out=ginv[:], in_=cumS[:], func=AF.Exp, scale=CW), r=[cum_b], w=[ginv_b])
            kb.op("pool", lambda e: e.tensor_tensor(out=t2[:], in0=cumS[:], in1=sgw[:], op=ALU.subtract), r=[cum_b, sgw_b], w=[t2_b])
            kb.op("act", lambda e: e.activation(out=gexcl[:], in_=t2[:], func=AF.Exp, scale=-CW), r=[t2_b], w=[gexcl_b])
            v4 = lambda t: t[:].rearrange("p (c t) -> p c t", c=8)
            kb.op("dve", lambda e, z=z: e.scalar_tensor_tensor(out=AR[z][:, :, 0:64], in0=v4(kk), scalar=-1.0, in1=v4(gexcl), op0=ALU.mult, op1=ALU.mult),
                  r=[kk_b, gexcl_b], w=[AR_b[z]])
            kb.op("pool", lambda e, z=z: e.tensor_tensor(out=AR[z][:, :, 64:128], in0=v4(rm), in1=v4(gincl), op=ALU.mult),
                  r=[rm_b, gincl_b], w=[AR_b[z]])
            kb.op("dve", lambda e, z=z: e.tensor_tensor(out=BK[z][:, :, 0:64], in0=v4(bb), in1=v4(ginv), op=ALU.mult),
                  r=[bb_b, ginv_b], w=[BK_b[z]])
            kb.op("pool", lambda e, z=z: e.tensor_tensor(out=BK[z][:, :, 64:128], in0=v4(kmod), in1=v4(ginv), op=ALU.mult),
                  r=[kmod_b, ginv_b], w=[BK_b[z]])
            kb.op("act", lambda e, z=z: e.copy(out=vb[z][:], in_=vm[:]), r=[vm_b], w=[vb_b[z]])
            kb.op("dve", lambda e, C=C: e.scalar_tensor_tensor(out=t1[:], in0=rm[:], scalar=C(4), in1=kmod[:], op0=ALU.mult, op1=ALU.mult),
                  r=[rm_b, kmod_b, cst], w=[t1_b])
            kb.op("pe", lambda e: e.matmul(psL[0:64, :], lhsT=onesf[:, :], rhs=t1[:], start=True, stop=True), r=[t1_b, cst], w=[psL_b])
            kb.op("dve", lambda e: e.tensor_tensor(out=bon[:], in0=psL[0:64, :], in1=vm[:], op=ALU.mult), r=[psL_b, vm_b], w=[bon_b])

            for c in range(8):
                u = un % NS
                un += 1
                cs = slice(c * 64, c * 64 + 64)
                gC = gincl[:, c * 64 + 63:c * 64 + 64]
                srcs = [(BK[z][:, c, 0:64], BK_b[z]), (BK[z][:, c, 64:128], BK_b[z]), (vb[z][:, cs], vb_b[z]), (AR[z][:, c, 0:64], AR_b[z])]
                for k4, (src, sb) in enumerate(srcs):
                    kb.op("pe", lambda e, k4=k4, src=src: e.transpose(out=psT[0:64, k4 * 64:(k4 + 1) * 64], in_=src, identity=identb[:]),
                          r=[sb, cst], w=[psT_b])
                kb.op("act", lambda e, u=u: e.copy(out=TM[u][:, 0:256], in_=psT[0:64, 0:256]), r=[psT_b], w=[TM_b[u]])
                kb.op("pe", lambda e, z=z, c=c: e.matmul(psG[0:64, 0:128], lhsT=BK[z][:, c, 0:64], rhs=AR[z][:, c, :], start=True, stop=True),
                      r=[BK_b[z], AR_b[z]], w=[psG_b])
                kb.op("pe", lambda e, z=z, c=c: e.matmul(psG[0:64, 128:256], lhsT=BK[z][:, c, 64:128], rhs=AR[z][:, c, :], start=True, stop=True),
                      r=[BK_b[z], AR_b[z]], w=[psG_b])
                kb.op("pe", lambda e, z=z, c=c: e.matmul(psG[0:64, 256:320], lhsT=AR[z][:, c, 0:64], rhs=BK[z][:, c, 0:64], start=True, stop=True),
                      r=[BK_b[z], AR_b[z]], w=[psG_b])
                kb.op("dve", lambda e, u=u: e.tensor_tensor(out=NM[u][:], in0=psG[0:64, 0:320], in1=maskG[:], op=ALU.mult),
                      r=[psG_b, cst], w=[NM_b[u]])
                kb.op("pe", lambda e, u=u: e.matmul(psA[0:64, 0:64], lhsT=NM[u][:, 128:192], rhs=TM[u][:, 128:192], start=True, stop=True),
                      r=[NM_b[u], TM_b[u]], w=[psX_b])
                kb.op("act", lambda e, u=u: e.copy(out=TM[u][:, 256:320], in_=psA[0:64, 0:64]), r=[psX_b], w=[TMx_b[u]])
                d0, d1 = DB[u][0], DB[u][1]
                pd = psD[u % 2]
                pdb = psD_b[u % 2]
                kb.op("dve", lambda e, u=u, d1=d1: e.tensor_tensor(out=d1[:, 0:64], in0=NM[u][:, 0:64], in1=identb[:], op=ALU.add),
                      r=[NM_b[u], cst], w=[DB_b[u][1]])
                kb.op("pe", lambda e, u=u, pd=pd: e.matmul(pd[0:64, 64:128], lhsT=NM[u][:, 256:320], rhs=NM[u][:, 0:64], start=True, stop=True),
                      r=[NM_b[u]], w=[pdb])
                kb.op("pe", lambda e, u=u, pd=pd: e.matmul(pd[0:64, 128:192], lhsT=NM[u][:, 0:64], rhs=NM[u][:, 256:320], start=True, stop=True),
                      r=[NM_b[u]], w=[pdb])
                kb.op("act", lambda e, d1=d1, pd=pd: e.copy(out=d1[:, 64:192], in_=pd[0:64, 64:192]), r=[pdb], w=[DB_b[u][1]])
                ci = 1
                for lvl in range(1, 6):
                    dc, dn_ = DB[u][ci], DB[u][1 - ci]
                    dcb, dnb = DB_b[u][ci], DB_b[u][1 - ci]
                    kb.op("pe", lambda e, dc=dc, pd=pd: e.matmul(pd[0:64, 0:128], lhsT=dc[:, 128:192], rhs=dc[:, 0:128], start=True, stop=True),
                          r=[dcb], w=[pdb])
                    if lvl < 5:
                        kb.op("pe", lambda e, dc=dc, pd=pd: e.matmul(pd[0:64, 128:192], lhsT=dc[:, 64:128], rhs=dc[:, 128:192], start=True, stop=True),
                              r=[dcb], w=[pdb])
                    kb.op("dve", lambda e, dc=dc, dn_=dn_, pd=pd: e.tensor_tensor(out=dn_[:, 0:64], in0=pd[0:64, 0:64], in1=dc[:, 0:64], op=ALU.add),
                          r=[pdb, dcb], w=[dnb])
                    if lvl < 5:
                        kb.op("act", lambda e, dn_=dn_, pd=pd: e.copy(out=dn_[:, 64:192], in_=pd[0:64, 64:192]), r=[pdb], w=[dnb])
                    ci = 1 - ci
                Tm, Tm_b = DB[u][ci], DB_b[u][ci]
                kb.op("pe", lambda e, u=u, Tm=Tm: e.matmul(psA[0:64, 64:192], lhsT=Tm[:, 0:64], rhs=TM[u][:, 192:320], start=True, stop=True),
                      r=[Tm_b, TM_b[u], TMx_b[u]], w=[psAW_b])
                kb.op("act", lambda e, u=u: e.copy(out=AW[u][:], in_=psA[0:64, 64:192]), r=[psAW_b], w=[AW_b[u]])
                kb.op("pe", lambda e, u=u: e.matmul(psA[0:64, 192:256], lhsT=AW[u][:, 0:64], rhs=TM[u][:, 0:64], start=True, stop=True),
                      r=[AW_b[u], TM_b[u]], w=[psGp_b])
                kb.op("dve", lambda e, u=u: e.tensor_tensor(out=G1[u][:], in0=psA[0:64, 192:256], in1=identf[:], op=ALU.add),
                      r=[psGp_b, cst], w=[G1_b[u]])
                kb.op("pe", lambda e, u=u: e.matmul(psA[0:64, 256:320], lhsT=TM[u][:, 0:64], rhs=AW[u][:, 64:128], start=True, stop=False),
                      r=[AW_b[u], TM_b[u]], w=[psH_b])
                kb.op("pe", lambda e, u=u: e.matmul(psA[0:64, 256:320], lhsT=TM[u][:, 64:128], rhs=TM[u][:, 128:192], start=False, stop=True),
                      r=[TM_b[u]], w=[psH_b])
                kb.op("dve", lambda e, u=u, gC=gC: e.tensor_scalar(out=Hg[u][:], in0=psA[0:64, 256:320], scalar1=gC, scalar2=None, op0=ALU.mult),
                      r=[psH_b, gincl_b], w=[Hg_b[u]])
                kb.op("pe", lambda e, u=u: e.matmul(psA[0:64, 320:384], lhsT=AW[u][:, 0:64], rhs=NM[u][:, 64:128], start=True, stop=True),
                      r=[AW_b[u], NM_b[u]], w=[psR_b])
                kb.op("dve", lambda e, u=u, z=z, c=c: e.tensor_tensor(out=Ry[u][:], in0=psA[0:64, 320:384], in1=AR[z][:, c, 64:128], op=ALU.add),
                      r=[psR_b, AR_b[z]], w=[Ry_b[u]])
                sc = (tb * 8 + c) % 2
                So, Sn = ST[h][sc], ST[h][1 - sc]
                Sob, Snb = ST_b[h][sc], ST_b[h][1 - sc]
                kb.op("pe", lambda e, u=u, cs=cs: e.matmul(psY[0:64, cs], lhsT=AW[u][:, 64:128], rhs=NM[u][:, 64:128], start=True, stop=False, skip_group_check=True),
                      r=[AW_b[u], NM_b[u]], w=[psY_b])
                kb.op("pe", lambda e, u=u, cs=cs: e.matmul(psY[0:64, cs], lhsT=TM[u][:, 128:192], rhs=NM[u][:, 192:256], start=False, stop=False, skip_group_check=True),
                      r=[TM_b[u], NM_b[u]], w=[psY_b])
                kb.op("pe", lambda e, u=u, cs=cs, So=So: e.matmul(psY[0:64, cs], lhsT=So[:], rhs=Ry[u][:], start=False, stop=True, skip_group_check=True),
                      r=[Sob, Ry_b[u]], w=[psY_b])
                kb.op("pe", lambda e, u=u, So=So: e.matmul(psS[0:64, 0:64], lhsT=G1[u][:], rhs=So[:], start=True, stop=True),
                      r=[G1_b[u], Sob], w=[psS_b])
                kb.op("dve", lambda e, u=u, Sn=Sn, gC=gC: e.scalar_tensor_tensor(out=Sn[:], in0=psS[0:64, 0:64], scalar=gC, in1=Hg[u][:], op0=ALU.mult, op1=ALU.add),
                      r=[psS_b, Hg_b[u], gincl_b], w=[Snb])
            kb.op("act", lambda e: e.copy(out=ysb[:], in_=psY[0:64, :]), r=[psY_b], w=[ysb_b])
            kb.op("pool", lambda e: e.tensor_tensor(out=ysq[:], in0=ysb[:], in1=ysb[:], op=ALU.mult), r=[ysb_b], w=[ysq_b])
            kb.op("pe", lambda e: e.matmul(psL[0:64, :], lhsT=onesf[:, :], rhs=ysb[:], start=True, stop=True), r=[ysb_b, cst], w=[psL_b])
            kb.op("act", lambda e: e.activation(out=ymean[:], in_=psL[0:64, :], func=AF.Identity, scale=1.0 / 64), r=[psL_b], w=[ymean_b])
            kb.op("pe", lambda e: e.matmul(psL[0:64, :], lhsT=onesf[:, :], rhs=ysq[:], start=True, stop=True), r=[ysq_b, cst], w=[psL_b])
            kb.op("pool", lambda e: e.tensor_tensor(out=ysq[:], in0=ymean[:], in1=ymean[:], op=ALU.mult), r=[ymean_b, ysq_b], w=[ysq_b])
            kb.op("dve", lambda e: e.scalar_tensor_tensor(out=yvar[:], in0=psL[0:64, :], scalar=1.0 / 64, in1=ysq[:], op0=ALU.mult, op1=ALU.subtract),
                  r=[psL_b, ysq_b], w=[yvar_b])
            kb.op("act", lambda e: e.activation(out=yvar[:], in_=yvar[:], func=AF.Sqrt, bias=epsgn[:], scale=1.0), r=[yvar_b, cst], w=[yvar_b])
            kb.op("dve", lambda e: e.reciprocal(out=yvar[:], in_=yvar[:]), r=[yvar_b], w=[yvar_b])
            kb.op("pool", lambda e: e.tensor_tensor(out=ysb[:], in0=ysb[:], in1=ymean[:], op=ALU.subtract), r=[ysb_b, ymean_b], w=[ysb_b])
            kb.op("dve", lambda e, C=C: e.scalar_tensor_tensor(out=ysb[:], in0=ysb[:], scalar=C(5), in1=yvar[:], op0=ALU.mult, op1=ALU.mult),
                  r=[ysb_b, yvar_b, cst], w=[ysb_b])
            kb.op("dve", lambda e, C=C: e.scalar_tensor_tensor(out=ysb[:], in0=ysb[:], scalar=C(6), in1=bon[:], op0=ALU.add, op1=ALU.add),
                  r=[ysb_b, bon_b, cst], w=[ysb_b])
            yo = pz % 2
            kb.op("pool", lambda e, yo=yo: e.tensor_tensor(out=yout[yo][:], in0=ysb[:], in1=gg[:], op=ALU.mult), r=[ysb_b, gg_b], w=[yout_b[yo]])
            kb.op("sp", lambda e, yo=yo, h=h, tb=tb: e.dma_start(out=g.yT[64 * h:64 * h + 64, tb * 512:(tb + 1) * 512], in_=yout[yo][:]),
                  r=[yout_b[yo]], dma=True)


def setup4(nc, g, out_name="out"):
    ei = lambda n, s, d=F32: nc.dram_tensor(n, list(s), d, kind="ExternalInput").ap()
    T = g.T
    g.w_o = ei("w_o", [D, D])
    g.ln1_w = ei("ln1_w", [1, D]); g.ln1_b = ei("ln1_b", [1, D])
    g.ln2_w = ei("ln2_w", [1, D]); g.ln2_b = ei("ln2_b", [1, D])
    g.router_w = ei("router_w", [D, 32]); g.router_b = ei("router_b", [1, 32])
    g.exp_w1 = ei("exp_w1", [32, D, 2048]); g.exp_b1 = ei("exp_b1", [32, 2048])
    g.exp_w2 = ei("exp_w2", [32, D, D]); g.exp_b2 = ei("exp_b2", [32, D])
    g.h1f = g.scratch("h1f", [T, D])
    g.h1T = g.scratch("h1T", [D, T], BF16)
    g.yacc = g.scratch("yacc", [T, D])
    g.out = nc.dram_tensor(out_name, [T, D], F32, kind="ExternalOutput").ap()


def _ln_tile(kb, src, src_b, dst, dst_b, st, mv, rs, st_b, epsc, cst, gam, bet):
    for hf in range(2):
        kb.op("dve", lambda e, hf=hf: e.bn_stats(out=st[:, hf, :], in_=src[:, hf * 512:(hf + 1) * 512]), r=[src_b], w=[st_b])
    kb.op("dve", lambda e: e.bn_aggr(out=mv[:], in_=st[:].rearrange("p a b -> p (a b)")), r=[st_b], w=[st_b])
    kb.op("act", lambda e: e.activation(out=rs[:], in_=mv[:, 1:2], func=AF.Sqrt, bias=epsc[:], scale=1.0), r=[st_b, cst], w=[st_b])
    kb.op("dve", lambda e: e.reciprocal(out=rs[:], in_=rs[:]), r=[st_b], w=[st_b])
    kb.op("dve", lambda e: e.tensor_scalar(out=dst[:], in0=src[:], scalar1=mv[:, 0:1], scalar2=rs[:], op0=ALU.subtract, op1=ALU.mult),
          r=[src_b, st_b], w=[dst_b])
    kb.op("dve", lambda e: e.tensor_tensor(out=dst[:], in0=dst[:], in1=gam[:], op=ALU.mult), r=[dst_b, cst], w=[dst_b])
    kb.op("dve", lambda e: e.tensor_tensor(out=dst[:], in0=dst[:], in1=bet[:], op=ALU.add), r=[dst_b, cst], w=[dst_b])


def phase4(kb, g, n_exp=32, sections="ABC"):
    nc, T, NT, NB = g.nc, g.T, g.NT, g.NB
    kb.ps_stack.close(); kb.ps_stack = None
    gates = kb.sbuf("m4_gates", [128, NT, 32], F32); gates_b = kb.buf("gates")
    kb.begin_phase()
    A = lambda n, s, d=F32: kb.sbuf("m4_" + n, s, d)
    cst = kb.buf("cst4")
    wo = A("wo", [128, KC, D], BF16)
    rwt = A("rwt", [128, KC, 32]); rbt = A("rbt", [128, 32])
    gam1 = A("gam1", [128, D]); bet1 = A("bet1", [128, D])
    identf = A("identf", [128, 128]); epsc = A("epsc", [128, 1])
    kb.op("dve", lambda e: e.memset(epsc[:], LN_EPS), w=[cst])
    kb.op("sp", lambda e: e.dma_start(out=identf[:], in_=g.ident_f), w=[cst], dma=True)
    for nm, t_, src in (("g1", gam1, g.ln1_w), ("b1", bet1, g.ln1_b)):
        kb.op("sp", lambda e, t_=t_, src=src: e.dma_start(out=t_[:], in_=src.broadcast_to([128, D])), w=[cst], dma=True)
    kb.op("sp", lambda e: e.dma_start(out=rbt[:], in_=g.router_b.broadcast_to([128, 32])), w=[cst], dma=True)
    kb.op("sp", lambda e: e.dma_start(out=rwt[:], in_=g.router_w.rearrange("(kc p) n -> p kc n", p=128)), w=[cst], dma=True)
    for kc in range(KC):
        kb.op("pool", lambda e, kc=kc: e.dma_start(out=wo[:, kc, :], in_=g.w_o[kc * 128:(kc + 1) * 128, :]), w=[cst], dma=True)

    ymt = [A(f"ymt{i}", [128, KC, 128], BF16) for i in range(2)]; ymt_b = kb.bufs(2, "ymt")
    h0t = [A(f"h0t{i}", [128, D]) for i in range(2)]; h0t_b = kb.bufs(2, "h0t")
    sres = [A(f"sres{i}", [128, D]) for i in range(2)]; sres_b = kb.bufs(2, "sres")
    h1t = [A(f"h1t{i}", [128, D]) for i in range(2)]; h1t_b = kb.bufs(2, "h1t")
    st = A("st", [128, 2, 6]); mv = A("mv", [128, 2]); rs = A("rs", [128, 1]); st_b = kb.buf("st")
    h1Tf = A("h1Tf", [128, KC, 128]); h1Tf_b = kb.buf("h1Tf")
    h1Tb = [A(f"h1Tb{i}", [128, KC, 128], BF16) for i in range(2)]; h1Tb_b = kb.bufs(2, "h1Tb")
    lg = A("lg", [128, 32]); mx8 = A("mx8", [128, 8]); msk = A("msk", [128, 32]); ee = A("ee", [128, 32]); ssum = A("ssum", [128, 1])
    nm0 = A("nm0", [128, 1]); lg_b = kb.buf("lg")
    ps = [kb.psum(f"m4ps{i}", [128, 512], F32) for i in range(8)]; ps_b = kb.bufs(8, "m4ps")

    import os
    STOPAT = int(os.environ.get('STOPAT', '99'))
    for i in range(NT):
        s = i % 2
        tsl = slice(i * 128, (i + 1) * 128)
        kb.op("sp", lambda e, s=s, tsl=tsl: e.dma_start(out=ymt[s][:], in_=g.yT[:, tsl].rearrange("(kc p) t -> p kc t", p=128)), w=[ymt_b[s]], dma=True)
        kb.op("sp", lambda e, s=s, tsl=tsl: e.dma_start(out=h0t[s][:], in_=g.h0f[tsl, :]), w=[h0t_b[s]], dma=True)
        if STOPAT <= 1:
            continue
        for hf in range(2):
            for kc in range(KC):
                kb.op("pe", lambda e, s=s, hf=hf, kc=kc: e.matmul(ps[hf][:, :], lhsT=ymt[s][:, kc, :], rhs=wo[:, kc, hf * 512:(hf + 1) * 512],
                                                                 start=(kc == 0), stop=(kc == KC - 1)), r=[ymt_b[s], cst], w=[ps_b[hf]])
            kb.op("dve", lambda e, s=s, hf=hf: e.scalar_tensor_tensor(out=sres[s][:, hf * 512:(hf + 1) * 512], in0=h0t[s][:, hf * 512:(hf + 1) * 512],
                                                                     scalar=ALPHA, in1=ps[hf][:, :], op0=ALU.mult, op1=ALU.add),
                  r=[h0t_b[s], ps_b[hf]], w=[sres_b[s]])
        if STOPAT <= 2:
            continue
        _ln_tile(kb, sres[s], sres_b[s], h1t[s], h1t_b[s], st, mv, rs, st_b, epsc, cst, gam1, bet1)
        if STOPAT <= 3:
            continue
        kb.op("sp", lambda e, s=s, tsl=tsl: e.dma_start(out=g.h1f[tsl, :], in_=h1t[s][:]), r=[h1t_b[s]], dma=True)
        if STOPAT <= 4:
            continue
        for hf in range(2):
            p = 2 + hf
            for c in range(4):
                kc = hf * 4 + c
                kb.op("pe", lambda e, s=s, p=p, c=c, kc=kc: e.transpose(out=ps[p][:, c * 128:(c + 1) * 128], in_=h1t[s][:, kc * 128:(kc + 1) * 128], identity=identf[:]),
                      r=[h1t_b[s], cst], w=[ps_b[p]])
            kb.op("act", lambda e, p=p, hf=hf: e.copy(out=h1Tf[:, hf * 4:(hf + 1) * 4, :], in_=ps[p][:].rearrange("p (c t) -> p c t", c=4)), r=[ps_b[p]], w=[h1Tf_b])
            kb.op("dve", lambda e, hf=hf, s=s: e.tensor_copy(out=h1Tb[s][:, hf * 4:(hf + 1) * 4, :], in_=h1Tf[:, hf * 4:(hf + 1) * 4, :]),
                  r=[h1Tf_b], w=[h1Tb_b[s]])
        if STOPAT <= 5:
            continue
        kb.op("sp", lambda e, s=s, tsl=tsl: e.dma_start(out=g.h1T[:, tsl].rearrange("(kc p) t -> p kc t", p=128), in_=h1Tb[s][:]), r=[h1Tb_b[s]], dma=True)
        import os
        if os.environ.get('SKIP_ROUTER'):
            continue
        for kc in range(KC):
            kb.op("pe", lambda e, kc=kc: e.matmul(ps[4][:, 0:32], lhsT=h1Tf[:, kc, :], rhs=rwt[:, kc, :], start=(kc == 0), stop=(kc == KC - 1)),
                  r=[h1Tf_b, cst], w=[ps_b[4]])
        kb.op("dve", lambda e: e.tensor_tensor(out=lg[:], in0=ps[4][:, 0:32], in1=rbt[:], op=ALU.add), r=[ps_b[4], cst], w=[lg_b])
        kb.op("dve", lambda e: e.max(out=mx8[:], in_=lg[:]), r=[lg_b], w=[lg_b])
        kb.op("dve", lambda e: e.tensor_scalar(out=msk[:], in0=lg[:], scalar1=mx8[:, 3:4], scalar2=None, op0=ALU.is_ge), r=[lg_b], w=[lg_b])
        kb.op("dve", lambda e: e.tensor_scalar(out=nm0[:], in0=mx8[:, 0:1], scalar1=-1.0, scalar2=None, op0=ALU.mult), r=[lg_b], w=[lg_b])
        kb.op("act", lambda e: e.activation(out=ee[:], in_=lg[:], func=AF.Exp, bias=nm0[:], scale=1.0), r=[lg_b], w=[lg_b])
        kb.op("dve", lambda e: e.tensor_tensor(out=ee[:], in0=ee[:], in1=msk[:], op=ALU.mult), r=[lg_b], w=[lg_b])
        kb.op("dve", lambda e: e.reduce_sum(out=ssum[:], in_=ee[:], axis=AX.X), r=[lg_b], w=[lg_b])
        kb.op("dve", lambda e: e.reciprocal(out=ssum[:], in_=ssum[:]), r=[lg_b], w=[lg_b])
        kb.op("dve", lambda e, i=i: e.tensor_scalar(out=gates[:, i, :], in0=ee[:], scalar1=ssum[:, 0:1], scalar2=None, op0=ALU.mult), r=[lg_b], w=[gates_b])

    if "B" not in sections:
        return
    kb.end_phase(); kb.begin_phase()
    A = lambda n, s, d=F32: kb.sbuf("m4b_" + n, s, d)
    cst = kb.buf("cst4b")
    b1t = A("b1t", [128, 32, 8, 2])
    for ex in range(32):
        kb.op("sp", lambda e, ex=ex: e.dma_start(out=b1t[:, ex, :, :], in_=g.exp_b1[ex:ex + 1, :].rearrange("o (c p two) -> p (o c) two", p=128, two=2)),
              w=[cst], dma=True)
    ps = [kb.psum(f"m4bps{i}", [128, 512], F32) for i in range(8)]; ps_b = kb.bufs(8, "m4bps")
    W1 = [A(f"W1_{i}", [128, KC, 2048], BF16) for i in range(2)]; W2 = [A(f"W2_{i}", [128, KC, D], BF16) for i in range(2)]
    W_b = kb.bufs(2, "W")
    b2t = [A(f"b2t{i}", [128, D]) for i in range(2)]
    xT = [A(f"xT{i}", [128, KC, 512], BF16) for i in range(2)]; xT_b = kb.bufs(2, "xT")
    actT = [A(f"actT{i}", [128, 8, 512], BF16) for i in range(2)]; actT_b = kb.bufs(2, "actT")
    xg = [A(f"xg{i}", [128, 512]) for i in range(2)]; sg = [A(f"sgm{i}", [128, 512]) for i in range(2)]; xl = [A(f"xl{i}", [128, 512]) for i in range(2)]
    el_b = kb.bufs(2, "el")
    ot = [A(f"ot{i}", [128, 512]) for i in range(3)]; ot_b = kb.bufs(3, "ot")
    yacc_b = kb.bufs(NT, "yacc")

    def load_w(ex):
        z = ex % 2
        for kc in range(KC):
            kb.op("pool", lambda e, z=z, kc=kc, ex=ex: e.dma_start(out=W1[z][:, kc, :], in_=g.exp_w1[ex, kc * 128:(kc + 1) * 128, :]), w=[W_b[z]], dma=True)
        for kc in range(KC):
            kb.op("pool", lambda e, z=z, kc=kc, ex=ex: e.dma_start(out=W2[z][:, kc, :], in_=g.exp_w2[ex, kc * 128:(kc + 1) * 128, :]), w=[W_b[z]], dma=True)
        kb.op("sp", lambda e, z=z, ex=ex: e.dma_start(out=b2t[z][:], in_=g.exp_b2[ex:ex + 1, :].broadcast_to([128, D])), w=[W_b[z]], dma=True)

    load_w(0)
    n = 0
    on = 0
    for ex in range(n_exp):
        z = ex % 2
        if ex + 1 < n_exp:
            load_w(ex + 1)
        for tb in range(NB):
            xs = n % 2
            n += 1
            kb.op("sp", lambda e, xs=xs, tb=tb: e.dma_start(out=xT[xs][:], in_=g.h1T[:, tb * 512:(tb + 1) * 512].rearrange("(kc p) t -> p kc t", p=128)),
                  w=[xT_b[xs]], dma=True)
            for fc in range(8):
                q = fc % 2
                pg, pl = ps[2 * q], ps[2 * q + 1]
                for which, pp, ppb in ((0, pg, ps_b[2 * q]), (1, pl, ps_b[2 * q + 1])):
                    for kc in range(KC):
                        kb.op("pe", lambda e, z=z, kc=kc, fc=fc, which=which, pp=pp, xs=xs: e.matmul(
                            pp[:, :], lhsT=W1[z][:, kc, fc * 256 + which:fc * 256 + 256:2], rhs=xT[xs][:, kc, :], start=(kc == 0), stop=(kc == KC - 1)),
                            r=[W_b[z], xT_b[xs]], w=[ppb])
                kb.op("dve", lambda e, q=q, pg=pg, ex=ex, fc=fc: e.tensor_scalar(out=xg[q][:], in0=pg[:, :], scalar1=b1t[:, ex, fc, 0:1], scalar2=7.0, op0=ALU.add, op1=ALU.min),
                      r=[ps_b[2 * q], cst], w=[el_b[q]])
                kb.op("act", lambda e, q=q: e.activation(out=sg[q][:], in_=xg[q][:], func=AF.Sigmoid, scale=1.702), r=[el_b[q]], w=[el_b[q]])
                kb.op("dve", lambda e, q=q, pl=pl, ex=ex, fc=fc: e.tensor_scalar(out=xl[q][:], in0=pl[:, :], scalar1=b1t[:, ex, fc, 1:2], scalar2=7.0, op0=ALU.add, op1=ALU.min),
                      r=[ps_b[2 * q + 1], cst], w=[el_b[q]])
                kb.op("dve", lambda e, q=q: e.tensor_scalar(out=xl[q][:], in0=xl[q][:], scalar1=-7.0, scalar2=1.0, op0=ALU.max, op1=ALU.add), r=[el_b[q]], w=[el_b[q]])
                kb.op("dve", lambda e, q=q: e.tensor_tensor(out=xg[q][:], in0=xg[q][:], in1=sg[q][:], op=ALU.mult), r=[el_b[q]], w=[el_b[q]])
                kb.op("dve", lambda e, q=q, xs=xs, fc=fc: e.tensor_tensor(out=actT[xs][:, fc, :], in0=xg[q][:], in1=xl[q][:], op=ALU.mult),
                      r=[el_b[q]], w=[actT_b[xs]])
            for tt in range(4):
                ti = tb * 4 + tt
                for hf in range(2):
                    pp = 4 + (on % 4)
                    oi = on % 3
                    on += 1
                    for fc in range(8):
                        kb.op("pe", lambda e, pp=pp, xs=xs, fc=fc, tt=tt, z=z, hf=hf: e.matmul(
                            ps[pp][:, :], lhsT=actT[xs][:, fc, tt * 128:(tt + 1) * 128], rhs=W2[z][:, fc, hf * 512:(hf + 1) * 512], start=(fc == 0), stop=(fc == 7)),
                            r=[actT_b[xs], W_b[z]], w=[ps_b[pp]])
                    kb.op("dve", lambda e, pp=pp, oi=oi, z=z, hf=hf: e.tensor_tensor(out=ot[oi][:], in0=ps[pp][:, :], in1=b2t[z][:, hf * 512:(hf + 1) * 512], op=ALU.add),
                          r=[ps_b[pp], W_b[z]], w=[ot_b[oi]])
                    kb.op("dve", lambda e, oi=oi, ti=ti, ex=ex: e.tensor_scalar(out=ot[oi][:], in0=ot[oi][:], scalar1=gates[:, ti, ex:ex + 1], scalar2=None, op0=ALU.mult),
                          r=[ot_b[oi], gates_b], w=[ot_b[oi]])
                    if ex == 0:
                        kb.op("pool", lambda e, oi=oi, ti=ti, hf=hf: e.dma_start(out=g.yacc[ti * 128:(ti + 1) * 128, hf * 512:(hf + 1) * 512], in_=ot[oi][:]),
                              r=[ot_b[oi]], w=[yacc_b[ti]], dma=True)
                    else:
                        kb.op("pool", lambda e, oi=oi, ti=ti, hf=hf: e.dma_start(out=g.yacc[ti * 128:(ti + 1) * 128, hf * 512:(hf + 1) * 512], in_=ot[oi][:], accum_op=ALU.add),
                              r=[ot_b[oi]], w=[yacc_b[ti]], dma=True)

    if "C" not in sections:
        return
    kb.end_phase(); kb.begin_phase()
    A = lambda n, s, d=F32: kb.sbuf("m4c_" + n, s, d)
    cst = kb.buf("cst4c")
    gam2 = A("gam2", [128, D]); bet2 = A("bet2", [128, D]); epsc = A("epsc", [128, 1])
    kb.op("dve", lambda e: e.memset(epsc[:], LN_EPS), w=[cst])
    kb.op("sp", lambda e: e.dma_start(out=gam2[:], in_=g.ln2_w.broadcast_to([128, D])), w=[cst], dma=True)
    kb.op("sp", lambda e: e.dma_start(out=bet2[:], in_=g.ln2_b.broadcast_to([128, D])), w=[cst], dma=True)
    h0t = [A(f"h0t{i}", [128, D]) for i in range(2)]; h0t_b = kb.bufs(2, "h0tc")
    sres = [A(f"sres{i}", [128, D]) for i in range(2)]; sres_b = kb.bufs(2, "sresc")
    h1t = [A(f"h1t{i}", [128, D]) for i in range(2)]; h1t_b = kb.bufs(2, "h1tc")
    st = A("st", [128, 2, 6]); mv = A("mv", [128, 2]); rs = A("rs", [128, 1]); st_b = kb.buf("stc")
    for i in range(NT):
        s = i % 2
        tsl = slice(i * 128, (i + 1) * 128)
        kb.op("sp", lambda e, s=s, tsl=tsl: e.dma_start(out=h0t[s][:], in_=g.h1f[tsl, :]), w=[h0t_b[s]], dma=True)
        kb.op("sp", lambda e, s=s, tsl=tsl: e.dma_start(out=h1t[s][:], in_=g.yacc[tsl, :]), r=[yacc_b[i]], w=[h1t_b[s]], dma=True)
        kb.op("dve", lambda e, s=s: e.scalar_tensor_tensor(out=sres[s][:], in0=h0t[s][:], scalar=ALPHA, in1=h1t[s][:], op0=ALU.mult, op1=ALU.add),
              r=[h0t_b[s], h1t_b[s]], w=[sres_b[s]])
        _ln_tile(kb, sres[s], sres_b[s], h1t[s], h1t_b[s], st, mv, rs, st_b, epsc, cst, gam2, bet2)
        kb.op("sp", lambda e, s=s, tsl=tsl: e.dma_start(out=g.out[tsl, :], in_=h1t[s][:]), r=[h1t_b[s]], dma=True)


def make_inputs(d, b, T):
    im = {"x": d['x'][b, :T], "ln_in_w": d['ln_in_w'][None], "ln_in_b": d['ln_in_b'][None], "w_in": d['w_in'][0],
          "fx_b_f": d['fx_b_f'][0][:, None], "fx_q_norm": d['fx_q_norm'][0][:, None], "fx_k_norm": d['fx_k_norm'][0][:, None],
          "w_o": d['w_o'][0], "ln1_w": d['ln1_w'], "ln1_b": d['ln1_b'], "ln2_w": d['ln2_w'], "ln2_b": d['ln2_b'],
          "router_w": d['router_w'][0], "router_b": d['router_b'], "exp_w1": d['exp_w1'][0], "exp_b1": d['exp_b1'][0],
          "exp_w2": d['exp_w2'][0], "exp_b2": d['exp_b2'][0]}
    im.update(consts_np()); im.update(consts2_np()); im.update(consts3_np(d))
    return {k: np.ascontiguousarray(v) for k, v in im.items()}


_T = 4096


def _build(T):
    nc = bass.Bass("TRN2", target_bir_lowering=False)
    g = setup(nc, T)
    setup2(nc, g); setup3(nc, g); setup4(nc, g)
    kb = KB(nc)
    kb.begin_phase(); phase01(kb, g); kb.end_phase()
    kb.begin_phase(); phase2(kb, g); kb.end_phase()
    kb.begin_phase(); phase3(kb, g); kb.end_phase()
    kb.begin_phase(); phase4(kb, g); kb.end_phase()
    kb.finish(); kb.close()
    return nc


def kernel(**inputs):
    d = {k: np.asarray(v) for k, v in inputs.items()}
    B = d["x"].shape[0]
    T = d["x"].shape[1]
    nc = _build(T)
    maps = [make_inputs(d, b % B, T) for b in range(8)]
    res = run_bass_kernel_spmd(nc, maps, core_ids=list(range(8)))
    out = np.stack([np.asarray(res.results[b]["out"]) for b in range(B)], axis=0)
    return out.astype(np.float32)
```

```python
import ml_dtypes
import contextlib
import numpy as np
import concourse.bass as bass
import concourse.mybir as mybir
from concourse.bass_utils import run_bass_kernel_spmd

F32 = mybir.dt.float32
BF16 = mybir.dt.bfloat16
I32 = mybir.dt.int32
U32 = mybir.dt.uint32
AF = mybir.ActivationFunctionType
ALU = mybir.AluOpType
AX = mybir.AxisListType

ENGS = ("pe", "act", "dve", "pool", "sp")


class Buf:
    __slots__ = ("name", "w", "rs")

    def __init__(self, name):
        self.name = name
        self.w = None
        self.rs = {}


class _Op:
    __slots__ = ("fn", "waits", "ev", "dma", "marked")

    def __init__(self, fn, waits, ev, dma):
        self.fn = fn
        self.waits = waits
        self.ev = ev
        self.dma = dma
        self.marked = False


class KB:
    def __init__(self, nc, n_dma_sems=20):
        self.nc = nc
        self.es = contextlib.ExitStack()
        self.q = {e: [] for e in ENGS}
        self.n_dma_sems = n_dma_sems
        self.dma_sems = {}
        self.dma_tgt = {}
        self.dma_rr = {}
        self.eng_sem = {}
        self.nbuf = 0
        self.allbufs = []
        self.base = {e: 0 for e in ENGS}
        self.barrier = []
        self.ps_stack = None
        for e in ENGS:
            self.eng_sem[e] = self.es.enter_context(nc.semaphore("es_" + e))
        for e in ("sp", "pool", "act"):
            self.dma_sems[e] = [self.es.enter_context(nc.semaphore(f"ds_{e}_{i}")) for i in range(n_dma_sems)]
            self.dma_tgt[e] = [0] * n_dma_sems
            self.dma_rr[e] = 0

    def begin_phase(self):
        self.ps_stack = contextlib.ExitStack()

    def end_phase(self):
        self.replay()
        self.ps_stack.close()
        self.ps_stack = None

    def sbuf(self, name, shape, dtype):
        st = self.ps_stack if self.ps_stack is not None else self.es
        return st.enter_context(self.nc.sbuf_tensor(name, list(shape), dtype))

    def psum(self, name, shape, dtype):
        st = self.ps_stack if self.ps_stack is not None else self.es
        return st.enter_context(self.nc.psum_tensor(name, list(shape), dtype))

    def buf(self, name=None):
        self.nbuf += 1
        b = Buf(name or f"b{self.nbuf}")
        self.allbufs.append(b)
        return b

    def bufs(self, n, name="b"):
        return [self.buf(f"{name}{i}") for i in range(n)]

    def op(self, eng, fn, r=(), w=(), dma=False):
        waits = []
        if not self.q[eng] and self.barrier:
            waits.extend(self.barrier)
        for b in r:
            if b.w is not None:
                waits.append(b.w)
        for b in w:
            if b.w is not None:
                waits.append(b.w)
            waits.extend(b.rs.values())
        if dma:
            i = self.dma_rr[eng]
            self.dma_rr[eng] = (i + 1) % self.n_dma_sems
            prev = self.dma_tgt[eng][i]
            if prev > 0:
                waits.append(("d", eng, i, prev))
            tgt = prev + 16
            self.dma_tgt[eng][i] = tgt
            ev = ("d", eng, i, tgt)
        else:
            ev = ("c", eng, len(self.q[eng]))
        self.q[eng].append(_Op(fn, waits, ev, dma))
        key = ev[:3] if ev[0] == "d" else ev[:2]
        for b in r:
            b.rs[key] = ev
        for b in w:
            b.w = ev
            b.rs = {}
        return ev

    def replay(self):
        nc = self.nc
        for e in ENGS:
            if self.q[e]:
                self.q[e][-1].marked = True
            for o in self.q[e]:
                for ev in o.waits:
                    if ev[0] == "c":
                        if ev[1] == "pe" and e == "pe":
                            continue
                        self.q[ev[1]][ev[2]].marked = True
        cnt = {}
        for e in ENGS:
            c = self.base[e]
            arr = []
            for o in self.q[e]:
                if o.marked and not o.dma:
                    c += 1
                arr.append(c)
            cnt[e] = arr

        def resolve(ev):
            if ev[0] == "c":
                return ("c", ev[1]), self.eng_sem[ev[1]], cnt[ev[1]][ev[2]]
            if ev[0] == "a":
                return ("c", ev[1]), self.eng_sem[ev[1]], ev[2]
            return ("d", ev[1], ev[2]), self.dma_sems[ev[1]][ev[2]], ev[3]

        if not hasattr(self, "seen"):
            self.seen = {e: {} for e in ENGS}

        def run_engine(ename, eobj):
            seen = self.seen[ename]
            for o in self.q[ename]:
                need = {}
                for ev in o.waits:
                    if ev[0] in ("c", "a") and ev[1] == "pe" and ename == "pe":
                        continue
                    k, sem, val = resolve(ev)
                    if seen.get(k, 0) >= val:
                        continue
                    if k not in need or need[k][1] < val:
                        need[k] = (sem, val)
                for k, (sem, val) in need.items():
                    eobj.wait_ge(sem, val)
                    seen[k] = val
                ins = o.fn(eobj)
                if o.dma:
                    ins.then_inc(self.dma_sems[o.ev[1]][o.ev[2]], 16)
                elif o.marked:
                    ins.then_inc(self.eng_sem[ename], 1)

        with nc.Block() as block:
            @block.tensor
            def _(t):
                run_engine("pe", t)

            @block.scalar
            def _(s):
                run_engine("act", s)

            @block.vector
            def _(v):
                run_engine("dve", v)

            @block.gpsimd
            def _(g):
                run_engine("pool", g)

            @block.sync
            def _(sy):
                run_engine("sp", sy)

        def absolutize(ev):
            if ev[0] == "c":
                return ("a", ev[1], cnt[ev[1]][ev[2]] if self.q[ev[1]][ev[2]].marked else cnt[ev[1]][-1])
            return ev
        for b in self.allbufs:
            if b.w is not None:
                b.w = absolutize(b.w)
            b.rs = {k: absolutize(v) for k, v in b.rs.items()}
        for e in ENGS:
            if self.q[e]:
                self.base[e] = cnt[e][-1]
            self.q[e] = []
        bar = [("a", e, self.base[e]) for e in ENGS if self.base[e] > 0]
        for e in ("sp", "pool", "act"):
            for i, t in enumerate(self.dma_tgt[e]):
                if t > 0:
                    bar.append(("d", e, i, t))
        self.barrier = bar

    def finish(self):
        nc = self.nc
        bar = list(self.barrier)
        with nc.Block() as block:
            @block.sync
            def _(sy):
                for ev in bar:
                    if ev[0] == "a":
                        sy.wait_ge(self.eng_sem[ev[1]], ev[2])
                    else:
                        sy.wait_ge(self.dma_sems[ev[1]][ev[2]], ev[3])

    def close(self):
        self.es.close()


D = 1024
KC = 8
RW_COLS = 1696
FX0 = RW_COLS
IN_COLS = 3752
LN_EPS = 1e-5
ALPHA = 2 ** 0.25


def col_tiles():
    t = []
    for nm, base in (("r", 0), ("k", 512), ("v", 1024)):
        for h in range(8):
            t.append((f"rw_{nm}{h}", base + 64 * h, 64))
    t.append(("rw_wa", 1536, 64))
    t.append(("rw_g", 1600, 96))
    for nm, base in (("q", 0), ("k", 512), ("og", 1536)):
        for h in range(8):
            t.append((f"fx_{nm}{h}", FX0 + base + 64 * h, 64))
    t.append(("fx_fz", FX0 + 2048, 8))
    return t


def consts_np():
    c = {}
    c["ident_f"] = np.eye(128, dtype=np.float32)
    c["ident_b"] = np.eye(128).astype(ml_dtypes.bfloat16)
    return c


class Ctx:
    pass


def setup(nc, T, ext_out=()):
    g = Ctx()
    g.nc = nc
    g.T = T
    g.NT = T // 128
    g.NB = T // 512
    ei = lambda n, s, d=F32: nc.dram_tensor(n, list(s), d, kind="ExternalInput").ap()
    g.x = ei("x", [T, D])
    g.ln_in_w = ei("ln_in_w", [1, D])
    g.ln_in_b = ei("ln_in_b", [1, D])
    g.w_in = ei("w_in", [D, IN_COLS])
    g.ident_f = ei("ident_f", [128, 128])
    g.ident_b = ei("ident_b", [128, 128], BF16)

    def scratch(n, s, d=F32):
        kind = "ExternalOutput" if n in ext_out else "Internal"
        return nc.dram_tensor(n, list(s), d, kind=kind).ap()

    g.scratch = scratch
    g.h0f = scratch("h0f", [T, D])
    g.pT = scratch("pT", [IN_COLS, T])
    g.vtok = scratch("vtok", [T, 512], BF16)
    return g


def phase01(kb, g):
    nc, T, NT, NB = g.nc, g.T, g.NT, g.NB
    h0T = kb.sbuf("h0T", [128, KC, T], BF16)
    h0T_b = kb.bufs(NT, "h0T")
    wbf = kb.sbuf("wbf", [128, KC, IN_COLS], BF16)
    wbf_b = kb.bufs(KC, "wbf")
    gam = kb.sbuf("gam", [128, D], F32)
    bet = kb.sbuf("bet", [128, D], F32)
    gb_b = kb.buf("gb")
    identf = kb.sbuf("identf", [128, 128], F32)
    id_b = kb.buf("id")
    epsc = kb.sbuf("epsc", [128, 1], F32)
    eps_b = kb.buf("eps")
    xt = [kb.sbuf(f"xt{i}", [128, D], F32) for i in range(2)]
    xt_b = kb.bufs(2, "xt")
    hn = [kb.sbuf(f"hn{i}", [128, D], F32) for i in range(2)]
    hn_b = kb.bufs(2, "hn")
    st = [kb.sbuf(f"st{i}", [128, 2, 6], F32) for i in range(2)]
    mv = [kb.sbuf(f"mv{i}", [128, 2], F32) for i in range(2)]
    rs = [kb.sbuf(f"rs{i}", [128, 1], F32) for i in range(2)]
    st_b = kb.bufs(2, "st")
    ps = [kb.psum(f"ps{i}", [128, 512], F32) for i in range(4)]
    ps_b = kb.bufs(4, "ps")
    stage = [kb.sbuf(f"stage{i}", [128, 512], F32) for i in range(3)]
    stage_b = kb.bufs(3, "stage")
    vst = [kb.sbuf(f"vst{i}", [128, 512], BF16) for i in range(2)]
    vst_b = kb.bufs(2, "vst")

    kb.op("sp", lambda e: e.dma_start(out=gam[:], in_=g.ln_in_w.broadcast_to([128, D])), w=[gb_b], dma=True)
    kb.op("sp", lambda e: e.dma_start(out=bet[:], in_=g.ln_in_b.broadcast_to([128, D])), w=[gb_b], dma=True)
    kb.op("sp", lambda e: e.dma_start(out=identf[:], in_=g.ident_f), w=[id_b], dma=True)
    kb.op("pool", lambda e: e.memset(epsc[:], LN_EPS), w=[eps_b])
    half = IN_COLS // 2
    for kc in range(KC):
        for hf in range(2):
            kb.op("pool", lambda e, kc=kc, hf=hf: e.dma_start(
                out=wbf[:, kc, hf * half:(hf + 1) * half],
                in_=g.w_in[kc * 128:(kc + 1) * 128, hf * half:(hf + 1) * half]),
                w=[wbf_b[kc]], dma=True)

    for i in range(NT):
        s = i % 2
        kb.op("sp", lambda e, i=i, s=s: e.dma_start(out=xt[s][:], in_=g.x[i * 128:(i + 1) * 128, :]), w=[xt_b[s]], dma=True)
        for hf in range(2):
            kb.op("dve", lambda e, s=s, hf=hf: e.bn_stats(out=st[s][:, hf, :], in_=xt[s][:, hf * 512:(hf + 1) * 512]),
                  r=[xt_b[s]], w=[st_b[s]])
        kb.op("dve", lambda e, s=s: e.bn_aggr(out=mv[s][:], in_=st[s][:].rearrange("p a b -> p (a b)")), r=[st_b[s]], w=[st_b[s]])
        kb.op("act", lambda e, s=s: e.activation(out=rs[s][:], in_=mv[s][:, 1:2], func=AF.Sqrt, bias=epsc[:], scale=1.0),
              r=[st_b[s], eps_b], w=[st_b[s]])
        kb.op("dve", lambda e, s=s: e.reciprocal(out=rs[s][:], in_=rs[s][:]), r=[st_b[s]], w=[st_b[s]])
        kb.op("dve", lambda e, s=s: e.tensor_scalar(out=hn[s][:], in0=xt[s][:], scalar1=mv[s][:, 0:1], scalar2=rs[s][:],
                                                    op0=ALU.subtract, op1=ALU.mult),
              r=[xt_b[s], st_b[s]], w=[hn_b[s]])
        kb.op("dve", lambda e, s=s: e.tensor_tensor(out=hn[s][:], in0=hn[s][:], in1=gam[:], op=ALU.mult), r=[hn_b[s], gb_b], w=[hn_b[s]])
        kb.op("pool", lambda e, s=s: e.tensor_tensor(out=hn[s][:], in0=hn[s][:], in1=bet[:], op=ALU.add), r=[hn_b[s], gb_b], w=[hn_b[s]])
        kb.op("sp", lambda e, i=i, s=s: e.dma_start(out=g.h0f[i * 128:(i + 1) * 128, :], in_=hn[s][:]), r=[hn_b[s]], dma=True)
        for hf in range(2):
            p = hf
            for c in range(4):
                kc = hf * 4 + c
                kb.op("pe", lambda e, s=s, p=p, c=c, kc=kc: e.transpose(out=ps[p][:, c * 128:(c + 1) * 128],
                                                                       in_=hn[s][:, kc * 128:(kc + 1) * 128], identity=identf[:]),
                      r=[hn_b[s], id_b], w=[ps_b[p]])
            eng = "act" if hf == 0 else "dve"
            if eng == "act":
                kb.op("act", lambda e, i=i, p=p, hf=hf: e.copy(out=h0T[:, hf * 4:(hf + 1) * 4, i * 128:(i + 1) * 128],
                                                               in_=ps[p][:].rearrange("p (c t) -> p c t", c=4)),
                      r=[ps_b[p]], w=[h0T_b[i]])
            else:
                kb.op("dve", lambda e, i=i, p=p, hf=hf: e.tensor_copy(out=h0T[:, hf * 4:(hf + 1) * 4, i * 128:(i + 1) * 128],
                                                                      in_=ps[p][:].rearrange("p (c t) -> p c t", c=4)),
                      r=[ps_b[p]], w=[h0T_b[i]])

    n = 0
    for (nm, c0, ncol) in col_tiles():
        for tb in range(NB):
            p = 2 + (n % 2)
            sg = n % 3
            for kc in range(KC):
                kb.op("pe", lambda e, p=p, kc=kc, c0=c0, ncol=ncol, tb=tb: e.matmul(
                    ps[p][0:ncol, :], lhsT=wbf[:, kc, c0:c0 + ncol], rhs=h0T[:, kc, tb * 512:(tb + 1) * 512],
                    start=(kc == 0), stop=(kc == KC - 1)),
                    r=[wbf_b[kc]] + h0T_b[tb * 4:(tb + 1) * 4], w=[ps_b[p]])
            if n % 2 == 0:
                kb.op("act", lambda e, p=p, sg=sg, ncol=ncol: e.copy(out=stage[sg][0:ncol, :], in_=ps[p][0:ncol, :]),
                      r=[ps_b[p]], w=[stage_b[sg]])
            else:
                kb.op("dve", lambda e, p=p, sg=sg, ncol=ncol: e.tensor_copy(out=stage[sg][0:ncol, :], in_=ps[p][0:ncol, :]),
                      r=[ps_b[p]], w=[stage_b[sg]])
            kb.op("sp", lambda e, sg=sg, c0=c0, ncol=ncol, tb=tb: e.dma_start(
                out=g.pT[c0:c0 + ncol, tb * 512:(tb + 1) * 512], in_=stage[sg][0:ncol, :]),
                r=[stage_b[sg]], dma=True)
            n += 1
    vc0 = FX0 + 1024
    for i in range(NT):
        p = 2 + (i % 2)
        s = i % 2
        for kc in range(KC):
            kb.op("pe", lambda e, p=p, kc=kc, i=i: e.matmul(
                ps[p][:, :], lhsT=h0T[:, kc, i * 128:(i + 1) * 128], rhs=wbf[:, kc, vc0:vc0 + 512],
                start=(kc == 0), stop=(kc == KC - 1)),
                r=[wbf_b[kc], h0T_b[i]], w=[ps_b[p]])
        kb.op("act", lambda e, p=p, s=s: e.copy(out=vst[s][:], in_=ps[p][:]), r=[ps_b[p]], w=[vst_b[s]])
        kb.op("sp", lambda e, s=s, i=i: e.dma_start(out=g.vtok[i * 128:(i + 1) * 128, :], in_=vst[s][:]), r=[vst_b[s]], dma=True)


def setup2(nc, g):
    ei = lambda n, s, d=F32: nc.dram_tensor(n, list(s), d, kind="ExternalInput").ap()
    g.fx_b_f = ei("fx_b_f", [8, 1])
    g.fx_q_norm = ei("fx_q_norm", [64, 1])
    g.fx_k_norm = ei("fx_k_norm", [64, 1])
    g.tri = ei("tri", [128, 128], BF16)
    g.yT = g.scratch("yT", [1024, g.T], BF16)


def consts2_np():
    c = {}
    k = np.arange(128)[:, None]
    q = np.arange(128)[None, :]
    c["tri"] = (k <= q).astype(np.float32).astype(ml_dtypes.bfloat16)
    return c


def phase2(kb, g):
    nc, T, NT, NB = g.nc, g.T, g.NT, g.NB
    onesf = kb.sbuf("onesf", [128, 128], F32)
    identf = kb.sbuf("identf2", [128, 128], F32)
    tri = kb.sbuf("tri_sb", [128, 128], BF16)
    cst_b = kb.buf("cst2")
    epsq = kb.sbuf("epsq", [128, 1], F32)
    negb = kb.sbuf("negb", [8, 1], F32)
    qw = kb.sbuf("qw", [64, 1], F32)
    kw = kb.sbuf("kw", [64, 1], F32)
    fz = kb.sbuf("fz", [8, T], F32)
    cp = kb.sbuf("cp", [8, T], F32)
    ones8 = kb.sbuf("ones8", [8, T], F32)
    cq8 = kb.sbuf("cq8", [8, T], F32)
    fz_b = kb.buf("fz")
    cq8_b = kb.buf("cq8")
    negc = kb.sbuf("negc", [128, NT, 8], F32)
    negc_b = kb.buf("negc")
    qrow = kb.sbuf("qrow", [65, T], F32)
    qrow_b = kb.buf("qrow")
    qraw = kb.sbuf("qraw", [64, T], F32)
    kraw = kb.sbuf("kraw", [64, T], F32)
    sq = kb.sbuf("sq", [64, T], F32)
    raw_b = {"q": kb.buf("qraw"), "k": kb.buf("kraw")}
    sq_b = kb.buf("sq")
    ograw = kb.sbuf("ograw", [64, T], F32)
    og_b = kb.buf("ograw")
    sg = kb.sbuf("sg", [64, T], BF16)
    sg_b = kb.buf("sg")
    Qa = kb.sbuf("Qa", [65, T], BF16)
    Ka = kb.sbuf("Ka", [65, T], BF16)
    Qa_b = kb.buf("Qa")
    Ka_b = kb.buf("Ka")
    Va = kb.sbuf("Va", [128, NT, 65], BF16)
    Va_b = kb.buf("Va")
    rst = [kb.sbuf(f"rst{i}", [64, 512], F32) for i in range(2)]
    rst_b = kb.bufs(2, "rst")
    PT = [kb.sbuf(f"PT{i}", [128, 512], BF16) for i in range(3)]
    PT_b = kb.bufs(3, "PT")
    dn = kb.sbuf("dn", [65, 512], F32)
    dn_b = kb.buf("dn")
    bcs = kb.sbuf("bcs", [64, 512], F32)
    bcs_b = kb.buf("bcs")
    o1 = kb.sbuf("o1", [64, 512], F32)
    o1_b = kb.buf("o1")
    yfx = [kb.sbuf(f"yfx{i}", [64, 512], BF16) for i in range(2)]
    yfx_b = kb.bufs(2, "yfx")
    psS = [kb.psum(f"psS{i}", [128, 512], F32) for i in range(2)]
    psS_b = kb.bufs(2, "psS")
    psO = [kb.psum(f"psO{i}", [128, 512], F32) for i in range(2)]
    psO_b = kb.bufs(2, "psO")
    psB = kb.psum("psB", [128, 512], F32)
    psB_b = kb.buf("psB")
    psN = [kb.psum(f"psN{i}", [128, 512], F32) for i in range(2)]
    psN_b = kb.bufs(2, "psN")

    kb.op("pool", lambda e: e.memset(onesf[:], 1.0), w=[cst_b])
    kb.op("pool", lambda e: e.memset(ones8[:], 1.0), w=[cst_b])
    kb.op("pool", lambda e: e.memset(epsq[:], 1e-6), w=[cst_b])
    kb.op("pool", lambda e: e.memset(Ka[64:65, :], 1.0), w=[Ka_b])
    kb.op("pool", lambda e: e.memset(Va[:, :, 64:65], 1.0), w=[Va_b])
    kb.op("sp", lambda e: e.dma_start(out=identf[:], in_=g.ident_f), w=[cst_b], dma=True)
    kb.op("sp", lambda e: e.dma_start(out=tri[:], in_=g.tri), w=[cst_b], dma=True)
    kb.op("sp", lambda e: e.dma_start(out=negb[:], in_=g.fx_b_f), w=[cst_b], dma=True)
    kb.op("sp", lambda e: e.dma_start(out=qw[:], in_=g.fx_q_norm), w=[cst_b], dma=True)
    kb.op("sp", lambda e: e.dma_start(out=kw[:], in_=g.fx_k_norm), w=[cst_b], dma=True)
    kb.op("dve", lambda e: e.tensor_scalar(out=negb[:], in0=negb[:], scalar1=-1.0, scalar2=None, op0=ALU.mult), r=[cst_b], w=[cst_b])
    fzr = FX0 + 2048
    kb.op("sp", lambda e: e.dma_start(out=fz[:], in_=g.pT[fzr:fzr + 8, :]), w=[fz_b], dma=True)
    kb.op("act", lambda e: e.activation(out=fz[:], in_=fz[:], func=AF.Exp, bias=negb[:], scale=-1.0), r=[fz_b, cst_b], w=[fz_b])
    kb.op("act", lambda e: e.activation(out=fz[:], in_=fz[:], func=AF.Ln, bias=1.0, scale=1.0), r=[fz_b], w=[fz_b])
    kb.op("dve", lambda e: e.tensor_tensor_scan(out=cp[:], data0=ones8[:], data1=fz[:], initial=0.0, op0=ALU.mult, op1=ALU.add),
          r=[fz_b, cst_b], w=[cq8_b])
    kb.op("dve", lambda e: e.tensor_scalar(out=cq8[:], in0=cp[:], scalar1=-8.0, scalar2=None, op0=ALU.mult), r=[cq8_b], w=[cq8_b])
    for i in range(NT):
        kb.op("pe", lambda e, i=i: e.transpose(out=psB[:, i * 8:(i + 1) * 8], in_=cp[:, i * 128:(i + 1) * 128], identity=identf[0:8, 0:8]),
              r=[cq8_b, cst_b], w=[psB_b])
    kb.op("act", lambda e: e.copy(out=negc[:].rearrange("p a b -> p (a b)"), in_=psB[:, 0:NT * 8]), r=[psB_b], w=[negc_b])

    for h in range(8):
        kb.op("sp", lambda e, h=h: e.dma_start(out=qraw[:], in_=g.pT[FX0 + 64 * h:FX0 + 64 * h + 64, :]), w=[raw_b["q"]], dma=True)
        kb.op("sp", lambda e, h=h: e.dma_start(out=kraw[:], in_=g.pT[FX0 + 512 + 64 * h:FX0 + 512 + 64 * h + 64, :]), w=[raw_b["k"]], dma=True)
        kb.op("sp", lambda e, h=h: e.dma_start(out=ograw[:], in_=g.pT[FX0 + 1536 + 64 * h:FX0 + 1536 + 64 * h + 64, :]), w=[og_b], dma=True)
        kb.op("sp", lambda e, h=h: e.dma_start(out=Va[:, :, 0:64], in_=g.vtok[:, 64 * h:64 * h + 64].rearrange("(j p) d -> p j d", p=128)),
              w=[Va_b], dma=True)
        kb.op("sp", lambda e, h=h: e.dma_start(out=qrow[64:65, :], in_=cq8[h:h + 1, :]), r=[cq8_b], w=[qrow_b], dma=True)
        kb.op("act", lambda e: e.copy(out=Qa[64:65, :], in_=qrow[64:65, :]), r=[qrow_b], w=[Qa_b])
        kb.op("act", lambda e: e.activation(out=sg[:], in_=ograw[:], func=AF.Sigmoid), r=[og_b], w=[sg_b])
        n = 0
        for nm, raw, wcol, dst, dst_b in (("q", qraw, qw, Qa, Qa_b), ("k", kraw, kw, Ka, Ka_b)):
            kb.op("act", lambda e, raw=raw: e.activation(out=sq[:], in_=raw[:], func=AF.Square), r=[raw_b[nm]], w=[sq_b])
            for tb in range(NB):
                p = n % 2
                n += 1
                sl = slice(tb * 512, (tb + 1) * 512)
                kb.op("pe", lambda e, p=p, sl=sl: e.matmul(psN[p][0:64, :], lhsT=onesf[0:64, 0:64], rhs=sq[:, sl], start=True, stop=True),
                      r=[sq_b, cst_b], w=[psN_b[p]])
                kb.op("act", lambda e, p=p: e.activation(out=rst[p][:], in_=psN[p][0:64, :], func=AF.Sqrt, bias=epsq[0:64, :], scale=1.0 / 64),
                      r=[psN_b[p], cst_b], w=[rst_b[p]])
                kb.op("dve", lambda e, p=p: e.reciprocal(out=rst[p][:], in_=rst[p][:]), r=[rst_b[p]], w=[rst_b[p]])
                kb.op("dve", lambda e, p=p, sl=sl, raw=raw, wcol=wcol, dst=dst: e.scalar_tensor_tensor(
                    out=dst[0:64, sl], in0=raw[:, sl], scalar=wcol[:, 0:1], in1=rst[p][:], op0=ALU.mult, op1=ALU.mult),
                    r=[raw_b[nm], rst_b[p], cst_b], w=[dst_b])
        it = 0
        for gq in range(NB):
            po = gq % 2
            jmax = 4 * gq + 3
            for j in range(jmax + 1):
                col0 = max(0, j - 4 * gq) * 128
                ps_i = it % 2
                pt_i = it % 3
                it += 1
                kb.op("pe", lambda e, ps_i=ps_i, j=j, gq=gq, col0=col0: e.matmul(
                    psS[ps_i][:, col0:512], lhsT=Ka[0:65, j * 128:(j + 1) * 128], rhs=Qa[0:65, gq * 512 + col0:(gq + 1) * 512],
                    start=True, stop=True), r=[Ka_b, Qa_b], w=[psS_b[ps_i]])
                kb.op("act", lambda e, ps_i=ps_i, pt_i=pt_i, j=j, h=h, col0=col0: e.activation(
                    out=PT[pt_i][:, col0:512], in_=psS[ps_i][:, col0:512], func=AF.Exp, bias=negc[:, j, h:h + 1], scale=0.125),
                    r=[psS_b[ps_i], negc_b], w=[PT_b[pt_i]])
                if j >= 4 * gq:
                    kb.op("pool", lambda e, pt_i=pt_i, col0=col0: e.tensor_tensor(
                        out=PT[pt_i][:, col0:col0 + 128], in0=PT[pt_i][:, col0:col0 + 128], in1=tri[:], op=ALU.mult),
                        r=[PT_b[pt_i], cst_b], w=[PT_b[pt_i]])
                kb.op("pe", lambda e, po=po, pt_i=pt_i, j=j, col0=col0, jmax=jmax: e.matmul(
                    psO[po][0:65, col0:512], lhsT=Va[:, j, 0:65], rhs=PT[pt_i][:, col0:512],
                    start=(j == 0), stop=(j == jmax), skip_group_check=True), r=[Va_b, PT_b[pt_i]], w=[psO_b[po]])
            kb.op("act", lambda e, po=po: e.copy(out=dn[64:65, :], in_=psO[po][64:65, :]), r=[psO_b[po]], w=[dn_b])
            kb.op("dve", lambda e: e.reciprocal(out=dn[64:65, :], in_=dn[64:65, :]), r=[dn_b], w=[dn_b])
            kb.op("pe", lambda e: e.matmul(psB[0:64, :], lhsT=onesf[64:65, 0:64], rhs=dn[64:65, :], start=True, stop=True),
                  r=[dn_b, cst_b], w=[psB_b])
            kb.op("act", lambda e: e.copy(out=bcs[:], in_=psB[0:64, :]), r=[psB_b], w=[bcs_b])
            kb.op("dve", lambda e, po=po: e.tensor_tensor(out=o1[:], in0=psO[po][0:64, :], in1=bcs[:], op=ALU.mult),
                  r=[psO_b[po], bcs_b], w=[o1_b])
            yi = gq % 2
            kb.op("pool", lambda e, yi=yi, gq=gq: e.tensor_tensor(out=yfx[yi][:], in0=o1[:], in1=sg[:, gq * 512:(gq + 1) * 512], op=ALU.mult),
                  r=[o1_b, sg_b], w=[yfx_b[yi]])
            kb.op("sp", lambda e, yi=yi, gq=gq, h=h: e.dma_start(out=g.yT[512 + 64 * h:512 + 64 * h + 64, gq * 512:(gq + 1) * 512], in_=yfx[yi][:]),
                  r=[yfx_b[yi]], dma=True)


CW = 0.6065306597126334


def setup3(nc, g):
    ei = lambda n, s, d=F32: nc.dram_tensor(n, list(s), d, kind="ExternalInput").ap()
    g.rw_mu = ei("rw_mu", [RW_COLS, 1])
    g.rw_w2a2 = ei("rw_w2a2", [64, 512])
    g.rw_g2 = ei("rw_g2", [96, 512])
    g.rw_cols = ei("rw_cols", [64, 8, 8])
    g.maskG = ei("maskG", [64, 320])
    g.resetm = ei("resetm", [64, 512])


def consts3_np(d):
    c = {}
    i = np.arange(64)[:, None]
    t = np.arange(64)[None, :]
    SU = (i < t).astype(np.float32)
    U = (i <= t).astype(np.float32)
    SL = (i > t).astype(np.float32)
    c["maskG"] = np.concatenate([SU, U, SU, U, SL], axis=1)
    rm = np.ones((64, 512), np.float32)
    rm[:, ::64] = 0.0
    c["resetm"] = rm
    c["rw_mu"] = d["rw_mu"][0][:, None]
    c["rw_w2a2"] = np.concatenate([d["rw_w2"][0], d["rw_a2"][0]], axis=0)
    c["rw_g2"] = d["rw_g2"][0]
    cols = np.zeros((64, 8, 8), np.float32)
    for j, nm in enumerate(["rw_w0", "rw_a0", "rw_k_k", "rw_k_a", "rw_r_k", "rw_gn_w", "rw_gn_b"]):
        cols[:, :, j] = d[nm][0].reshape(8, 64).T
    c["rw_cols"] = cols
    return c


def phase3(kb, g):
    nc, T, NT, NB = g.nc, g.T, g.NT, g.NB
    A = lambda n, s, d=F32: kb.sbuf("r3_" + n, s, d)
    onesf = A("onesf", [64, 64]); identb = A("identb", [64, 64], BF16); identf = A("identf", [64, 64])
    maskG = A("maskG", [64, 320]); resetm = A("resetm", [64, 512])
    w2a2 = A("w2a2", [32, 512]); a2t = A("a2t", [32, 512]); g2 = A("g2", [96, 512]); cols = A("cols", [64, 8, 8])
    mu_rkv = A("mu_rkv", [64, 24]); mu_wa = A("mu_wa", [32, 1]); mu_ad = A("mu_ad", [32, 1]); mu_g = A("mu_g", [96, 1])
    eps12 = A("eps12", [64, 1]); epsgn = A("epsgn", [64, 1])
    cst = kb.buf("cst3")
    kb.op("pool", lambda e: e.memset(onesf[:], 1.0), w=[cst])
    kb.op("pool", lambda e: e.memset(eps12[:], 0.0), w=[cst])
    kb.op("pool", lambda e: e.memset(epsgn[:], 64e-5), w=[cst])
    kb.op("sp", lambda e: e.dma_start(out=identb[:], in_=g.ident_b[0:64, 0:64]), w=[cst], dma=True)
    kb.op("sp", lambda e: e.dma_start(out=identf[:], in_=g.ident_f[0:64, 0:64]), w=[cst], dma=True)
    kb.op("sp", lambda e: e.dma_start(out=maskG[:], in_=g.maskG), w=[cst], dma=True)
    kb.op("sp", lambda e: e.dma_start(out=resetm[:], in_=g.resetm), w=[cst], dma=True)
    kb.op("sp", lambda e: e.dma_start(out=w2a2[:], in_=g.rw_w2a2[0:32, :]), w=[cst], dma=True)
    kb.op("sp", lambda e: e.dma_start(out=a2t[:], in_=g.rw_w2a2[32:64, :]), w=[cst], dma=True)
    kb.op("sp", lambda e: e.dma_start(out=g2[:], in_=g.rw_g2), w=[cst], dma=True)
    kb.op("sp", lambda e: e.dma_start(out=cols[:], in_=g.rw_cols), w=[cst], dma=True)
    for j in range(24):
        kb.op("sp", lambda e, j=j: e.dma_start(out=mu_rkv[:, j:j + 1], in_=g.rw_mu[64 * j:64 * j + 64, :]), w=[cst], dma=True)
    kb.op("sp", lambda e: e.dma_start(out=mu_wa[:], in_=g.rw_mu[1536:1568, :]), w=[cst], dma=True)
    kb.op("sp", lambda e: e.dma_start(out=mu_ad[:], in_=g.rw_mu[1568:1600, :]), w=[cst], dma=True)
    kb.op("sp", lambda e: e.dma_start(out=mu_g[:], in_=g.rw_mu[1600:1696, :]), w=[cst], dma=True)

    NS = 2
    cur = [A(f"cur{i}", [96, 512]) for i in range(3)]; prv = [A(f"prv{i}", [96, 512]) for i in range(3)]
    ld_b = kb.bufs(3, "ld")
    ldn = [0]
    wam = A("wam", [32, 512]); adm = A("adm", [32, 512]); gdm = A("gdm", [96, 512]); wam_b = kb.buf("wam"); adm_b = kb.buf("adm"); gdm_b = kb.buf("gdm")
    rm = A("rm", [64, 512]); km = A("km", [64, 512]); vm = A("vm", [64, 512])
    rm_b = kb.buf("rm"); km_b = kb.buf("km"); vm_b = kb.buf("vm")
    sgw = A("sgw", [64, 512]); asig = A("asig", [64, 512]); gg = A("gg", [64, 512])
    sgw_b = kb.buf("sgw"); asig_b = kb.buf("asig"); gg_b = kb.buf("gg")
    kk = A("kk", [64, 512]); t1 = A("t1", [64, 512]); t2 = A("t2", [64, 512]); kmod = A("kmod", [64, 512]); bb = A("bb", [64, 512])
    kk_b = kb.buf("kk"); t1_b = kb.buf("t1"); t2_b = kb.buf("t2"); kmod_b = kb.buf("kmod"); bb_b = kb.buf("bb")
    cumS = A("cumS", [64, 512]); gincl = A("gincl", [64, 512]); ginv = A("ginv", [64, 512]); gexcl = A("gexcl", [64, 512])
    cum_b = kb.buf("cum"); gincl_b = kb.buf("gincl"); ginv_b = kb.buf("ginv"); gexcl_b = kb.buf("gexcl")
    bon = A("bon", [64, 512]); bon_b = kb.buf("bon")
    AR = [A(f"AR{i}", [64, 8, 128], BF16) for i in range(2)]; BK = [A(f"BK{i}", [64, 8, 128], BF16) for i in range(2)]
    vb = [A(f"vb{i}", [64, 512], BF16) for i in range(2)]
    AR_b = kb.bufs(2, "AR"); BK_b = kb.bufs(2, "BK"); vb_b = kb.bufs(2, "vb")
    TM = [A(f"TM{i}", [64, 320], BF16) for i in range(NS)]; TM_b = kb.bufs(NS, "TM"); TMx_b = kb.bufs(NS, "TMx")
    NM = [A(f"NM{i}", [64, 320], BF16) for i in range(NS)]; NM_b = kb.bufs(NS, "NM")
    DB = [[A(f"DB{i}_{j}", [64, 192], BF16) for j in range(2)] for i in range(NS)]
    DB_b = [kb.bufs(2, f"DB{i}") for i in range(NS)]
    AW = [A(f"AW{i}", [64, 128], BF16) for i in range(NS)]; AW_b = kb.bufs(NS, "AW")
    G1 = [A(f"G1{i}", [64, 64]) for i in range(NS)]; G1_b = kb.bufs(NS, "G1")
    Hg = [A(f"Hg{i}", [64, 64]) for i in range(NS)]; Hg_b = kb.bufs(NS, "Hg")
    Ry = [A(f"Ry{i}", [64, 64]) for i in range(NS)]; Ry_b = kb.bufs(NS, "Ry")
    ST = [[A(f"ST{h}_{j}", [64, 64]) for j in range(2)] for h in range(8)]
    ST_b = [kb.bufs(2, f"ST{h}") for h in range(8)]
    ysb = A("ysb", [64, 512]); ysq = A("ysq", [64, 512]); ymean = A("ymean", [64, 512]); yvar = A("yvar", [64, 512])
    ysb_b = kb.buf("ysb"); ysq_b = kb.buf("ysq"); ymean_b = kb.buf("ymean"); yvar_b = kb.buf("yvar")
    yout = [A(f"yout{i}", [64, 512], BF16) for i in range(2)]; yout_b = kb.bufs(2, "yout")
    tw = A("tw", [32, 512]); sgd = A("sgd", [96, 512]); tw_b = kb.buf("tw"); sgd_b = kb.buf("sgd")
    psT = kb.psum("r3psT", [128, 1024], BF16); psT_b = kb.buf("psT")
    psG = kb.psum("r3psG", [128, 512], F32); psG_b = kb.buf("psG")
    psD = [kb.psum(f"r3psD{i}", [128, 512], F32) for i in range(2)]; psD_b = kb.bufs(2, "psD")
    psA = kb.psum("r3psA", [128, 512], F32)
    psX_b = kb.buf("psA"); psAW_b = psX_b; psGp_b = psX_b; psH_b = psX_b; psR_b = psX_b
    psY = kb.psum("r3psY", [128, 512], F32); psY_b = kb.buf("psY")
    psS = kb.psum("r3psS", [128, 512], F32); psS_b = kb.buf("psS")
    psL = kb.psum("r3psL", [128, 512], F32); psL_b = kb.buf("psL")

    for h in range(8):
        kb.op("pool", lambda e, h=h: e.memset(ST[h][0][:], 0.0), w=[ST_b[h][0]])

    def load_mixed(rows0, nrows, tb, mucol, dst, dst_b):
        i = ldn[0] % 3
        ldn[0] += 1
        t0 = tb * 512
        kb.op("sp", lambda e: e.dma_start(out=cur[i][0:nrows, :], in_=g.pT[rows0:rows0 + nrows, t0:t0 + 512]), w=[ld_b[i]], dma=True)
        if tb == 0:
            kb.op("pool", lambda e: e.memset(prv[i][0:nrows, 0:1], 0.0), w=[ld_b[i]])
            kb.op("sp", lambda e: e.dma_start(out=prv[i][0:nrows, 1:512], in_=g.pT[rows0:rows0 + nrows, 0:511]), w=[ld_b[i]], dma=True)
        else:
            kb.op("sp", lambda e: e.dma_start(out=prv[i][0:nrows, :], in_=g.pT[rows0:rows0 + nrows, t0 - 1:t0 + 511]), w=[ld_b[i]], dma=True)
        kb.op("pool", lambda e: e.tensor_tensor(out=prv[i][0:nrows, :], in0=prv[i][0:nrows, :], in1=cur[i][0:nrows, :], op=ALU.subtract),
              r=[ld_b[i]], w=[ld_b[i]])
        kb.op("dve", lambda e: e.scalar_tensor_tensor(out=dst[0:nrows, :], in0=prv[i][0:nrows, :], scalar=mucol, in1=cur[i][0:nrows, :],
                                                       op0=ALU.mult, op1=ALU.add), r=[ld_b[i], cst], w=[dst_b])

    un = 0
    for tb in range(NB):
        load_mixed(1536, 32, tb, mu_wa[:, 0:1], wam, wam_b)
        load_mixed(1568, 32, tb, mu_ad[:, 0:1], adm, adm_b)
        load_mixed(1600, 96, tb, mu_g[:, 0:1], gdm, gdm_b)
        kb.op("act", lambda e: e.activation(out=tw[0:32, :], in_=wam[0:32, :], func=AF.Tanh), r=[wam_b], w=[tw_b])
        kb.op("act", lambda e: e.activation(out=sgd[:], in_=gdm[:], func=AF.Sigmoid), r=[gdm_b], w=[sgd_b])
        for h in range(8):
            hc = slice(64 * h, 64 * h + 64)
            pz = tb * 8 + h
            z = pz % 2
            C = lambda j, h=h: cols[:, h, j:j + 1]
            load_mixed(64 * h, 64, tb, mu_rkv[:, h:h + 1], rm, rm_b)
            load_mixed(512 + 64 * h, 64, tb, mu_rkv[:, 8 + h:9 + h], km, km_b)
            load_mixed(1024 + 64 * h, 64, tb, mu_rkv[:, 16 + h:17 + h], vm, vm_b)
            kb.op("pe", lambda e, hc=hc: e.matmul(psL[0:64, :], lhsT=w2a2[0:32, hc], rhs=tw[0:32, :], start=True, stop=True),
                  r=[tw_b, cst], w=[psL_b])
            kb.op("act", lambda e, C=C: e.activation(out=sgw[:], in_=psL[0:64, :], func=AF.Sigmoid, bias=C(0), scale=1.0),
                  r=[psL_b, cst], w=[sgw_b])
            kb.op("pe", lambda e, hc=hc: e.matmul(psL[0:64, :], lhsT=a2t[0:32, hc], rhs=adm[0:32, :], start=True, stop=True),
                  r=[adm_b, cst], w=[psL_b])
            kb.op("act", lambda e, C=C: e.activation(out=asig[:], in_=psL[0:64, :], func=AF.Sigmoid, bias=C(1), scale=1.0),
                  r=[psL_b, cst], w=[asig_b])
            kb.op("pe", lambda e, hc=hc: e.matmul(psL[0:64, :], lhsT=g2[0:96, hc], rhs=sgd[0:96, :], start=True, stop=True),
                  r=[sgd_b, cst], w=[psL_b])
            kb.op("act", lambda e: e.copy(out=gg[:], in_=psL[0:64, :]), r=[psL_b], w=[gg_b])
            kb.op("dve", lambda e, C=C: e.tensor_scalar(out=kk[:], in0=km[:], scalar1=C(2), scalar2=None, op0=ALU.mult), r=[km_b, cst], w=[kk_b])
            kb.op("pool", lambda e: e.tensor_tensor(out=t1[:], in0=kk[:], in1=kk[:], op=ALU.mult), r=[kk_b], w=[t1_b])
            kb.op("pe", lambda e: e.matmul(psL[0:64, :], lhsT=onesf[:, :], rhs=t1[:], start=True, stop=True), r=[t1_b, cst], w=[psL_b])
            kb.op("act", lambda e: e.activation(out=t2[:], in_=psL[0:64, :], func=AF.Sqrt, bias=eps12[:], scale=1.0), r=[psL_b, cst], w=[t2_b])
            kb.op("dve", lambda e: e.tensor_scalar(out=t2[:], in0=t2[:], scalar1=1e-12, scalar2=None, op0=ALU.max), r=[t2_b], w=[t2_b])
            kb.op("dve", lambda e: e.reciprocal(out=t2[:], in_=t2[:]), r=[t2_b], w=[t2_b])
            kb.op("dve", lambda e: e.tensor_tensor(out=kk[:], in0=kk[:], in1=t2[:], op=ALU.mult), r=[kk_b, t2_b], w=[kk_b])
            kb.op("dve", lambda e, C=C: e.tensor_scalar(out=t1[:], in0=asig[:], scalar1=-1.0, scalar2=C(3), op0=ALU.add, op1=ALU.mult),
                  r=[asig_b, cst], w=[t1_b])
            kb.op("dve", lambda e: e.scalar_tensor_tensor(out=kmod[:], in0=t1[:], scalar=1.0, in1=km[:], op0=ALU.add, op1=ALU.mult),
                  r=[t1_b, km_b], w=[kmod_b])
            kb.op("pool", lambda e: e.tensor_tensor(out=bb[:], in0=kk[:], in1=asig[:], op=ALU.mult), r=[kk_b, asig_b], w=[bb_b])
            kb.op("dve", lambda e: e.tensor_tensor_scan(out=cumS[:], data0=resetm[:], data1=sgw[:], initial=0.0, op0=ALU.mult, op1=ALU.add),
                  r=[sgw_b, cst], w=[cum_b])
            kb.op("act", lambda e: e.activation(out=gincl[:], in_=cumS[:], func=AF.Exp, scale=-CW), r=[cum_b], w=[gincl_b])
            kb.op("act", lambda e: e.activation(out=ginv[:], in_=cumS[:], func=AF.Exp, scale=CW), r=[cum_b], w=[ginv_b])
            kb.op("pool", lambda e: e.tensor_tensor(out=t2[:], in0=cumS[:], in1=sgw[:], op=ALU.subtract), r=[cum_b, sgw_b], w=[t2_b])
            kb.op("act", lambda e: e.activation(out=gexcl[:], in_=t2[:], func=AF.Exp, scale=-CW), r=[t2_b], w=[gexcl_b])
            v4 = lambda t: t[:].rearrange("p (c t) -> p c t", c=8)
            kb.op("dve", lambda e, z=z: e.scalar_tensor_tensor(out=AR[z][:, :, 0:64], in0=v4(kk), scalar=-1.0, in1=v4(gexcl), op0=ALU.mult, op1=ALU.mult),
                  r=[kk_b, gexcl_b], w=[AR_b[z]])
            kb.op("pool", lambda e, z=z: e.tensor_tensor(out=AR[z][:, :, 64:128], in0=v4(rm), in1=v4(gincl), op=ALU.mult),
                  r=[rm_b, gincl_b], w=[AR_b[z]])
            kb.op("dve", lambda e, z=z: e.tensor_tensor(out=BK[z][:, :, 0:64], in0=v4(bb), in1=v4(ginv), op=ALU.mult),
                  r=[bb_b, ginv_b], w=[BK_b[z]])
            kb.op("pool", lambda e, z=z: e.tensor_tensor(out=BK[z][:, :, 64:128], in0=v4(kmod), in1=v4(ginv), op=ALU.mult),
                  r=[kmod_b, ginv_b], w=[BK_b[z]])
            kb.op("act", lambda e, z=z: e.copy(out=vb[z][:], in_=vm[:]), r=[vm_b], w=[vb_b[z]])
            kb.op("dve", lambda e, C=C: e.scalar_tensor_tensor(out=t1[:], in0=rm[:], scalar=C(4), in1=kmod[:], op0=ALU.mult, op1=ALU.mult),
                  r=[rm_b, kmod_b, cst], w=[t1_b])
            kb.op("pe", lambda e: e.matmul(psL[0:64, :], lhsT=onesf[:, :], rhs=t1[:], start=True, stop=True), r=[t1_b, cst], w=[psL_b])
            kb.op("dve", lambda e: e.tensor_tensor(out=bon[:], in0=psL[0:64, :], in1=vm[:], op=ALU.mult), r=[psL_b, vm_b], w=[bon_b])

            for c in range(8):
                u = un % NS
                un += 1
                cs = slice(c * 64, c * 64 + 64)
                gC = gincl[:, c * 64 + 63:c * 64 + 64]
                srcs = [(BK[z][:, c, 0:64], BK_b[z]), (BK[z][:, c, 64:128], BK_b[z]), (vb[z][:, cs], vb_b[z]), (AR[z][:, c, 0:64], AR_b[z])]
                for k4, (src, sb) in enumerate(srcs):
                    kb.op("pe", lambda e, k4=k4, src=src: e.transpose(out=psT[0:64, k4 * 64:(k4 + 1) * 64], in_=src, identity=identb[:]),
                          r=[sb, cst], w=[psT_b])
                kb.op("act", lambda e, u=u: e.copy(out=TM[u][:, 0:256], in_=psT[0:64, 0:256]), r=[psT_b], w=[TM_b[u]])
                kb.op("pe", lambda e, z=z, c=c: e.matmul(psG[0:64, 0:128], lhsT=BK[z][:, c, 0:64], rhs=AR[z][:, c, :], start=True, stop=True),
                      r=[BK_b[z], AR_b[z]], w=[psG_b])
                kb.op("pe", lambda e, z=z, c=c: e.matmul(psG[0:64, 128:256], lhsT=BK[z][:, c, 64:128], rhs=AR[z][:, c, :], start=True, stop=True),
                      r=[BK_b[z], AR_b[z]], w=[psG_b])
                kb.op("pe", lambda e, z=z, c=c: e.matmul(psG[0:64, 256:320], lhsT=AR[z][:, c, 0:64], rhs=BK[z][:, c, 0:64], start=True, stop=True),
                      r=[BK_b[z], AR_b[z]], w=[psG_b])
                kb.op("dve", lambda e, u=u: e.tensor_tensor(out=NM[u][:], in0=psG[0:64, 0:320], in1=maskG[:], op=ALU.mult),
                      r=[psG_b, cst], w=[NM_b[u]])
                kb.op("pe", lambda e, u=u: e.matmul(psA[0:64, 0:64], lhsT=NM[u][:, 128:192], rhs=TM[u][:, 128:192], start=True, stop=True),
                      r=[NM_b[u], TM_b[u]], w=[psX_b])
                kb.op("act", lambda e, u=u: e.copy(out=TM[u][:, 256:320], in_=psA[0:64, 0:64]), r=[psX_b], w=[TMx_b[u]])
                d0, d1 = DB[u][0], DB[u][1]
                pd = psD[u % 2]
                pdb = psD_b[u % 2]
                kb.op("dve", lambda e, u=u, d1=d1: e.tensor_tensor(out=d1[:, 0:64], in0=NM[u][:, 0:64], in1=identb[:], op=ALU.add),
                      r=[NM_b[u], cst], w=[DB_b[u][1]])
                kb.op("pe", lambda e, u=u, pd=pd: e.matmul(pd[0:64, 64:128], lhsT=NM[u][:, 256:320], rhs=NM[u][:, 0:64], start=True, stop=True),
                      r=[NM_b[u]], w=[pdb])
                kb.op("pe", lambda e, u=u, pd=pd: e.matmul(pd[0:64, 128:192], lhsT=NM[u][:, 0:64], rhs=NM[u][:, 256:320], start=True, stop=True),
                      r=[NM_b[u]], w=[pdb])
                kb.op("act", lambda e, d1=d1, pd=pd: e.copy(out=d1[:, 64:192], in_=pd[0:64, 64:192]), r=[pdb], w=[DB_b[u][1]])
                ci = 1
                for lvl in range(1, 6):
                    dc, dn_ = DB[u][ci], DB[u][1 - ci]
                    dcb, dnb = DB_b[u][ci], DB_b[u][1 - ci]
                    kb.op("pe", lambda e, dc=dc, pd=pd: e.matmul(pd[0:64, 0:128], lhsT=dc[:, 128:192], rhs=dc[:, 0:128], start=True, stop=True),
                          r=[dcb], w=[pdb])
                    if lvl < 5:
                        kb.op("pe", lambda e, dc=dc, pd=pd: e.matmul(pd[0:64, 128:192], lhsT=dc[:, 64:128], rhs=dc[:, 128:192], start=True, stop=True),
                              r=[dcb], w=[pdb])
                    kb.op("dve", lambda e, dc=dc, dn_=dn_, pd=pd: e.tensor_tensor(out=dn_[:, 0:64], in0=pd[0:64, 0:64], in1=dc[:, 0:64], op=ALU.add),
                          r=[pdb, dcb], w=[dnb])
                    if lvl < 5:
                        kb.op("act", lambda e, dn_=dn_, pd=pd: e.copy(out=dn_[:, 64:192], in_=pd[0:64, 64:192]), r=[pdb], w=[dnb])
                    ci = 1 - ci
                Tm, Tm_b = DB[u][ci], DB_b[u][ci]
                kb.op("pe", lambda e, u=u, Tm=Tm: e.matmul(psA[0:64, 64:192], lhsT=Tm[:, 0:64], rhs=TM[u][:, 192:320], start=True, stop=True),
                      r=[Tm_b, TM_b[u], TMx_b[u]], w=[psAW_b])
                kb.op("act", lambda e, u=u: e.copy(out=AW[u][:], in_=psA[0:64, 64:192]), r=[psAW_b], w=[AW_b[u]])
                kb.op("pe", lambda e, u=u: e.matmul(psA[0:64, 192:256], lhsT=AW[u][:, 0:64], rhs=TM[u][:, 0:64], start=True, stop=True),
                      r=[AW_b[u], TM_b[u]], w=[psGp_b])
                kb.op("dve", lambda e, u=u: e.tensor_tensor(out=G1[u][:], in0=psA[0:64, 192:256], in1=identf[:], op=ALU.add),
                      r=[psGp_b, cst], w=[G1_b[u]])
                kb.op("pe", lambda e, u=u: e.matmul(psA[0:64, 256:320], lhsT=TM[u][:, 0:64], rhs=AW[u][:, 64:128], start=True, stop=False),
                      r=[AW_b[u], TM_b[u]], w=[psH_b])
                kb.op("pe", lambda e, u=u: e.matmul(psA[0:64, 256:320], lhsT=TM[u][:, 64:128], rhs=TM[u][:, 128:192], start=False, stop=True),
                      r=[TM_b[u]], w=[psH_b])
                kb.op("dve", lambda e, u=u, gC=gC: e.tensor_scalar(out=Hg[u][:], in0=psA[0:64, 256:320], scalar1=gC, scalar2=None, op0=ALU.mult),
                      r=[psH_b, gincl_b], w=[Hg_b[u]])
                kb.op("pe", lambda e, u=u: e.matmul(psA[0:64, 320:384], lhsT=AW[u][:, 0:64], rhs=NM[u][:, 64:128], start=True, stop=True),
                      r=[AW_b[u], NM_b[u]], w=[psR_b])
                kb.op("dve", lambda e, u=u, z=z, c=c: e.tensor_tensor(out=Ry[u][:], in0=psA[0:64, 320:384], in1=AR[z][:, c, 64:128], op=ALU.add),
                      r=[psR_b, AR_b[z]], w=[Ry_b[u]])
                sc = (tb * 8 + c) % 2
                So, Sn = ST[h][sc], ST[h][1 - sc]
                Sob, Snb = ST_b[h][sc], ST_b[h][1 - sc]
                kb.op("pe", lambda e, u=u, cs=cs: e.matmul(psY[0:64, cs], lhsT=AW[u][:, 64:128], rhs=NM[u][:, 64:128], start=True, stop=False, skip_group_check=True),
                      r=[AW_b[u], NM_b[u]], w=[psY_b])
                kb.op("pe", lambda e, u=u, cs=cs: e.matmul(psY[0:64, cs], lhsT=TM[u][:, 128:192], rhs=NM[u][:, 192:256], start=False, stop=False, skip_group_check=True),
                      r=[TM_b[u], NM_b[u]], w=[psY_b])
                kb.op("pe", lambda e, u=u, cs=cs, So=So: e.matmul(psY[0:64, cs], lhsT=So[:], rhs=Ry[u][:], start=False, stop=True, skip_group_check=True),
                      r=[Sob, Ry_b[u]], w=[psY_b])
                kb.op("pe", lambda e, u=u, So=So: e.matmul(psS[0:64, 0:64], lhsT=G1[u][:], rhs=So[:], start=True, stop=True),
                      r=[G1_b[u], Sob], w=[psS_b])
                kb.op("dve", lambda e, u=u, Sn=Sn, gC=gC: e.scalar_tensor_tensor(out=Sn[:], in0=psS[0:64, 0:64], scalar=gC, in1=Hg[u][:], op0=ALU.mult, op1=ALU.add),
                      r=[psS_b, Hg_b[u], gincl_b], w=[Snb])
            kb.op("act", lambda e: e.copy(out=ysb[:], in_=psY[0:64, :]), r=[psY_b], w=[ysb_b])
            kb.op("pool", lambda e: e.tensor_tensor(out=ysq[:], in0=ysb[:], in1=ysb[:], op=ALU.mult), r=[ysb_b], w=[ysq_b])
            kb.op("pe", lambda e: e.matmul(psL[0:64, :], lhsT=onesf[:, :], rhs=ysb[:], start=True, stop=True), r=[ysb_b, cst], w=[psL_b])
            kb.op("act", lambda e: e.activation(out=ymean[:], in_=psL[0:64, :], func=AF.Identity, scale=1.0 / 64), r=[psL_b], w=[ymean_b])
            kb.op("pe", lambda e: e.matmul(psL[0:64, :], lhsT=onesf[:, :], rhs=ysq[:], start=True, stop=True), r=[ysq_b, cst], w=[psL_b])
            kb.op("pool", lambda e: e.tensor_tensor(out=ysq[:], in0=ymean[:], in1=ymean[:], op=ALU.mult), r=[ymean_b, ysq_b], w=[ysq_b])
            kb.op("dve", lambda e: e.scalar_tensor_tensor(out=yvar[:], in0=psL[0:64, :], scalar=1.0 / 64, in1=ysq[:], op0=ALU.mult, op1=ALU.subtract),
                  r=[psL_b, ysq_b], w=[yvar_b])
            kb.op("act", lambda e: e.activation(out=yvar[:], in_=yvar[:], func=AF.Sqrt, bias=epsgn[:], scale=1.0), r=[yvar_b, cst], w=[yvar_b])
            kb.op("dve", lambda e: e.reciprocal(out=yvar[:], in_=yvar[:]), r=[yvar_b], w=[yvar_b])
            kb.op("pool", lambda e: e.tensor_tensor(out=ysb[:], in0=ysb[:], in1=ymean[:], op=ALU.subtract), r=[ysb_b, ymean_b], w=[ysb_b])
            kb.op("dve", lambda e, C=C: e.scalar_tensor_tensor(out=ysb[:], in0=ysb[:], scalar=C(5), in1=yvar[:], op0=ALU.mult, op1=ALU.mult),
                  r=[ysb_b, yvar_b, cst], w=[ysb_b])
            kb.op("dve", lambda e, C=C: e.scalar_tensor_tensor(out=ysb[:], in0=ysb[:], scalar=C(6), in1=bon[:], op0=ALU.add, op1=ALU.add),
                  r=[ysb_b, bon_b, cst], w=[ysb_b])
            yo = pz % 2
            kb.op("pool", lambda e, yo=yo: e.tensor_tensor(out=yout[yo][:], in0=ysb[:], in1=gg[:], op=ALU.mult), r=[ysb_b, gg_b], w=[yout_b[yo]])
            kb.op("sp", lambda e, yo=yo, h=h, tb=tb: e.dma_start(out=g.yT[64 * h:64 * h + 64, tb * 512:(tb + 1) * 512], in_=yout[yo][:]),
                  r=[yout_b[yo]], dma=True)


def setup4(nc, g, out_name="out"):
    ei = lambda n, s, d=F32: nc.dram_tensor(n, list(s), d, kind="ExternalInput").ap()
    T = g.T
    g.w_o = ei("w_o", [D, D])
    g.ln1_w = ei("ln1_w", [1, D]); g.ln1_b = ei("ln1_b", [1, D])
    g.ln2_w = ei("ln2_w", [1, D]); g.ln2_b = ei("ln2_b", [1, D])
    g.router_w = ei("router_w", [D, 32]); g.router_b = ei("router_b", [1, 32])
    g.exp_w1 = ei("exp_w1", [32, D, 2048]); g.exp_b1 = ei("exp_b1", [32, 2048])
    g.exp_w2 = ei("exp_w2", [32, D, D]); g.exp_b2 = ei("exp_b2", [32, D])
    g.h1f = g.scratch("h1f", [T, D])
    g.h1T = g.scratch("h1T", [D, T], BF16)
    g.yacc = g.scratch("yacc", [T, D])
    g.out = nc.dram_tensor(out_name, [T, D], F32, kind="ExternalOutput").ap()


def _ln_tile(kb, src, src_b, dst, dst_b, st, mv, rs, st_b, epsc, cst, gam, bet):
    for hf in range(2):
        kb.op("dve", lambda e, hf=hf: e.bn_stats(out=st[:, hf, :], in_=src[:, hf * 512:(hf + 1) * 512]), r=[src_b], w=[st_b])
    kb.op("dve", lambda e: e.bn_aggr(out=mv[:], in_=st[:].rearrange("p a b -> p (a b)")), r=[st_b], w=[st_b])
    kb.op("act", lambda e: e.activation(out=rs[:], in_=mv[:, 1:2], func=AF.Sqrt, bias=epsc[:], scale=1.0), r=[st_b, cst], w=[st_b])
    kb.op("dve", lambda e: e.reciprocal(out=rs[:], in_=rs[:]), r=[st_b], w=[st_b])
    kb.op("dve", lambda e: e.tensor_scalar(out=dst[:], in0=src[:], scalar1=mv[:, 0:1], scalar2=rs[:], op0=ALU.subtract, op1=ALU.mult),
          r=[src_b, st_b], w=[dst_b])
    kb.op("dve", lambda e: e.tensor_tensor(out=dst[:], in0=dst[:], in1=gam[:], op=ALU.mult), r=[dst_b, cst], w=[dst_b])
    kb.op("dve", lambda e: e.tensor_tensor(out=dst[:], in0=dst[:], in1=bet[:], op=ALU.add), r=[dst_b, cst], w=[dst_b])


def phase4(kb, g, n_exp=32, sections="ABC"):
    nc, T, NT, NB = g.nc, g.T, g.NT, g.NB
    kb.ps_stack.close(); kb.ps_stack = None
    gates = kb.sbuf("m4_gates", [128, NT, 32], F32); gates_b = kb.buf("gates")
    kb.begin_phase()
    A = lambda n, s, d=F32: kb.sbuf("m4_" + n, s, d)
    cst = kb.buf("cst4")
    wo = A("wo", [128, KC, D], BF16)
    rwt = A("rwt", [128, KC, 32]); rbt = A("rbt", [128, 32])
    gam1 = A("gam1", [128, D]); bet1 = A("bet1", [128, D])
    identf = A("identf", [128, 128]); epsc = A("epsc", [128, 1])
    kb.op("dve", lambda e: e.memset(epsc[:], LN_EPS), w=[cst])
    kb.op("sp", lambda e: e.dma_start(out=identf[:], in_=g.ident_f), w=[cst], dma=True)
    for nm, t_, src in (("g1", gam1, g.ln1_w), ("b1", bet1, g.ln1_b)):
        kb.op("sp", lambda e, t_=t_, src=src: e.dma_start(out=t_[:], in_=src.broadcast_to([128, D])), w=[cst], dma=True)
    kb.op("sp", lambda e: e.dma_start(out=rbt[:], in_=g.router_b.broadcast_to([128, 32])), w=[cst], dma=True)
    kb.op("sp", lambda e: e.dma_start(out=rwt[:], in_=g.router_w.rearrange("(kc p) n -> p kc n", p=128)), w=[cst], dma=True)
    for kc in range(KC):
        kb.op("pool", lambda e, kc=kc: e.dma_start(out=wo[:, kc, :], in_=g.w_o[kc * 128:(kc + 1) * 128, :]), w=[cst], dma=True)

    ymt = [A(f"ymt{i}", [128, KC, 128], BF16) for i in range(2)]; ymt_b = kb.bufs(2, "ymt")
    h0t = [A(f"h0t{i}", [128, D]) for i in range(2)]; h0t_b = kb.bufs(2, "h0t")
    sres = [A(f"sres{i}", [128, D]) for i in range(2)]; sres_b = kb.bufs(2, "sres")
    h1t = [A(f"h1t{i}", [128, D]) for i in range(2)]; h1t_b = kb.bufs(2, "h1t")
    st = A("st", [128, 2, 6]); mv = A("mv", [128, 2]); rs = A("rs", [128, 1]); st_b = kb.buf("st")
    h1Tf = A("h1Tf", [128, KC, 128]); h1Tf_b = kb.buf("h1Tf")
    h1Tb = [A(f"h1Tb{i}", [128, KC, 128], BF16) for i in range(2)]; h1Tb_b = kb.bufs(2, "h1Tb")
    lg = A("lg", [128, 32]); mx8 = A("mx8", [128, 8]); msk = A("msk", [128, 32]); ee = A("ee", [128, 32]); ssum = A("ssum", [128, 1])
    nm0 = A("nm0", [128, 1]); lg_b = kb.buf("lg")
    ps = [kb.psum(f"m4ps{i}", [128, 512], F32) for i in range(8)]; ps_b = kb.bufs(8, "m4ps")

    import os
    STOPAT = int(os.environ.get('STOPAT', '99'))
    for i in range(NT):
        s = i % 2
        tsl = slice(i * 128, (i + 1) * 128)
        kb.op("sp", lambda e, s=s, tsl=tsl: e.dma_start(out=ymt[s][:], in_=g.yT[:, tsl].rearrange("(kc p) t -> p kc t", p=128)), w=[ymt_b[s]], dma=True)
        kb.op("sp", lambda e, s=s, tsl=tsl: e.dma_start(out=h0t[s][:], in_=g.h0f[tsl, :]), w=[h0t_b[s]], dma=True)
        if STOPAT <= 1:
            continue
        for hf in range(2):
            for kc in range(KC):
                kb.op("pe", lambda e, s=s, hf=hf, kc=kc: e.matmul(ps[hf][:, :], lhsT=ymt[s][:, kc, :], rhs=wo[:, kc, hf * 512:(hf + 1) * 512],
                                                                 start=(kc == 0), stop=(kc == KC - 1)), r=[ymt_b[s], cst], w=[ps_b[hf]])
            kb.op("dve", lambda e, s=s, hf=hf: e.scalar_tensor_tensor(out=sres[s][:, hf * 512:(hf + 1) * 512], in0=h0t[s][:, hf * 512:(hf + 1) * 512],
                                                                     scalar=ALPHA, in1=ps[hf][:, :], op0=ALU.mult, op1=ALU.add),
                  r=[h0t_b[s], ps_b[hf]], w=[sres_b[s]])
        if STOPAT <= 2:
            continue
        _ln_tile(kb, sres[s], sres_b[s], h1t[s], h1t_b[s], st, mv, rs, st_b, epsc, cst, gam1, bet1)
        if STOPAT <= 3:
            continue
        kb.op("sp", lambda e, s=s, tsl=tsl: e.dma_start(out=g.h1f[tsl, :], in_=h1t[s][:]), r=[h1t_b[s]], dma=True)
        if STOPAT <= 4:
            continue
        for hf in range(2):
            p = 2 + hf
            for c in range(4):
                kc = hf * 4 + c
                kb.op("pe", lambda e, s=s, p=p, c=c, kc=kc: e.transpose(out=ps[p][:, c * 128:(c + 1) * 128], in_=h1t[s][:, kc * 128:(kc + 1) * 128], identity=identf[:]),
                      r=[h1t_b[s], cst], w=[ps_b[p]])
            kb.op("act", lambda e, p=p, hf=hf: e.copy(out=h1Tf[:, hf * 4:(hf + 1) * 4, :], in_=ps[p][:].rearrange("p (c t) -> p c t", c=4)), r=[ps_b[p]], w=[h1Tf_b])
            kb.op("dve", lambda e, hf=hf, s=s: e.tensor_copy(out=h1Tb[s][:, hf * 4:(hf + 1) * 4, :], in_=h1Tf[:, hf * 4:(hf + 1) * 4, :]),
                  r=[h1Tf_b], w=[h1Tb_b[s]])
        if STOPAT <= 5:
            continue
        kb.op("sp", lambda e, s=s, tsl=tsl: e.dma_start(out=g.h1T[:, tsl].rearrange("(kc p) t -> p kc t", p=128), in_=h1Tb[s][:]), r=[h1Tb_b[s]], dma=True)
        import os
        if os.environ.get('SKIP_ROUTER'):
            continue
        for kc in range(KC):
            kb.op("pe", lambda e, kc=kc: e.matmul(ps[4][:, 0:32], lhsT=h1Tf[:, kc, :], rhs=rwt[:, kc, :], start=(kc == 0), stop=(kc == KC - 1)),
                  r=[h1Tf_b, cst], w=[ps_b[4]])
        kb.op("dve", lambda e: e.tensor_tensor(out=lg[:], in0=ps[4][:, 0:32], in1=rbt[:], op=ALU.add), r=[ps_b[4], cst], w=[lg_b])
        kb.op("dve", lambda e: e.max(out=mx8[:], in_=lg[:]), r=[lg_b], w=[lg_b])
        kb.op("dve", lambda e: e.tensor_scalar(out=msk[:], in0=lg[:], scalar1=mx8[:, 3:4], scalar2=None, op0=ALU.is_ge), r=[lg_b], w=[lg_b])
        kb.op("dve", lambda e: e.tensor_scalar(out=nm0[:], in0=mx8[:, 0:1], scalar1=-1.0, scalar2=None, op0=ALU.mult), r=[lg_b], w=[lg_b])
        kb.op("act", lambda e: e.activation(out=ee[:], in_=lg[:], func=AF.Exp, bias=nm0[:], scale=1.0), r=[lg_b], w=[lg_b])
        kb.op("dve", lambda e: e.tensor_tensor(out=ee[:], in0=ee[:], in1=msk[:], op=ALU.mult), r=[lg_b], w=[lg_b])
        kb.op("dve", lambda e: e.reduce_sum(out=ssum[:], in_=ee[:], axis=AX.X), r=[lg_b], w=[lg_b])
        kb.op("dve", lambda e: e.reciprocal(out=ssum[:], in_=ssum[:]), r=[lg_b], w=[lg_b])
        kb.op("dve", lambda e, i=i: e.tensor_scalar(out=gates[:, i, :], in0=ee[:], scalar1=ssum[:, 0:1], scalar2=None, op0=ALU.mult), r=[lg_b], w=[gates_b])

    if "B" not in sections:
        return
    kb.end_phase(); kb.begin_phase()
    A = lambda n, s, d=F32: kb.sbuf("m4b_" + n, s, d)
    cst = kb.buf("cst4b")
    b1t = A("b1t", [128, 32, 8, 2])
    for ex in range(32):
        kb.op("sp", lambda e, ex=ex: e.dma_start(out=b1t[:, ex, :, :], in_=g.exp_b1[ex:ex + 1, :].rearrange("o (c p two) -> p (o c) two", p=128, two=2)),
              w=[cst], dma=True)
    ps = [kb.psum(f"m4bps{i}", [128, 512], F32) for i in range(8)]; ps_b = kb.bufs(8, "m4bps")
    W1 = [A(f"W1_{i}", [128, KC, 2048], BF16) for i in range(2)]; W2 = [A(f"W2_{i}", [128, KC, D], BF16) for i in range(2)]
    W_b = kb.bufs(2, "W")
    b2t = [A(f"b2t{i}", [128, D]) for i in range(2)]
    xT = [A(f"xT{i}", [128, KC, 512], BF16) for i in range(2)]; xT_b = kb.bufs(2, "xT")
    actT = [A(f"actT{i}", [128, 8, 512], BF16) for i in range(2)]; actT_b = kb.bufs(2, "actT")
    xg = [A(f"xg{i}", [128, 512]) for i in range(2)]; sg = [A(f"sgm{i}", [128, 512]) for i in range(2)]; xl = [A(f"xl{i}", [128, 512]) for i in range(2)]
    el_b = kb.bufs(2, "el")
    ot = [A(f"ot{i}", [128, 512]) for i in range(3)]; ot_b = kb.bufs(3, "ot")
    yacc_b = kb.bufs(NT, "yacc")

    def load_w(ex):
        z = ex % 2
        for kc in range(KC):
            kb.op("pool", lambda e, z=z, kc=kc, ex=ex: e.dma_start(out=W1[z][:, kc, :], in_=g.exp_w1[ex, kc * 128:(kc + 1) * 128, :]), w=[W_b[z]], dma=True)
        for kc in range(KC):
            kb.op("pool", lambda e, z=z, kc=kc, ex=ex: e.dma_start(out=W2[z][:, kc, :], in_=g.exp_w2[ex, kc * 128:(kc + 1) * 128, :]), w=[W_b[z]], dma=True)
        kb.op("sp", lambda e, z=z, ex=ex: e.dma_start(out=b2t[z][:], in_=g.exp_b2[ex:ex + 1, :].broadcast_to([128, D])), w=[W_b[z]], dma=True)

    load_w(0)
    n = 0
    on = 0
    for ex in range(n_exp):
        z = ex % 2
        if ex + 1 < n_exp:
            load_w(ex + 1)
        for tb in range(NB):
            xs = n % 2
            n += 1
            kb.op("sp", lambda e, xs=xs, tb=tb: e.dma_start(out=xT[xs][:], in_=g.h1T[:, tb * 512:(tb + 1) * 512].rearrange("(kc p) t -> p kc t", p=128)),
                  w=[xT_b[xs]], dma=True)
            for fc in range(8):
                q = fc % 2
                pg, pl = ps[2 * q], ps[2 * q + 1]
                for which, pp, ppb in ((0, pg, ps_b[2 * q]), (1, pl, ps_b[2 * q + 1])):
                    for kc in range(KC):
                        kb.op("pe", lambda e, z=z, kc=kc, fc=fc, which=which, pp=pp, xs=xs: e.matmul(
                            pp[:, :], lhsT=W1[z][:, kc, fc * 256 + which:fc * 256 + 256:2], rhs=xT[xs][:, kc, :], start=(kc == 0), stop=(kc == KC - 1)),
                            r=[W_b[z], xT_b[xs]], w=[ppb])
                kb.op("dve", lambda e, q=q, pg=pg, ex=ex, fc=fc: e.tensor_scalar(out=xg[q][:], in0=pg[:, :], scalar1=b1t[:, ex, fc, 0:1], scalar2=7.0, op0=ALU.add, op1=ALU.min),
                      r=[ps_b[2 * q], cst], w=[el_b[q]])
                kb.op("act", lambda e, q=q: e.activation(out=sg[q][:], in_=xg[q][:], func=AF.Sigmoid, scale=1.702), r=[el_b[q]], w=[el_b[q]])
                kb.op("dve", lambda e, q=q, pl=pl, ex=ex, fc=fc: e.tensor_scalar(out=xl[q][:], in0=pl[:, :], scalar1=b1t[:, ex, fc, 1:2], scalar2=7.0, op0=ALU.add, op1=ALU.min),
                      r=[ps_b[2 * q + 1], cst], w=[el_b[q]])
                kb.op("dve", lambda e, q=q: e.tensor_scalar(out=xl[q][:], in0=xl[q][:], scalar1=-7.0, scalar2=1.0, op0=ALU.max, op1=ALU.add), r=[el_b[q]], w=[el_b[q]])
                kb.op("dve", lambda e, q=q: e.tensor_tensor(out=xg[q][:], in0=xg[q][:], in1=sg[q][:], op=ALU.mult), r=[el_b[q]], w=[el_b[q]])
                kb.op("dve", lambda e, q=q, xs=xs, fc=fc: e.tensor_tensor(out=actT[xs][:, fc, :], in0=xg[q][:], in1=xl[q][:], op=ALU.mult),
                      r=[el_b[q]], w=[actT_b[xs]])
            for tt in range(4):
                ti = tb * 4 + tt
                for hf in range(2):
                    pp = 4 + (on % 4)
                    oi = on % 3
                    on += 1
                    for fc in range(8):
                        kb.op("pe", lambda e, pp=pp, xs=xs, fc=fc, tt=tt, z=z, hf=hf: e.matmul(
                            ps[pp][:, :], lhsT=actT[xs][:, fc, tt * 128:(tt + 1) * 128], rhs=W2[z][:, fc, hf * 512:(hf + 1) * 512], start=(fc == 0), stop=(fc == 7)),
                            r=[actT_b[xs], W_b[z]], w=[ps_b[pp]])
                    kb.op("dve", lambda e, pp=pp, oi=oi, z=z, hf=hf: e.tensor_tensor(out=ot[oi][:], in0=ps[pp][:, :], in1=b2t[z][:, hf * 512:(hf + 1) * 512], op=ALU.add),
                          r=[ps_b[pp], W_b[z]], w=[ot_b[oi]])
                    kb.op("dve", lambda e, oi=oi, ti=ti, ex=ex: e.tensor_scalar(out=ot[oi][:], in0=ot[oi][:], scalar1=gates[:, ti, ex:ex + 1], scalar2=None, op0=ALU.mult),
                          r=[ot_b[oi], gates_b], w=[ot_b[oi]])
                    if ex == 0:
                        kb.op("pool", lambda e, oi=oi, ti=ti, hf=hf: e.dma_start(out=g.yacc[ti * 128:(ti + 1) * 128, hf * 512:(hf + 1) * 512], in_=ot[oi][:]),
                              r=[ot_b[oi]], w=[yacc_b[ti]], dma=True)
                    else:
                        kb.op("pool", lambda e, oi=oi, ti=ti, hf=hf: e.dma_start(out=g.yacc[ti * 128:(ti + 1) * 128, hf * 512:(hf + 1) * 512], in_=ot[oi][:], accum_op=ALU.add),
                              r=[ot_b[oi]], w=[yacc_b[ti]], dma=True)

    if "C" not in sections:
        return
    kb.end_phase(); kb.begin_phase()
    A = lambda n, s, d=F32: kb.sbuf("m4c_" + n, s, d)
    cst = kb.buf("cst4c")
    gam2 = A("gam2", [128, D]); bet2 = A("bet2", [128, D]); epsc = A("epsc", [128, 1])
    kb.op("dve", lambda e: e.memset(epsc[:], LN_EPS), w=[cst])
    kb.op("sp", lambda e: e.dma_start(out=gam2[:], in_=g.ln2_w.broadcast_to([128, D])), w=[cst], dma=True)
    kb.op("sp", lambda e: e.dma_start(out=bet2[:], in_=g.ln2_b.broadcast_to([128, D])), w=[cst], dma=True)
    h0t = [A(f"h0t{i}", [128, D]) for i in range(2)]; h0t_b = kb.bufs(2, "h0tc")
    sres = [A(f"sres{i}", [128, D]) for i in range(2)]; sres_b = kb.bufs(2, "sresc")
    h1t = [A(f"h1t{i}", [128, D]) for i in range(2)]; h1t_b = kb.bufs(2, "h1tc")
    st = A("st", [128, 2, 6]); mv = A("mv", [128, 2]); rs = A("rs", [128, 1]); st_b = kb.buf("stc")
    for i in range(NT):
        s = i % 2
        tsl = slice(i * 128, (i + 1) * 128)
        kb.op("sp", lambda e, s=s, tsl=tsl: e.dma_start(out=h0t[s][:], in_=g.h1f[tsl, :]), w=[h0t_b[s]], dma=True)
        kb.op("sp", lambda e, s=s, tsl=tsl: e.dma_start(out=h1t[s][:], in_=g.yacc[tsl, :]), r=[yacc_b[i]], w=[h1t_b[s]], dma=True)
        kb.op("dve", lambda e, s=s: e.scalar_tensor_tensor(out=sres[s][:], in0=h0t[s][:], scalar=ALPHA, in1=h1t[s][:], op0=ALU.mult, op1=ALU.add),
              r=[h0t_b[s], h1t_b[s]], w=[sres_b[s]])
        _ln_tile(kb, sres[s], sres_b[s], h1t[s], h1t_b[s], st, mv, rs, st_b, epsc, cst, gam2, bet2)
        kb.op("sp", lambda e, s=s, tsl=tsl: e.dma_start(out=g.out[tsl, :], in_=h1t[s][:]), r=[h1t_b[s]], dma=True)


def make_inputs(d, b, T):
    im = {"x": d['x'][b, :T], "ln_in_w": d['ln_in_w'][None], "ln_in_b": d['ln_in_b'][None], "w_in": d['w_in'][0],
          "fx_b_f": d['fx_b_f'][0][:, None], "fx_q_norm": d['fx_q_norm'][0][:, None], "fx_k_norm": d['fx_k_norm'][0][:, None],
          "w_o": d['w_o'][0], "ln1_w": d['ln1_w'], "ln1_b": d['ln1_b'], "ln2_w": d['ln2_w'], "ln2_b": d['ln2_b'],
          "router_w": d['router_w'][0], "router_b": d['router_b'], "exp_w1": d['exp_w1'][0], "exp_b1": d['exp_b1'][0],
          "exp_w2": d['exp_w2'][0], "exp_b2": d['exp_b2'][0]}
    im.update(consts_np()); im.update(consts2_np()); im.update(consts3_np(d)); im.update(consts4g_np(T))
    return {k: np.ascontiguousarray(v) for k, v in im.items()}


def moe_cap(T):
    mean = T * 4 // 32
    return 128 * int(np.ceil(1.25 * mean / 128.0))


def setup4g(nc, g):
    ei = lambda n, s, d=F32: nc.dram_tensor(n, list(s), d, kind="ExternalInput").ap()
    T = g.T
    C = moe_cap(T)
    g.C = C
    g.NR = 32 * C + 128
    g.ec_iota = ei("ec_iota", [128, 32])
    g.tok16 = ei("tok16", [128, 16])
    g.table_init = ei("table_init", [g.NR, 16])
    g.su_b = ei("su_b", [128, 128], BF16)
    g.table = g.scratch("table", [g.NR, 16])
    g.h1b = g.scratch("h1b", [T + 128, D], BF16)
    g.out_all = g.scratch("out_all", [g.NR, D])


def consts4g_np(T):
    C = moe_cap(T)
    NR = 32 * C + 128
    c = {}
    c["ec_iota"] = np.tile((np.arange(32) * C).astype(np.float32)[None, :], (128, 1))
    c["tok16"] = np.tile(np.arange(128, dtype=np.float32)[:, None], (1, 16))
    ti = np.zeros((NR, 16), np.float32)
    ti[:, 0] = T
    c["table_init"] = ti
    a = np.arange(128)
    c["su_b"] = (a[:, None] < a[None, :]).astype(np.float32).astype(ml_dtypes.bfloat16)
    return c


def phase4g(kb, g):
    nc, T, NT, NB = g.nc, g.T, g.NT, g.NB
    C = g.C
    TRASH = 32 * C
    NS = C // 128
    kb.ps_stack.close(); kb.ps_stack = None
    rowi = kb.sbuf("m4_rowi", [128, NT, 4], I32); gsel = kb.sbuf("m4_gsel", [128, NT, 4], F32); sel_b = kb.buf("sel")
    kb.begin_phase()
    A = lambda n, s, d=F32: kb.sbuf("m4_" + n, s, d)
    cst = kb.buf("cst4")
    wo = A("wo", [128, KC, D], BF16)
    rwt = A("rwt", [128, KC, 32]); rbt = A("rbt", [128, 32])
    gam1 = A("gam1", [128, D]); bet1 = A("bet1", [128, D])
    identf = A("identf", [128, 128]); epsc = A("epsc", [128, 1])
    ecio = A("ecio", [128, 32]); tok0 = A("tok0", [128, 16]); sub = A("sub", [128, 128], BF16); onesb = A("onesb", [128, 128], BF16)
    tin = A("tin", [128, g.NR // 128, 16]); zrow = A("zrow", [128, D], BF16); zrowf = A("zrowf", [128, D])
    table_b = kb.buf("table"); h1b_b = kb.buf("h1b"); outall_b = kb.buf("outall")
    kb.op("dve", lambda e: e.memset(epsc[:], LN_EPS), w=[cst])
    kb.op("dve", lambda e: e.memset(onesb[:], 1.0), w=[cst])
    kb.op("dve", lambda e: e.memset(zrow[:], 0.0), w=[cst])
    kb.op("dve", lambda e: e.memset(zrowf[:], 0.0), w=[cst])
    kb.op("sp", lambda e: e.dma_start(out=identf[:], in_=g.ident_f), w=[cst], dma=True)
    kb.op("sp", lambda e: e.dma_start(out=ecio[:], in_=g.ec_iota), w=[cst], dma=True)
    kb.op("sp", lambda e: e.dma_start(out=tok0[:], in_=g.tok16), w=[cst], dma=True)
    kb.op("sp", lambda e: e.dma_start(out=sub[:], in_=g.su_b), w=[cst], dma=True)
    kb.op("sp", lambda e: e.dma_start(out=tin[:], in_=g.table_init.rearrange("(j p) w -> p j w", p=128)), w=[cst], dma=True)
    kb.op("sp", lambda e: e.dma_start(out=g.table.rearrange("(j p) w -> p j w", p=128), in_=tin[:]), r=[cst], w=[table_b], dma=True)
    kb.op("sp", lambda e: e.dma_start(out=g.h1b[T:T + 128, :], in_=zrow[:]), r=[cst], w=[h1b_b], dma=True)
    kb.op("sp", lambda e: e.dma_start(out=g.out_all[TRASH:TRASH + 128, :], in_=zrowf[:]), r=[cst], w=[outall_b], dma=True)
    for nm, t_, src in (("g1", gam1, g.ln1_w), ("b1", bet1, g.ln1_b)):
        kb.op("sp", lambda e, t_=t_, src=src: e.dma_start(out=t_[:], in_=src.broadcast_to([128, D])), w=[cst], dma=True)
    kb.op("sp", lambda e: e.dma_start(out=rbt[:], in_=g.router_b.broadcast_to([128, 32])), w=[cst], dma=True)
    kb.op("sp", lambda e: e.dma_start(out=rwt[:], in_=g.router_w.rearrange("(kc p) n -> p kc n", p=128)), w=[cst], dma=True)
    for kc in range(KC):
        kb.op("pool", lambda e, kc=kc: e.dma_start(out=wo[:, kc, :], in_=g.w_o[kc * 128:(kc + 1) * 128, :]), w=[cst], dma=True)

    ymt = [A(f"ymt{i}", [128, KC, 128], BF16) for i in range(2)]; ymt_b = kb.bufs(2, "ymt")
    h0t = [A(f"h0t{i}", [128, D]) for i in range(2)]; h0t_b = kb.bufs(2, "h0t")
    sres = [A(f"sres{i}", [128, D]) for i in range(2)]; sres_b = kb.bufs(2, "sres")
    h1t = [A(f"h1t{i}", [128, D]) for i in range(2)]; h1t_b = kb.bufs(2, "h1t")
    h1bt = [A(f"h1bt{i}", [128, D], BF16) for i in range(2)]; h1bt_b = kb.bufs(2, "h1bt")
    st = A("st", [128, 2, 6]); mv = A("mv", [128, 2]); rs = A("rs", [128, 1]); st_b = kb.buf("st")
    h1Tf = A("h1Tf", [128, KC, 128]); h1Tf_b = kb.buf("h1Tf")
    lg = A("lg", [128, 32]); mx8 = A("mx8", [128, 8]); msk = A("msk", [128, 32]); ee = A("ee", [128, 32]); ssum = A("ssum", [128, 1])
    gt = A("gt", [128, 32]); nm0 = A("nm0", [128, 1]); lg_b = kb.buf("lg")
    mskb = A("mskb", [128, NT, 32], BF16); mskb_b = kb.bufs(NT, "mskb")
    ridx = A("ridx", [128, 32]); okm = A("okm", [128, 32]); selk = A("selk", [128, 32]); tmpk = A("tmpk", [128, 32])
    rowf = A("rowf", [128, NT, 4]); rt_b = kb.buf("rt")
    tokt = [A(f"tokt{i}", [128, 16]) for i in range(2)]; tokt_b = kb.bufs(2, "tokt")
    ps = [kb.psum(f"m4ps{i}", [128, 512], F32) for i in range(8)]; ps_b = kb.bufs(8, "m4ps")

    for i in range(NT):
        s = i % 2
        tsl = slice(i * 128, (i + 1) * 128)
        kb.op("sp", lambda e, s=s, tsl=tsl: e.dma_start(out=ymt[s][:], in_=g.yT[:, tsl].rearrange("(kc p) t -> p kc t", p=128)), w=[ymt_b[s]], dma=True)
        kb.op("sp", lambda e, s=s, tsl=tsl: e.dma_start(out=h0t[s][:], in_=g.h0f[tsl, :]), w=[h0t_b[s]], dma=True)
        for hf in range(2):
            for kc in range(KC):
                kb.op("pe", lambda e, s=s, hf=hf, kc=kc: e.matmul(ps[hf][:, :], lhsT=ymt[s][:, kc, :], rhs=wo[:, kc, hf * 512:(hf + 1) * 512],
                                                                 start=(kc == 0), stop=(kc == KC - 1)), r=[ymt_b[s], cst], w=[ps_b[hf]])
            kb.op("dve", lambda e, s=s, hf=hf: e.scalar_tensor_tensor(out=sres[s][:, hf * 512:(hf + 1) * 512], in0=h0t[s][:, hf * 512:(hf + 1) * 512],
                                                                     scalar=ALPHA, in1=ps[hf][:, :], op0=ALU.mult, op1=ALU.add),
                  r=[h0t_b[s], ps_b[hf]], w=[sres_b[s]])
        _ln_tile(kb, sres[s], sres_b[s], h1t[s], h1t_b[s], st, mv, rs, st_b, epsc, cst, gam1, bet1)
        kb.op("sp", lambda e, s=s, tsl=tsl: e.dma_start(out=g.h1f[tsl, :], in_=h1t[s][:]), r=[h1t_b[s]], dma=True)
        kb.op("act", lambda e, s=s: e.copy(out=h1bt[s][:], in_=h1t[s][:]), r=[h1t_b[s]], w=[h1bt_b[s]])
        kb.op("sp", lambda e, s=s, tsl=tsl: e.dma_start(out=g.h1b[tsl, :], in_=h1bt[s][:]), r=[h1bt_b[s]], w=[h1b_b], dma=True)
        for hf in range(2):
            p = 2 + hf
            for c in range(4):
                kc = hf * 4 + c
                kb.op("pe", lambda e, s=s, p=p, c=c, kc=kc: e.transpose(out=ps[p][:, c * 128:(c + 1) * 128], in_=h1t[s][:, kc * 128:(kc + 1) * 128], identity=identf[:]),
                      r=[h1t_b[s], cst], w=[ps_b[p]])
            kb.op("act", lambda e, p=p, hf=hf: e.copy(out=h1Tf[:, hf * 4:(hf + 1) * 4, :], in_=ps[p][:].rearrange("p (c t) -> p c t", c=4)), r=[ps_b[p]], w=[h1Tf_b])
        for kc in range(KC):
            kb.op("pe", lambda e, kc=kc: e.matmul(ps[4][:, 0:32], lhsT=h1Tf[:, kc, :], rhs=rwt[:, kc, :], start=(kc == 0), stop=(kc == KC - 1)),
                  r=[h1Tf_b, cst], w=[ps_b[4]])
        kb.op("dve", lambda e: e.tensor_tensor(out=lg[:], in0=ps[4][:, 0:32], in1=rbt[:], op=ALU.add), r=[ps_b[4], cst], w=[lg_b])
        kb.op("dve", lambda e: e.max(out=mx8[:], in_=lg[:]), r=[lg_b], w=[lg_b])
        kb.op("dve", lambda e: e.tensor_scalar(out=msk[:], in0=lg[:], scalar1=mx8[:, 3:4], scalar2=None, op0=ALU.is_ge), r=[lg_b], w=[lg_b])
        kb.op("dve", lambda e: e.tensor_scalar(out=nm0[:], in0=mx8[:, 0:1], scalar1=-1.0, scalar2=None, op0=ALU.mult), r=[lg_b], w=[lg_b])
        kb.op("act", lambda e: e.activation(out=ee[:], in_=lg[:], func=AF.Exp, bias=nm0[:], scale=1.0), r=[lg_b], w=[lg_b])
        kb.op("dve", lambda e: e.tensor_tensor(out=ee[:], in0=ee[:], in1=msk[:], op=ALU.mult), r=[lg_b], w=[lg_b])
        kb.op("dve", lambda e: e.reduce_sum(out=ssum[:], in_=ee[:], axis=AX.X), r=[lg_b], w=[lg_b])
        kb.op("dve", lambda e: e.reciprocal(out=ssum[:], in_=ssum[:]), r=[lg_b], w=[lg_b])
        kb.op("dve", lambda e: e.tensor_scalar(out=gt[:], in0=ee[:], scalar1=ssum[:, 0:1], scalar2=None, op0=ALU.mult), r=[lg_b], w=[lg_b])
        kb.op("dve", lambda e, i=i: e.tensor_copy(out=mskb[:, i, :], in_=msk[:]), r=[lg_b], w=[mskb_b[i]])
        kb.op("pe", lambda e, i=i: e.matmul(ps[5][:, 0:32], lhsT=sub[:], rhs=mskb[:, i, :], start=True, stop=(i == 0)), r=[mskb_b[i], cst], w=[ps_b[5]])
        for j in range(i):
            kb.op("pe", lambda e, j=j, i=i: e.matmul(ps[5][:, 0:32], lhsT=onesb[:], rhs=mskb[:, j, :], start=False, stop=(j == i - 1)),
                  r=[mskb_b[j], cst], w=[ps_b[5]])
        kb.op("dve", lambda e: e.tensor_tensor(out=ridx[:], in0=ps[5][:, 0:32], in1=ecio[:], op=ALU.add), r=[ps_b[5], cst], w=[rt_b])
        kb.op("dve", lambda e: e.tensor_scalar(out=okm[:], in0=ps[5][:, 0:32], scalar1=float(C), scalar2=None, op0=ALU.is_lt), r=[ps_b[5]], w=[rt_b])
        kb.op("dve", lambda e: e.tensor_tensor(out=okm[:], in0=okm[:], in1=msk[:], op=ALU.mult), r=[rt_b, lg_b], w=[rt_b])
        kb.op("dve", lambda e: e.tensor_scalar(out=ridx[:], in0=ridx[:], scalar1=-float(TRASH), scalar2=None, op0=ALU.add), r=[rt_b], w=[rt_b])
        kb.op("dve", lambda e: e.tensor_tensor(out=ridx[:], in0=ridx[:], in1=okm[:], op=ALU.mult), r=[rt_b], w=[rt_b])
        kb.op("dve", lambda e: e.tensor_scalar(out=ridx[:], in0=ridx[:], scalar1=float(TRASH), scalar2=None, op0=ALU.add), r=[rt_b], w=[rt_b])
        for k in range(4):
            kb.op("dve", lambda e, k=k: e.tensor_scalar(out=selk[:], in0=lg[:], scalar1=mx8[:, k:k + 1], scalar2=None, op0=ALU.is_equal), r=[lg_b], w=[rt_b])
            kb.op("dve", lambda e: e.tensor_tensor(out=tmpk[:], in0=selk[:], in1=ridx[:], op=ALU.mult), r=[rt_b], w=[rt_b])
            kb.op("dve", lambda e, i=i, k=k: e.reduce_sum(out=rowf[:, i, k:k + 1], in_=tmpk[:], axis=AX.X), r=[rt_b], w=[rt_b])
            kb.op("dve", lambda e: e.tensor_tensor(out=tmpk[:], in0=selk[:], in1=gt[:], op=ALU.mult), r=[rt_b, lg_b], w=[rt_b])
            kb.op("dve", lambda e, i=i, k=k: e.reduce_sum(out=gsel[:, i, k:k + 1], in_=tmpk[:], axis=AX.X), r=[rt_b], w=[sel_b])
        kb.op("dve", lambda e, i=i: e.tensor_copy(out=rowi[:, i, :], in_=rowf[:, i, :]), r=[rt_b], w=[sel_b])
        kb.op("dve", lambda e, i=i, s=s: e.tensor_scalar(out=tokt[s][:], in0=tok0[:], scalar1=float(i * 128), scalar2=None, op0=ALU.add), r=[cst], w=[tokt_b[s]])
        for k in range(4):
            kb.op("pool", lambda e, i=i, k=k, s=s: e.indirect_dma_start(
                out=g.table[:, :], out_offset=bass.IndirectOffsetOnAxis(ap=rowi[:, i, k:k + 1], axis=0), in_=tokt[s][:, :], in_offset=None),
                r=[sel_b, tokt_b[s]], w=[table_b], dma=True)

    kb.end_phase(); kb.begin_phase()
    A = lambda n, s, d=F32: kb.sbuf("m4b_" + n, s, d)
    cst = kb.buf("cst4b")
    b1t = A("b1t", [128, 32, 8, 2]); identb = A("identb", [128, 128], BF16)
    kb.op("sp", lambda e: e.dma_start(out=identb[:], in_=g.ident_b), w=[cst], dma=True)
    for ex in range(32):
        kb.op("sp", lambda e, ex=ex: e.dma_start(out=b1t[:, ex, :, :], in_=g.exp_b1[ex:ex + 1, :].rearrange("o (c p two) -> p (o c) two", p=128, two=2)),
              w=[cst], dma=True)
    ps = [kb.psum(f"m4bps{i}", [128, 512], F32) for i in range(7)]; ps_b = kb.bufs(7, "m4bps")
    psTb = kb.psum("m4bpsT", [128, 1024], BF16); psTb_b = kb.buf("psTb")
    W1 = [A(f"W1_{i}", [128, KC, 2048], BF16) for i in range(2)]; W2 = [A(f"W2_{i}", [128, KC, D], BF16) for i in range(2)]
    W_b = kb.bufs(2, "W")
    b2t = [A(f"b2t{i}", [128, D]) for i in range(2)]
    idxf = [A(f"idxf{i}", [128, 16]) for i in range(2)]; idxi = [A(f"idxi{i}", [128, 1], I32) for i in range(2)]; idx_b = kb.bufs(2, "idx")
    Xg = [A(f"Xg{i}", [128, D], BF16) for i in range(2)]; Xg_b = kb.bufs(2, "Xg")
    xT = [A(f"xT{i}", [128, KC, C], BF16) for i in range(2)]; xT_b = kb.bufs(2, "xT")
    actT = [A(f"actT{i}", [128, 8, 512], BF16) for i in range(2)]; actT_b = kb.bufs(2, "actT")
    xg = [A(f"xg{i}", [128, 512]) for i in range(2)]; sg = [A(f"sgm{i}", [128, 512]) for i in range(2)]; xl = [A(f"xl{i}", [128, 512]) for i in range(2)]
    el_b = kb.bufs(2, "el")
    ot = [A(f"ot{i}", [128, 512]) for i in range(3)]; ot_b = kb.bufs(3, "ot")
    blocks = []
    c0 = 0
    while c0 < C:
        w = min(512, C - c0)
        blocks.append((c0, w))
        c0 += w

    def load_w(ex):
        z = ex % 2
        for kc in range(KC):
            kb.op("pool", lambda e, z=z, kc=kc, ex=ex: e.dma_start(out=W1[z][:, kc, :], in_=g.exp_w1[ex, kc * 128:(kc + 1) * 128, :]), w=[W_b[z]], dma=True)
        for kc in range(KC):
            kb.op("pool", lambda e, z=z, kc=kc, ex=ex: e.dma_start(out=W2[z][:, kc, :], in_=g.exp_w2[ex, kc * 128:(kc + 1) * 128, :]), w=[W_b[z]], dma=True)
        kb.op("sp", lambda e, z=z, ex=ex: e.dma_start(out=b2t[z][:], in_=g.exp_b2[ex:ex + 1, :].broadcast_to([128, D])), w=[W_b[z]], dma=True)

    def gather_x(ex):
        xz = ex % 2
        for s_ in range(NS):
            q = (ex * NS + s_) % 2
            r0 = ex * C + s_ * 128
            kb.op("sp", lambda e, q=q, r0=r0: e.dma_start(out=idxf[q][:], in_=g.table[r0:r0 + 128, :]), r=[table_b], w=[idx_b[q]], dma=True)
            kb.op("dve", lambda e, q=q: e.tensor_copy(out=idxi[q][:], in_=idxf[q][:, 0:1]), r=[idx_b[q]], w=[idx_b[q]])
            kb.op("pool", lambda e, q=q: e.indirect_dma_start(
                out=Xg[q][:, :], out_offset=None, in_=g.h1b[:, :], in_offset=bass.IndirectOffsetOnAxis(ap=idxi[q][:, 0:1], axis=0)),
                r=[idx_b[q], h1b_b], w=[Xg_b[q]], dma=True)
            for kc in range(KC):
                kb.op("pe", lambda e, q=q, kc=kc: e.transpose(out=psTb[:, kc * 128:(kc + 1) * 128], in_=Xg[q][:, kc * 128:(kc + 1) * 128], identity=identb[:]),
                      r=[Xg_b[q], cst], w=[psTb_b])
            kb.op("act", lambda e, xz=xz, s_=s_: e.copy(out=xT[xz][:, :, s_ * 128:(s_ + 1) * 128], in_=psTb[:].rearrange("p (c t) -> p c t", c=8)),
                  r=[psTb_b], w=[xT_b[xz]])

    load_w(0)
    gather_x(0)
    on = 0
    an = 0
    for ex in range(32):
        z = ex % 2
        xz = ex % 2
        if ex + 1 < 32:
            load_w(ex + 1)
            gather_x(ex + 1)
        for (c0, w) in blocks:
            xs = an % 2
            an += 1
            for fc in range(8):
                q = fc % 2
                pg, pl = ps[2 * q], ps[2 * q + 1]
                for which, pp, ppb in ((0, pg, ps_b[2 * q]), (1, pl, ps_b[2 * q + 1])):
                    for kc in range(KC):
                        kb.op("pe", lambda e, z=z, kc=kc, fc=fc, which=which, pp=pp, xz=xz, c0=c0, w=w: e.matmul(
                            pp[:, 0:w], lhsT=W1[z][:, kc, fc * 256 + which:fc * 256 + 256:2], rhs=xT[xz][:, kc, c0:c0 + w], start=(kc == 0), stop=(kc == KC - 1)),
                            r=[W_b[z], xT_b[xz]], w=[ppb])
                kb.op("dve", lambda e, q=q, pg=pg, ex=ex, fc=fc, w=w: e.tensor_scalar(out=xg[q][:, 0:w], in0=pg[:, 0:w], scalar1=b1t[:, ex, fc, 0:1], scalar2=7.0, op0=ALU.add, op1=ALU.min),
                      r=[ps_b[2 * q], cst], w=[el_b[q]])
                kb.op("act", lambda e, q=q, w=w: e.activation(out=sg[q][:, 0:w], in_=xg[q][:, 0:w], func=AF.Sigmoid, scale=1.702), r=[el_b[q]], w=[el_b[q]])
                kb.op("dve", lambda e, q=q, pl=pl, ex=ex, fc=fc, w=w: e.tensor_scalar(out=xl[q][:, 0:w], in0=pl[:, 0:w], scalar1=b1t[:, ex, fc, 1:2], scalar2=7.0, op0=ALU.add, op1=ALU.min),
                      r=[ps_b[2 * q + 1], cst], w=[el_b[q]])
                kb.op("dve", lambda e, q=q, w=w: e.tensor_scalar(out=xl[q][:, 0:w], in0=xl[q][:, 0:w], scalar1=-7.0, scalar2=1.0, op0=ALU.max, op1=ALU.add), r=[el_b[q]], w=[el_b[q]])
                kb.op("dve", lambda e, q=q, w=w: e.tensor_tensor(out=xg[q][:, 0:w], in0=xg[q][:, 0:w], in1=sg[q][:, 0:w], op=ALU.mult), r=[el_b[q]], w=[el_b[q]])
                kb.op("dve", lambda e, q=q, xs=xs, fc=fc, w=w: e.tensor_tensor(out=actT[xs][:, fc, 0:w], in0=xg[q][:, 0:w], in1=xl[q][:, 0:w], op=ALU.mult),
                      r=[el_b[q]], w=[actT_b[xs]])
            for tt in range(w // 128):
                r0 = ex * C + c0 + tt * 128
                for hf in range(2):
                    pp = 4 + (on % 3)
                    oi = on % 3
                    on += 1
                    for fc in range(8):
                        kb.op("pe", lambda e, pp=pp, xs=xs, fc=fc, tt=tt, z=z, hf=hf: e.matmul(
                            ps[pp][:, :], lhsT=actT[xs][:, fc, tt * 128:(tt + 1) * 128], rhs=W2[z][:, fc, hf * 512:(hf + 1) * 512], start=(fc == 0), stop=(fc == 7)),
                            r=[actT_b[xs], W_b[z]], w=[ps_b[pp]])
                    kb.op("dve", lambda e, pp=pp, oi=oi, z=z, hf=hf: e.tensor_tensor(out=ot[oi][:], in0=ps[pp][:, :], in1=b2t[z][:, hf * 512:(hf + 1) * 512], op=ALU.add),
                          r=[ps_b[pp], W_b[z]], w=[ot_b[oi]])
                    kb.op("sp", lambda e, oi=oi, r0=r0, hf=hf: e.dma_start(out=g.out_all[r0:r0 + 128, hf * 512:(hf + 1) * 512], in_=ot[oi][:]),
                          r=[ot_b[oi]], w=[outall_b], dma=True)

    kb.end_phase(); kb.begin_phase()
    A = lambda n, s, d=F32: kb.sbuf("m4c_" + n, s, d)
    cst = kb.buf("cst4c")
    gam2 = A("gam2", [128, D]); bet2 = A("bet2", [128, D]); epsc = A("epsc", [128, 1])
    kb.op("dve", lambda e: e.memset(epsc[:], LN_EPS), w=[cst])
    kb.op("sp", lambda e: e.dma_start(out=gam2[:], in_=g.ln2_w.broadcast_to([128, D])), w=[cst], dma=True)
    kb.op("sp", lambda e: e.dma_start(out=bet2[:], in_=g.ln2_b.broadcast_to([128, D])), w=[cst], dma=True)
    h0t = [A(f"h0t{i}", [128, D]) for i in range(2)]; h0t_b = kb.bufs(2, "h0tc")
    sres = [A(f"sres{i}", [128, D]) for i in range(2)]; sres_b = kb.bufs(2, "sresc")
    h1t = [A(f"h1t{i}", [128, D]) for i in range(2)]; h1t_b = kb.bufs(2, "h1tc")
    Rk = [A(f"Rk{i}", [128, D]) for i in range(4)]; Rk_b = kb.bufs(4, "Rk")
    st = A("st", [128, 2, 6]); mv = A("mv", [128, 2]); rs = A("rs", [128, 1]); st_b = kb.buf("stc")
    for i in range(NT):
        s = i % 2
        tsl = slice(i * 128, (i + 1) * 128)
        kb.op("sp", lambda e, s=s, tsl=tsl: e.dma_start(out=h0t[s][:], in_=g.h1f[tsl, :]), w=[h0t_b[s]], dma=True)
        for k in range(4):
            kb.op("pool", lambda e, i=i, k=k: e.indirect_dma_start(
                out=Rk[k][:, :], out_offset=None, in_=g.out_all[:, :], in_offset=bass.IndirectOffsetOnAxis(ap=rowi[:, i, k:k + 1], axis=0)),
                r=[sel_b, outall_b], w=[Rk_b[k]], dma=True)
        kb.op("dve", lambda e, s=s: e.tensor_scalar(out=sres[s][:], in0=h0t[s][:], scalar1=ALPHA, scalar2=None, op0=ALU.mult), r=[h0t_b[s]], w=[sres_b[s]])
        for k in range(4):
            kb.op("dve", lambda e, s=s, i=i, k=k: e.scalar_tensor_tensor(out=sres[s][:], in0=Rk[k][:], scalar=gsel[:, i, k:k + 1], in1=sres[s][:], op0=ALU.mult, op1=ALU.add),
                  r=[Rk_b[k], sel_b, sres_b[s]], w=[sres_b[s]])
        _ln_tile(kb, sres[s], sres_b[s], h1t[s], h1t_b[s], st, mv, rs, st_b, epsc, cst, gam2, bet2)
        kb.op("sp", lambda e, s=s, tsl=tsl: e.dma_start(out=g.out[tsl, :], in_=h1t[s][:]), r=[h1t_b[s]], dma=True)


_T = 4096


def _build(T):
    nc = bass.Bass("TRN2", target_bir_lowering=False)
    g = setup(nc, T)
    setup2(nc, g); setup3(nc, g); setup4(nc, g); setup4g(nc, g)
    kb = KB(nc)
    kb.begin_phase(); phase01(kb, g); kb.end_phase()
    kb.begin_phase(); phase2(kb, g); kb.end_phase()
    kb.begin_phase(); phase3(kb, g); kb.end_phase()
    kb.begin_phase(); phase4g(kb, g); kb.end_phase()
    kb.finish(); kb.close()
    return nc


def kernel(**inputs):
    d = {k: np.asarray(v) for k, v in inputs.items()}
    B = d["x"].shape[0]
    T = d["x"].shape[1]
    nc = _build(T)
    maps = [make_inputs(d, b % B, T) for b in range(8)]
    res = run_bass_kernel_spmd(nc, maps, core_ids=list(range(8)))
    out = np.stack([np.asarray(res.results[b]["out"]) for b in range(B)], axis=0)
    return out.astype(np.float32)
```

```python
import ml_dtypes
import contextlib
import numpy as np
import concourse.bass as bass
import concourse.mybir as mybir
from concourse.bass_utils import run_bass_kernel_spmd

F32 = mybir.dt.float32
BF16 = mybir.dt.bfloat16
I32 = mybir.dt.int32
U32 = mybir.dt.uint32
AF = mybir.ActivationFunctionType
ALU = mybir.AluOpType
AX = mybir.AxisListType

ENGS = ("pe", "act", "dve", "pool", "sp")


class Buf:
    __slots__ = ("name", "w", "rs")

    def __init__(self, name):
        self.name = name
        self.w = None
        self.rs = {}


class _Op:
    __slots__ = ("fn", "waits", "ev", "dma", "marked")

    def __init__(self, fn, waits, ev, dma):
        self.fn = fn
        self.waits = waits
        self.ev = ev
        self.dma = dma
        self.marked = False


class KB:
    def __init__(self, nc, n_dma_sems=20):
        self.nc = nc
        self.es = contextlib.ExitStack()
        self.q = {e: [] for e in ENGS}
        self.n_dma_sems = n_dma_sems
        self.dma_sems = {}
        self.dma_tgt = {}
        self.dma_rr = {}
        self.eng_sem = {}
        self.nbuf = 0
        self.allbufs = []
        self.base = {e: 0 for e in ENGS}
        self.barrier = []
        self.ps_stack = None
        for e in ENGS:
            self.eng_sem[e] = self.es.enter_context(nc.semaphore("es_" + e))
        for e in ("sp", "pool", "act"):
            self.dma_sems[e] = [self.es.enter_context(nc.semaphore(f"ds_{e}_{i}")) for i in range(n_dma_sems)]
            self.dma_tgt[e] = [0] * n_dma_sems
            self.dma_rr[e] = 0

    def begin_phase(self):
        self.ps_stack = contextlib.ExitStack()

    def end_phase(self):
        self.replay()
        self.ps_stack.close()
        self.ps_stack = None

    def sbuf(self, name, shape, dtype):
        st = self.ps_stack if self.ps_stack is not None else self.es
        return st.enter_context(self.nc.sbuf_tensor(name, list(shape), dtype))

    def psum(self, name, shape, dtype):
        st = self.ps_stack if self.ps_stack is not None else self.es
        return st.enter_context(self.nc.psum_tensor(name, list(shape), dtype))

    def buf(self, name=None):
        self.nbuf += 1
        b = Buf(name or f"b{self.nbuf}")
        self.allbufs.append(b)
        return b

    def bufs(self, n, name="b"):
        return [self.buf(f"{name}{i}") for i in range(n)]

    def op(self, eng, fn, r=(), w=(), dma=False):
        waits = []
        if not self.q[eng] and self.barrier:
            waits.extend(self.barrier)
        for b in r:
            if b.w is not None:
                waits.append(b.w)
        for b in w:
            if b.w is not None:
                waits.append(b.w)
            waits.extend(b.rs.values())
        if dma:
            i = self.dma_rr[eng]
            self.dma_rr[eng] = (i + 1) % self.n_dma_sems
            prev = self.dma_tgt[eng][i]
            if prev > 0:
                waits.append(("d", eng, i, prev))
            tgt = prev + 16
            self.dma_tgt[eng][i] = tgt
            ev = ("d", eng, i, tgt)
        else:
            ev = ("c", eng, len(self.q[eng]))
        self.q[eng].append(_Op(fn, waits, ev, dma))
        key = ev[:3] if ev[0] == "d" else ev[:2]
        for b in r:
            b.rs[key] = ev
        for b in w:
            b.w = ev
            b.rs = {}
        return ev

    def replay(self):
        nc = self.nc
        for e in ENGS:
            if self.q[e]:
                self.q[e][-1].marked = True
            for o in self.q[e]:
                for ev in o.waits:
                    if ev[0] == "c":
                        if ev[1] == "pe" and e == "pe":
                            continue
                        self.q[ev[1]][ev[2]].marked = True
        cnt = {}
        for e in ENGS:
            c = self.base[e]
            arr = []
            for o in self.q[e]:
                if o.marked and not o.dma:
                    c += 1
                arr.append(c)
            cnt[e] = arr

        def resolve(ev):
            if ev[0] == "c":
                return ("c", ev[1]), self.eng_sem[ev[1]], cnt[ev[1]][ev[2]]
            if ev[0] == "a":
                return ("c", ev[1]), self.eng_sem[ev[1]], ev[2]
            return ("d", ev[1], ev[2]), self.dma_sems[ev[1]][ev[2]], ev[3]

        if not hasattr(self, "seen"):
            self.seen = {e: {} for e in ENGS}

        def run_engine(ename, eobj):
            seen = self.seen[ename]
            for o in self.q[ename]:
                need = {}
                for ev in o.waits:
                    if ev[0] in ("c", "a") and ev[1] == "pe" and ename == "pe":
                        continue
                    k, sem, val = resolve(ev)
                    if seen.get(k, 0) >= val:
                        continue
                    if k not in need or need[k][1] < val:
                        need[k] = (sem, val)
                for k, (sem, val) in need.items():
                    eobj.wait_ge(sem, val)
                    seen[k] = val
                ins = o.fn(eobj)
                if o.dma:
                    ins.then_inc(self.dma_sems[o.ev[1]][o.ev[2]], 16)
                elif o.marked:
                    ins.then_inc(self.eng_sem[ename], 1)

        with nc.Block() as block:
            @block.tensor
            def _(t):
                run_engine("pe", t)

            @block.scalar
            def _(s):
                run_engine("act", s)

            @block.vector
            def _(v):
                run_engine("dve", v)

            @block.gpsimd
            def _(g):
                run_engine("pool", g)

            @block.sync
            def _(sy):
                run_engine("sp", sy)

        def absolutize(ev):
            if ev[0] == "c":
                return ("a", ev[1], cnt[ev[1]][ev[2]] if self.q[ev[1]][ev[2]].marked else cnt[ev[1]][-1])
            return ev
        for b in self.allbufs:
            if b.w is not None:
                b.w = absolutize(b.w)
            b.rs = {k: absolutize(v) for k, v in b.rs.items()}
        for e in ENGS:
            if self.q[e]:
                self.base[e] = cnt[e][-1]
            self.q[e] = []
        bar = [("a", e, self.base[e]) for e in ENGS if self.base[e] > 0]
        for e in ("sp", "pool", "act"):
            for i, t in enumerate(self.dma_tgt[e]):
                if t > 0:
                    bar.append(("d", e, i, t))
        self.barrier = bar

    def finish(self):
        nc = self.nc
        bar = list(self.barrier)
        with nc.Block() as block:
            @block.sync
            def _(sy):
                for ev in bar:
                    if ev[0] == "a":
                        sy.wait_ge(self.eng_sem[ev[1]], ev[2])
                    else:
                        sy.wait_ge(self.dma_sems[ev[1]][ev[2]], ev[3])

    def close(self):
        self.es.close()


D = 1024
KC = 8
RW_COLS = 1696
FX0 = RW_COLS
IN_COLS = 3752
LN_EPS = 1e-5
ALPHA = 2 ** 0.25


def col_tiles():
    t = []
    for nm, base in (("r", 0), ("k", 512), ("v", 1024)):
        for h in range(8):
            t.append((f"rw_{nm}{h}", base + 64 * h, 64))
    t.append(("rw_wa", 1536, 64))
    t.append(("rw_g", 1600, 96))
    for nm, base in (("q", 0), ("k", 512), ("og", 1536)):
        for h in range(8):
            t.append((f"fx_{nm}{h}", FX0 + base + 64 * h, 64))
    t.append(("fx_fz", FX0 + 2048, 8))
    return t


def consts_np():
    c = {}
    c["ident_f"] = np.eye(128, dtype=np.float32)
    c["ident_b"] = np.eye(128).astype(ml_dtypes.bfloat16)
    return c


class Ctx:
    pass


def setup(nc, T, ext_out=()):
    g = Ctx()
    g.nc = nc
    g.T = T
    g.NT = T // 128
    g.NB = T // 512
    ei = lambda n, s, d=F32: nc.dram_tensor(n, list(s), d, kind="ExternalInput").ap()
    g.x = ei("x", [T, D])
    g.ln_in_w = ei("ln_in_w", [1, D])
    g.ln_in_b = ei("ln_in_b", [1, D])
    g.w_in = ei("w_in", [D, IN_COLS])
    g.ident_f = ei("ident_f", [128, 128])
    g.ident_b = ei("ident_b", [128, 128], BF16)

    def scratch(n, s, d=F32):
        kind = "ExternalOutput" if n in ext_out else "Internal"
        return nc.dram_tensor(n, list(s), d, kind=kind).ap()

    g.scratch = scratch
    g.h0f = scratch("h0f", [T, D])
    g.pT = scratch("pT", [IN_COLS, T])
    g.vtok = scratch("vtok", [T, 512], BF16)
    return g


def phase01(kb, g):
    nc, T, NT, NB = g.nc, g.T, g.NT, g.NB
    h0T = kb.sbuf("h0T", [128, KC, T], BF16)
    h0T_b = kb.bufs(NT, "h0T")
    wbf = kb.sbuf("wbf", [128, KC, IN_COLS], BF16)
    wbf_b = kb.bufs(KC, "wbf")
    gam = kb.sbuf("gam", [128, D], F32)
    bet = kb.sbuf("bet", [128, D], F32)
    gb_b = kb.buf("gb")
    identf = kb.sbuf("identf", [128, 128], F32)
    id_b = kb.buf("id")
    epsc = kb.sbuf("epsc", [128, 1], F32)
    eps_b = kb.buf("eps")
    xt = [kb.sbuf(f"xt{i}", [128, D], F32) for i in range(2)]
    xt_b = kb.bufs(2, "xt")
    hn = [kb.sbuf(f"hn{i}", [128, D], F32) for i in range(2)]
    hn_b = kb.bufs(2, "hn")
    st = [kb.sbuf(f"st{i}", [128, 2, 6], F32) for i in range(2)]
    mv = [kb.sbuf(f"mv{i}", [128, 2], F32) for i in range(2)]
    rs = [kb.sbuf(f"rs{i}", [128, 1], F32) for i in range(2)]
    st_b = kb.bufs(2, "st")
    ps = [kb.psum(f"ps{i}", [128, 512], F32) for i in range(4)]
    ps_b = kb.bufs(4, "ps")
    stage = [kb.sbuf(f"stage{i}", [128, 512], F32) for i in range(3)]
    stage_b = kb.bufs(3, "stage")
    vst = [kb.sbuf(f"vst{i}", [128, 512], BF16) for i in range(2)]
    vst_b = kb.bufs(2, "vst")

    kb.op("sp", lambda e: e.dma_start(out=gam[:], in_=g.ln_in_w.broadcast_to([128, D])), w=[gb_b], dma=True)
    kb.op("sp", lambda e: e.dma_start(out=bet[:], in_=g.ln_in_b.broadcast_to([128, D])), w=[gb_b], dma=True)
    kb.op("sp", lambda e: e.dma_start(out=identf[:], in_=g.ident_f), w=[id_b], dma=True)
    kb.op("pool", lambda e: e.memset(epsc[:], LN_EPS), w=[eps_b])
    half = IN_COLS // 2
    for kc in range(KC):
        for hf in range(2):
            kb.op("pool", lambda e, kc=kc, hf=hf: e.dma_start(
                out=wbf[:, kc, hf * half:(hf + 1) * half],
                in_=g.w_in[kc * 128:(kc + 1) * 128, hf * half:(hf + 1) * half]),
                w=[wbf_b[kc]], dma=True)

    for i in range(NT):
        s = i % 2
        kb.op("sp", lambda e, i=i, s=s: e.dma_start(out=xt[s][:], in_=g.x[i * 128:(i + 1) * 128, :]), w=[xt_b[s]], dma=True)
        for hf in range(2):
            kb.op("dve", lambda e, s=s, hf=hf: e.bn_stats(out=st[s][:, hf, :], in_=xt[s][:, hf * 512:(hf + 1) * 512]),
                  r=[xt_b[s]], w=[st_b[s]])
        kb.op("dve", lambda e, s=s: e.bn_aggr(out=mv[s][:], in_=st[s][:].rearrange("p a b -> p (a b)")), r=[st_b[s]], w=[st_b[s]])
        kb.op("act", lambda e, s=s: e.activation(out=rs[s][:], in_=mv[s][:, 1:2], func=AF.Sqrt, bias=epsc[:], scale=1.0),
              r=[st_b[s], eps_b], w=[st_b[s]])
        kb.op("dve", lambda e, s=s: e.reciprocal(out=rs[s][:], in_=rs[s][:]), r=[st_b[s]], w=[st_b[s]])
        kb.op("dve", lambda e, s=s: e.tensor_scalar(out=hn[s][:], in0=xt[s][:], scalar1=mv[s][:, 0:1], scalar2=rs[s][:],
                                                    op0=ALU.subtract, op1=ALU.mult),
              r=[xt_b[s], st_b[s]], w=[hn_b[s]])
        kb.op("dve", lambda e, s=s: e.tensor_tensor(out=hn[s][:], in0=hn[s][:], in1=gam[:], op=ALU.mult), r=[hn_b[s], gb_b], w=[hn_b[s]])
        kb.op("pool", lambda e, s=s: e.tensor_tensor(out=hn[s][:], in0=hn[s][:], in1=bet[:], op=ALU.add), r=[hn_b[s], gb_b], w=[hn_b[s]])
        kb.op("sp", lambda e, i=i, s=s: e.dma_start(out=g.h0f[i * 128:(i + 1) * 128, :], in_=hn[s][:]), r=[hn_b[s]], dma=True)
        for hf in range(2):
            p = hf
            for c in range(4):
                kc = hf * 4 + c
                kb.op("pe", lambda e, s=s, p=p, c=c, kc=kc: e.transpose(out=ps[p][:, c * 128:(c + 1) * 128],
                                                                       in_=hn[s][:, kc * 128:(kc + 1) * 128], identity=identf[:]),
                      r=[hn_b[s], id_b], w=[ps_b[p]])
            eng = "act" if hf == 0 else "dve"
            if eng == "act":
                kb.op("act", lambda e, i=i, p=p, hf=hf: e.copy(out=h0T[:, hf * 4:(hf + 1) * 4, i * 128:(i + 1) * 128],
                                                               in_=ps[p][:].rearrange("p (c t) -> p c t", c=4)),
                      r=[ps_b[p]], w=[h0T_b[i]])
            else:
                kb.op("dve", lambda e, i=i, p=p, hf=hf: e.tensor_copy(out=h0T[:, hf * 4:(hf + 1) * 4, i * 128:(i + 1) * 128],
                                                                      in_=ps[p][:].rearrange("p (c t) -> p c t", c=4)),
                      r=[ps_b[p]], w=[h0T_b[i]])

    n = 0
    for (nm, c0, ncol) in col_tiles():
        for tb in range(NB):
            p = 2 + (n % 2)
            sg = n % 3
            for kc in range(KC):
                kb.op("pe", lambda e, p=p, kc=kc, c0=c0, ncol=ncol, tb=tb: e.matmul(
                    ps[p][0:ncol, :], lhsT=wbf[:, kc, c0:c0 + ncol], rhs=h0T[:, kc, tb * 512:(tb + 1) * 512],
                    start=(kc == 0), stop=(kc == KC - 1)),
                    r=[wbf_b[kc]] + h0T_b[tb * 4:(tb + 1) * 4], w=[ps_b[p]])
            if n % 2 == 0:
                kb.op("act", lambda e, p=p, sg=sg, ncol=ncol: e.copy(out=stage[sg][0:ncol, :], in_=ps[p][0:ncol, :]),
                      r=[ps_b[p]], w=[stage_b[sg]])
            else:
                kb.op("dve", lambda e, p=p, sg=sg, ncol=ncol: e.tensor_copy(out=stage[sg][0:ncol, :], in_=ps[p][0:ncol, :]),
                      r=[ps_b[p]], w=[stage_b[sg]])
            kb.op("sp", lambda e, sg=sg, c0=c0, ncol=ncol, tb=tb: e.dma_start(
                out=g.pT[c0:c0 + ncol, tb * 512:(tb + 1) * 512], in_=stage[sg][0:ncol, :]),
                r=[stage_b[sg]], dma=True)
            n += 1
    vc0 = FX0 + 1024
    for i in range(NT):
        p = 2 + (i % 2)
        s = i % 2
        for kc in range(KC):
            kb.op("pe", lambda e, p=p, kc=kc, i=i: e.matmul(
                ps[p][:, :], lhsT=h0T[:, kc, i * 128:(i + 1) * 128], rhs=wbf[:, kc, vc0:vc0 + 512],
                start=(kc == 0), stop=(kc == KC - 1)),
                r=[wbf_b[kc], h0T_b[i]], w=[ps_b[p]])
        kb.op("act", lambda e, p=p, s=s: e.copy(out=vst[s][:], in_=ps[p][:]), r=[ps_b[p]], w=[vst_b[s]])
        kb.op("sp", lambda e, s=s, i=i: e.dma_start(out=g.vtok[i * 128:(i + 1) * 128, :], in_=vst[s][:]), r=[vst_b[s]], dma=True)


def setup2(nc, g):
    ei = lambda n, s, d=F32: nc.dram_tensor(n, list(s), d, kind="ExternalInput").ap()
    g.fx_b_f = ei("fx_b_f", [8, 1])
    g.fx_q_norm = ei("fx_q_norm", [64, 1])
    g.fx_k_norm = ei("fx_k_norm", [64, 1])
    g.tri = ei("tri", [128, 128], BF16)
    g.yT = g.scratch("yT", [1024, g.T], BF16)


def consts2_np():
    c = {}
    k = np.arange(128)[:, None]
    q = np.arange(128)[None, :]
    c["tri"] = (k <= q).astype(np.float32).astype(ml_dtypes.bfloat16)
    return c


def phase2(kb, g):
    nc, T, NT, NB = g.nc, g.T, g.NT, g.NB
    onesf = kb.sbuf("onesf", [128, 128], F32)
    identf = kb.sbuf("identf2", [128, 128], F32)
    tri = kb.sbuf("tri_sb", [128, 128], BF16)
    cst_b = kb.buf("cst2")
    epsq = kb.sbuf("epsq", [128, 1], F32)
    negb = kb.sbuf("negb", [8, 1], F32)
    qw = kb.sbuf("qw", [64, 1], F32)
    kw = kb.sbuf("kw", [64, 1], F32)
    fz = kb.sbuf("fz", [8, T], F32)
    cp = kb.sbuf("cp", [8, T], F32)
    ones8 = kb.sbuf("ones8", [8, T], F32)
    cq8 = kb.sbuf("cq8", [8, T], F32)
    fz_b = kb.buf("fz")
    cq8_b = kb.buf("cq8")
    negc = kb.sbuf("negc", [128, NT, 8], F32)
    negc_b = kb.buf("negc")
    qrow = kb.sbuf("qrow", [65, T], F32)
    qrow_b = kb.buf("qrow")
    qraw = kb.sbuf("qraw", [64, T], F32)
    kraw = kb.sbuf("kraw", [64, T], F32)
    sq = kb.sbuf("sq", [64, T], F32)
    raw_b = {"q": kb.buf("qraw"), "k": kb.buf("kraw")}
    sq_b = kb.buf("sq")
    ograw = kb.sbuf("ograw", [64, T], F32)
    og_b = kb.buf("ograw")
    sg = kb.sbuf("sg", [64, T], BF16)
    sg_b = kb.buf("sg")
    Qa = kb.sbuf("Qa", [65, T], BF16)
    Ka = kb.sbuf("Ka", [65, T], BF16)
    Qa_b = kb.buf("Qa")
    Ka_b = kb.buf("Ka")
    Va = kb.sbuf("Va", [128, NT, 65], BF16)
    Va_b = kb.buf("Va")
    rst = [kb.sbuf(f"rst{i}", [64, 512], F32) for i in range(2)]
    rst_b = kb.bufs(2, "rst")
    PT = [kb.sbuf(f"PT{i}", [128, 512], BF16) for i in range(3)]
    PT_b = kb.bufs(3, "PT")
    dn = kb.sbuf("dn", [65, 512], F32)
    dn_b = kb.buf("dn")
    bcs = kb.sbuf("bcs", [64, 512], F32)
    bcs_b = kb.buf("bcs")
    o1 = kb.sbuf("o1", [64, 512], F32)
    o1_b = kb.buf("o1")
    yfx = [kb.sbuf(f"yfx{i}", [64, 512], BF16) for i in range(2)]
    yfx_b = kb.bufs(2, "yfx")
    psS = [kb.psum(f"psS{i}", [128, 512], F32) for i in range(2)]
    psS_b = kb.bufs(2, "psS")
    psO = [kb.psum(f"psO{i}", [128, 512], F32) for i in range(2)]
    psO_b = kb.bufs(2, "psO")
    psB = kb.psum("psB", [128, 512], F32)
    psB_b = kb.buf("psB")
    psN = [kb.psum(f"psN{i}", [128, 512], F32) for i in range(2)]
    psN_b = kb.bufs(2, "psN")

    kb.op("pool", lambda e: e.memset(onesf[:], 1.0), w=[cst_b])
    kb.op("pool", lambda e: e.memset(ones8[:], 1.0), w=[cst_b])
    kb.op("pool", lambda e: e.memset(epsq[:], 1e-6), w=[cst_b])
    kb.op("pool", lambda e: e.memset(Ka[64:65, :], 1.0), w=[Ka_b])
    kb.op("pool", lambda e: e.memset(Va[:, :, 64:65], 1.0), w=[Va_b])
    kb.op("sp", lambda e: e.dma_start(out=identf[:], in_=g.ident_f), w=[cst_b], dma=True)
    kb.op("sp", lambda e: e.dma_start(out=tri[:], in_=g.tri), w=[cst_b], dma=True)
    kb.op("sp", lambda e: e.dma_start(out=negb[:], in_=g.fx_b_f), w=[cst_b], dma=True)
    kb.op("sp", lambda e: e.dma_start(out=qw[:], in_=g.fx_q_norm), w=[cst_b], dma=True)
    kb.op("sp", lambda e: e.dma_start(out=kw[:], in_=g.fx_k_norm), w=[cst_b], dma=True)
    kb.op("dve", lambda e: e.tensor_scalar(out=negb[:], in0=negb[:], scalar1=-1.0, scalar2=None, op0=ALU.mult), r=[cst_b], w=[cst_b])
    fzr = FX0 + 2048
    kb.op("sp", lambda e: e.dma_start(out=fz[:], in_=g.pT[fzr:fzr + 8, :]), w=[fz_b], dma=True)
    kb.op("act", lambda e: e.activation(out=fz[:], in_=fz[:], func=AF.Exp, bias=negb[:], scale=-1.0), r=[fz_b, cst_b], w=[fz_b])
    kb.op("act", lambda e: e.activation(out=fz[:], in_=fz[:], func=AF.Ln, bias=1.0, scale=1.0), r=[fz_b], w=[fz_b])
    kb.op("dve", lambda e: e.tensor_tensor_scan(out=cp[:], data0=ones8[:], data1=fz[:], initial=0.0, op0=ALU.mult, op1=ALU.add),
          r=[fz_b, cst_b], w=[cq8_b])
    kb.op("dve", lambda e: e.tensor_scalar(out=cq8[:], in0=cp[:], scalar1=-8.0, scalar2=None, op0=ALU.mult), r=[cq8_b], w=[cq8_b])
    for i in range(NT):
        kb.op("pe", lambda e, i=i: e.transpose(out=psB[:, i * 8:(i + 1) * 8], in_=cp[:, i * 128:(i + 1) * 128], identity=identf[0:8, 0:8]),
              r=[cq8_b, cst_b], w=[psB_b])
    kb.op("act", lambda e: e.copy(out=negc[:].rearrange("p a b -> p (a b)"), in_=psB[:, 0:NT * 8]), r=[psB_b], w=[negc_b])

    for h in range(8):
        kb.op("sp", lambda e, h=h: e.dma_start(out=qraw[:], in_=g.pT[FX0 + 64 * h:FX0 + 64 * h + 64, :]), w=[raw_b["q"]], dma=True)
        kb.op("sp", lambda e, h=h: e.dma_start(out=kraw[:], in_=g.pT[FX0 + 512 + 64 * h:FX0 + 512 + 64 * h + 64, :]), w=[raw_b["k"]], dma=True)
        kb.op("sp", lambda e, h=h: e.dma_start(out=ograw[:], in_=g.pT[FX0 + 1536 + 64 * h:FX0 + 1536 + 64 * h + 64, :]), w=[og_b], dma=True)
        kb.op("sp", lambda e, h=h: e.dma_start(out=Va[:, :, 0:64], in_=g.vtok[:, 64 * h:64 * h + 64].rearrange("(j p) d -> p j d", p=128)),
              w=[Va_b], dma=True)
        kb.op("sp", lambda e, h=h: e.dma_start(out=qrow[64:65, :], in_=cq8[h:h + 1, :]), r=[cq8_b], w=[qrow_b], dma=True)
        kb.op("act", lambda e: e.copy(out=Qa[64:65, :], in_=qrow[64:65, :]), r=[qrow_b], w=[Qa_b])
        kb.op("act", lambda e: e.activation(out=sg[:], in_=ograw[:], func=AF.Sigmoid), r=[og_b], w=[sg_b])
        n = 0
        for nm, raw, wcol, dst, dst_b in (("q", qraw, qw, Qa, Qa_b), ("k", kraw, kw, Ka, Ka_b)):
            kb.op("act", lambda e, raw=raw: e.activation(out=sq[:], in_=raw[:], func=AF.Square), r=[raw_b[nm]], w=[sq_b])
            for tb in range(NB):
                p = n % 2
                n += 1
                sl = slice(tb * 512, (tb + 1) * 512)
                kb.op("pe", lambda e, p=p, sl=sl: e.matmul(psN[p][0:64, :], lhsT=onesf[0:64, 0:64], rhs=sq[:, sl], start=True, stop=True),
                      r=[sq_b, cst_b], w=[psN_b[p]])
                kb.op("act", lambda e, p=p: e.activation(out=rst[p][:], in_=psN[p][0:64, :], func=AF.Sqrt, bias=epsq[0:64, :], scale=1.0 / 64),
                      r=[psN_b[p], cst_b], w=[rst_b[p]])
                kb.op("dve", lambda e, p=p: e.reciprocal(out=rst[p][:], in_=rst[p][:]), r=[rst_b[p]], w=[rst_b[p]])
                kb.op("dve", lambda e, p=p, sl=sl, raw=raw, wcol=wcol, dst=dst: e.scalar_tensor_tensor(
                    out=dst[0:64, sl], in0=raw[:, sl], scalar=wcol[:, 0:1], in1=rst[p][:], op0=ALU.mult, op1=ALU.mult),
                    r=[raw_b[nm], rst_b[p], cst_b], w=[dst_b])
        iters = [(gq, j) for gq in range(NB) for j in range(4 * gq + 4)]

        def emit_S(n, h=h):
            gq, j = iters[n]
            col0 = max(0, j - 4 * gq) * 128
            ps_i = n % 2
            kb.op("pe", lambda e, ps_i=ps_i, j=j, gq=gq, col0=col0: e.matmul(
                psS[ps_i][:, col0:512], lhsT=Ka[0:65, j * 128:(j + 1) * 128], rhs=Qa[0:65, gq * 512 + col0:(gq + 1) * 512],
                start=True, stop=True), r=[Ka_b, Qa_b], w=[psS_b[ps_i]])

        def emit_rest(n, h=h):
            gq, j = iters[n]
            po = gq % 2
            jmax = 4 * gq + 3
            col0 = max(0, j - 4 * gq) * 128
            ps_i = n % 2
            pt_i = n % 3
            kb.op("act", lambda e, ps_i=ps_i, pt_i=pt_i, j=j, h=h, col0=col0: e.activation(
                out=PT[pt_i][:, col0:512], in_=psS[ps_i][:, col0:512], func=AF.Exp, bias=negc[:, j, h:h + 1], scale=0.125),
                r=[psS_b[ps_i], negc_b], w=[PT_b[pt_i]])
            if j >= 4 * gq:
                kb.op("pool", lambda e, pt_i=pt_i, col0=col0: e.tensor_tensor(
                    out=PT[pt_i][:, col0:col0 + 128], in0=PT[pt_i][:, col0:col0 + 128], in1=tri[:], op=ALU.mult),
                    r=[PT_b[pt_i], cst_b], w=[PT_b[pt_i]])
            kb.op("pe", lambda e, po=po, pt_i=pt_i, j=j, col0=col0, jmax=jmax: e.matmul(
                psO[po][0:65, col0:512], lhsT=Va[:, j, 0:65], rhs=PT[pt_i][:, col0:512],
                start=(j == 0), stop=(j == jmax), skip_group_check=True), r=[Va_b, PT_b[pt_i]], w=[psO_b[po]])
            if j == jmax:
                kb.op("act", lambda e, po=po: e.copy(out=dn[64:65, :], in_=psO[po][64:65, :]), r=[psO_b[po]], w=[dn_b])
                kb.op("dve", lambda e: e.reciprocal(out=dn[64:65, :], in_=dn[64:65, :]), r=[dn_b], w=[dn_b])
                kb.op("pe", lambda e: e.matmul(psB[0:64, :], lhsT=onesf[64:65, 0:64], rhs=dn[64:65, :], start=True, stop=True),
                      r=[dn_b, cst_b], w=[psB_b])
                kb.op("act", lambda e: e.copy(out=bcs[:], in_=psB[0:64, :]), r=[psB_b], w=[bcs_b])
                kb.op("dve", lambda e, po=po: e.tensor_tensor(out=o1[:], in0=psO[po][0:64, :], in1=bcs[:], op=ALU.mult),
                      r=[psO_b[po], bcs_b], w=[o1_b])
                yi = gq % 2
                kb.op("pool", lambda e, yi=yi, gq=gq: e.tensor_tensor(out=yfx[yi][:], in0=o1[:], in1=sg[:, gq * 512:(gq + 1) * 512], op=ALU.mult),
                      r=[o1_b, sg_b], w=[yfx_b[yi]])
                kb.op("sp", lambda e, yi=yi, gq=gq, h=h: e.dma_start(out=g.yT[512 + 64 * h:512 + 64 * h + 64, gq * 512:(gq + 1) * 512], in_=yfx[yi][:]),
                      r=[yfx_b[yi]], dma=True)


        emit_S(0)
        for n in range(len(iters)):
            if n + 1 < len(iters):
                emit_S(n + 1)
            emit_rest(n)


CW = 0.6065306597126334


def setup3(nc, g):
    ei = lambda n, s, d=F32: nc.dram_tensor(n, list(s), d, kind="ExternalInput").ap()
    g.rw_mu = ei("rw_mu", [RW_COLS, 1])
    g.rw_w2a2 = ei("rw_w2a2", [64, 512])
    g.rw_g2 = ei("rw_g2", [96, 512])
    g.rw_cols = ei("rw_cols", [64, 8, 8])
    g.maskG = ei("maskG", [64, 320])
    g.resetm = ei("resetm", [64, 512])


def consts3_np(d):
    c = {}
    i = np.arange(64)[:, None]
    t = np.arange(64)[None, :]
    SU = (i < t).astype(np.float32)
    U = (i <= t).astype(np.float32)
    SL = (i > t).astype(np.float32)
    c["maskG"] = np.concatenate([SU, U, SU, U, SL], axis=1)
    rm = np.ones((64, 512), np.float32)
    rm[:, ::64] = 0.0
    c["resetm"] = rm
    c["rw_mu"] = d["rw_mu"][0][:, None]
    c["rw_w2a2"] = np.concatenate([d["rw_w2"][0], d["rw_a2"][0]], axis=0)
    c["rw_g2"] = d["rw_g2"][0]
    cols = np.zeros((64, 8, 8), np.float32)
    for j, nm in enumerate(["rw_w0", "rw_a0", "rw_k_k", "rw_k_a", "rw_r_k", "rw_gn_w", "rw_gn_b"]):
        cols[:, :, j] = d[nm][0].reshape(8, 64).T
    c["rw_cols"] = cols
    return c


def phase3(kb, g):
    nc, T, NT, NB = g.nc, g.T, g.NT, g.NB
    A = lambda n, s, d=F32: kb.sbuf("r3_" + n, s, d)
    onesf = A("onesf", [64, 64]); identb = A("identb", [64, 64], BF16); identf = A("identf", [64, 64])
    maskG = A("maskG", [64, 320]); resetm = A("resetm", [64, 512])
    w2a2 = A("w2a2", [32, 512]); a2t = A("a2t", [32, 512]); g2 = A("g2", [96, 512]); cols = A("cols", [64, 8, 8])
    mu_rkv = A("mu_rkv", [64, 24]); mu_wa = A("mu_wa", [32, 1]); mu_ad = A("mu_ad", [32, 1]); mu_g = A("mu_g", [96, 1])
    eps12 = A("eps12", [64, 1]); epsgn = A("epsgn", [64, 1])
    cst = kb.buf("cst3")
    kb.op("pool", lambda e: e.memset(onesf[:], 1.0), w=[cst])
    kb.op("pool", lambda e: e.memset(eps12[:], 0.0), w=[cst])
    kb.op("pool", lambda e: e.memset(epsgn[:], 64e-5), w=[cst])
    kb.op("sp", lambda e: e.dma_start(out=identb[:], in_=g.ident_b[0:64, 0:64]), w=[cst], dma=True)
    kb.op("sp", lambda e: e.dma_start(out=identf[:], in_=g.ident_f[0:64, 0:64]), w=[cst], dma=True)
    kb.op("sp", lambda e: e.dma_start(out=maskG[:], in_=g.maskG), w=[cst], dma=True)
    kb.op("sp", lambda e: e.dma_start(out=resetm[:], in_=g.resetm), w=[cst], dma=True)
    kb.op("sp", lambda e: e.dma_start(out=w2a2[:], in_=g.rw_w2a2[0:32, :]), w=[cst], dma=True)
    kb.op("sp", lambda e: e.dma_start(out=a2t[:], in_=g.rw_w2a2[32:64, :]), w=[cst], dma=True)
    kb.op("sp", lambda e: e.dma_start(out=g2[:], in_=g.rw_g2), w=[cst], dma=True)
    kb.op("sp", lambda e: e.dma_start(out=cols[:], in_=g.rw_cols), w=[cst], dma=True)
    for j in range(24):
        kb.op("sp", lambda e, j=j: e.dma_start(out=mu_rkv[:, j:j + 1], in_=g.rw_mu[64 * j:64 * j + 64, :]), w=[cst], dma=True)
    kb.op("sp", lambda e: e.dma_start(out=mu_wa[:], in_=g.rw_mu[1536:1568, :]), w=[cst], dma=True)
    kb.op("sp", lambda e: e.dma_start(out=mu_ad[:], in_=g.rw_mu[1568:1600, :]), w=[cst], dma=True)
    kb.op("sp", lambda e: e.dma_start(out=mu_g[:], in_=g.rw_mu[1600:1696, :]), w=[cst], dma=True)

    NS = 2
    cur = [A(f"cur{i}", [96, 512]) for i in range(3)]; prv = [A(f"prv{i}", [96, 512]) for i in range(3)]
    ld_b = kb.bufs(3, "ld")
    ldn = [0]
    wam = A("wam", [32, 512]); adm = A("adm", [32, 512]); gdm = A("gdm", [96, 512]); wam_b = kb.buf("wam"); adm_b = kb.buf("adm"); gdm_b = kb.buf("gdm")
    rm = A("rm", [64, 512]); km = A("km", [64, 512]); vm = A("vm", [64, 512])
    rm_b = kb.buf("rm"); km_b = kb.buf("km"); vm_b = kb.buf("vm")
    sgw = A("sgw", [64, 512]); asig = A("asig", [64, 512]); ggL = [A(f"gg{i}", [64, 512]) for i in range(2)]
    sgw_b = kb.buf("sgw"); asig_b = kb.buf("asig"); ggL_b = kb.bufs(2, "gg")
    kk = A("kk", [64, 512]); t1 = A("t1", [64, 512]); t2 = A("t2", [64, 512]); kmod = A("kmod", [64, 512]); bb = A("bb", [64, 512])
    kk_b = kb.buf("kk"); t1_b = kb.buf("t1"); t2_b = kb.buf("t2"); kmod_b = kb.buf("kmod"); bb_b = kb.buf("bb")
    cumS = A("cumS", [64, 512]); ginclL = [A(f"gincl{i}", [64, 512]) for i in range(2)]; ginv = A("ginv", [64, 512]); gexcl = A("gexcl", [64, 512])
    cum_b = kb.buf("cum"); ginclL_b = kb.bufs(2, "gincl"); ginv_b = kb.buf("ginv"); gexcl_b = kb.buf("gexcl")
    bonL = [A(f"bon{i}", [64, 512]) for i in range(2)]; bonL_b = kb.bufs(2, "bon")
    AR = [A(f"AR{i}", [64, 8, 128], BF16) for i in range(2)]; BK = [A(f"BK{i}", [64, 8, 128], BF16) for i in range(2)]
    vb = [A(f"vb{i}", [64, 512], BF16) for i in range(2)]
    AR_b = kb.bufs(2, "AR"); BK_b = kb.bufs(2, "BK"); vb_b = kb.bufs(2, "vb")
    TM = [A(f"TM{i}", [64, 320], BF16) for i in range(NS)]; TM_b = kb.bufs(NS, "TM"); TMx_b = kb.bufs(NS, "TMx")
    NM = [A(f"NM{i}", [64, 320], BF16) for i in range(NS)]; NM_b = kb.bufs(NS, "NM")
    DB = [[A(f"DB{i}_{j}", [64, 192], BF16) for j in range(2)] for i in range(NS)]
    DB_b = [kb.bufs(2, f"DB{i}") for i in range(NS)]
    AW = [A(f"AW{i}", [64, 128], BF16) for i in range(NS)]; AW_b = kb.bufs(NS, "AW")
    G1 = [A(f"G1{i}", [64, 64]) for i in range(NS)]; G1_b = kb.bufs(NS, "G1")
    Hg = [A(f"Hg{i}", [64, 64]) for i in range(NS)]; Hg_b = kb.bufs(NS, "Hg")
    Ry = [A(f"Ry{i}", [64, 64]) for i in range(NS)]; Ry_b = kb.bufs(NS, "Ry")
    ST = [[A(f"ST{h}_{j}", [64, 64]) for j in range(2)] for h in range(8)]
    ST_b = [kb.bufs(2, f"ST{h}") for h in range(8)]
    ysb = A("ysb", [64, 512]); ysq = A("ysq", [64, 512]); ymean = A("ymean", [64, 512]); yvar = A("yvar", [64, 512])
    ysb_b = kb.buf("ysb"); ysq_b = kb.buf("ysq"); ymean_b = kb.buf("ymean"); yvar_b = kb.buf("yvar")
    yout = [A(f"yout{i}", [64, 512], BF16) for i in range(2)]; yout_b = kb.bufs(2, "yout")
    tw = A("tw", [32, 512]); sgd = A("sgd", [96, 512]); tw_b = kb.buf("tw"); sgd_b = kb.buf("sgd")
    psT = kb.psum("r3psT", [128, 1024], BF16); psT_b = kb.buf("psT")
    psG = kb.psum("r3psG", [128, 512], F32); psG_b = kb.buf("psG")
    psD = [kb.psum(f"r3psD{i}", [128, 512], F32) for i in range(2)]; psD_b = kb.bufs(2, "psD")
    psA = kb.psum("r3psA", [128, 512], F32)
    psX_b = kb.buf("psA"); psAW_b = psX_b; psGp_b = psX_b; psH_b = psX_b; psR_b = psX_b
    psYL = [kb.psum(f"r3psY{i}", [128, 512], F32) for i in range(2)]; psYL_b = kb.bufs(2, "psY")
    psL = kb.psum("r3psL", [128, 512], F32); psL_b = kb.buf("psL")

    for h in range(8):
        kb.op("pool", lambda e, h=h: e.memset(ST[h][0][:], 0.0), w=[ST_b[h][0]])

    def load_mixed(rows0, nrows, tb, mucol, dst, dst_b):
        i = ldn[0] % 3
        ldn[0] += 1
        t0 = tb * 512
        kb.op("sp", lambda e: e.dma_start(out=cur[i][0:nrows, :], in_=g.pT[rows0:rows0 + nrows, t0:t0 + 512]), w=[ld_b[i]], dma=True)
        if tb == 0:
            kb.op("pool", lambda e: e.memset(prv[i][0:nrows, 0:1], 0.0), w=[ld_b[i]])
            kb.op("sp", lambda e: e.dma_start(out=prv[i][0:nrows, 1:512], in_=g.pT[rows0:rows0 + nrows, 0:511]), w=[ld_b[i]], dma=True)
        else:
            kb.op("sp", lambda e: e.dma_start(out=prv[i][0:nrows, :], in_=g.pT[rows0:rows0 + nrows, t0 - 1:t0 + 511]), w=[ld_b[i]], dma=True)
        kb.op("pool", lambda e: e.tensor_tensor(out=prv[i][0:nrows, :], in0=prv[i][0:nrows, :], in1=cur[i][0:nrows, :], op=ALU.subtract),
              r=[ld_b[i]], w=[ld_b[i]])
        kb.op("dve", lambda e: e.scalar_tensor_tensor(out=dst[0:nrows, :], in0=prv[i][0:nrows, :], scalar=mucol, in1=cur[i][0:nrows, :],
                                                       op0=ALU.mult, op1=ALU.add), r=[ld_b[i], cst], w=[dst_b])

    def r1_stage(h, tb):
        z = h % 2
        pz = tb * 8 + h
        gincl = ginclL[z]; gincl_b = ginclL_b[z]; bon = bonL[z]; bon_b = bonL_b[z]; gg = ggL[z]; gg_b = ggL_b[z]
        psY = psYL[z]; psY_b = psYL_b[z]
        C = lambda j, h=h: cols[:, h, j:j + 1]
        hc = slice(64 * h, 64 * h + 64)
        load_mixed(64 * h, 64, tb, mu_rkv[:, h:h + 1], rm, rm_b)
        load_mixed(512 + 64 * h, 64, tb, mu_rkv[:, 8 + h:9 + h], km, km_b)
        load_mixed(1024 + 64 * h, 64, tb, mu_rkv[:, 16 + h:17 + h], vm, vm_b)
        kb.op("pe", lambda e, hc=hc: e.matmul(psL[0:64, :], lhsT=w2a2[0:32, hc], rhs=tw[0:32, :], start=True, stop=True),
              r=[tw_b, cst], w=[psL_b])
        kb.op("act", lambda e, C=C: e.activation(out=sgw[:], in_=psL[0:64, :], func=AF.Sigmoid, bias=C(0), scale=1.0),
              r=[psL_b, cst], w=[sgw_b])
        kb.op("pe", lambda e, hc=hc: e.matmul(psL[0:64, :], lhsT=a2t[0:32, hc], rhs=adm[0:32, :], start=True, stop=True),
              r=[adm_b, cst], w=[psL_b])
        kb.op("act", lambda e, C=C: e.activation(out=asig[:], in_=psL[0:64, :], func=AF.Sigmoid, bias=C(1), scale=1.0),
              r=[psL_b, cst], w=[asig_b])
        kb.op("pe", lambda e, hc=hc: e.matmul(psL[0:64, :], lhsT=g2[0:96, hc], rhs=sgd[0:96, :], start=True, stop=True),
              r=[sgd_b, cst], w=[psL_b])
        kb.op("act", lambda e: e.copy(out=gg[:], in_=psL[0:64, :]), r=[psL_b], w=[gg_b])
        kb.op("dve", lambda e, C=C: e.tensor_scalar(out=kk[:], in0=km[:], scalar1=C(2), scalar2=None, op0=ALU.mult), r=[km_b, cst], w=[kk_b])
        kb.op("pool", lambda e: e.tensor_tensor(out=t1[:], in0=kk[:], in1=kk[:], op=ALU.mult), r=[kk_b], w=[t1_b])
        kb.op("pe", lambda e: e.matmul(psL[0:64, :], lhsT=onesf[:, :], rhs=t1[:], start=True, stop=True), r=[t1_b, cst], w=[psL_b])
        kb.op("act", lambda e: e.activation(out=t2[:], in_=psL[0:64, :], func=AF.Sqrt, bias=eps12[:], scale=1.0), r=[psL_b, cst], w=[t2_b])
        kb.op("dve", lambda e: e.tensor_scalar(out=t2[:], in0=t2[:], scalar1=1e-12, scalar2=None, op0=ALU.max), r=[t2_b], w=[t2_b])
        kb.op("dve", lambda e: e.reciprocal(out=t2[:], in_=t2[:]), r=[t2_b], w=[t2_b])
        kb.op("dve", lambda e: e.tensor_tensor(out=kk[:], in0=kk[:], in1=t2[:], op=ALU.mult), r=[kk_b, t2_b], w=[kk_b])
        kb.op("dve", lambda e, C=C: e.tensor_scalar(out=t1[:], in0=asig[:], scalar1=-1.0, scalar2=C(3), op0=ALU.add, op1=ALU.mult),
              r=[asig_b, cst], w=[t1_b])
        kb.op("dve", lambda e: e.scalar_tensor_tensor(out=kmod[:], in0=t1[:], scalar=1.0, in1=km[:], op0=ALU.add, op1=ALU.mult),
              r=[t1_b, km_b], w=[kmod_b])
        kb.op("pool", lambda e: e.tensor_tensor(out=bb[:], in0=kk[:], in1=asig[:], op=ALU.mult), r=[kk_b, asig_b], w=[bb_b])
        kb.op("dve", lambda e: e.tensor_tensor_scan(out=cumS[:], data0=resetm[:], data1=sgw[:], initial=0.0, op0=ALU.mult, op1=ALU.add),
              r=[sgw_b, cst], w=[cum_b])
        kb.op("act", lambda e: e.activation(out=gincl[:], in_=cumS[:], func=AF.Exp, scale=-CW), r=[cum_b], w=[gincl_b])
        kb.op("act", lambda e: e.activation(out=ginv[:], in_=cumS[:], func=AF.Exp, scale=CW), r=[cum_b], w=[ginv_b])
        kb.op("pool", lambda e: e.tensor_tensor(out=t2[:], in0=cumS[:], in1=sgw[:], op=ALU.subtract), r=[cum_b, sgw_b], w=[t2_b])
        kb.op("act", lambda e: e.activation(out=gexcl[:], in_=t2[:], func=AF.Exp, scale=-CW), r=[t2_b], w=[gexcl_b])
        v4 = lambda t: t[:].rearrange("p (c t) -> p c t", c=8)
        kb.op("dve", lambda e, z=z: e.scalar_tensor_tensor(out=AR[z][:, :, 0:64], in0=v4(kk), scalar=-1.0, in1=v4(gexcl), op0=ALU.mult, op1=ALU.mult),
              r=[kk_b, gexcl_b], w=[AR_b[z]])
        kb.op("pool", lambda e, z=z: e.tensor_tensor(out=AR[z][:, :, 64:128], in0=v4(rm), in1=v4(gincl), op=ALU.mult),
              r=[rm_b, gincl_b], w=[AR_b[z]])
        kb.op("dve", lambda e, z=z: e.tensor_tensor(out=BK[z][:, :, 0:64], in0=v4(bb), in1=v4(ginv), op=ALU.mult),
              r=[bb_b, ginv_b], w=[BK_b[z]])
        kb.op("pool", lambda e, z=z: e.tensor_tensor(out=BK[z][:, :, 64:128], in0=v4(kmod), in1=v4(ginv), op=ALU.mult),
              r=[kmod_b, ginv_b], w=[BK_b[z]])
        kb.op("act", lambda e, z=z: e.copy(out=vb[z][:], in_=vm[:]), r=[vm_b], w=[vb_b[z]])
        kb.op("dve", lambda e, C=C: e.scalar_tensor_tensor(out=t1[:], in0=rm[:], scalar=C(4), in1=kmod[:], op0=ALU.mult, op1=ALU.mult),
              r=[rm_b, kmod_b, cst], w=[t1_b])
        kb.op("pe", lambda e: e.matmul(psL[0:64, :], lhsT=onesf[:, :], rhs=t1[:], start=True, stop=True), r=[t1_b, cst], w=[psL_b])
        kb.op("dve", lambda e: e.tensor_tensor(out=bon[:], in0=psL[0:64, :], in1=vm[:], op=ALU.mult), r=[psL_b, vm_b], w=[bon_b])


    def unit_chain(h, tb):
        z = h % 2
        pz = tb * 8 + h
        gincl = ginclL[z]; gincl_b = ginclL_b[z]; bon = bonL[z]; bon_b = bonL_b[z]; gg = ggL[z]; gg_b = ggL_b[z]
        psY = psYL[z]; psY_b = psYL_b[z]
        C = lambda j, h=h: cols[:, h, j:j + 1]
        for c in range(8):
            u = z
            cs = slice(c * 64, c * 64 + 64)
            gC = gincl[:, c * 64 + 63:c * 64 + 64]
            srcs = [(BK[z][:, c, 0:64], BK_b[z]), (BK[z][:, c, 64:128], BK_b[z]), (vb[z][:, cs], vb_b[z]), (AR[z][:, c, 0:64], AR_b[z])]
            for k4, (src, sb) in enumerate(srcs):
                kb.op("pe", lambda e, k4=k4, src=src: e.transpose(out=psT[0:64, k4 * 64:(k4 + 1) * 64], in_=src, identity=identb[:]),
                      r=[sb, cst], w=[psT_b])
            kb.op("act", lambda e, u=u: e.copy(out=TM[u][:, 0:256], in_=psT[0:64, 0:256]), r=[psT_b], w=[TM_b[u]])
            yield
            kb.op("pe", lambda e, z=z, c=c: e.matmul(psG[0:64, 0:128], lhsT=BK[z][:, c, 0:64], rhs=AR[z][:, c, :], start=True, stop=True),
                  r=[BK_b[z], AR_b[z]], w=[psG_b])
            kb.op("pe", lambda e, z=z, c=c: e.matmul(psG[0:64, 128:256], lhsT=BK[z][:, c, 64:128], rhs=AR[z][:, c, :], start=True, stop=True),
                  r=[BK_b[z], AR_b[z]], w=[psG_b])
            kb.op("pe", lambda e, z=z, c=c: e.matmul(psG[0:64, 256:320], lhsT=AR[z][:, c, 0:64], rhs=BK[z][:, c, 0:64], start=True, stop=True),
                  r=[BK_b[z], AR_b[z]], w=[psG_b])
            kb.op("dve", lambda e, u=u: e.tensor_tensor(out=NM[u][:], in0=psG[0:64, 0:320], in1=maskG[:], op=ALU.mult),
                  r=[psG_b, cst], w=[NM_b[u]])
            yield
            kb.op("pe", lambda e, u=u: e.matmul(psA[0:64, 0:64], lhsT=NM[u][:, 128:192], rhs=TM[u][:, 128:192], start=True, stop=True),
                  r=[NM_b[u], TM_b[u]], w=[psX_b])
            kb.op("act", lambda e, u=u: e.copy(out=TM[u][:, 256:320], in_=psA[0:64, 0:64]), r=[psX_b], w=[TMx_b[u]])
            yield
            d0, d1 = DB[u][0], DB[u][1]
            pd = psD[u % 2]
            pdb = psD_b[u % 2]
            kb.op("dve", lambda e, u=u, d1=d1: e.tensor_tensor(out=d1[:, 0:64], in0=NM[u][:, 0:64], in1=identb[:], op=ALU.add),
                  r=[NM_b[u], cst], w=[DB_b[u][1]])
            kb.op("pe", lambda e, u=u, pd=pd: e.matmul(pd[0:64, 64:128], lhsT=NM[u][:, 256:320], rhs=NM[u][:, 0:64], start=True, stop=True),
                  r=[NM_b[u]], w=[pdb])
            kb.op("pe", lambda e, u=u, pd=pd: e.matmul(pd[0:64, 128:192], lhsT=NM[u][:, 0:64], rhs=NM[u][:, 256:320], start=True, stop=True),
                  r=[NM_b[u]], w=[pdb])
            kb.op("act", lambda e, d1=d1, pd=pd: e.copy(out=d1[:, 64:192], in_=pd[0:64, 64:192]), r=[pdb], w=[DB_b[u][1]])
            ci = 1
            for lvl in range(1, 6):
                dc, dn_ = DB[u][ci], DB[u][1 - ci]
                dcb, dnb = DB_b[u][ci], DB_b[u][1 - ci]
                kb.op("pe", lambda e, dc=dc, pd=pd: e.matmul(pd[0:64, 0:128], lhsT=dc[:, 128:192], rhs=dc[:, 0:128], start=True, stop=True),
                      r=[dcb], w=[pdb])
                if lvl < 5:
                    kb.op("pe", lambda e, dc=dc, pd=pd: e.matmul(pd[0:64, 128:192], lhsT=dc[:, 64:128], rhs=dc[:, 128:192], start=True, stop=True),
                          r=[dcb], w=[pdb])
                kb.op("dve", lambda e, dc=dc, dn_=dn_, pd=pd: e.tensor_tensor(out=dn_[:, 0:64], in0=pd[0:64, 0:64], in1=dc[:, 0:64], op=ALU.add),
                      r=[pdb, dcb], w=[dnb])
                if lvl < 5:
                    kb.op("act", lambda e, dn_=dn_, pd=pd: e.copy(out=dn_[:, 64:192], in_=pd[0:64, 64:192]), r=[pdb], w=[dnb])
                yield
                ci = 1 - ci
            Tm, Tm_b = DB[u][ci], DB_b[u][ci]
            yield
            kb.op("pe", lambda e, u=u, Tm=Tm: e.matmul(psA[0:64, 64:192], lhsT=Tm[:, 0:64], rhs=TM[u][:, 192:320], start=True, stop=True),
                  r=[Tm_b, TM_b[u], TMx_b[u]], w=[psAW_b])
            kb.op("act", lambda e, u=u: e.copy(out=AW[u][:], in_=psA[0:64, 64:192]), r=[psAW_b], w=[AW_b[u]])
            yield
            kb.op("pe", lambda e, u=u: e.matmul(psA[0:64, 192:256], lhsT=AW[u][:, 0:64], rhs=TM[u][:, 0:64], start=True, stop=True),
                  r=[AW_b[u], TM_b[u]], w=[psGp_b])
            kb.op("dve", lambda e, u=u: e.tensor_tensor(out=G1[u][:], in0=psA[0:64, 192:256], in1=identf[:], op=ALU.add),
                  r=[psGp_b, cst], w=[G1_b[u]])
            yield
            kb.op("pe", lambda e, u=u: e.matmul(psA[0:64, 256:320], lhsT=TM[u][:, 0:64], rhs=AW[u][:, 64:128], start=True, stop=False),
                  r=[AW_b[u], TM_b[u]], w=[psH_b])
            kb.op("pe", lambda e, u=u: e.matmul(psA[0:64, 256:320], lhsT=TM[u][:, 64:128], rhs=TM[u][:, 128:192], start=False, stop=True),
                  r=[TM_b[u]], w=[psH_b])
            kb.op("dve", lambda e, u=u, gC=gC: e.tensor_scalar(out=Hg[u][:], in0=psA[0:64, 256:320], scalar1=gC, scalar2=None, op0=ALU.mult),
                  r=[psH_b, gincl_b], w=[Hg_b[u]])
            yield
            kb.op("pe", lambda e, u=u: e.matmul(psA[0:64, 320:384], lhsT=AW[u][:, 0:64], rhs=NM[u][:, 64:128], start=True, stop=True),
                  r=[AW_b[u], NM_b[u]], w=[psR_b])
            kb.op("dve", lambda e, u=u, z=z, c=c: e.tensor_tensor(out=Ry[u][:], in0=psA[0:64, 320:384], in1=AR[z][:, c, 64:128], op=ALU.add),
                  r=[psR_b, AR_b[z]], w=[Ry_b[u]])
            yield
            sc = (tb * 8 + c) % 2
            So, Sn = ST[h][sc], ST[h][1 - sc]
            Sob, Snb = ST_b[h][sc], ST_b[h][1 - sc]
            kb.op("pe", lambda e, u=u, cs=cs: e.matmul(psY[0:64, cs], lhsT=AW[u][:, 64:128], rhs=NM[u][:, 64:128], start=True, stop=False, skip_group_check=True),
                  r=[AW_b[u], NM_b[u]], w=[psY_b])
            kb.op("pe", lambda e, u=u, cs=cs: e.matmul(psY[0:64, cs], lhsT=TM[u][:, 128:192], rhs=NM[u][:, 192:256], start=False, stop=False, skip_group_check=True),
                  r=[TM_b[u], NM_b[u]], w=[psY_b])
            kb.op("pe", lambda e, u=u, cs=cs, So=So: e.matmul(psY[0:64, cs], lhsT=So[:], rhs=Ry[u][:], start=False, stop=True, skip_group_check=True),
                  r=[Sob, Ry_b[u]], w=[psY_b])
            yield
            kb.op("pe", lambda e, u=u, So=So: e.matmul(psA[0:64, 384:448], lhsT=G1[u][:], rhs=So[:], start=True, stop=True),
                  r=[G1_b[u], Sob], w=[psX_b])
            kb.op("dve", lambda e, u=u, Sn=Sn, gC=gC: e.scalar_tensor_tensor(out=Sn[:], in0=psA[0:64, 384:448], scalar=gC, in1=Hg[u][:], op0=ALU.mult, op1=ALU.add),
                  r=[psX_b, Hg_b[u], gincl_b], w=[Snb])
            yield

    def gn_stage(h, tb):
        z = h % 2
        pz = tb * 8 + h
        gincl = ginclL[z]; gincl_b = ginclL_b[z]; bon = bonL[z]; bon_b = bonL_b[z]; gg = ggL[z]; gg_b = ggL_b[z]
        psY = psYL[z]; psY_b = psYL_b[z]
        C = lambda j, h=h: cols[:, h, j:j + 1]
        kb.op("act", lambda e: e.copy(out=ysb[:], in_=psY[0:64, :]), r=[psY_b], w=[ysb_b])
        kb.op("pool", lambda e: e.tensor_tensor(out=ysq[:], in0=ysb[:], in1=ysb[:], op=ALU.mult), r=[ysb_b], w=[ysq_b])
        kb.op("pe", lambda e: e.matmul(psL[0:64, :], lhsT=onesf[:, :], rhs=ysb[:], start=True, stop=True), r=[ysb_b, cst], w=[psL_b])
        kb.op("act", lambda e: e.activation(out=ymean[:], in_=psL[0:64, :], func=AF.Identity, scale=1.0 / 64), r=[psL_b], w=[ymean_b])
        kb.op("pe", lambda e: e.matmul(psL[0:64, :], lhsT=onesf[:, :], rhs=ysq[:], start=True, stop=True), r=[ysq_b, cst], w=[psL_b])
        kb.op("pool", lambda e: e.tensor_tensor(out=ysq[:], in0=ymean[:], in1=ymean[:], op=ALU.mult), r=[ymean_b, ysq_b], w=[ysq_b])
        kb.op("dve", lambda e: e.scalar_tensor_tensor(out=yvar[:], in0=psL[0:64, :], scalar=1.0 / 64, in1=ysq[:], op0=ALU.mult, op1=ALU.subtract),
              r=[psL_b, ysq_b], w=[yvar_b])
        kb.op("act", lambda e: e.activation(out=yvar[:], in_=yvar[:], func=AF.Sqrt, bias=epsgn[:], scale=1.0), r=[yvar_b, cst], w=[yvar_b])
        kb.op("dve", lambda e: e.reciprocal(out=yvar[:], in_=yvar[:]), r=[yvar_b], w=[yvar_b])
        kb.op("pool", lambda e: e.tensor_tensor(out=ysb[:], in0=ysb[:], in1=ymean[:], op=ALU.subtract), r=[ysb_b, ymean_b], w=[ysb_b])
        kb.op("dve", lambda e, C=C: e.scalar_tensor_tensor(out=ysb[:], in0=ysb[:], scalar=C(5), in1=yvar[:], op0=ALU.mult, op1=ALU.mult),
              r=[ysb_b, yvar_b, cst], w=[ysb_b])
        kb.op("dve", lambda e, C=C: e.scalar_tensor_tensor(out=ysb[:], in0=ysb[:], scalar=C(6), in1=bon[:], op0=ALU.add, op1=ALU.add),
              r=[ysb_b, bon_b, cst], w=[ysb_b])
        yo = pz % 2
        kb.op("pool", lambda e, yo=yo: e.tensor_tensor(out=yout[yo][:], in0=ysb[:], in1=gg[:], op=ALU.mult), r=[ysb_b, gg_b], w=[yout_b[yo]])
        kb.op("sp", lambda e, yo=yo, h=h, tb=tb: e.dma_start(out=g.yT[64 * h:64 * h + 64, tb * 512:(tb + 1) * 512], in_=yout[yo][:]),
              r=[yout_b[yo]], dma=True)
    for tb in range(NB):
        load_mixed(1536, 32, tb, mu_wa[:, 0:1], wam, wam_b)
        load_mixed(1568, 32, tb, mu_ad[:, 0:1], adm, adm_b)
        load_mixed(1600, 96, tb, mu_g[:, 0:1], gdm, gdm_b)
        kb.op("act", lambda e: e.activation(out=tw[0:32, :], in_=wam[0:32, :], func=AF.Tanh), r=[wam_b], w=[tw_b])
        kb.op("act", lambda e: e.activation(out=sgd[:], in_=gdm[:], func=AF.Sigmoid), r=[gdm_b], w=[sgd_b])
        for hp in range(4):
            heads = (2 * hp, 2 * hp + 1)
            for h in heads:
                r1_stage(h, tb)
            alive = [unit_chain(h, tb) for h in heads]
            while alive:
                for gen in list(alive):
                    try:
                        next(gen)
                    except StopIteration:
                        alive.remove(gen)
            for h in heads:
                gn_stage(h, tb)


def setup4(nc, g, out_name="out"):
    ei = lambda n, s, d=F32: nc.dram_tensor(n, list(s), d, kind="ExternalInput").ap()
    T = g.T
    g.w_o = ei("w_o", [D, D])
    g.ln1_w = ei("ln1_w", [1, D]); g.ln1_b = ei("ln1_b", [1, D])
    g.ln2_w = ei("ln2_w", [1, D]); g.ln2_b = ei("ln2_b", [1, D])
    g.router_w = ei("router_w", [D, 32]); g.router_b = ei("router_b", [1, 32])
    g.exp_w1 = ei("exp_w1", [32, D, 2048]); g.exp_b1 = ei("exp_b1", [32, 2048])
    g.exp_w2 = ei("exp_w2", [32, D, D]); g.exp_b2 = ei("exp_b2", [32, D])
    g.h1f = g.scratch("h1f", [T, D])
    g.h1T = g.scratch("h1T", [D, T], BF16)
    g.yacc = g.scratch("yacc", [T, D])
    g.out = nc.dram_tensor(out_name, [T, D], F32, kind="ExternalOutput").ap()


def _ln_tile(kb, src, src_b, dst, dst_b, st, mv, rs, st_b, epsc, cst, gam, bet):
    for hf in range(2):
        kb.op("dve", lambda e, hf=hf: e.bn_stats(out=st[:, hf, :], in_=src[:, hf * 512:(hf + 1) * 512]), r=[src_b], w=[st_b])
    kb.op("dve", lambda e: e.bn_aggr(out=mv[:], in_=st[:].rearrange("p a b -> p (a b)")), r=[st_b], w=[st_b])
    kb.op("act", lambda e: e.activation(out=rs[:], in_=mv[:, 1:2], func=AF.Sqrt, bias=epsc[:], scale=1.0), r=[st_b, cst], w=[st_b])
    kb.op("dve", lambda e: e.reciprocal(out=rs[:], in_=rs[:]), r=[st_b], w=[st_b])
    kb.op("dve", lambda e: e.tensor_scalar(out=dst[:], in0=src[:], scalar1=mv[:, 0:1], scalar2=rs[:], op0=ALU.subtract, op1=ALU.mult),
          r=[src_b, st_b], w=[dst_b])
    kb.op("dve", lambda e: e.tensor_tensor(out=dst[:], in0=dst[:], in1=gam[:], op=ALU.mult), r=[dst_b, cst], w=[dst_b])
    kb.op("dve", lambda e: e.tensor_tensor(out=dst[:], in0=dst[:], in1=bet[:], op=ALU.add), r=[dst_b, cst], w=[dst_b])


def phase4(kb, g, n_exp=32, sections="ABC"):
    nc, T, NT, NB = g.nc, g.T, g.NT, g.NB
    kb.ps_stack.close(); kb.ps_stack = None
    gates = kb.sbuf("m4_gates", [128, NT, 32], F32); gates_b = kb.buf("gates")
    kb.begin_phase()
    A = lambda n, s, d=F32: kb.sbuf("m4_" + n, s, d)
    cst = kb.buf("cst4")
    wo = A("wo", [128, KC, D], BF16)
    rwt = A("rwt", [128, KC, 32]); rbt = A("rbt", [128, 32])
    gam1 = A("gam1", [128, D]); bet1 = A("bet1", [128, D])
    identf = A("identf", [128, 128]); epsc = A("epsc", [128, 1])
    kb.op("dve", lambda e: e.memset(epsc[:], LN_EPS), w=[cst])
    kb.op("sp", lambda e: e.dma_start(out=identf[:], in_=g.ident_f), w=[cst], dma=True)
    for nm, t_, src in (("g1", gam1, g.ln1_w), ("b1", bet1, g.ln1_b)):
        kb.op("sp", lambda e, t_=t_, src=src: e.dma_start(out=t_[:], in_=src.broadcast_to([128, D])), w=[cst], dma=True)
    kb.op("sp", lambda e: e.dma_start(out=rbt[:], in_=g.router_b.broadcast_to([128, 32])), w=[cst], dma=True)
    kb.op("sp", lambda e: e.dma_start(out=rwt[:], in_=g.router_w.rearrange("(kc p) n -> p kc n", p=128)), w=[cst], dma=True)
    for kc in range(KC):
        kb.op("pool", lambda e, kc=kc: e.dma_start(out=wo[:, kc, :], in_=g.w_o[kc * 128:(kc + 1) * 128, :]), w=[cst], dma=True)

    ymt = [A(f"ymt{i}", [128, KC, 128], BF16) for i in range(2)]; ymt_b = kb.bufs(2, "ymt")
    h0t = [A(f"h0t{i}", [128, D]) for i in range(2)]; h0t_b = kb.bufs(2, "h0t")
    sres = [A(f"sres{i}", [128, D]) for i in range(2)]; sres_b = kb.bufs(2, "sres")
    h1t = [A(f"h1t{i}", [128, D]) for i in range(2)]; h1t_b = kb.bufs(2, "h1t")
    st = A("st", [128, 2, 6]); mv = A("mv", [128, 2]); rs = A("rs", [128, 1]); st_b = kb.buf("st")
    h1Tf = A("h1Tf", [128, KC, 128]); h1Tf_b = kb.buf("h1Tf")
    h1Tb = [A(f"h1Tb{i}", [128, KC, 128], BF16) for i in range(2)]; h1Tb_b = kb.bufs(2, "h1Tb")
    lg = A("lg", [128, 32]); mx8 = A("mx8", [128, 8]); msk = A("msk", [128, 32]); ee = A("ee", [128, 32]); ssum = A("ssum", [128, 1])
    nm0 = A("nm0", [128, 1]); lg_b = kb.buf("lg")
    ps = [kb.psum(f"m4ps{i}", [128, 512], F32) for i in range(8)]; ps_b = kb.bufs(8, "m4ps")

    import os
    STOPAT = int(os.environ.get('STOPAT', '99'))
    for i in range(NT):
        s = i % 2
        tsl = slice(i * 128, (i + 1) * 128)
        kb.op("sp", lambda e, s=s, tsl=tsl: e.dma_start(out=ymt[s][:], in_=g.yT[:, tsl].rearrange("(kc p) t -> p kc t", p=128)), w=[ymt_b[s]], dma=True)
        kb.op("sp", lambda e, s=s, tsl=tsl: e.dma_start(out=h0t[s][:], in_=g.h0f[tsl, :]), w=[h0t_b[s]], dma=True)
        if STOPAT <= 1:
            continue
        for hf in range(2):
            for kc in range(KC):
                kb.op("pe", lambda e, s=s, hf=hf, kc=kc: e.matmul(ps[hf][:, :], lhsT=ymt[s][:, kc, :], rhs=wo[:, kc, hf * 512:(hf + 1) * 512],
                                                                 start=(kc == 0), stop=(kc == KC - 1)), r=[ymt_b[s], cst], w=[ps_b[hf]])
            kb.op("dve", lambda e, s=s, hf=hf: e.scalar_tensor_tensor(out=sres[s][:, hf * 512:(hf + 1) * 512], in0=h0t[s][:, hf * 512:(hf + 1) * 512],
                                                                     scalar=ALPHA, in1=ps[hf][:, :], op0=ALU.mult, op1=ALU.add),
                  r=[h0t_b[s], ps_b[hf]], w=[sres_b[s]])
        if STOPAT <= 2:
            continue
        _ln_tile(kb, sres[s], sres_b[s], h1t[s], h1t_b[s], st, mv, rs, st_b, epsc, cst, gam1, bet1)
        if STOPAT <= 3:
            continue
        kb.op("sp", lambda e, s=s, tsl=tsl: e.dma_start(out=g.h1f[tsl, :], in_=h1t[s][:]), r=[h1t_b[s]], dma=True)
        if STOPAT <= 4:
            continue
        for hf in range(2):
            p = 2 + hf
            for c in range(4):
                kc = hf * 4 + c
                kb.op("pe", lambda e, s=s, p=p, c=c, kc=kc: e.transpose(out=ps[p][:, c * 128:(c + 1) * 128], in_=h1t[s][:, kc * 128:(kc + 1) * 128], identity=identf[:]),
                      r=[h1t_b[s], cst], w=[ps_b[p]])
            kb.op("act", lambda e, p=p, hf=hf: e.copy(out=h1Tf[:, hf * 4:(hf + 1) * 4, :], in_=ps[p][:].rearrange("p (c t) -> p c t", c=4)), r=[ps_b[p]], w=[h1Tf_b])
            kb.op("dve", lambda e, hf=hf, s=s: e.tensor_copy(out=h1Tb[s][:, hf * 4:(hf + 1) * 4, :], in_=h1Tf[:, hf * 4:(hf + 1) * 4, :]),
                  r=[h1Tf_b], w=[h1Tb_b[s]])
        if STOPAT <= 5:
            continue
        kb.op("sp", lambda e, s=s, tsl=tsl: e.dma_start(out=g.h1T[:, tsl].rearrange("(kc p) t -> p kc t", p=128), in_=h1Tb[s][:]), r=[h1Tb_b[s]], dma=True)
        import os
        if os.environ.get('SKIP_ROUTER'):
            continue
        for kc in range(KC):
            kb.op("pe", lambda e, kc=kc: e.matmul(ps[4][:, 0:32], lhsT=h1Tf[:, kc, :], rhs=rwt[:, kc, :], start=(kc == 0), stop=(kc == KC - 1)),
                  r=[h1Tf_b, cst], w=[ps_b[4]])
        kb.op("dve", lambda e: e.tensor_tensor(out=lg[:], in0=ps[4][:, 0:32], in1=rbt[:], op=ALU.add), r=[ps_b[4], cst], w=[lg_b])
        kb.op("dve", lambda e: e.max(out=mx8[:], in_=lg[:]), r=[lg_b], w=[lg_b])
        kb.op("dve", lambda e: e.tensor_scalar(out=msk[:], in0=lg[:], scalar1=mx8[:, 3:4], scalar2=None, op0=ALU.is_ge), r=[lg_b], w=[lg_b])
        kb.op("dve", lambda e: e.tensor_scalar(out=nm0[:], in0=mx8[:, 0:1], scalar1=-1.0, scalar2=None, op0=ALU.mult), r=[lg_b], w=[lg_b])
        kb.op("act", lambda e: e.activation(out=ee[:], in_=lg[:], func=AF.Exp, bias=nm0[:], scale=1.0), r=[lg_b], w=[lg_b])
        kb.op("dve", lambda e: e.tensor_tensor(out=ee[:], in0=ee[:], in1=msk[:], op=ALU.mult), r=[lg_b], w=[lg_b])
        kb.op("dve", lambda e: e.reduce_sum(out=ssum[:], in_=ee[:], axis=AX.X), r=[lg_b], w=[lg_b])
        kb.op("dve", lambda e: e.reciprocal(out=ssum[:], in_=ssum[:]), r=[lg_b], w=[lg_b])
        kb.op("dve", lambda e, i=i: e.tensor_scalar(out=gates[:, i, :], in0=ee[:], scalar1=ssum[:, 0:1], scalar2=None, op0=ALU.mult), r=[lg_b], w=[gates_b])

    if "B" not in sections:
        return
    kb.end_phase(); kb.begin_phase()
    A = lambda n, s, d=F32: kb.sbuf("m4b_" + n, s, d)
    cst = kb.buf("cst4b")
    b1t = A("b1t", [128, 32, 8, 2])
    for ex in range(32):
        kb.op("sp", lambda e, ex=ex: e.dma_start(out=b1t[:, ex, :, :], in_=g.exp_b1[ex:ex + 1, :].rearrange("o (c p two) -> p (o c) two", p=128, two=2)),
              w=[cst], dma=True)
    ps = [kb.psum(f"m4bps{i}", [128, 512], F32) for i in range(8)]; ps_b = kb.bufs(8, "m4bps")
    W1 = [A(f"W1_{i}", [128, KC, 2048], BF16) for i in range(2)]; W2 = [A(f"W2_{i}", [128, KC, D], BF16) for i in range(2)]
    W_b = kb.bufs(2, "W")
    b2t = [A(f"b2t{i}", [128, D]) for i in range(2)]
    xT = [A(f"xT{i}", [128, KC, 512], BF16) for i in range(2)]; xT_b = kb.bufs(2, "xT")
    actT = [A(f"actT{i}", [128, 8, 512], BF16) for i in range(2)]; actT_b = kb.bufs(2, "actT")
    xg = [A(f"xg{i}", [128, 512]) for i in range(2)]; sg = [A(f"sgm{i}", [128, 512]) for i in range(2)]; xl = [A(f"xl{i}", [128, 512]) for i in range(2)]
    el_b = kb.bufs(2, "el")
    ot = [A(f"ot{i}", [128, 512]) for i in range(3)]; ot_b = kb.bufs(3, "ot")
    yacc_b = kb.bufs(NT, "yacc")

    def load_w(ex):
        z = ex % 2
        for kc in range(KC):
            kb.op("pool", lambda e, z=z, kc=kc, ex=ex: e.dma_start(out=W1[z][:, kc, :], in_=g.exp_w1[ex, kc * 128:(kc + 1) * 128, :]), w=[W_b[z]], dma=True)
        for kc in range(KC):
            kb.op("pool", lambda e, z=z, kc=kc, ex=ex: e.dma_start(out=W2[z][:, kc, :], in_=g.exp_w2[ex, kc * 128:(kc + 1) * 128, :]), w=[W_b[z]], dma=True)
        kb.op("sp", lambda e, z=z, ex=ex: e.dma_start(out=b2t[z][:], in_=g.exp_b2[ex:ex + 1, :].broadcast_to([128, D])), w=[W_b[z]], dma=True)

    load_w(0)
    n = 0
    on = 0
    for ex in range(n_exp):
        z = ex % 2
        if ex + 1 < n_exp:
            load_w(ex + 1)
        for tb in range(NB):
            xs = n % 2
            n += 1
            kb.op("sp", lambda e, xs=xs, tb=tb: e.dma_start(out=xT[xs][:], in_=g.h1T[:, tb * 512:(tb + 1) * 512].rearrange("(kc p) t -> p kc t", p=128)),
                  w=[xT_b[xs]], dma=True)
            for fc in range(8):
                q = fc % 2
                pg, pl = ps[2 * q], ps[2 * q + 1]
                for which, pp, ppb in ((0, pg, ps_b[2 * q]), (1, pl, ps_b[2 * q + 1])):
                    for kc in range(KC):
                        kb.op("pe", lambda e, z=z, kc=kc, fc=fc, which=which, pp=pp, xs=xs: e.matmul(
                            pp[:, :], lhsT=W1[z][:, kc, fc * 256 + which:fc * 256 + 256:2], rhs=xT[xs][:, kc, :], start=(kc == 0), stop=(kc == KC - 1)),
                            r=[W_b[z], xT_b[xs]], w=[ppb])
                kb.op("dve", lambda e, q=q, pg=pg, ex=ex, fc=fc: e.tensor_scalar(out=xg[q][:], in0=pg[:, :], scalar1=b1t[:, ex, fc, 0:1], scalar2=7.0, op0=ALU.add, op1=ALU.min),
                      r=[ps_b[2 * q], cst], w=[el_b[q]])
                kb.op("act", lambda e, q=q: e.activation(out=sg[q][:], in_=xg[q][:], func=AF.Sigmoid, scale=1.702), r=[el_b[q]], w=[el_b[q]])
                kb.op("dve", lambda e, q=q, pl=pl, ex=ex, fc=fc: e.tensor_scalar(out=xl[q][:], in0=pl[:, :], scalar1=b1t[:, ex, fc, 1:2], scalar2=7.0, op0=ALU.add, op1=ALU.min),
                      r=[ps_b[2 * q + 1], cst], w=[el_b[q]])
                kb.op("dve", lambda e, q=q: e.tensor_scalar(out=xl[q][:], in0=xl[q][:], scalar1=-7.0, scalar2=1.0, op0=ALU.max, op1=ALU.add), r=[el_b[q]], w=[el_b[q]])
                kb.op("dve", lambda e, q=q: e.tensor_tensor(out=xg[q][:], in0=xg[q][:], in1=sg[q][:], op=ALU.mult), r=[el_b[q]], w=[el_b[q]])
                kb.op("dve", lambda e, q=q, xs=xs, fc=fc: e.tensor_tensor(out=actT[xs][:, fc, :], in0=xg[q][:], in1=xl[q][:], op=ALU.mult),
                      r=[el_b[q]], w=[actT_b[xs]])
            for tt in range(4):
                ti = tb * 4 + tt
                for hf in range(2):
                    pp = 4 + (on % 4)
                    oi = on % 3
                    on += 1
                    for fc in range(8):
                        kb.op("pe", lambda e, pp=pp, xs=xs, fc=fc, tt=tt, z=z, hf=hf: e.matmul(
                            ps[pp][:, :], lhsT=actT[xs][:, fc, tt * 128:(tt + 1) * 128], rhs=W2[z][:, fc, hf * 512:(hf + 1) * 512], start=(fc == 0), stop=(fc == 7)),
                            r=[actT_b[xs], W_b[z]], w=[ps_b[pp]])
                    kb.op("dve", lambda e, pp=pp, oi=oi, z=z, hf=hf: e.tensor_tensor(out=ot[oi][:], in0=ps[pp][:, :], in1=b2t[z][:, hf * 512:(hf + 1) * 512], op=ALU.add),
                          r=[ps_b[pp], W_b[z]], w=[ot_b[oi]])
                    kb.op("dve", lambda e, oi=oi, ti=ti, ex=ex: e.tensor_scalar(out=ot[oi][:], in0=ot[oi][:], scalar1=gates[:, ti, ex:ex + 1], scalar2=None, op0=ALU.mult),
                          r=[ot_b[oi], gates_b], w=[ot_b[oi]])
                    if ex == 0:
                        kb.op("pool", lambda e, oi=oi, ti=ti, hf=hf: e.dma_start(out=g.yacc[ti * 128:(ti + 1) * 128, hf * 512:(hf + 1) * 512], in_=ot[oi][:]),
                              r=[ot_b[oi]], w=[yacc_b[ti]], dma=True)
                    else:
                        kb.op("pool", lambda e, oi=oi, ti=ti, hf=hf: e.dma_start(out=g.yacc[ti * 128:(ti + 1) * 128, hf * 512:(hf + 1) * 512], in_=ot[oi][:], accum_op=ALU.add),
                              r=[ot_b[oi]], w=[yacc_b[ti]], dma=True)

    if "C" not in sections:
        return
    kb.end_phase(); kb.begin_phase()
    A = lambda n, s, d=F32: kb.sbuf("m4c_" + n, s, d)
    cst = kb.buf("cst4c")
    gam2 = A("gam2", [128, D]); bet2 = A("bet2", [128, D]); epsc = A("epsc", [128, 1])
    kb.op("dve", lambda e: e.memset(epsc[:], LN_EPS), w=[cst])
    kb.op("sp", lambda e: e.dma_start(out=gam2[:], in_=g.ln2_w.broadcast_to([128, D])), w=[cst], dma=True)
    kb.op("sp", lambda e: e.dma_start(out=bet2[:], in_=g.ln2_b.broadcast_to([128, D])), w=[cst], dma=True)
    h0t = [A(f"h0t{i}", [128, D]) for i in range(2)]; h0t_b = kb.bufs(2, "h0tc")
    sres = [A(f"sres{i}", [128, D]) for i in range(2)]; sres_b = kb.bufs(2, "sresc")
    h1t = [A(f"h1t{i}", [128, D]) for i in range(2)]; h1t_b = kb.bufs(2, "h1tc")
    st = A("st", [128, 2, 6]); mv = A("mv", [128, 2]); rs = A("rs", [128, 1]); st_b = kb.buf("stc")
    for i in range(NT):
        s = i % 2
        tsl = slice(i * 128, (i + 1) * 128)
        kb.op("sp", lambda e, s=s, tsl=tsl: e.dma_start(out=h0t[s][:], in_=g.h1f[tsl, :]), w=[h0t_b[s]], dma=True)
        kb.op("sp", lambda e, s=s, tsl=tsl: e.dma_start(out=h1t[s][:], in_=g.yacc[tsl, :]), r=[yacc_b[i]], w=[h1t_b[s]], dma=True)
        kb.op("dve", lambda e, s=s: e.scalar_tensor_tensor(out=sres[s][:], in0=h0t[s][:], scalar=ALPHA, in1=h1t[s][:], op0=ALU.mult, op1=ALU.add),
              r=[h0t_b[s], h1t_b[s]], w=[sres_b[s]])
        _ln_tile(kb, sres[s], sres_b[s], h1t[s], h1t_b[s], st, mv, rs, st_b, epsc, cst, gam2, bet2)
        kb.op("sp", lambda e, s=s, tsl=tsl: e.dma_start(out=g.out[tsl, :], in_=h1t[s][:]), r=[h1t_b[s]], dma=True)


def make_inputs(d, b, T):
    im = {"x": d['x'][b, :T], "ln_in_w": d['ln_in_w'][None], "ln_in_b": d['ln_in_b'][None], "w_in": d['w_in'][0],
          "fx_b_f": d['fx_b_f'][0][:, None], "fx_q_norm": d['fx_q_norm'][0][:, None], "fx_k_norm": d['fx_k_norm'][0][:, None],
          "w_o": d['w_o'][0], "ln1_w": d['ln1_w'], "ln1_b": d['ln1_b'], "ln2_w": d['ln2_w'], "ln2_b": d['ln2_b'],
          "router_w": d['router_w'][0], "router_b": d['router_b'], "exp_w1": d['exp_w1'][0], "exp_b1": d['exp_b1'][0],
          "exp_w2": d['exp_w2'][0], "exp_b2": d['exp_b2'][0]}
    im.update(consts_np()); im.update(consts2_np()); im.update(consts3_np(d)); im.update(consts4g_np(T))
    return {k: np.ascontiguousarray(v) for k, v in im.items()}


def moe_cap(T):
    mean = T * 4 // 32
    return 128 * int(np.ceil(1.25 * mean / 128.0))


def setup4g(nc, g):
    ei = lambda n, s, d=F32: nc.dram_tensor(n, list(s), d, kind="ExternalInput").ap()
    T = g.T
    C = moe_cap(T)
    g.C = C
    g.NR = 32 * C + 128
    g.ec_iota = ei("ec_iota", [128, 32])
    g.tok16 = ei("tok16", [128, 16])
    g.table_init = ei("table_init", [g.NR, 16])
    g.su_b = ei("su_b", [128, 128], BF16)
    g.table = g.scratch("table", [g.NR, 16])
    g.h1b = g.scratch("h1b", [T + 128, D], BF16)
    g.out_all = g.scratch("out_all", [g.NR, D])


def consts4g_np(T):
    C = moe_cap(T)
    NR = 32 * C + 128
    c = {}
    c["ec_iota"] = np.tile((np.arange(32) * C).astype(np.float32)[None, :], (128, 1))
    c["tok16"] = np.tile(np.arange(128, dtype=np.float32)[:, None], (1, 16))
    ti = np.zeros((NR, 16), np.float32)
    ti[:, 0] = T
    c["table_init"] = ti
    a = np.arange(128)
    c["su_b"] = (a[:, None] < a[None, :]).astype(np.float32).astype(ml_dtypes.bfloat16)
    return c


def phase4g(kb, g):
    nc, T, NT, NB = g.nc, g.T, g.NT, g.NB
    C = g.C
    TRASH = 32 * C
    NS = C // 128
    kb.ps_stack.close(); kb.ps_stack = None
    rowi = kb.sbuf("m4_rowi", [128, NT, 4], I32); gsel = kb.sbuf("m4_gsel", [128, NT, 4], F32); sel_b = kb.buf("sel")
    kb.begin_phase()
    A = lambda n, s, d=F32: kb.sbuf("m4_" + n, s, d)
    cst = kb.buf("cst4")
    wo = A("wo", [128, KC, D], BF16)
    rwt = A("rwt", [128, KC, 32]); rbt = A("rbt", [128, 32])
    gam1 = A("gam1", [128, D]); bet1 = A("bet1", [128, D])
    identf = A("identf", [128, 128]); epsc = A("epsc", [128, 1])
    ecio = A("ecio", [128, 32]); tok0 = A("tok0", [128, 16]); sub = A("sub", [128, 128], BF16); onesb = A("onesb", [128, 128], BF16)
    tin = A("tin", [128, g.NR // 128, 16]); zrow = A("zrow", [128, D], BF16); zrowf = A("zrowf", [128, D])
    table_b = kb.buf("table"); h1b_b = kb.buf("h1b"); outall_b = kb.buf("outall")
    kb.op("dve", lambda e: e.memset(epsc[:], LN_EPS), w=[cst])
    kb.op("dve", lambda e: e.memset(onesb[:], 1.0), w=[cst])
    kb.op("dve", lambda e: e.memset(zrow[:], 0.0), w=[cst])
    kb.op("dve", lambda e: e.memset(zrowf[:], 0.0), w=[cst])
    kb.op("sp", lambda e: e.dma_start(out=identf[:], in_=g.ident_f), w=[cst], dma=True)
    kb.op("sp", lambda e: e.dma_start(out=ecio[:], in_=g.ec_iota), w=[cst], dma=True)
    kb.op("sp", lambda e: e.dma_start(out=tok0[:], in_=g.tok16), w=[cst], dma=True)
    kb.op("sp", lambda e: e.dma_start(out=sub[:], in_=g.su_b), w=[cst], dma=True)
    kb.op("sp", lambda e: e.dma_start(out=tin[:], in_=g.table_init.rearrange("(j p) w -> p j w", p=128)), w=[cst], dma=True)
    kb.op("sp", lambda e: e.dma_start(out=g.table.rearrange("(j p) w -> p j w", p=128), in_=tin[:]), r=[cst], w=[table_b], dma=True)
    kb.op("sp", lambda e: e.dma_start(out=g.h1b[T:T + 128, :], in_=zrow[:]), r=[cst], w=[h1b_b], dma=True)
    kb.op("sp", lambda e: e.dma_start(out=g.out_all[TRASH:TRASH + 128, :], in_=zrowf[:]), r=[cst], w=[outall_b], dma=True)
    for nm, t_, src in (("g1", gam1, g.ln1_w), ("b1", bet1, g.ln1_b)):
        kb.op("sp", lambda e, t_=t_, src=src: e.dma_start(out=t_[:], in_=src.broadcast_to([128, D])), w=[cst], dma=True)
    kb.op("sp", lambda e: e.dma_start(out=rbt[:], in_=g.router_b.broadcast_to([128, 32])), w=[cst], dma=True)
    kb.op("sp", lambda e: e.dma_start(out=rwt[:], in_=g.router_w.rearrange("(kc p) n -> p kc n", p=128)), w=[cst], dma=True)
    for kc in range(KC):
        kb.op("pool", lambda e, kc=kc: e.dma_start(out=wo[:, kc, :], in_=g.w_o[kc * 128:(kc + 1) * 128, :]), w=[cst], dma=True)

    ymt = [A(f"ymt{i}", [128, KC, 128], BF16) for i in range(2)]; ymt_b = kb.bufs(2, "ymt")
    h0t = [A(f"h0t{i}", [128, D]) for i in range(2)]; h0t_b = kb.bufs(2, "h0t")
    sres = [A(f"sres{i}", [128, D]) for i in range(2)]; sres_b = kb.bufs(2, "sres")
    h1t = [A(f"h1t{i}", [128, D]) for i in range(2)]; h1t_b = kb.bufs(2, "h1t")
    h1bt = [A(f"h1bt{i}", [128, D], BF16) for i in range(2)]; h1bt_b = kb.bufs(2, "h1bt")
    st = A("st", [128, 2, 6]); mv = A("mv", [128, 2]); rs = A("rs", [128, 1]); st_b = kb.buf("st")
    h1Tf = A("h1Tf", [128, KC, 128]); h1Tf_b = kb.buf("h1Tf")
    lg = A("lg", [128, 32]); mx8 = A("mx8", [128, 8]); msk = A("msk", [128, 32]); ee = A("ee", [128, 32]); ssum = A("ssum", [128, 1])
    gt = A("gt", [128, 32]); nm0 = A("nm0", [128, 1]); lg_b = kb.buf("lg")
    mskb = A("mskb", [128, NT, 32], BF16); mskb_b = kb.bufs(NT, "mskb")
    ridx = A("ridx", [128, 32]); okm = A("okm", [128, 32]); selk = A("selk", [128, 32]); tmpk = A("tmpk", [128, 32])
    rowf = A("rowf", [128, NT, 4]); rt_b = kb.buf("rt")
    tokt = [A(f"tokt{i}", [128, 16]) for i in range(2)]; tokt_b = kb.bufs(2, "tokt")
    ps = [kb.psum(f"m4ps{i}", [128, 512], F32) for i in range(8)]; ps_b = kb.bufs(8, "m4ps")

    for i in range(NT):
        s = i % 2
        tsl = slice(i * 128, (i + 1) * 128)
        kb.op("sp", lambda e, s=s, tsl=tsl: e.dma_start(out=ymt[s][:], in_=g.yT[:, tsl].rearrange("(kc p) t -> p kc t", p=128)), w=[ymt_b[s]], dma=True)
        kb.op("sp", lambda e, s=s, tsl=tsl: e.dma_start(out=h0t[s][:], in_=g.h0f[tsl, :]), w=[h0t_b[s]], dma=True)
        for hf in range(2):
            for kc in range(KC):
                kb.op("pe", lambda e, s=s, hf=hf, kc=kc: e.matmul(ps[hf][:, :], lhsT=ymt[s][:, kc, :], rhs=wo[:, kc, hf * 512:(hf + 1) * 512],
                                                                 start=(kc == 0), stop=(kc == KC - 1)), r=[ymt_b[s], cst], w=[ps_b[hf]])
            kb.op("dve", lambda e, s=s, hf=hf: e.scalar_tensor_tensor(out=sres[s][:, hf * 512:(hf + 1) * 512], in0=h0t[s][:, hf * 512:(hf + 1) * 512],
                                                                     scalar=ALPHA, in1=ps[hf][:, :], op0=ALU.mult, op1=ALU.add),
                  r=[h0t_b[s], ps_b[hf]], w=[sres_b[s]])
        _ln_tile(kb, sres[s], sres_b[s], h1t[s], h1t_b[s], st, mv, rs, st_b, epsc, cst, gam1, bet1)
        kb.op("sp", lambda e, s=s, tsl=tsl: e.dma_start(out=g.h1f[tsl, :], in_=h1t[s][:]), r=[h1t_b[s]], dma=True)
        kb.op("act", lambda e, s=s: e.copy(out=h1bt[s][:], in_=h1t[s][:]), r=[h1t_b[s]], w=[h1bt_b[s]])
        kb.op("sp", lambda e, s=s, tsl=tsl: e.dma_start(out=g.h1b[tsl, :], in_=h1bt[s][:]), r=[h1bt_b[s]], w=[h1b_b], dma=True)
        for hf in range(2):
            p = 2 + hf
            for c in range(4):
                kc = hf * 4 + c
                kb.op("pe", lambda e, s=s, p=p, c=c, kc=kc: e.transpose(out=ps[p][:, c * 128:(c + 1) * 128], in_=h1t[s][:, kc * 128:(kc + 1) * 128], identity=identf[:]),
                      r=[h1t_b[s], cst], w=[ps_b[p]])
            kb.op("act", lambda e, p=p, hf=hf: e.copy(out=h1Tf[:, hf * 4:(hf + 1) * 4, :], in_=ps[p][:].rearrange("p (c t) -> p c t", c=4)), r=[ps_b[p]], w=[h1Tf_b])
        for kc in range(KC):
            kb.op("pe", lambda e, kc=kc: e.matmul(ps[4][:, 0:32], lhsT=h1Tf[:, kc, :], rhs=rwt[:, kc, :], start=(kc == 0), stop=(kc == KC - 1)),
                  r=[h1Tf_b, cst], w=[ps_b[4]])
        kb.op("dve", lambda e: e.tensor_tensor(out=lg[:], in0=ps[4][:, 0:32], in1=rbt[:], op=ALU.add), r=[ps_b[4], cst], w=[lg_b])
        kb.op("dve", lambda e: e.max(out=mx8[:], in_=lg[:]), r=[lg_b], w=[lg_b])
        kb.op("dve", lambda e: e.tensor_scalar(out=msk[:], in0=lg[:], scalar1=mx8[:, 3:4], scalar2=None, op0=ALU.is_ge), r=[lg_b], w=[lg_b])
        kb.op("dve", lambda e: e.tensor_scalar(out=nm0[:], in0=mx8[:, 0:1], scalar1=-1.0, scalar2=None, op0=ALU.mult), r=[lg_b], w=[lg_b])
        kb.op("act", lambda e: e.activation(out=ee[:], in_=lg[:], func=AF.Exp, bias=nm0[:], scale=1.0), r=[lg_b], w=[lg_b])
        kb.op("dve", lambda e: e.tensor_tensor(out=ee[:], in0=ee[:], in1=msk[:], op=ALU.mult), r=[lg_b], w=[lg_b])
        kb.op("dve", lambda e: e.reduce_sum(out=ssum[:], in_=ee[:], axis=AX.X), r=[lg_b], w=[lg_b])
        kb.op("dve", lambda e: e.reciprocal(out=ssum[:], in_=ssum[:]), r=[lg_b], w=[lg_b])
        kb.op("dve", lambda e: e.tensor_scalar(out=gt[:], in0=ee[:], scalar1=ssum[:, 0:1], scalar2=None, op0=ALU.mult), r=[lg_b], w=[lg_b])
        kb.op("dve", lambda e, i=i: e.tensor_copy(out=mskb[:, i, :], in_=msk[:]), r=[lg_b], w=[mskb_b[i]])
        kb.op("pe", lambda e, i=i: e.matmul(ps[5][:, 0:32], lhsT=sub[:], rhs=mskb[:, i, :], start=True, stop=(i == 0)), r=[mskb_b[i], cst], w=[ps_b[5]])
        for j in range(i):
            kb.op("pe", lambda e, j=j, i=i: e.matmul(ps[5][:, 0:32], lhsT=onesb[:], rhs=mskb[:, j, :], start=False, stop=(j == i - 1)),
                  r=[mskb_b[j], cst], w=[ps_b[5]])
        kb.op("dve", lambda e: e.tensor_tensor(out=ridx[:], in0=ps[5][:, 0:32], in1=ecio[:], op=ALU.add), r=[ps_b[5], cst], w=[rt_b])
        kb.op("dve", lambda e: e.tensor_scalar(out=okm[:], in0=ps[5][:, 0:32], scalar1=float(C), scalar2=None, op0=ALU.is_lt), r=[ps_b[5]], w=[rt_b])
        kb.op("dve", lambda e: e.tensor_tensor(out=okm[:], in0=okm[:], in1=msk[:], op=ALU.mult), r=[rt_b, lg_b], w=[rt_b])
        kb.op("dve", lambda e: e.tensor_scalar(out=ridx[:], in0=ridx[:], scalar1=-float(TRASH), scalar2=None, op0=ALU.add), r=[rt_b], w=[rt_b])
        kb.op("dve", lambda e: e.tensor_tensor(out=ridx[:], in0=ridx[:], in1=okm[:], op=ALU.mult), r=[rt_b], w=[rt_b])
        kb.op("dve", lambda e: e.tensor_scalar(out=ridx[:], in0=ridx[:], scalar1=float(TRASH), scalar2=None, op0=ALU.add), r=[rt_b], w=[rt_b])
        for k in range(4):
            kb.op("dve", lambda e, k=k: e.tensor_scalar(out=selk[:], in0=lg[:], scalar1=mx8[:, k:k + 1], scalar2=None, op0=ALU.is_equal), r=[lg_b], w=[rt_b])
            kb.op("dve", lambda e: e.tensor_tensor(out=tmpk[:], in0=selk[:], in1=ridx[:], op=ALU.mult), r=[rt_b], w=[rt_b])
            kb.op("dve", lambda e, i=i, k=k: e.reduce_sum(out=rowf[:, i, k:k + 1], in_=tmpk[:], axis=AX.X), r=[rt_b], w=[rt_b])
            kb.op("dve", lambda e: e.tensor_tensor(out=tmpk[:], in0=selk[:], in1=gt[:], op=ALU.mult), r=[rt_b, lg_b], w=[rt_b])
            kb.op("dve", lambda e, i=i, k=k: e.reduce_sum(out=gsel[:, i, k:k + 1], in_=tmpk[:], axis=AX.X), r=[rt_b], w=[sel_b])
        kb.op("dve", lambda e, i=i: e.tensor_copy(out=rowi[:, i, :], in_=rowf[:, i, :]), r=[rt_b], w=[sel_b])
        kb.op("dve", lambda e, i=i, s=s: e.tensor_scalar(out=tokt[s][:], in0=tok0[:], scalar1=float(i * 128), scalar2=None, op0=ALU.add), r=[cst], w=[tokt_b[s]])
        for k in range(4):
            kb.op("pool", lambda e, i=i, k=k, s=s: e.indirect_dma_start(
                out=g.table[:, :], out_offset=bass.IndirectOffsetOnAxis(ap=rowi[:, i, k:k + 1], axis=0), in_=tokt[s][:, :], in_offset=None),
                r=[sel_b, tokt_b[s]], w=[table_b], dma=True)

    kb.end_phase(); kb.begin_phase()
    A = lambda n, s, d=F32: kb.sbuf("m4b_" + n, s, d)
    cst = kb.buf("cst4b")
    b1t = A("b1t", [128, 32, 8, 2]); identb = A("identb", [128, 128], BF16)
    kb.op("sp", lambda e: e.dma_start(out=identb[:], in_=g.ident_b), w=[cst], dma=True)
    for ex in range(32):
        kb.op("sp", lambda e, ex=ex: e.dma_start(out=b1t[:, ex, :, :], in_=g.exp_b1[ex:ex + 1, :].rearrange("o (c p two) -> p (o c) two", p=128, two=2)),
              w=[cst], dma=True)
    ps = [kb.psum(f"m4bps{i}", [128, 512], F32) for i in range(7)]; ps_b = kb.bufs(7, "m4bps")
    psTb = kb.psum("m4bpsT", [128, 1024], BF16); psTb_b = kb.buf("psTb")
    W1 = [A(f"W1_{i}", [128, KC, 2048], BF16) for i in range(2)]; W2 = [A(f"W2_{i}", [128, KC, D], BF16) for i in range(2)]
    W_b = kb.bufs(2, "W")
    b2t = [A(f"b2t{i}", [128, D]) for i in range(2)]
    idxf = [A(f"idxf{i}", [128, 16]) for i in range(2)]; idxi = [A(f"idxi{i}", [128, 1], I32) for i in range(2)]; idx_b = kb.bufs(2, "idx")
    Xg = [A(f"Xg{i}", [128, D], BF16) for i in range(2)]; Xg_b = kb.bufs(2, "Xg")
    xT = [A(f"xT{i}", [128, KC, C], BF16) for i in range(2)]; xT_b = kb.bufs(2, "xT")
    actT = [A(f"actT{i}", [128, 8, 512], BF16) for i in range(2)]; actT_b = kb.bufs(2, "actT")
    xg = [A(f"xg{i}", [128, 512]) for i in range(2)]; sg = [A(f"sgm{i}", [128, 512]) for i in range(2)]; xl = [A(f"xl{i}", [128, 512]) for i in range(2)]
    el_b = kb.bufs(2, "el")
    ot = [A(f"ot{i}", [128, 512]) for i in range(3)]; ot_b = kb.bufs(3, "ot")
    blocks = []
    c0 = 0
    while c0 < C:
        w = min(512, C - c0)
        blocks.append((c0, w))
        c0 += w

    def load_w(ex):
        z = ex % 2
        for kc in range(KC):
            kb.op("pool", lambda e, z=z, kc=kc, ex=ex: e.dma_start(out=W1[z][:, kc, :], in_=g.exp_w1[ex, kc * 128:(kc + 1) * 128, :]), w=[W_b[z]], dma=True)
        for kc in range(KC):
            kb.op("pool", lambda e, z=z, kc=kc, ex=ex: e.dma_start(out=W2[z][:, kc, :], in_=g.exp_w2[ex, kc * 128:(kc + 1) * 128, :]), w=[W_b[z]], dma=True)
        kb.op("sp", lambda e, z=z, ex=ex: e.dma_start(out=b2t[z][:], in_=g.exp_b2[ex:ex + 1, :].broadcast_to([128, D])), w=[W_b[z]], dma=True)

    def gather_x(ex):
        xz = ex % 2
        for s_ in range(NS):
            q = (ex * NS + s_) % 2
            r0 = ex * C + s_ * 128
            kb.op("sp", lambda e, q=q, r0=r0: e.dma_start(out=idxf[q][:], in_=g.table[r0:r0 + 128, :]), r=[table_b], w=[idx_b[q]], dma=True)
            kb.op("dve", lambda e, q=q: e.tensor_copy(out=idxi[q][:], in_=idxf[q][:, 0:1]), r=[idx_b[q]], w=[idx_b[q]])
            kb.op("pool", lambda e, q=q: e.indirect_dma_start(
                out=Xg[q][:, :], out_offset=None, in_=g.h1b[:, :], in_offset=bass.IndirectOffsetOnAxis(ap=idxi[q][:, 0:1], axis=0)),
                r=[idx_b[q], h1b_b], w=[Xg_b[q]], dma=True)
            for kc in range(KC):
                kb.op("pe", lambda e, q=q, kc=kc: e.transpose(out=psTb[:, kc * 128:(kc + 1) * 128], in_=Xg[q][:, kc * 128:(kc + 1) * 128], identity=identb[:]),
                      r=[Xg_b[q], cst], w=[psTb_b])
            kb.op("act", lambda e, xz=xz, s_=s_: e.copy(out=xT[xz][:, :, s_ * 128:(s_ + 1) * 128], in_=psTb[:].rearrange("p (c t) -> p c t", c=8)),
                  r=[psTb_b], w=[xT_b[xz]])

    load_w(0)
    gather_x(0)
    on = 0
    an = 0
    for ex in range(32):
        z = ex % 2
        xz = ex % 2
        if ex + 1 < 32:
            load_w(ex + 1)
            gather_x(ex + 1)
        for (c0, w) in blocks:
            xs = an % 2
            an += 1
            for fc in range(8):
                q = fc % 2
                pg, pl = ps[2 * q], ps[2 * q + 1]
                for which, pp, ppb in ((0, pg, ps_b[2 * q]), (1, pl, ps_b[2 * q + 1])):
                    for kc in range(KC):
                        kb.op("pe", lambda e, z=z, kc=kc, fc=fc, which=which, pp=pp, xz=xz, c0=c0, w=w: e.matmul(
                            pp[:, 0:w], lhsT=W1[z][:, kc, fc * 256 + which:fc * 256 + 256:2], rhs=xT[xz][:, kc, c0:c0 + w], start=(kc == 0), stop=(kc == KC - 1)),
                            r=[W_b[z], xT_b[xz]], w=[ppb])
                kb.op("dve", lambda e, q=q, pg=pg, ex=ex, fc=fc, w=w: e.tensor_scalar(out=xg[q][:, 0:w], in0=pg[:, 0:w], scalar1=b1t[:, ex, fc, 0:1], scalar2=7.0, op0=ALU.add, op1=ALU.min),
                      r=[ps_b[2 * q], cst], w=[el_b[q]])
                kb.op("act", lambda e, q=q, w=w: e.activation(out=sg[q][:, 0:w], in_=xg[q][:, 0:w], func=AF.Sigmoid, scale=1.702), r=[el_b[q]], w=[el_b[q]])
                kb.op("dve", lambda e, q=q, pl=pl, ex=ex, fc=fc, w=w: e.tensor_scalar(out=xl[q][:, 0:w], in0=pl[:, 0:w], scalar1=b1t[:, ex, fc, 1:2], scalar2=7.0, op0=ALU.add, op1=ALU.min),
                      r=[ps_b[2 * q + 1], cst], w=[el_b[q]])
                kb.op("dve", lambda e, q=q, w=w: e.tensor_scalar(out=xl[q][:, 0:w], in0=xl[q][:, 0:w], scalar1=-7.0, scalar2=1.0, op0=ALU.max, op1=ALU.add), r=[el_b[q]], w=[el_b[q]])
                kb.op("dve", lambda e, q=q, w=w: e.tensor_tensor(out=xg[q][:, 0:w], in0=xg[q][:, 0:w], in1=sg[q][:, 0:w], op=ALU.mult), r=[el_b[q]], w=[el_b[q]])
                kb.op("dve", lambda e, q=q, xs=xs, fc=fc, w=w: e.tensor_tensor(out=actT[xs][:, fc, 0:w], in0=xg[q][:, 0:w], in1=xl[q][:, 0:w], op=ALU.mult),
                      r=[el_b[q]], w=[actT_b[xs]])
            for tt in range(w // 128):
                r0 = ex * C + c0 + tt * 128
                for hf in range(2):
                    pp = 4 + (on % 3)
                    oi = on % 3
                    on += 1
                    for fc in range(8):
                        kb.op("pe", lambda e, pp=pp, xs=xs, fc=fc, tt=tt, z=z, hf=hf: e.matmul(
                            ps[pp][:, :], lhsT=actT[xs][:, fc, tt * 128:(tt + 1) * 128], rhs=W2[z][:, fc, hf * 512:(hf + 1) * 512], start=(fc == 0), stop=(fc == 7)),
                            r=[actT_b[xs], W_b[z]], w=[ps_b[pp]])
                    kb.op("dve", lambda e, pp=pp, oi=oi, z=z, hf=hf: e.tensor_tensor(out=ot[oi][:], in0=ps[pp][:, :], in1=b2t[z][:, hf * 512:(hf + 1) * 512], op=ALU.add),
                          r=[ps_b[pp], W_b[z]], w=[ot_b[oi]])
                    kb.op("sp", lambda e, oi=oi, r0=r0, hf=hf: e.dma_start(out=g.out_all[r0:r0 + 128, hf * 512:(hf + 1) * 512], in_=ot[oi][:]),
                          r=[ot_b[oi]], w=[outall_b], dma=True)

    kb.end_phase(); kb.begin_phase()
    A = lambda n, s, d=F32: kb.sbuf("m4c_" + n, s, d)
    cst = kb.buf("cst4c")
    gam2 = A("gam2", [128, D]); bet2 = A("bet2", [128, D]); epsc = A("epsc", [128, 1])
    kb.op("dve", lambda e: e.memset(epsc[:], LN_EPS), w=[cst])
    kb.op("sp", lambda e: e.dma_start(out=gam2[:], in_=g.ln2_w.broadcast_to([128, D])), w=[cst], dma=True)
    kb.op("sp", lambda e: e.dma_start(out=bet2[:], in_=g.ln2_b.broadcast_to([128, D])), w=[cst], dma=True)
    h0t = [A(f"h0t{i}", [128, D]) for i in range(2)]; h0t_b = kb.bufs(2, "h0tc")
    sres = [A(f"sres{i}", [128, D]) for i in range(2)]; sres_b = kb.bufs(2, "sresc")
    h1t = [A(f"h1t{i}", [128, D]) for i in range(2)]; h1t_b = kb.bufs(2, "h1tc")
    Rk = [A(f"Rk{i}", [128, D]) for i in range(4)]; Rk_b = kb.bufs(4, "Rk")
    st = A("st", [128, 2, 6]); mv = A("mv", [128, 2]); rs = A("rs", [128, 1]); st_b = kb.buf("stc")
    for i in range(NT):
        s = i % 2
        tsl = slice(i * 128, (i + 1) * 128)
        kb.op("sp", lambda e, s=s, tsl=tsl: e.dma_start(out=h0t[s][:], in_=g.h1f[tsl, :]), w=[h0t_b[s]], dma=True)
        for k in range(4):
            kb.op("pool", lambda e, i=i, k=k: e.indirect_dma_start(
                out=Rk[k][:, :], out_offset=None, in_=g.out_all[:, :], in_offset=bass.IndirectOffsetOnAxis(ap=rowi[:, i, k:k + 1], axis=0)),
                r=[sel_b, outall_b], w=[Rk_b[k]], dma=True)
        kb.op("dve", lambda e, s=s: e.tensor_scalar(out=sres[s][:], in0=h0t[s][:], scalar1=ALPHA, scalar2=None, op0=ALU.mult), r=[h0t_b[s]], w=[sres_b[s]])
        for k in range(4):
            kb.op("dve", lambda e, s=s, i=i, k=k: e.scalar_tensor_tensor(out=sres[s][:], in0=Rk[k][:], scalar=gsel[:, i, k:k + 1], in1=sres[s][:], op0=ALU.mult, op1=ALU.add),
                  r=[Rk_b[k], sel_b, sres_b[s]], w=[sres_b[s]])
        _ln_tile(kb, sres[s], sres_b[s], h1t[s], h1t_b[s], st, mv, rs, st_b, epsc, cst, gam2, bet2)
        kb.op("sp", lambda e, s=s, tsl=tsl: e.dma_start(out=g.out[tsl, :], in_=h1t[s][:]), r=[h1t_b[s]], dma=True)


_T = 4096


def _build(T):
    nc = bass.Bass("TRN2", target_bir_lowering=False)
    g = setup(nc, T)
    setup2(nc, g); setup3(nc, g); setup4(nc, g); setup4g(nc, g)
    kb = KB(nc)
    kb.begin_phase(); phase01(kb, g); kb.end_phase()
    kb.begin_phase(); phase2(kb, g); kb.end_phase()
    kb.begin_phase(); phase3(kb, g); kb.end_phase()
    kb.begin_phase(); phase4g(kb, g); kb.end_phase()
    kb.finish(); kb.close()
    return nc


def kernel(**inputs):
    d = {k: np.asarray(v) for k, v in inputs.items()}
    B = d["x"].shape[0]
    T = d["x"].shape[1]
    nc = _build(T)
    maps = [make_inputs(d, b % B, T) for b in range(8)]
    res = run_bass_kernel_spmd(nc, maps, core_ids=list(range(8)))
    out = np.stack([np.asarray(res.results[b]["out"]) for b in range(B)], axis=0)
    return out.astype(np.float32)
```

```python
import ml_dtypes
import contextlib
import numpy as np
import concourse.bass as bass
import concourse.mybir as mybir
from concourse.bass_utils import run_bass_kernel_spmd

F32 = mybir.dt.float32
BF16 = mybir.dt.bfloat16
I32 = mybir.dt.int32
U32 = mybir.dt.uint32
AF = mybir.ActivationFunctionType
ALU = mybir.AluOpType
AX = mybir.AxisListType

ENGS = ("pe", "act", "dve", "pool", "sp")


class Buf:
    __slots__ = ("name", "w", "rs")

    def __init__(self, name):
        self.name = name
        self.w = None
        self.rs = {}


class _Op:
    __slots__ = ("fn", "waits", "ev", "dma", "marked")

    def __init__(self, fn, waits, ev, dma):
        self.fn = fn
        self.waits = waits
        self.ev = ev
        self.dma = dma
        self.marked = False


class KB:
    def __init__(self, nc, n_dma_sems=20):
        self.nc = nc
        self.es = contextlib.ExitStack()
        self.q = {e: [] for e in ENGS}
        self.n_dma_sems = n_dma_sems
        self.dma_sems = {}
        self.dma_tgt = {}
        self.dma_rr = {}
        self.eng_sem = {}
        self.nbuf = 0
        self.allbufs = []
        self.base = {e: 0 for e in ENGS}
        self.barrier = []
        self.ps_stack = None
        for e in ENGS:
            self.eng_sem[e] = self.es.enter_context(nc.semaphore("es_" + e))
        for e in ("sp", "pool", "act"):
            self.dma_sems[e] = [self.es.enter_context(nc.semaphore(f"ds_{e}_{i}")) for i in range(n_dma_sems)]
            self.dma_tgt[e] = [0] * n_dma_sems
            self.dma_rr[e] = 0

    def begin_phase(self):
        self.ps_stack = contextlib.ExitStack()

    def end_phase(self):
        self.replay()
        self.ps_stack.close()
        self.ps_stack = None

    def sbuf(self, name, shape, dtype):
        st = self.ps_stack if self.ps_stack is not None else self.es
        return st.enter_context(self.nc.sbuf_tensor(name, list(shape), dtype))

    def psum(self, name, shape, dtype):
        st = self.ps_stack if self.ps_stack is not None else self.es
        return st.enter_context(self.nc.psum_tensor(name, list(shape), dtype))

    def buf(self, name=None):
        self.nbuf += 1
        b = Buf(name or f"b{self.nbuf}")
        self.allbufs.append(b)
        return b

    def bufs(self, n, name="b"):
        return [self.buf(f"{name}{i}") for i in range(n)]

    def op(self, eng, fn, r=(), w=(), dma=False):
        waits = []
        if not self.q[eng] and self.barrier:
            waits.extend(self.barrier)
        for b in r:
            if b.w is not None:
                waits.append(b.w)
        for b in w:
            if b.w is not None:
                waits.append(b.w)
            waits.extend(b.rs.values())
        if dma:
            i = self.dma_rr[eng]
            self.dma_rr[eng] = (i + 1) % self.n_dma_sems
            prev = self.dma_tgt[eng][i]
            if prev > 0:
                waits.append(("d", eng, i, prev))
            tgt = prev + 16
            self.dma_tgt[eng][i] = tgt
            ev = ("d", eng, i, tgt)
        else:
            ev = ("c", eng, len(self.q[eng]))
        self.q[eng].append(_Op(fn, waits, ev, dma))
        key = ev[:3] if ev[0] == "d" else ev[:2]
        for b in r:
            b.rs[key] = ev
        for b in w:
            b.w = ev
            b.rs = {}
        return ev

    def replay(self):
        nc = self.nc
        for e in ENGS:
            if self.q[e]:
                self.q[e][-1].marked = True
            for o in self.q[e]:
                for ev in o.waits:
                    if ev[0] == "c":
                        if ev[1] == "pe" and e == "pe":
                            continue
                        self.q[ev[1]][ev[2]].marked = True
        cnt = {}
        for e in ENGS:
            c = self.base[e]
            arr = []
            for o in self.q[e]:
                if o.marked and not o.dma:
                    c += 1
                arr.append(c)
            cnt[e] = arr

        def resolve(ev):
            if ev[0] == "c":
                return ("c", ev[1]), self.eng_sem[ev[1]], cnt[ev[1]][ev[2]]
            if ev[0] == "a":
                return ("c", ev[1]), self.eng_sem[ev[1]], ev[2]
            return ("d", ev[1], ev[2]), self.dma_sems[ev[1]][ev[2]], ev[3]

        if not hasattr(self, "seen"):
            self.seen = {e: {} for e in ENGS}

        def run_engine(ename, eobj):
            seen = self.seen[ename]
            for o in self.q[ename]:
                need = {}
                for ev in o.waits:
                    if ev[0] in ("c", "a") and ev[1] == "pe" and ename == "pe":
                        continue
                    k, sem, val = resolve(ev)
                    if seen.get(k, 0) >= val:
                        continue
                    if k not in need or need[k][1] < val:
                        need[k] = (sem, val)
                for k, (sem, val) in need.items():
                    eobj.wait_ge(sem, val)
                    seen[k] = val
                ins = o.fn(eobj)
                if o.dma:
                    ins.then_inc(self.dma_sems[o.ev[1]][o.ev[2]], 16)
                elif o.marked:
                    ins.then_inc(self.eng_sem[ename], 1)

        with nc.Block() as block:
            @block.tensor
            def _(t):
                run_engine("pe", t)

            @block.scalar
            def _(s):
                run_engine("act", s)

            @block.vector
            def _(v):
                run_engine("dve", v)

            @block.gpsimd
            def _(g):
                run_engine("pool", g)

            @block.sync
            def _(sy):
                run_engine("sp", sy)

        def absolutize(ev):
            if ev[0] == "c":
                return ("a", ev[1], cnt[ev[1]][ev[2]] if self.q[ev[1]][ev[2]].marked else cnt[ev[1]][-1])
            return ev
        for b in self.allbufs:
            if b.w is not None:
                b.w = absolutize(b.w)
            b.rs = {k: absolutize(v) for k, v in b.rs.items()}
        for e in ENGS:
            if self.q[e]:
                self.base[e] = cnt[e][-1]
            self.q[e] = []
        bar = [("a", e, self.base[e]) for e in ENGS if self.base[e] > 0]
        for e in ("sp", "pool", "act"):
            for i, t in enumerate(self.dma_tgt[e]):
                if t > 0:
                    bar.append(("d", e, i, t))
        self.barrier = bar

    def finish(self):
        nc = self.nc
        bar = list(self.barrier)
        with nc.Block() as block:
            @block.sync
            def _(sy):
                for ev in bar:
                    if ev[0] == "a":
                        sy.wait_ge(self.eng_sem[ev[1]], ev[2])
                    else:
                        sy.wait_ge(self.dma_sems[ev[1]][ev[2]], ev[3])

    def close(self):
        self.es.close()


D = 1024
KC = 8
RW_COLS = 1696
FX0 = RW_COLS
IN_COLS = 3752
LN_EPS = 1e-5
ALPHA = 2 ** 0.25


def col_tiles():
    t = []
    for nm, base in (("r", 0), ("k", 512), ("v", 1024)):
        for h in range(8):
            t.append((f"rw_{nm}{h}", base + 64 * h, 64))
    t.append(("rw_wa", 1536, 64))
    t.append(("rw_g", 1600, 96))
    for nm, base in (("q", 0), ("k", 512), ("og", 1536)):
        for h in range(8):
            t.append((f"fx_{nm}{h}", FX0 + base + 64 * h, 64))
    t.append(("fx_fz", FX0 + 2048, 8))
    return t


def consts_np():
    c = {}
    c["ident_f"] = np.eye(128, dtype=np.float32)
    c["ident_b"] = np.eye(128).astype(ml_dtypes.bfloat16)
    return c


class Ctx:
    pass


def setup(nc, T, ext_out=()):
    g = Ctx()
    g.nc = nc
    g.T = T
    g.NT = T // 128
    g.NB = T // 512
    ei = lambda n, s, d=F32: nc.dram_tensor(n, list(s), d, kind="ExternalInput").ap()
    g.x = ei("x", [T, D])
    g.ln_in_w = ei("ln_in_w", [1, D])
    g.ln_in_b = ei("ln_in_b", [1, D])
    g.w_in = ei("w_in", [D, IN_COLS])
    g.ident_f = ei("ident_f", [128, 128])
    g.ident_b = ei("ident_b", [128, 128], BF16)

    def scratch(n, s, d=F32):
        kind = "ExternalOutput" if n in ext_out else "Internal"
        return nc.dram_tensor(n, list(s), d, kind=kind).ap()

    g.scratch = scratch
    g.h0f = scratch("h0f", [T, D])
    g.pT = scratch("pT", [IN_COLS, T])
    g.vtok = scratch("vtok", [T, 512], BF16)
    return g


def phase01(kb, g):
    nc, T, NT, NB = g.nc, g.T, g.NT, g.NB
    h0T = kb.sbuf("h0T", [128, KC, T], BF16)
    h0T_b = kb.bufs(NT, "h0T")
    wbf = kb.sbuf("wbf", [128, KC, IN_COLS], BF16)
    wbf_b = kb.bufs(KC, "wbf")
    gam = kb.sbuf("gam", [128, D], F32)
    bet = kb.sbuf("bet", [128, D], F32)
    gb_b = kb.buf("gb")
    identf = kb.sbuf("identf", [128, 128], F32)
    id_b = kb.buf("id")
    epsc = kb.sbuf("epsc", [128, 1], F32)
    eps_b = kb.buf("eps")
    xt = [kb.sbuf(f"xt{i}", [128, D], F32) for i in range(2)]
    xt_b = kb.bufs(2, "xt")
    hn = [kb.sbuf(f"hn{i}", [128, D], F32) for i in range(2)]
    hn_b = kb.bufs(2, "hn")
    st = [kb.sbuf(f"st{i}", [128, 2, 6], F32) for i in range(2)]
    mv = [kb.sbuf(f"mv{i}", [128, 2], F32) for i in range(2)]
    rs = [kb.sbuf(f"rs{i}", [128, 1], F32) for i in range(2)]
    st_b = kb.bufs(2, "st")
    ps = [kb.psum(f"ps{i}", [128, 512], F32) for i in range(4)]
    ps_b = kb.bufs(4, "ps")
    stage = [kb.sbuf(f"stage{i}", [128, 512], F32) for i in range(3)]
    stage_b = kb.bufs(3, "stage")
    vst = [kb.sbuf(f"vst{i}", [128, 512], BF16) for i in range(2)]
    vst_b = kb.bufs(2, "vst")

    kb.op("sp", lambda e: e.dma_start(out=gam[:], in_=g.ln_in_w.broadcast_to([128, D])), w=[gb_b], dma=True)
    kb.op("sp", lambda e: e.dma_start(out=bet[:], in_=g.ln_in_b.broadcast_to([128, D])), w=[gb_b], dma=True)
    kb.op("sp", lambda e: e.dma_start(out=identf[:], in_=g.ident_f), w=[id_b], dma=True)
    kb.op("pool", lambda e: e.memset(epsc[:], LN_EPS), w=[eps_b])
    half = IN_COLS // 2
    for kc in range(KC):
        for hf in range(2):
            kb.op("pool", lambda e, kc=kc, hf=hf: e.dma_start(
                out=wbf[:, kc, hf * half:(hf + 1) * half],
                in_=g.w_in[kc * 128:(kc + 1) * 128, hf * half:(hf + 1) * half]),
                w=[wbf_b[kc]], dma=True)

    for i in range(NT):
        s = i % 2
        kb.op("sp", lambda e, i=i, s=s: e.dma_start(out=xt[s][:], in_=g.x[i * 128:(i + 1) * 128, :]), w=[xt_b[s]], dma=True)
        for hf in range(2):
            kb.op("dve", lambda e, s=s, hf=hf: e.bn_stats(out=st[s][:, hf, :], in_=xt[s][:, hf * 512:(hf + 1) * 512]),
                  r=[xt_b[s]], w=[st_b[s]])
        kb.op("dve", lambda e, s=s: e.bn_aggr(out=mv[s][:], in_=st[s][:].rearrange("p a b -> p (a b)")), r=[st_b[s]], w=[st_b[s]])
        kb.op("act", lambda e, s=s: e.activation(out=rs[s][:], in_=mv[s][:, 1:2], func=AF.Sqrt, bias=epsc[:], scale=1.0),
              r=[st_b[s], eps_b], w=[st_b[s]])
        kb.op("dve", lambda e, s=s: e.reciprocal(out=rs[s][:], in_=rs[s][:]), r=[st_b[s]], w=[st_b[s]])
        kb.op("dve", lambda e, s=s: e.tensor_scalar(out=hn[s][:], in0=xt[s][:], scalar1=mv[s][:, 0:1], scalar2=rs[s][:],
                                                    op0=ALU.subtract, op1=ALU.mult),
              r=[xt_b[s], st_b[s]], w=[hn_b[s]])
        kb.op("dve", lambda e, s=s: e.tensor_tensor(out=hn[s][:], in0=hn[s][:], in1=gam[:], op=ALU.mult), r=[hn_b[s], gb_b], w=[hn_b[s]])
        kb.op("pool", lambda e, s=s: e.tensor_tensor(out=hn[s][:], in0=hn[s][:], in1=bet[:], op=ALU.add), r=[hn_b[s], gb_b], w=[hn_b[s]])
        kb.op("sp", lambda e, i=i, s=s: e.dma_start(out=g.h0f[i * 128:(i + 1) * 128, :], in_=hn[s][:]), r=[hn_b[s]], dma=True)
        for hf in range(2):
            p = hf
            for c in range(4):
                kc = hf * 4 + c
                kb.op("pe", lambda e, s=s, p=p, c=c, kc=kc: e.transpose(out=ps[p][:, c * 128:(c + 1) * 128],
                                                                       in_=hn[s][:, kc * 128:(kc + 1) * 128], identity=identf[:]),
                      r=[hn_b[s], id_b], w=[ps_b[p]])
            eng = "act" if hf == 0 else "dve"
            if eng == "act":
                kb.op("act", lambda e, i=i, p=p, hf=hf: e.copy(out=h0T[:, hf * 4:(hf + 1) * 4, i * 128:(i + 1) * 128],
                                                               in_=ps[p][:].rearrange("p (c t) -> p c t", c=4)),
                      r=[ps_b[p]], w=[h0T_b[i]])
            else:
                kb.op("dve", lambda e, i=i, p=p, hf=hf: e.tensor_copy(out=h0T[:, hf * 4:(hf + 1) * 4, i * 128:(i + 1) * 128],
                                                                      in_=ps[p][:].rearrange("p (c t) -> p c t", c=4)),
                      r=[ps_b[p]], w=[h0T_b[i]])

    n = 0
    for (nm, c0, ncol) in col_tiles():
        for tb in range(NB):
            p = 2 + (n % 2)
            sg = n % 3
            for kc in range(KC):
                kb.op("pe", lambda e, p=p, kc=kc, c0=c0, ncol=ncol, tb=tb: e.matmul(
                    ps[p][0:ncol, :], lhsT=wbf[:, kc, c0:c0 + ncol], rhs=h0T[:, kc, tb * 512:(tb + 1) * 512],
                    start=(kc == 0), stop=(kc == KC - 1)),
                    r=[wbf_b[kc]] + h0T_b[tb * 4:(tb + 1) * 4], w=[ps_b[p]])
            if n % 2 == 0:
                kb.op("act", lambda e, p=p, sg=sg, ncol=ncol: e.copy(out=stage[sg][0:ncol, :], in_=ps[p][0:ncol, :]),
                      r=[ps_b[p]], w=[stage_b[sg]])
            else:
                kb.op("dve", lambda e, p=p, sg=sg, ncol=ncol: e.tensor_copy(out=stage[sg][0:ncol, :], in_=ps[p][0:ncol, :]),
                      r=[ps_b[p]], w=[stage_b[sg]])
            kb.op("sp", lambda e, sg=sg, c0=c0, ncol=ncol, tb=tb: e.dma_start(
                out=g.pT[c0:c0 + ncol, tb * 512:(tb + 1) * 512], in_=stage[sg][0:ncol, :]),
                r=[stage_b[sg]], dma=True)
            n += 1
    vc0 = FX0 + 1024
    for i in range(NT):
        p = 2 + (i % 2)
        s = i % 2
        for kc in range(KC):
            kb.op("pe", lambda e, p=p, kc=kc, i=i: e.matmul(
                ps[p][:, :], lhsT=h0T[:, kc, i * 128:(i + 1) * 128], rhs=wbf[:, kc, vc0:vc0 + 512],
                start=(kc == 0), stop=(kc == KC - 1)),
                r=[wbf_b[kc], h0T_b[i]], w=[ps_b[p]])
        kb.op("act", lambda e, p=p, s=s: e.copy(out=vst[s][:], in_=ps[p][:]), r=[ps_b[p]], w=[vst_b[s]])
        kb.op("sp", lambda e, s=s, i=i: e.dma_start(out=g.vtok[i * 128:(i + 1) * 128, :], in_=vst[s][:]), r=[vst_b[s]], dma=True)


def setup2(nc, g):
    ei = lambda n, s, d=F32: nc.dram_tensor(n, list(s), d, kind="ExternalInput").ap()
    g.fx_b_f = ei("fx_b_f", [8, 1])
    g.fx_q_norm = ei("fx_q_norm", [64, 1])
    g.fx_k_norm = ei("fx_k_norm", [64, 1])
    g.tri = ei("tri", [128, 128], BF16)
    g.yT = g.scratch("yT", [1024, g.T], BF16)


def consts2_np():
    c = {}
    k = np.arange(128)[:, None]
    q = np.arange(128)[None, :]
    c["tri"] = (k <= q).astype(np.float32).astype(ml_dtypes.bfloat16)
    return c


def phase2(kb, g):
    nc, T, NT, NB = g.nc, g.T, g.NT, g.NB
    onesf = kb.sbuf("onesf", [128, 128], F32)
    identf = kb.sbuf("identf2", [128, 128], F32)
    tri = kb.sbuf("tri_sb", [128, 128], BF16)
    cst_b = kb.buf("cst2")
    epsq = kb.sbuf("epsq", [128, 1], F32)
    negb = kb.sbuf("negb", [8, 1], F32)
    qw = kb.sbuf("qw", [64, 1], F32)
    kw = kb.sbuf("kw", [64, 1], F32)
    fz = kb.sbuf("fz", [8, T], F32)
    cp = kb.sbuf("cp", [8, T], F32)
    ones8 = kb.sbuf("ones8", [8, T], F32)
    cq8 = kb.sbuf("cq8", [8, T], F32)
    fz_b = kb.buf("fz")
    cq8_b = kb.buf("cq8")
    negc = kb.sbuf("negc", [128, NT, 8], F32)
    negc_b = kb.buf("negc")
    qrow = kb.sbuf("qrow", [65, T], F32)
    qrow_b = kb.buf("qrow")
    qraw = kb.sbuf("qraw", [64, T], F32)
    kraw = kb.sbuf("kraw", [64, T], F32)
    sq = kb.sbuf("sq", [64, T], F32)
    raw_b = {"q": kb.buf("qraw"), "k": kb.buf("kraw")}
    sq_b = kb.buf("sq")
    ograw = kb.sbuf("ograw", [64, T], F32)
    og_b = kb.buf("ograw")
    sg = kb.sbuf("sg", [64, T], BF16)
    sg_b = kb.buf("sg")
    Qa = kb.sbuf("Qa", [65, T], BF16)
    Ka = kb.sbuf("Ka", [65, T], BF16)
    Qa_b = kb.buf("Qa")
    Ka_b = kb.buf("Ka")
    Va = kb.sbuf("Va", [128, NT, 65], BF16)
    Va_b = kb.buf("Va")
    rst = [kb.sbuf(f"rst{i}", [64, 512], F32) for i in range(2)]
    rst_b = kb.bufs(2, "rst")
    PT = [kb.sbuf(f"PT{i}", [128, 512], BF16) for i in range(3)]
    PT_b = kb.bufs(3, "PT")
    dn = kb.sbuf("dn", [65, 512], F32)
    dn_b = kb.buf("dn")
    bcs = kb.sbuf("bcs", [64, 512], F32)
    bcs_b = kb.buf("bcs")
    o1 = kb.sbuf("o1", [64, 512], F32)
    o1_b = kb.buf("o1")
    yfx = [kb.sbuf(f"yfx{i}", [64, 512], BF16) for i in range(2)]
    yfx_b = kb.bufs(2, "yfx")
    psS = [kb.psum(f"psS{i}", [128, 512], F32) for i in range(2)]
    psS_b = kb.bufs(2, "psS")
    psO = [kb.psum(f"psO{i}", [128, 512], F32) for i in range(2)]
    psO_b = kb.bufs(2, "psO")
    psB = kb.psum("psB", [128, 512], F32)
    psB_b = kb.buf("psB")
    psN = [kb.psum(f"psN{i}", [128, 512], F32) for i in range(2)]
    psN_b = kb.bufs(2, "psN")

    kb.op("pool", lambda e: e.memset(onesf[:], 1.0), w=[cst_b])
    kb.op("pool", lambda e: e.memset(ones8[:], 1.0), w=[cst_b])
    kb.op("pool", lambda e: e.memset(epsq[:], 1e-6), w=[cst_b])
    kb.op("pool", lambda e: e.memset(Ka[64:65, :], 1.0), w=[Ka_b])
    kb.op("pool", lambda e: e.memset(Va[:, :, 64:65], 1.0), w=[Va_b])
    kb.op("sp", lambda e: e.dma_start(out=identf[:], in_=g.ident_f), w=[cst_b], dma=True)
    kb.op("sp", lambda e: e.dma_start(out=tri[:], in_=g.tri), w=[cst_b], dma=True)
    kb.op("sp", lambda e: e.dma_start(out=negb[:], in_=g.fx_b_f), w=[cst_b], dma=True)
    kb.op("sp", lambda e: e.dma_start(out=qw[:], in_=g.fx_q_norm), w=[cst_b], dma=True)
    kb.op("sp", lambda e: e.dma_start(out=kw[:], in_=g.fx_k_norm), w=[cst_b], dma=True)
    kb.op("dve", lambda e: e.tensor_scalar(out=negb[:], in0=negb[:], scalar1=-1.0, scalar2=None, op0=ALU.mult), r=[cst_b], w=[cst_b])
    fzr = FX0 + 2048
    kb.op("sp", lambda e: e.dma_start(out=fz[:], in_=g.pT[fzr:fzr + 8, :]), w=[fz_b], dma=True)
    kb.op("act", lambda e: e.activation(out=fz[:], in_=fz[:], func=AF.Exp, bias=negb[:], scale=-1.0), r=[fz_b, cst_b], w=[fz_b])
    kb.op("act", lambda e: e.activation(out=fz[:], in_=fz[:], func=AF.Ln, bias=1.0, scale=1.0), r=[fz_b], w=[fz_b])
    kb.op("dve", lambda e: e.tensor_tensor_scan(out=cp[:], data0=ones8[:], data1=fz[:], initial=0.0, op0=ALU.mult, op1=ALU.add),
          r=[fz_b, cst_b], w=[cq8_b])
    kb.op("dve", lambda e: e.tensor_scalar(out=cq8[:], in0=cp[:], scalar1=-8.0, scalar2=None, op0=ALU.mult), r=[cq8_b], w=[cq8_b])
    for i in range(NT):
        kb.op("pe", lambda e, i=i: e.transpose(out=psB[:, i * 8:(i + 1) * 8], in_=cp[:, i * 128:(i + 1) * 128], identity=identf[0:8, 0:8]),
              r=[cq8_b, cst_b], w=[psB_b])
    kb.op("act", lambda e: e.copy(out=negc[:].rearrange("p a b -> p (a b)"), in_=psB[:, 0:NT * 8]), r=[psB_b], w=[negc_b])

    for h in range(8):
        kb.op("sp", lambda e, h=h: e.dma_start(out=qraw[:], in_=g.pT[FX0 + 64 * h:FX0 + 64 * h + 64, :]), w=[raw_b["q"]], dma=True)
        kb.op("sp", lambda e, h=h: e.dma_start(out=kraw[:], in_=g.pT[FX0 + 512 + 64 * h:FX0 + 512 + 64 * h + 64, :]), w=[raw_b["k"]], dma=True)
        kb.op("sp", lambda e, h=h: e.dma_start(out=ograw[:], in_=g.pT[FX0 + 1536 + 64 * h:FX0 + 1536 + 64 * h + 64, :]), w=[og_b], dma=True)
        kb.op("sp", lambda e, h=h: e.dma_start(out=Va[:, :, 0:64], in_=g.vtok[:, 64 * h:64 * h + 64].rearrange("(j p) d -> p j d", p=128)),
              w=[Va_b], dma=True)
        kb.op("sp", lambda e, h=h: e.dma_start(out=qrow[64:65, :], in_=cq8[h:h + 1, :]), r=[cq8_b], w=[qrow_b], dma=True)
        kb.op("act", lambda e: e.copy(out=Qa[64:65, :], in_=qrow[64:65, :]), r=[qrow_b], w=[Qa_b])
        kb.op("act", lambda e: e.activation(out=sg[:], in_=ograw[:], func=AF.Sigmoid), r=[og_b], w=[sg_b])
        n = 0
        for nm, raw, wcol, dst, dst_b in (("q", qraw, qw, Qa, Qa_b), ("k", kraw, kw, Ka, Ka_b)):
            kb.op("act", lambda e, raw=raw: e.activation(out=sq[:], in_=raw[:], func=AF.Square), r=[raw_b[nm]], w=[sq_b])
            for tb in range(NB):
                p = n % 2
                n += 1
                sl = slice(tb * 512, (tb + 1) * 512)
                kb.op("pe", lambda e, p=p, sl=sl: e.matmul(psN[p][0:64, :], lhsT=onesf[0:64, 0:64], rhs=sq[:, sl], start=True, stop=True),
                      r=[sq_b, cst_b], w=[psN_b[p]])
                kb.op("act", lambda e, p=p: e.activation(out=rst[p][:], in_=psN[p][0:64, :], func=AF.Sqrt, bias=epsq[0:64, :], scale=1.0 / 64),
                      r=[psN_b[p], cst_b], w=[rst_b[p]])
                kb.op("dve", lambda e, p=p: e.reciprocal(out=rst[p][:], in_=rst[p][:]), r=[rst_b[p]], w=[rst_b[p]])
                kb.op("dve", lambda e, p=p, sl=sl, raw=raw, wcol=wcol, dst=dst: e.scalar_tensor_tensor(
                    out=dst[0:64, sl], in0=raw[:, sl], scalar=wcol[:, 0:1], in1=rst[p][:], op0=ALU.mult, op1=ALU.mult),
                    r=[raw_b[nm], rst_b[p], cst_b], w=[dst_b])
        iters = [(gq, j) for gq in range(NB) for j in range(4 * gq + 4)]

        def emit_S(n, h=h):
            gq, j = iters[n]
            col0 = max(0, j - 4 * gq) * 128
            ps_i = n % 2
            kb.op("pe", lambda e, ps_i=ps_i, j=j, gq=gq, col0=col0: e.matmul(
                psS[ps_i][:, col0:512], lhsT=Ka[0:65, j * 128:(j + 1) * 128], rhs=Qa[0:65, gq * 512 + col0:(gq + 1) * 512],
                start=True, stop=True), r=[Ka_b, Qa_b], w=[psS_b[ps_i]])

        def emit_rest(n, h=h):
            gq, j = iters[n]
            po = gq % 2
            jmax = 4 * gq + 3
            col0 = max(0, j - 4 * gq) * 128
            ps_i = n % 2
            pt_i = n % 3
            kb.op("act", lambda e, ps_i=ps_i, pt_i=pt_i, j=j, h=h, col0=col0: e.activation(
                out=PT[pt_i][:, col0:512], in_=psS[ps_i][:, col0:512], func=AF.Exp, bias=negc[:, j, h:h + 1], scale=0.125),
                r=[psS_b[ps_i], negc_b], w=[PT_b[pt_i]])
            if j >= 4 * gq:
                kb.op("pool", lambda e, pt_i=pt_i, col0=col0: e.tensor_tensor(
                    out=PT[pt_i][:, col0:col0 + 128], in0=PT[pt_i][:, col0:col0 + 128], in1=tri[:], op=ALU.mult),
                    r=[PT_b[pt_i], cst_b], w=[PT_b[pt_i]])
            kb.op("pe", lambda e, po=po, pt_i=pt_i, j=j, col0=col0, jmax=jmax: e.matmul(
                psO[po][0:65, col0:512], lhsT=Va[:, j, 0:65], rhs=PT[pt_i][:, col0:512],
                start=(j == 0), stop=(j == jmax), skip_group_check=True), r=[Va_b, PT_b[pt_i]], w=[psO_b[po]])
            if j == jmax:
                kb.op("act", lambda e, po=po: e.copy(out=dn[64:65, :], in_=psO[po][64:65, :]), r=[psO_b[po]], w=[dn_b])
                kb.op("dve", lambda e: e.reciprocal(out=dn[64:65, :], in_=dn[64:65, :]), r=[dn_b], w=[dn_b])
                kb.op("pe", lambda e: e.matmul(psB[0:64, :], lhsT=onesf[64:65, 0:64], rhs=dn[64:65, :], start=True, stop=True),
                      r=[dn_b, cst_b], w=[psB_b])
                kb.op("act", lambda e: e.copy(out=bcs[:], in_=psB[0:64, :]), r=[psB_b], w=[bcs_b])
                kb.op("dve", lambda e, po=po: e.tensor_tensor(out=o1[:], in0=psO[po][0:64, :], in1=bcs[:], op=ALU.mult),
                      r=[psO_b[po], bcs_b], w=[o1_b])
                yi = gq % 2
                kb.op("pool", lambda e, yi=yi, gq=gq: e.tensor_tensor(out=yfx[yi][:], in0=o1[:], in1=sg[:, gq * 512:(gq + 1) * 512], op=ALU.mult),
                      r=[o1_b, sg_b], w=[yfx_b[yi]])
                kb.op("sp", lambda e, yi=yi, gq=gq, h=h: e.dma_start(out=g.yT[512 + 64 * h:512 + 64 * h + 64, gq * 512:(gq + 1) * 512], in_=yfx[yi][:]),
                      r=[yfx_b[yi]], dma=True)


        emit_S(0)
        for n in range(len(iters)):
            if n + 1 < len(iters):
                emit_S(n + 1)
            emit_rest(n)


CW = 0.6065306597126334


def setup3(nc, g):
    ei = lambda n, s, d=F32: nc.dram_tensor(n, list(s), d, kind="ExternalInput").ap()
    g.rw_mu = ei("rw_mu", [RW_COLS, 1])
    g.rw_w2a2 = ei("rw_w2a2", [64, 512])
    g.rw_g2 = ei("rw_g2", [96, 512])
    g.rw_cols = ei("rw_cols", [64, 8, 8])
    g.maskG = ei("maskG", [64, 320])
    g.resetm = ei("resetm", [64, 512])


def consts3_np(d):
    c = {}
    i = np.arange(64)[:, None]
    t = np.arange(64)[None, :]
    SU = (i < t).astype(np.float32)
    U = (i <= t).astype(np.float32)
    SL = (i > t).astype(np.float32)
    c["maskG"] = np.concatenate([SU, U, SU, U, SL], axis=1)
    rm = np.ones((64, 512), np.float32)
    rm[:, ::64] = 0.0
    c["resetm"] = rm
    c["rw_mu"] = d["rw_mu"][0][:, None]
    c["rw_w2a2"] = np.concatenate([d["rw_w2"][0], d["rw_a2"][0]], axis=0)
    c["rw_g2"] = d["rw_g2"][0]
    cols = np.zeros((64, 8, 8), np.float32)
    for j, nm in enumerate(["rw_w0", "rw_a0", "rw_k_k", "rw_k_a", "rw_r_k", "rw_gn_w", "rw_gn_b"]):
        cols[:, :, j] = d[nm][0].reshape(8, 64).T
    c["rw_cols"] = cols
    return c


def phase3(kb, g):
    nc, T, NT, NB = g.nc, g.T, g.NT, g.NB
    A = lambda n, s, d=F32: kb.sbuf("r3_" + n, s, d)
    onesf = A("onesf", [64, 64]); identb = A("identb", [64, 64], BF16); identf = A("identf", [64, 64])
    maskG = A("maskG", [64, 320]); resetm = A("resetm", [64, 512])
    w2a2 = A("w2a2", [32, 512]); a2t = A("a2t", [32, 512]); g2 = A("g2", [96, 512]); cols = A("cols", [64, 8, 8])
    mu_rkv = A("mu_rkv", [64, 24]); mu_wa = A("mu_wa", [32, 1]); mu_ad = A("mu_ad", [32, 1]); mu_g = A("mu_g", [96, 1])
    eps12 = A("eps12", [64, 1]); epsgn = A("epsgn", [64, 1])
    cst = kb.buf("cst3")
    kb.op("pool", lambda e: e.memset(onesf[:], 1.0), w=[cst])
    kb.op("pool", lambda e: e.memset(eps12[:], 0.0), w=[cst])
    kb.op("pool", lambda e: e.memset(epsgn[:], 64e-5), w=[cst])
    kb.op("sp", lambda e: e.dma_start(out=identb[:], in_=g.ident_b[0:64, 0:64]), w=[cst], dma=True)
    kb.op("sp", lambda e: e.dma_start(out=identf[:], in_=g.ident_f[0:64, 0:64]), w=[cst], dma=True)
    kb.op("sp", lambda e: e.dma_start(out=maskG[:], in_=g.maskG), w=[cst], dma=True)
    kb.op("sp", lambda e: e.dma_start(out=resetm[:], in_=g.resetm), w=[cst], dma=True)
    kb.op("sp", lambda e: e.dma_start(out=w2a2[:], in_=g.rw_w2a2[0:32, :]), w=[cst], dma=True)
    kb.op("sp", lambda e: e.dma_start(out=a2t[:], in_=g.rw_w2a2[32:64, :]), w=[cst], dma=True)
    kb.op("sp", lambda e: e.dma_start(out=g2[:], in_=g.rw_g2), w=[cst], dma=True)
    kb.op("sp", lambda e: e.dma_start(out=cols[:], in_=g.rw_cols), w=[cst], dma=True)
    for j in range(24):
        kb.op("sp", lambda e, j=j: e.dma_start(out=mu_rkv[:, j:j + 1], in_=g.rw_mu[64 * j:64 * j + 64, :]), w=[cst], dma=True)
    kb.op("sp", lambda e: e.dma_start(out=mu_wa[:], in_=g.rw_mu[1536:1568, :]), w=[cst], dma=True)
    kb.op("sp", lambda e: e.dma_start(out=mu_ad[:], in_=g.rw_mu[1568:1600, :]), w=[cst], dma=True)
    kb.op("sp", lambda e: e.dma_start(out=mu_g[:], in_=g.rw_mu[1600:1696, :]), w=[cst], dma=True)

    NS = 2
    cur = [A(f"cur{i}", [96, 512]) for i in range(3)]; prv = [A(f"prv{i}", [96, 512]) for i in range(3)]
    ld_b = kb.bufs(3, "ld")
    ldn = [0]
    wam = A("wam", [32, 512]); adm = A("adm", [32, 512]); gdm = A("gdm", [96, 512]); wam_b = kb.buf("wam"); adm_b = kb.buf("adm"); gdm_b = kb.buf("gdm")
    rm = A("rm", [64, 512]); km = A("km", [64, 512]); vm = A("vm", [64, 512])
    rm_b = kb.buf("rm"); km_b = kb.buf("km"); vm_b = kb.buf("vm")
    sgw = A("sgw", [64, 512]); asig = A("asig", [64, 512]); ggL = [A(f"gg{i}", [64, 512]) for i in range(6)]
    sgw_b = kb.buf("sgw"); asig_b = kb.buf("asig"); ggL_b = kb.bufs(6, "gg")
    kk = A("kk", [64, 512]); t1 = A("t1", [64, 512]); t2 = A("t2", [64, 512]); kmod = A("kmod", [64, 512]); bb = A("bb", [64, 512])
    kk_b = kb.buf("kk"); t1_b = kb.buf("t1"); t2_b = kb.buf("t2"); kmod_b = kb.buf("kmod"); bb_b = kb.buf("bb")
    cumS = A("cumS", [64, 512]); ginclL = [A(f"gincl{i}", [64, 512]) for i in range(4)]; ginv = A("ginv", [64, 512]); gexcl = A("gexcl", [64, 512])
    cum_b = kb.buf("cum"); ginclL_b = kb.bufs(4, "gincl"); ginv_b = kb.buf("ginv"); gexcl_b = kb.buf("gexcl")
    bonL = [A(f"bon{i}", [64, 512]) for i in range(6)]; bonL_b = kb.bufs(6, "bon")
    AR = [A(f"AR{i}", [64, 8, 128], BF16) for i in range(4)]; BK = [A(f"BK{i}", [64, 8, 128], BF16) for i in range(4)]
    vb = [A(f"vb{i}", [64, 512], BF16) for i in range(4)]
    AR_b = kb.bufs(4, "AR"); BK_b = kb.bufs(4, "BK"); vb_b = kb.bufs(4, "vb")
    TM = [A(f"TM{i}", [64, 320], BF16) for i in range(NS)]; TM_b = kb.bufs(NS, "TM"); TMx_b = kb.bufs(NS, "TMx")
    NM = [A(f"NM{i}", [64, 320], BF16) for i in range(NS)]; NM_b = kb.bufs(NS, "NM")
    DB = [[A(f"DB{i}_{j}", [64, 192], BF16) for j in range(2)] for i in range(NS)]
    DB_b = [kb.bufs(2, f"DB{i}") for i in range(NS)]
    AW = [A(f"AW{i}", [64, 128], BF16) for i in range(NS)]; AW_b = kb.bufs(NS, "AW")
    G1 = [A(f"G1{i}", [64, 64]) for i in range(NS)]; G1_b = kb.bufs(NS, "G1")
    Hg = [A(f"Hg{i}", [64, 64]) for i in range(NS)]; Hg_b = kb.bufs(NS, "Hg")
    Ry = [A(f"Ry{i}", [64, 64]) for i in range(NS)]; Ry_b = kb.bufs(NS, "Ry")
    ST = [[A(f"ST{h}_{j}", [64, 64]) for j in range(2)] for h in range(8)]
    ST_b = [kb.bufs(2, f"ST{h}") for h in range(8)]
    ysbL = [A(f"ysb{i}", [64, 512]) for i in range(2)]; ymsq = A("ymsq", [64, 512]); ysq = A("ysq", [64, 512]); ymean = A("ymean", [64, 512]); yvar = A("yvar", [64, 512])
    ysbL_b = kb.bufs(2, "ysb"); ymsq_b = kb.buf("ymsq"); ysq_b = kb.buf("ysq"); ymean_b = kb.buf("ymean"); yvar_b = kb.buf("yvar")
    yout = [A(f"yout{i}", [64, 512], BF16) for i in range(2)]; yout_b = kb.bufs(2, "yout")
    tw = A("tw", [32, 512]); sgd = A("sgd", [96, 512]); tw_b = kb.buf("tw"); sgd_b = kb.buf("sgd")
    psT = kb.psum("r3psT", [128, 1024], BF16); psT_b = kb.buf("psT")
    psG = kb.psum("r3psG", [128, 512], F32); psG_b = kb.buf("psG")
    psD = [kb.psum(f"r3psD{i}", [128, 512], F32) for i in range(2)]; psD_b = kb.bufs(2, "psD")
    psA = kb.psum("r3psA", [128, 512], F32)
    psX_b = kb.buf("psA"); psAW_b = psX_b; psGp_b = psX_b; psH_b = psX_b; psR_b = psX_b
    psYL = [kb.psum(f"r3psY{i}", [128, 512], F32) for i in range(2)]; psYL_b = kb.bufs(2, "psY")
    psL = kb.psum("r3psL", [128, 512], F32); psL_b = kb.buf("psL")

    for h in range(8):
        kb.op("pool", lambda e, h=h: e.memset(ST[h][0][:], 0.0), w=[ST_b[h][0]])

    def load_mixed(rows0, nrows, tb, mucol, dst, dst_b):
        i = ldn[0] % 3
        ldn[0] += 1
        t0 = tb * 512
        kb.op("sp", lambda e: e.dma_start(out=cur[i][0:nrows, :], in_=g.pT[rows0:rows0 + nrows, t0:t0 + 512]), w=[ld_b[i]], dma=True)
        if tb == 0:
            kb.op("pool", lambda e: e.memset(prv[i][0:nrows, 0:1], 0.0), w=[ld_b[i]])
            kb.op("sp", lambda e: e.dma_start(out=prv[i][0:nrows, 1:512], in_=g.pT[rows0:rows0 + nrows, 0:511]), w=[ld_b[i]], dma=True)
        else:
            kb.op("sp", lambda e: e.dma_start(out=prv[i][0:nrows, :], in_=g.pT[rows0:rows0 + nrows, t0 - 1:t0 + 511]), w=[ld_b[i]], dma=True)
        kb.op("pool", lambda e: e.tensor_tensor(out=prv[i][0:nrows, :], in0=prv[i][0:nrows, :], in1=cur[i][0:nrows, :], op=ALU.subtract),
              r=[ld_b[i]], w=[ld_b[i]])
        kb.op("dve", lambda e: e.scalar_tensor_tensor(out=dst[0:nrows, :], in0=prv[i][0:nrows, :], scalar=mucol, in1=cur[i][0:nrows, :],
                                                       op0=ALU.mult, op1=ALU.add), r=[ld_b[i], cst], w=[dst_b])

    def r1_stage(h, tb, zz, zb):
        z = h % 2
        pz = tb * 8 + h
        gincl = ginclL[zz]; gincl_b = ginclL_b[zz]; bon = bonL[zb]; bon_b = bonL_b[zb]; gg = ggL[zb]; gg_b = ggL_b[zb]
        psY = psYL[z]; psY_b = psYL_b[z]
        C = lambda j, h=h: cols[:, h, j:j + 1]
        hc = slice(64 * h, 64 * h + 64)
        yield
        load_mixed(64 * h, 64, tb, mu_rkv[:, h:h + 1], rm, rm_b)
        yield
        load_mixed(512 + 64 * h, 64, tb, mu_rkv[:, 8 + h:9 + h], km, km_b)
        yield
        load_mixed(1024 + 64 * h, 64, tb, mu_rkv[:, 16 + h:17 + h], vm, vm_b)
        yield
        kb.op("pe", lambda e, hc=hc: e.matmul(psL[0:64, :], lhsT=w2a2[0:32, hc], rhs=tw[0:32, :], start=True, stop=True),
              r=[tw_b, cst], w=[psL_b])
        kb.op("act", lambda e, C=C: e.activation(out=sgw[:], in_=psL[0:64, :], func=AF.Sigmoid, bias=C(0), scale=1.0),
              r=[psL_b, cst], w=[sgw_b])
        yield
        kb.op("pe", lambda e, hc=hc: e.matmul(psL[0:64, :], lhsT=a2t[0:32, hc], rhs=adm[0:32, :], start=True, stop=True),
              r=[adm_b, cst], w=[psL_b])
        kb.op("act", lambda e, C=C: e.activation(out=asig[:], in_=psL[0:64, :], func=AF.Sigmoid, bias=C(1), scale=1.0),
              r=[psL_b, cst], w=[asig_b])
        yield
        kb.op("pe", lambda e, hc=hc: e.matmul(psL[0:64, :], lhsT=g2[0:96, hc], rhs=sgd[0:96, :], start=True, stop=True),
              r=[sgd_b, cst], w=[psL_b])
        kb.op("act", lambda e: e.copy(out=gg[:], in_=psL[0:64, :]), r=[psL_b], w=[gg_b])
        yield
        kb.op("dve", lambda e, C=C: e.tensor_scalar(out=kk[:], in0=km[:], scalar1=C(2), scalar2=None, op0=ALU.mult), r=[km_b, cst], w=[kk_b])
        yield
        kb.op("pool", lambda e: e.tensor_tensor(out=t1[:], in0=kk[:], in1=kk[:], op=ALU.mult), r=[kk_b], w=[t1_b])
        yield
        kb.op("pe", lambda e: e.matmul(psL[0:64, :], lhsT=onesf[:, :], rhs=t1[:], start=True, stop=True), r=[t1_b, cst], w=[psL_b])
        kb.op("act", lambda e: e.activation(out=t2[:], in_=psL[0:64, :], func=AF.Sqrt, bias=eps12[:], scale=1.0), r=[psL_b, cst], w=[t2_b])
        yield
        kb.op("dve", lambda e: e.tensor_scalar(out=t2[:], in0=t2[:], scalar1=1e-12, scalar2=None, op0=ALU.max), r=[t2_b], w=[t2_b])
        yield
        kb.op("dve", lambda e: e.reciprocal(out=t2[:], in_=t2[:]), r=[t2_b], w=[t2_b])
        yield
        kb.op("dve", lambda e: e.tensor_tensor(out=kk[:], in0=kk[:], in1=t2[:], op=ALU.mult), r=[kk_b, t2_b], w=[kk_b])
        yield
        kb.op("dve", lambda e, C=C: e.tensor_scalar(out=t1[:], in0=asig[:], scalar1=-1.0, scalar2=C(3), op0=ALU.add, op1=ALU.mult),
              r=[asig_b, cst], w=[t1_b])
        yield
        kb.op("dve", lambda e: e.scalar_tensor_tensor(out=kmod[:], in0=t1[:], scalar=1.0, in1=km[:], op0=ALU.add, op1=ALU.mult),
              r=[t1_b, km_b], w=[kmod_b])
        yield
        kb.op("pool", lambda e: e.tensor_tensor(out=bb[:], in0=kk[:], in1=asig[:], op=ALU.mult), r=[kk_b, asig_b], w=[bb_b])
        yield
        kb.op("dve", lambda e: e.tensor_tensor_scan(out=cumS[:], data0=resetm[:], data1=sgw[:], initial=0.0, op0=ALU.mult, op1=ALU.add),
              r=[sgw_b, cst], w=[cum_b])
        yield
        kb.op("act", lambda e: e.activation(out=gincl[:], in_=cumS[:], func=AF.Exp, scale=-CW), r=[cum_b], w=[gincl_b])
        yield
        kb.op("act", lambda e: e.activation(out=ginv[:], in_=cumS[:], func=AF.Exp, scale=CW), r=[cum_b], w=[ginv_b])
        yield
        kb.op("pool", lambda e: e.tensor_tensor(out=t2[:], in0=cumS[:], in1=sgw[:], op=ALU.subtract), r=[cum_b, sgw_b], w=[t2_b])
        yield
        kb.op("act", lambda e: e.activation(out=gexcl[:], in_=t2[:], func=AF.Exp, scale=-CW), r=[t2_b], w=[gexcl_b])
        v4 = lambda t: t[:].rearrange("p (c t) -> p c t", c=8)
        yield
        kb.op("dve", lambda e, z=zz: e.scalar_tensor_tensor(out=AR[zz][:, :, 0:64], in0=v4(kk), scalar=-1.0, in1=v4(gexcl), op0=ALU.mult, op1=ALU.mult),
              r=[kk_b, gexcl_b], w=[AR_b[zz]])
        yield
        kb.op("pool", lambda e, z=zz: e.tensor_tensor(out=AR[zz][:, :, 64:128], in0=v4(rm), in1=v4(gincl), op=ALU.mult),
              r=[rm_b, gincl_b], w=[AR_b[zz]])
        yield
        kb.op("dve", lambda e, z=zz: e.tensor_tensor(out=BK[zz][:, :, 0:64], in0=v4(bb), in1=v4(ginv), op=ALU.mult),
              r=[bb_b, ginv_b], w=[BK_b[zz]])
        yield
        kb.op("pool", lambda e, z=zz: e.tensor_tensor(out=BK[zz][:, :, 64:128], in0=v4(kmod), in1=v4(ginv), op=ALU.mult),
              r=[kmod_b, ginv_b], w=[BK_b[zz]])
        yield
        kb.op("act", lambda e, z=zz: e.copy(out=vb[zz][:], in_=vm[:]), r=[vm_b], w=[vb_b[zz]])
        yield
        kb.op("dve", lambda e, C=C: e.scalar_tensor_tensor(out=t1[:], in0=rm[:], scalar=C(4), in1=kmod[:], op0=ALU.mult, op1=ALU.mult),
              r=[rm_b, kmod_b, cst], w=[t1_b])
        yield
        kb.op("pe", lambda e: e.matmul(psL[0:64, :], lhsT=onesf[:, :], rhs=t1[:], start=True, stop=True), r=[t1_b, cst], w=[psL_b])
        kb.op("dve", lambda e: e.tensor_tensor(out=bon[:], in0=psL[0:64, :], in1=vm[:], op=ALU.mult), r=[psL_b, vm_b], w=[bon_b])


    def unit_chain(h, tb, zz, zb):
        z = h % 2
        pz = tb * 8 + h
        gincl = ginclL[zz]; gincl_b = ginclL_b[zz]; bon = bonL[zb]; bon_b = bonL_b[zb]; gg = ggL[zb]; gg_b = ggL_b[zb]
        psY = psYL[z]; psY_b = psYL_b[z]
        C = lambda j, h=h: cols[:, h, j:j + 1]
        for c in range(8):
            u = z
            cs = slice(c * 64, c * 64 + 64)
            gC = gincl[:, c * 64 + 63:c * 64 + 64]
            srcs = [(BK[zz][:, c, 0:64], BK_b[zz]), (BK[zz][:, c, 64:128], BK_b[zz]), (vb[zz][:, cs], vb_b[zz]), (AR[zz][:, c, 0:64], AR_b[zz])]
            for k4, (src, sb) in enumerate(srcs):
                kb.op("pe", lambda e, k4=k4, src=src: e.transpose(out=psT[0:64, k4 * 64:(k4 + 1) * 64], in_=src, identity=identb[:]),
                      r=[sb, cst], w=[psT_b])
            kb.op("act", lambda e, u=u: e.copy(out=TM[u][:, 0:256], in_=psT[0:64, 0:256]), r=[psT_b], w=[TM_b[u]])
            yield
            kb.op("pe", lambda e, z=zz, c=c: e.matmul(psG[0:64, 0:128], lhsT=BK[zz][:, c, 0:64], rhs=AR[zz][:, c, :], start=True, stop=True),
                  r=[BK_b[zz], AR_b[zz]], w=[psG_b])
            kb.op("pe", lambda e, z=zz, c=c: e.matmul(psG[0:64, 128:256], lhsT=BK[zz][:, c, 64:128], rhs=AR[zz][:, c, :], start=True, stop=True),
                  r=[BK_b[zz], AR_b[zz]], w=[psG_b])
            kb.op("pe", lambda e, z=zz, c=c: e.matmul(psG[0:64, 256:320], lhsT=AR[zz][:, c, 0:64], rhs=BK[zz][:, c, 0:64], start=True, stop=True),
                  r=[BK_b[zz], AR_b[zz]], w=[psG_b])
            kb.op("dve", lambda e, u=u: e.tensor_tensor(out=NM[u][:], in0=psG[0:64, 0:320], in1=maskG[:], op=ALU.mult),
                  r=[psG_b, cst], w=[NM_b[u]])
            yield
            kb.op("pe", lambda e, u=u: e.matmul(psA[0:64, 0:64], lhsT=NM[u][:, 128:192], rhs=TM[u][:, 128:192], start=True, stop=True),
                  r=[NM_b[u], TM_b[u]], w=[psX_b])
            kb.op("act", lambda e, u=u: e.copy(out=TM[u][:, 256:320], in_=psA[0:64, 0:64]), r=[psX_b], w=[TMx_b[u]])
            yield
            d0, d1 = DB[u][0], DB[u][1]
            pd = psD[u % 2]
            pdb = psD_b[u % 2]
            kb.op("dve", lambda e, u=u, d1=d1: e.tensor_tensor(out=d1[:, 0:64], in0=NM[u][:, 0:64], in1=identb[:], op=ALU.add),
                  r=[NM_b[u], cst], w=[DB_b[u][1]])
            kb.op("pe", lambda e, u=u, pd=pd: e.matmul(pd[0:64, 64:128], lhsT=NM[u][:, 256:320], rhs=NM[u][:, 0:64], start=True, stop=True),
                  r=[NM_b[u]], w=[pdb])
            kb.op("pe", lambda e, u=u, pd=pd: e.matmul(pd[0:64, 128:192], lhsT=NM[u][:, 0:64], rhs=NM[u][:, 256:320], start=True, stop=True),
                  r=[NM_b[u]], w=[pdb])
            kb.op("act", lambda e, d1=d1, pd=pd: e.copy(out=d1[:, 64:192], in_=pd[0:64, 64:192]), r=[pdb], w=[DB_b[u][1]])
            ci = 1
            for lvl in range(1, 6):
                dc, dn_ = DB[u][ci], DB[u][1 - ci]
                dcb, dnb = DB_b[u][ci], DB_b[u][1 - ci]
                kb.op("pe", lambda e, dc=dc, pd=pd: e.matmul(pd[0:64, 0:128], lhsT=dc[:, 128:192], rhs=dc[:, 0:128], start=True, stop=True),
                      r=[dcb], w=[pdb])
                if lvl < 5:
                    kb.op("pe", lambda e, dc=dc, pd=pd: e.matmul(pd[0:64, 128:192], lhsT=dc[:, 64:128], rhs=dc[:, 128:192], start=True, stop=True),
                          r=[dcb], w=[pdb])
                kb.op("dve", lambda e, dc=dc, dn_=dn_, pd=pd: e.tensor_tensor(out=dn_[:, 0:64], in0=pd[0:64, 0:64], in1=dc[:, 0:64], op=ALU.add),
                      r=[pdb, dcb], w=[dnb])
                if lvl < 5:
                    kb.op("act", lambda e, dn_=dn_, pd=pd: e.copy(out=dn_[:, 64:192], in_=pd[0:64, 64:192]), r=[pdb], w=[dnb])
                yield
                ci = 1 - ci
            Tm, Tm_b = DB[u][ci], DB_b[u][ci]
            yield
            kb.op("pe", lambda e, u=u, Tm=Tm: e.matmul(psA[0:64, 64:192], lhsT=Tm[:, 0:64], rhs=TM[u][:, 192:320], start=True, stop=True),
                  r=[Tm_b, TM_b[u], TMx_b[u]], w=[psAW_b])
            kb.op("act", lambda e, u=u: e.copy(out=AW[u][:], in_=psA[0:64, 64:192]), r=[psAW_b], w=[AW_b[u]])
            yield
            kb.op("pe", lambda e, u=u: e.matmul(psA[0:64, 192:256], lhsT=AW[u][:, 0:64], rhs=TM[u][:, 0:64], start=True, stop=True),
                  r=[AW_b[u], TM_b[u]], w=[psGp_b])
            kb.op("dve", lambda e, u=u: e.tensor_tensor(out=G1[u][:], in0=psA[0:64, 192:256], in1=identf[:], op=ALU.add),
                  r=[psGp_b, cst], w=[G1_b[u]])
            yield
            kb.op("pe", lambda e, u=u: e.matmul(psA[0:64, 256:320], lhsT=TM[u][:, 0:64], rhs=AW[u][:, 64:128], start=True, stop=False),
                  r=[AW_b[u], TM_b[u]], w=[psH_b])
            kb.op("pe", lambda e, u=u: e.matmul(psA[0:64, 256:320], lhsT=TM[u][:, 64:128], rhs=TM[u][:, 128:192], start=False, stop=True),
                  r=[TM_b[u]], w=[psH_b])
            kb.op("dve", lambda e, u=u, gC=gC: e.tensor_scalar(out=Hg[u][:], in0=psA[0:64, 256:320], scalar1=gC, scalar2=None, op0=ALU.mult),
                  r=[psH_b, gincl_b], w=[Hg_b[u]])
            yield
            kb.op("pe", lambda e, u=u: e.matmul(psA[0:64, 320:384], lhsT=AW[u][:, 0:64], rhs=NM[u][:, 64:128], start=True, stop=True),
                  r=[AW_b[u], NM_b[u]], w=[psR_b])
            kb.op("dve", lambda e, u=u, z=zz, c=c: e.tensor_tensor(out=Ry[u][:], in0=psA[0:64, 320:384], in1=AR[zz][:, c, 64:128], op=ALU.add),
                  r=[psR_b, AR_b[zz]], w=[Ry_b[u]])
            yield
            sc = (tb * 8 + c) % 2
            So, Sn = ST[h][sc], ST[h][1 - sc]
            Sob, Snb = ST_b[h][sc], ST_b[h][1 - sc]
            kb.op("pe", lambda e, u=u, cs=cs: e.matmul(psY[0:64, cs], lhsT=AW[u][:, 64:128], rhs=NM[u][:, 64:128], start=True, stop=False, skip_group_check=True),
                  r=[AW_b[u], NM_b[u]], w=[psY_b])
            kb.op("pe", lambda e, u=u, cs=cs: e.matmul(psY[0:64, cs], lhsT=TM[u][:, 128:192], rhs=NM[u][:, 192:256], start=False, stop=False, skip_group_check=True),
                  r=[TM_b[u], NM_b[u]], w=[psY_b])
            kb.op("pe", lambda e, u=u, cs=cs, So=So: e.matmul(psY[0:64, cs], lhsT=So[:], rhs=Ry[u][:], start=False, stop=True, skip_group_check=True),
                  r=[Sob, Ry_b[u]], w=[psY_b])
            yield
            kb.op("pe", lambda e, u=u, So=So: e.matmul(psA[0:64, 384:448], lhsT=G1[u][:], rhs=So[:], start=True, stop=True),
                  r=[G1_b[u], Sob], w=[psX_b])
            kb.op("dve", lambda e, u=u, Sn=Sn, gC=gC: e.scalar_tensor_tensor(out=Sn[:], in0=psA[0:64, 384:448], scalar=gC, in1=Hg[u][:], op0=ALU.mult, op1=ALU.add),
                  r=[psX_b, Hg_b[u], gincl_b], w=[Snb])
            yield

    def gn_stage(h, tb, zz, zb):
        z = h % 2
        pz = tb * 8 + h
        gincl = ginclL[zz]; gincl_b = ginclL_b[zz]; bon = bonL[zb]; bon_b = bonL_b[zb]; gg = ggL[zb]; gg_b = ggL_b[zb]
        psY = psYL[z]; psY_b = psYL_b[z]
        ysb = ysbL[z]; ysb_b = ysbL_b[z]
        C = lambda j, h=h: cols[:, h, j:j + 1]
        yield
        kb.op("pool", lambda e: e.tensor_tensor(out=ysq[:], in0=ysb[:], in1=ysb[:], op=ALU.mult), r=[ysb_b], w=[ysq_b])
        yield
        kb.op("pe", lambda e: e.matmul(psL[0:64, :], lhsT=onesf[:, :], rhs=ysb[:], start=True, stop=True), r=[ysb_b, cst], w=[psL_b])
        kb.op("act", lambda e: e.activation(out=ymean[:], in_=psL[0:64, :], func=AF.Identity, scale=1.0 / 64), r=[psL_b], w=[ymean_b])
        yield
        kb.op("pool", lambda e: e.tensor_tensor(out=ymsq[:], in0=ymean[:], in1=ymean[:], op=ALU.mult), r=[ymean_b], w=[ymsq_b])
        yield
        kb.op("pe", lambda e: e.matmul(psL[0:64, :], lhsT=onesf[:, :], rhs=ysq[:], start=True, stop=True), r=[ysq_b, cst], w=[psL_b])
        kb.op("dve", lambda e: e.scalar_tensor_tensor(out=yvar[:], in0=psL[0:64, :], scalar=1.0 / 64, in1=ymsq[:], op0=ALU.mult, op1=ALU.subtract),
              r=[psL_b, ymsq_b], w=[yvar_b])
        yield
        kb.op("act", lambda e: e.activation(out=yvar[:], in_=yvar[:], func=AF.Sqrt, bias=epsgn[:], scale=1.0), r=[yvar_b, cst], w=[yvar_b])
        yield
        kb.op("dve", lambda e: e.reciprocal(out=yvar[:], in_=yvar[:]), r=[yvar_b], w=[yvar_b])
        yield
        kb.op("pool", lambda e: e.tensor_tensor(out=ysb[:], in0=ysb[:], in1=ymean[:], op=ALU.subtract), r=[ysb_b, ymean_b], w=[ysb_b])
        yield
        kb.op("dve", lambda e, C=C: e.scalar_tensor_tensor(out=ysb[:], in0=ysb[:], scalar=C(5), in1=yvar[:], op0=ALU.mult, op1=ALU.mult),
              r=[ysb_b, yvar_b, cst], w=[ysb_b])
        yield
        kb.op("dve", lambda e, C=C: e.scalar_tensor_tensor(out=ysb[:], in0=ysb[:], scalar=C(6), in1=bon[:], op0=ALU.add, op1=ALU.add),
              r=[ysb_b, bon_b, cst], w=[ysb_b])
        yo = pz % 2
        yield
        kb.op("pool", lambda e, yo=yo: e.tensor_tensor(out=yout[yo][:], in0=ysb[:], in1=gg[:], op=ALU.mult), r=[ysb_b, gg_b], w=[yout_b[yo]])
        yield
        kb.op("sp", lambda e, yo=yo, h=h, tb=tb: e.dma_start(out=g.yT[64 * h:64 * h + 64, tb * 512:(tb + 1) * 512], in_=yout[yo][:]),
              r=[yout_b[yo]], dma=True)
    def lora_stage(tb):
        load_mixed(1536, 32, tb, mu_wa[:, 0:1], wam, wam_b)
        load_mixed(1568, 32, tb, mu_ad[:, 0:1], adm, adm_b)
        load_mixed(1600, 96, tb, mu_g[:, 0:1], gdm, gdm_b)
        kb.op("act", lambda e: e.activation(out=tw[0:32, :], in_=wam[0:32, :], func=AF.Tanh), r=[wam_b], w=[tw_b])
        kb.op("act", lambda e: e.activation(out=sgd[:], in_=gdm[:], func=AF.Sigmoid), r=[gdm_b], w=[sgd_b])

    def chain2(*gens):
        for gen in gens:
            yield from gen

    def run_rr(gens):
        alive = list(gens)
        while alive:
            for gen in list(alive):
                try:
                    next(gen)
                except StopIteration:
                    alive.remove(gen)

    slots = [(tb, hp) for tb in range(NB) for hp in range(4)]

    def gn_pre(si):
        for z in range(2):
            kb.op("act", lambda e, z=z: e.copy(out=ysbL[z][:], in_=psYL[z][0:64, :]), r=[psYL_b[z]], w=[ysbL_b[z]])

    def heads_of(si):
        tb, hp = slots[si]
        return [(2 * hp + k, tb, 2 * (si % 2) + k, 2 * (si % 3) + k) for k in range(2)]

    lora_stage(0)
    run_rr([chain2(*[r1_stage(*a) for a in heads_of(0)])])
    for si in range(len(slots)):
        gens = [unit_chain(*a) for a in heads_of(si)]
        if si + 1 < len(slots):
            if slots[si + 1][1] == 0:
                lora_stage(slots[si + 1][0])
            gens.append(chain2(*[r1_stage(*a) for a in heads_of(si + 1)]))
        if si > 0:
            gn_pre(si - 1)
            gens.append(chain2(*[gn_stage(*a) for a in heads_of(si - 1)]))
        run_rr(gens)
    gn_pre(len(slots) - 1)
    run_rr([chain2(*[gn_stage(*a) for a in heads_of(len(slots) - 1)])])


def setup4(nc, g, out_name="out"):
    ei = lambda n, s, d=F32: nc.dram_tensor(n, list(s), d, kind="ExternalInput").ap()
    T = g.T
    g.w_o = ei("w_o", [D, D])
    g.ln1_w = ei("ln1_w", [1, D]); g.ln1_b = ei("ln1_b", [1, D])
    g.ln2_w = ei("ln2_w", [1, D]); g.ln2_b = ei("ln2_b", [1, D])
    g.router_w = ei("router_w", [D, 32]); g.router_b = ei("router_b", [1, 32])
    g.exp_w1 = ei("exp_w1", [32, D, 2048]); g.exp_b1 = ei("exp_b1", [32, 2048])
    g.exp_w2 = ei("exp_w2", [32, D, D]); g.exp_b2 = ei("exp_b2", [32, D])
    g.h1f = g.scratch("h1f", [T, D])
    g.h1T = g.scratch("h1T", [D, T], BF16)
    g.yacc = g.scratch("yacc", [T, D])
    g.out = nc.dram_tensor(out_name, [T, D], F32, kind="ExternalOutput").ap()


def _ln_tile(kb, src, src_b, dst, dst_b, st, mv, rs, st_b, epsc, cst, gam, bet):
    for hf in range(2):
        kb.op("dve", lambda e, hf=hf: e.bn_stats(out=st[:, hf, :], in_=src[:, hf * 512:(hf + 1) * 512]), r=[src_b], w=[st_b])
    kb.op("dve", lambda e: e.bn_aggr(out=mv[:], in_=st[:].rearrange("p a b -> p (a b)")), r=[st_b], w=[st_b])
    kb.op("act", lambda e: e.activation(out=rs[:], in_=mv[:, 1:2], func=AF.Sqrt, bias=epsc[:], scale=1.0), r=[st_b, cst], w=[st_b])
    kb.op("dve", lambda e: e.reciprocal(out=rs[:], in_=rs[:]), r=[st_b], w=[st_b])
    kb.op("dve", lambda e: e.tensor_scalar(out=dst[:], in0=src[:], scalar1=mv[:, 0:1], scalar2=rs[:], op0=ALU.subtract, op1=ALU.mult),
          r=[src_b, st_b], w=[dst_b])
    kb.op("dve", lambda e: e.tensor_tensor(out=dst[:], in0=dst[:], in1=gam[:], op=ALU.mult), r=[dst_b, cst], w=[dst_b])
    kb.op("dve", lambda e: e.tensor_tensor(out=dst[:], in0=dst[:], in1=bet[:], op=ALU.add), r=[dst_b, cst], w=[dst_b])


def phase4(kb, g, n_exp=32, sections="ABC"):
    nc, T, NT, NB = g.nc, g.T, g.NT, g.NB
    kb.ps_stack.close(); kb.ps_stack = None
    gates = kb.sbuf("m4_gates", [128, NT, 32], F32); gates_b = kb.buf("gates")
    kb.begin_phase()
    A = lambda n, s, d=F32: kb.sbuf("m4_" + n, s, d)
    cst = kb.buf("cst4")
    wo = A("wo", [128, KC, D], BF16)
    rwt = A("rwt", [128, KC, 32]); rbt = A("rbt", [128, 32])
    gam1 = A("gam1", [128, D]); bet1 = A("bet1", [128, D])
    identf = A("identf", [128, 128]); epsc = A("epsc", [128, 1])
    kb.op("dve", lambda e: e.memset(epsc[:], LN_EPS), w=[cst])
    kb.op("sp", lambda e: e.dma_start(out=identf[:], in_=g.ident_f), w=[cst], dma=True)
    for nm, t_, src in (("g1", gam1, g.ln1_w), ("b1", bet1, g.ln1_b)):
        kb.op("sp", lambda e, t_=t_, src=src: e.dma_start(out=t_[:], in_=src.broadcast_to([128, D])), w=[cst], dma=True)
    kb.op("sp", lambda e: e.dma_start(out=rbt[:], in_=g.router_b.broadcast_to([128, 32])), w=[cst], dma=True)
    kb.op("sp", lambda e: e.dma_start(out=rwt[:], in_=g.router_w.rearrange("(kc p) n -> p kc n", p=128)), w=[cst], dma=True)
    for kc in range(KC):
        kb.op("pool", lambda e, kc=kc: e.dma_start(out=wo[:, kc, :], in_=g.w_o[kc * 128:(kc + 1) * 128, :]), w=[cst], dma=True)

    ymt = [A(f"ymt{i}", [128, KC, 128], BF16) for i in range(2)]; ymt_b = kb.bufs(2, "ymt")
    h0t = [A(f"h0t{i}", [128, D]) for i in range(2)]; h0t_b = kb.bufs(2, "h0t")
    sres = [A(f"sres{i}", [128, D]) for i in range(2)]; sres_b = kb.bufs(2, "sres")
    h1t = [A(f"h1t{i}", [128, D]) for i in range(2)]; h1t_b = kb.bufs(2, "h1t")
    st = A("st", [128, 2, 6]); mv = A("mv", [128, 2]); rs = A("rs", [128, 1]); st_b = kb.buf("st")
    h1Tf = A("h1Tf", [128, KC, 128]); h1Tf_b = kb.buf("h1Tf")
    h1Tb = [A(f"h1Tb{i}", [128, KC, 128], BF16) for i in range(2)]; h1Tb_b = kb.bufs(2, "h1Tb")
    lg = A("lg", [128, 32]); mx8 = A("mx8", [128, 8]); msk = A("msk", [128, 32]); ee = A("ee", [128, 32]); ssum = A("ssum", [128, 1])
    nm0 = A("nm0", [128, 1]); lg_b = kb.buf("lg")
    ps = [kb.psum(f"m4ps{i}", [128, 512], F32) for i in range(8)]; ps_b = kb.bufs(8, "m4ps")

    import os
    STOPAT = int(os.environ.get('STOPAT', '99'))
    for i in range(NT):
        s = i % 2
        tsl = slice(i * 128, (i + 1) * 128)
        kb.op("sp", lambda e, s=s, tsl=tsl: e.dma_start(out=ymt[s][:], in_=g.yT[:, tsl].rearrange("(kc p) t -> p kc t", p=128)), w=[ymt_b[s]], dma=True)
        kb.op("sp", lambda e, s=s, tsl=tsl: e.dma_start(out=h0t[s][:], in_=g.h0f[tsl, :]), w=[h0t_b[s]], dma=True)
        if STOPAT <= 1:
            continue
        for hf in range(2):
            for kc in range(KC):
                kb.op("pe", lambda e, s=s, hf=hf, kc=kc: e.matmul(ps[hf][:, :], lhsT=ymt[s][:, kc, :], rhs=wo[:, kc, hf * 512:(hf + 1) * 512],
                                                                 start=(kc == 0), stop=(kc == KC - 1)), r=[ymt_b[s], cst], w=[ps_b[hf]])
            kb.op("dve", lambda e, s=s, hf=hf: e.scalar_tensor_tensor(out=sres[s][:, hf * 512:(hf + 1) * 512], in0=h0t[s][:, hf * 512:(hf + 1) * 512],
                                                                     scalar=ALPHA, in1=ps[hf][:, :], op0=ALU.mult, op1=ALU.add),
                  r=[h0t_b[s], ps_b[hf]], w=[sres_b[s]])
        if STOPAT <= 2:
            continue
        _ln_tile(kb, sres[s], sres_b[s], h1t[s], h1t_b[s], st, mv, rs, st_b, epsc, cst, gam1, bet1)
        if STOPAT <= 3:
            continue
        kb.op("sp", lambda e, s=s, tsl=tsl: e.dma_start(out=g.h1f[tsl, :], in_=h1t[s][:]), r=[h1t_b[s]], dma=True)
        if STOPAT <= 4:
            continue
        for hf in range(2):
            p = 2 + hf
            for c in range(4):
                kc = hf * 4 + c
                kb.op("pe", lambda e, s=s, p=p, c=c, kc=kc: e.transpose(out=ps[p][:, c * 128:(c + 1) * 128], in_=h1t[s][:, kc * 128:(kc + 1) * 128], identity=identf[:]),
                      r=[h1t_b[s], cst], w=[ps_b[p]])
            kb.op("act", lambda e, p=p, hf=hf: e.copy(out=h1Tf[:, hf * 4:(hf + 1) * 4, :], in_=ps[p][:].rearrange("p (c t) -> p c t", c=4)), r=[ps_b[p]], w=[h1Tf_b])
            kb.op("dve", lambda e, hf=hf, s=s: e.tensor_copy(out=h1Tb[s][:, hf * 4:(hf + 1) * 4, :], in_=h1Tf[:, hf * 4:(hf + 1) * 4, :]),
                  r=[h1Tf_b], w=[h1Tb_b[s]])
        if STOPAT <= 5:
            continue
        kb.op("sp", lambda e, s=s, tsl=tsl: e.dma_start(out=g.h1T[:, tsl].rearrange("(kc p) t -> p kc t", p=128), in_=h1Tb[s][:]), r=[h1Tb_b[s]], dma=True)
        import os
        if os.environ.get('SKIP_ROUTER'):
            continue
        for kc in range(KC):
            kb.op("pe", lambda e, kc=kc: e.matmul(ps[4][:, 0:32], lhsT=h1Tf[:, kc, :], rhs=rwt[:, kc, :], start=(kc == 0), stop=(kc == KC - 1)),
                  r=[h1Tf_b, cst], w=[ps_b[4]])
        kb.op("dve", lambda e: e.tensor_tensor(out=lg[:], in0=ps[4][:, 0:32], in1=rbt[:], op=ALU.add), r=[ps_b[4], cst], w=[lg_b])
        kb.op("dve", lambda e: e.max(out=mx8[:], in_=lg[:]), r=[lg_b], w=[lg_b])
        kb.op("dve", lambda e: e.tensor_scalar(out=msk[:], in0=lg[:], scalar1=mx8[:, 3:4], scalar2=None, op0=ALU.is_ge), r=[lg_b], w=[lg_b])
        kb.op("dve", lambda e: e.tensor_scalar(out=nm0[:], in0=mx8[:, 0:1], scalar1=-1.0, scalar2=None, op0=ALU.mult), r=[lg_b], w=[lg_b])
        kb.op("act", lambda e: e.activation(out=ee[:], in_=lg[:], func=AF.Exp, bias=nm0[:], scale=1.0), r=[lg_b], w=[lg_b])
        kb.op("dve", lambda e: e.tensor_tensor(out=ee[:], in0=ee[:], in1=msk[:], op=ALU.mult), r=[lg_b], w=[lg_b])
        kb.op("dve", lambda e: e.reduce_sum(out=ssum[:], in_=ee[:], axis=AX.X), r=[lg_b], w=[lg_b])
        kb.op("dve", lambda e: e.reciprocal(out=ssum[:], in_=ssum[:]), r=[lg_b], w=[lg_b])
        kb.op("dve", lambda e, i=i: e.tensor_scalar(out=gates[:, i, :], in0=ee[:], scalar1=ssum[:, 0:1], scalar2=None, op0=ALU.mult), r=[lg_b], w=[gates_b])

    if "B" not in sections:
        return
    kb.end_phase(); kb.begin_phase()
    A = lambda n, s, d=F32: kb.sbuf("m4b_" + n, s, d)
    cst = kb.buf("cst4b")
    b1t = A("b1t", [128, 32, 8, 2])
    for ex in range(32):
        kb.op("sp", lambda e, ex=ex: e.dma_start(out=b1t[:, ex, :, :], in_=g.exp_b1[ex:ex + 1, :].rearrange("o (c p two) -> p (o c) two", p=128, two=2)),
              w=[cst], dma=True)
    ps = [kb.psum(f"m4bps{i}", [128, 512], F32) for i in range(8)]; ps_b = kb.bufs(8, "m4bps")
    W1 = [A(f"W1_{i}", [128, KC, 2048], BF16) for i in range(2)]; W2 = [A(f"W2_{i}", [128, KC, D], BF16) for i in range(2)]
    W_b = kb.bufs(2, "W")
    b2t = [A(f"b2t{i}", [128, D]) for i in range(2)]
    xT = [A(f"xT{i}", [128, KC, 512], BF16) for i in range(2)]; xT_b = kb.bufs(2, "xT")
    actT = [A(f"actT{i}", [128, 8, 512], BF16) for i in range(2)]; actT_b = kb.bufs(2, "actT")
    xg = [A(f"xg{i}", [128, 512]) for i in range(2)]; sg = [A(f"sgm{i}", [128, 512]) for i in range(2)]; xl = [A(f"xl{i}", [128, 512]) for i in range(2)]
    el_b = kb.bufs(2, "el")
    ot = [A(f"ot{i}", [128, 512]) for i in range(3)]; ot_b = kb.bufs(3, "ot")
    yacc_b = kb.bufs(NT, "yacc")

    def load_w(ex):
        z = ex % 2
        for kc in range(KC):
            kb.op("pool", lambda e, z=z, kc=kc, ex=ex: e.dma_start(out=W1[z][:, kc, :], in_=g.exp_w1[ex, kc * 128:(kc + 1) * 128, :]), w=[W_b[z]], dma=True)
        for kc in range(KC):
            kb.op("pool", lambda e, z=z, kc=kc, ex=ex: e.dma_start(out=W2[z][:, kc, :], in_=g.exp_w2[ex, kc * 128:(kc + 1) * 128, :]), w=[W_b[z]], dma=True)
        kb.op("sp", lambda e, z=z, ex=ex: e.dma_start(out=b2t[z][:], in_=g.exp_b2[ex:ex + 1, :].broadcast_to([128, D])), w=[W_b[z]], dma=True)

    load_w(0)
    n = 0
    on = 0
    for ex in range(n_exp):
        z = ex % 2
        if ex + 1 < n_exp:
            load_w(ex + 1)
        for tb in range(NB):
            xs = n % 2
            n += 1
            kb.op("sp", lambda e, xs=xs, tb=tb: e.dma_start(out=xT[xs][:], in_=g.h1T[:, tb * 512:(tb + 1) * 512].rearrange("(kc p) t -> p kc t", p=128)),
                  w=[xT_b[xs]], dma=True)
            for fc in range(8):
                q = fc % 2
                pg, pl = ps[2 * q], ps[2 * q + 1]
                for which, pp, ppb in ((0, pg, ps_b[2 * q]), (1, pl, ps_b[2 * q + 1])):
                    for kc in range(KC):
                        kb.op("pe", lambda e, z=z, kc=kc, fc=fc, which=which, pp=pp, xs=xs: e.matmul(
                            pp[:, :], lhsT=W1[z][:, kc, fc * 256 + which:fc * 256 + 256:2], rhs=xT[xs][:, kc, :], start=(kc == 0), stop=(kc == KC - 1)),
                            r=[W_b[z], xT_b[xs]], w=[ppb])
                kb.op("dve", lambda e, q=q, pg=pg, ex=ex, fc=fc: e.tensor_scalar(out=xg[q][:], in0=pg[:, :], scalar1=b1t[:, ex, fc, 0:1], scalar2=7.0, op0=ALU.add, op1=ALU.min),
                      r=[ps_b[2 * q], cst], w=[el_b[q]])
                kb.op("act", lambda e, q=q: e.activation(out=sg[q][:], in_=xg[q][:], func=AF.Sigmoid, scale=1.702), r=[el_b[q]], w=[el_b[q]])
                kb.op("dve", lambda e, q=q, pl=pl, ex=ex, fc=fc: e.tensor_scalar(out=xl[q][:], in0=pl[:, :], scalar1=b1t[:, ex, fc, 1:2], scalar2=7.0, op0=ALU.add, op1=ALU.min),
                      r=[ps_b[2 * q + 1], cst], w=[el_b[q]])
                kb.op("dve", lambda e, q=q: e.tensor_scalar(out=xl[q][:], in0=xl[q][:], scalar1=-7.0, scalar2=1.0, op0=ALU.max, op1=ALU.add), r=[el_b[q]], w=[el_b[q]])
                kb.op("dve", lambda e, q=q: e.tensor_tensor(out=xg[q][:], in0=xg[q][:], in1=sg[q][:], op=ALU.mult), r=[el_b[q]], w=[el_b[q]])
                kb.op("dve", lambda e, q=q, xs=xs, fc=fc: e.tensor_tensor(out=actT[xs][:, fc, :], in0=xg[q][:], in1=xl[q][:], op=ALU.mult),
                      r=[el_b[q]], w=[actT_b[xs]])
            for tt in range(4):
                ti = tb * 4 + tt
                for hf in range(2):
                    pp = 4 + (on % 4)
                    oi = on % 3
                    on += 1
                    for fc in range(8):
                        kb.op("pe", lambda e, pp=pp, xs=xs, fc=fc, tt=tt, z=z, hf=hf: e.matmul(
                            ps[pp][:, :], lhsT=actT[xs][:, fc, tt * 128:(tt + 1) * 128], rhs=W2[z][:, fc, hf * 512:(hf + 1) * 512], start=(fc == 0), stop=(fc == 7)),
                            r=[actT_b[xs], W_b[z]], w=[ps_b[pp]])
                    kb.op("dve", lambda e, pp=pp, oi=oi, z=z, hf=hf: e.tensor_tensor(out=ot[oi][:], in0=ps[pp][:, :], in1=b2t[z][:, hf * 512:(hf + 1) * 512], op=ALU.add),
                          r=[ps_b[pp], W_b[z]], w=[ot_b[oi]])
                    kb.op("dve", lambda e, oi=oi, ti=ti, ex=ex: e.tensor_scalar(out=ot[oi][:], in0=ot[oi][:], scalar1=gates[:, ti, ex:ex + 1], scalar2=None, op0=ALU.mult),
                          r=[ot_b[oi], gates_b], w=[ot_b[oi]])
                    if ex == 0:
                        kb.op("pool", lambda e, oi=oi, ti=ti, hf=hf: e.dma_start(out=g.yacc[ti * 128:(ti + 1) * 128, hf * 512:(hf + 1) * 512], in_=ot[oi][:]),
                              r=[ot_b[oi]], w=[yacc_b[ti]], dma=True)
                    else:
                        kb.op("pool", lambda e, oi=oi, ti=ti, hf=hf: e.dma_start(out=g.yacc[ti * 128:(ti + 1) * 128, hf * 512:(hf + 1) * 512], in_=ot[oi][:], accum_op=ALU.add),
                              r=[ot_b[oi]], w=[yacc_b[ti]], dma=True)

    if "C" not in sections:
        return
    kb.end_phase(); kb.begin_phase()
    A = lambda n, s, d=F32: kb.sbuf("m4c_" + n, s, d)
    cst = kb.buf("cst4c")
    gam2 = A("gam2", [128, D]); bet2 = A("bet2", [128, D]); epsc = A("epsc", [128, 1])
    kb.op("dve", lambda e: e.memset(epsc[:], LN_EPS), w=[cst])
    kb.op("sp", lambda e: e.dma_start(out=gam2[:], in_=g.ln2_w.broadcast_to([128, D])), w=[cst], dma=True)
    kb.op("sp", lambda e: e.dma_start(out=bet2[:], in_=g.ln2_b.broadcast_to([128, D])), w=[cst], dma=True)
    h0t = [A(f"h0t{i}", [128, D]) for i in range(2)]; h0t_b = kb.bufs(2, "h0tc")
    sres = [A(f"sres{i}", [128, D]) for i in range(2)]; sres_b = kb.bufs(2, "sresc")
    h1t = [A(f"h1t{i}", [128, D]) for i in range(2)]; h1t_b = kb.bufs(2, "h1tc")
    st = A("st", [128, 2, 6]); mv = A("mv", [128, 2]); rs = A("rs", [128, 1]); st_b = kb.buf("stc")
    for i in range(NT):
        s = i % 2
        tsl = slice(i * 128, (i + 1) * 128)
        kb.op("sp", lambda e, s=s, tsl=tsl: e.dma_start(out=h0t[s][:], in_=g.h1f[tsl, :]), w=[h0t_b[s]], dma=True)
        kb.op("sp", lambda e, s=s, tsl=tsl: e.dma_start(out=h1t[s][:], in_=g.yacc[tsl, :]), r=[yacc_b[i]], w=[h1t_b[s]], dma=True)
        kb.op("dve", lambda e, s=s: e.scalar_tensor_tensor(out=sres[s][:], in0=h0t[s][:], scalar=ALPHA, in1=h1t[s][:], op0=ALU.mult, op1=ALU.add),
              r=[h0t_b[s], h1t_b[s]], w=[sres_b[s]])
        _ln_tile(kb, sres[s], sres_b[s], h1t[s], h1t_b[s], st, mv, rs, st_b, epsc, cst, gam2, bet2)
        kb.op("sp", lambda e, s=s, tsl=tsl: e.dma_start(out=g.out[tsl, :], in_=h1t[s][:]), r=[h1t_b[s]], dma=True)


def make_inputs(d, b, T):
    im = {"x": d['x'][b, :T], "ln_in_w": d['ln_in_w'][None], "ln_in_b": d['ln_in_b'][None], "w_in": d['w_in'][0],
          "fx_b_f": d['fx_b_f'][0][:, None], "fx_q_norm": d['fx_q_norm'][0][:, None], "fx_k_norm": d['fx_k_norm'][0][:, None],
          "w_o": d['w_o'][0], "ln1_w": d['ln1_w'], "ln1_b": d['ln1_b'], "ln2_w": d['ln2_w'], "ln2_b": d['ln2_b'],
          "router_w": d['router_w'][0], "router_b": d['router_b'], "exp_w1": d['exp_w1'][0], "exp_b1": d['exp_b1'][0],
          "exp_w2": d['exp_w2'][0], "exp_b2": d['exp_b2'][0]}
    im.update(consts_np()); im.update(consts2_np()); im.update(consts3_np(d)); im.update(consts4g_np(T))
    return {k: np.ascontiguousarray(v) for k, v in im.items()}


def moe_cap(T):
    mean = T * 4 // 32
    return 128 * int(np.ceil(1.25 * mean / 128.0))


def setup4g(nc, g):
    ei = lambda n, s, d=F32: nc.dram_tensor(n, list(s), d, kind="ExternalInput").ap()
    T = g.T
    C = moe_cap(T)
    g.C = C
    g.NR = 32 * C + 128
    g.ec_iota = ei("ec_iota", [128, 32])
    g.tok16 = ei("tok16", [128, 16])
    g.table_init = ei("table_init", [g.NR, 16])
    g.su_b = ei("su_b", [128, 128], BF16)
    g.table = g.scratch("table", [g.NR, 16])
    g.h1b = g.scratch("h1b", [T + 128, D], BF16)
    g.out_all = g.scratch("out_all", [g.NR, D])


def consts4g_np(T):
    C = moe_cap(T)
    NR = 32 * C + 128
    c = {}
    c["ec_iota"] = np.tile((np.arange(32) * C).astype(np.float32)[None, :], (128, 1))
    c["tok16"] = np.tile(np.arange(128, dtype=np.float32)[:, None], (1, 16))
    ti = np.zeros((NR, 16), np.float32)
    ti[:, 0] = T
    c["table_init"] = ti
    a = np.arange(128)
    c["su_b"] = (a[:, None] < a[None, :]).astype(np.float32).astype(ml_dtypes.bfloat16)
    return c


def phase4g(kb, g):
    nc, T, NT, NB = g.nc, g.T, g.NT, g.NB
    C = g.C
    TRASH = 32 * C
    NS = C // 128
    kb.ps_stack.close(); kb.ps_stack = None
    rowi = kb.sbuf("m4_rowi", [128, NT, 4], I32); gsel = kb.sbuf("m4_gsel", [128, NT, 4], F32); sel_b = kb.buf("sel")
    kb.begin_phase()
    A = lambda n, s, d=F32: kb.sbuf("m4_" + n, s, d)
    cst = kb.buf("cst4")
    wo = A("wo", [128, KC, D], BF16)
    rwt = A("rwt", [128, KC, 32]); rbt = A("rbt", [128, 32])
    gam1 = A("gam1", [128, D]); bet1 = A("bet1", [128, D])
    identf = A("identf", [128, 128]); epsc = A("epsc", [128, 1])
    ecio = A("ecio", [128, 32]); tok0 = A("tok0", [128, 16]); sub = A("sub", [128, 128], BF16); onesb = A("onesb", [128, 128], BF16)
    tin = A("tin", [128, g.NR // 128, 16]); zrow = A("zrow", [128, D], BF16); zrowf = A("zrowf", [128, D])
    table_b = kb.buf("table"); h1b_b = kb.buf("h1b"); outall_b = kb.buf("outall")
    kb.op("dve", lambda e: e.memset(epsc[:], LN_EPS), w=[cst])
    kb.op("dve", lambda e: e.memset(onesb[:], 1.0), w=[cst])
    kb.op("dve", lambda e: e.memset(zrow[:], 0.0), w=[cst])
    kb.op("dve", lambda e: e.memset(zrowf[:], 0.0), w=[cst])
    kb.op("sp", lambda e: e.dma_start(out=identf[:], in_=g.ident_f), w=[cst], dma=True)
    kb.op("sp", lambda e: e.dma_start(out=ecio[:], in_=g.ec_iota), w=[cst], dma=True)
    kb.op("sp", lambda e: e.dma_start(out=tok0[:], in_=g.tok16), w=[cst], dma=True)
    kb.op("sp", lambda e: e.dma_start(out=sub[:], in_=g.su_b), w=[cst], dma=True)
    kb.op("sp", lambda e: e.dma_start(out=tin[:], in_=g.table_init.rearrange("(j p) w -> p j w", p=128)), w=[cst], dma=True)
    kb.op("sp", lambda e: e.dma_start(out=g.table.rearrange("(j p) w -> p j w", p=128), in_=tin[:]), r=[cst], w=[table_b], dma=True)
    kb.op("sp", lambda e: e.dma_start(out=g.h1b[T:T + 128, :], in_=zrow[:]), r=[cst], w=[h1b_b], dma=True)
    kb.op("sp", lambda e: e.dma_start(out=g.out_all[TRASH:TRASH + 128, :], in_=zrowf[:]), r=[cst], w=[outall_b], dma=True)
    for nm, t_, src in (("g1", gam1, g.ln1_w), ("b1", bet1, g.ln1_b)):
        kb.op("sp", lambda e, t_=t_, src=src: e.dma_start(out=t_[:], in_=src.broadcast_to([128, D])), w=[cst], dma=True)
    kb.op("sp", lambda e: e.dma_start(out=rbt[:], in_=g.router_b.broadcast_to([128, 32])), w=[cst], dma=True)
    kb.op("sp", lambda e: e.dma_start(out=rwt[:], in_=g.router_w.rearrange("(kc p) n -> p kc n", p=128)), w=[cst], dma=True)
    for kc in range(KC):
        kb.op("pool", lambda e, kc=kc: e.dma_start(out=wo[:, kc, :], in_=g.w_o[kc * 128:(kc + 1) * 128, :]), w=[cst], dma=True)

    ymt = [A(f"ymt{i}", [128, KC, 128], BF16) for i in range(2)]; ymt_b = kb.bufs(2, "ymt")
    h0t = [A(f"h0t{i}", [128, D]) for i in range(2)]; h0t_b = kb.bufs(2, "h0t")
    sres = [A(f"sres{i}", [128, D]) for i in range(2)]; sres_b = kb.bufs(2, "sres")
    h1t = [A(f"h1t{i}", [128, D]) for i in range(2)]; h1t_b = kb.bufs(2, "h1t")
    h1bt = [A(f"h1bt{i}", [128, D], BF16) for i in range(2)]; h1bt_b = kb.bufs(2, "h1bt")
    st = A("st", [128, 2, 6]); mv = A("mv", [128, 2]); rs = A("rs", [128, 1]); st_b = kb.buf("st")
    h1Tf = A("h1Tf", [128, KC, 128]); h1Tf_b = kb.buf("h1Tf")
    lg = A("lg", [128, 32]); mx8 = A("mx8", [128, 8]); msk = A("msk", [128, 32]); ee = A("ee", [128, 32]); ssum = A("ssum", [128, 1])
    gt = A("gt", [128, 32]); nm0 = A("nm0", [128, 1]); lg_b = kb.buf("lg")
    mskb = A("mskb", [128, NT, 32], BF16); mskb_b = kb.bufs(NT, "mskb")
    ridx = A("ridx", [128, 32]); okm = A("okm", [128, 32]); selk = A("selk", [128, 32]); tmpk = A("tmpk", [128, 32])
    rowf = A("rowf", [128, NT, 4]); rt_b = kb.buf("rt")
    tokt = [A(f"tokt{i}", [128, 16]) for i in range(2)]; tokt_b = kb.bufs(2, "tokt")
    ps = [kb.psum(f"m4ps{i}", [128, 512], F32) for i in range(8)]; ps_b = kb.bufs(8, "m4ps")

    for i in range(NT):
        s = i % 2
        tsl = slice(i * 128, (i + 1) * 128)
        kb.op("sp", lambda e, s=s, tsl=tsl: e.dma_start(out=ymt[s][:], in_=g.yT[:, tsl].rearrange("(kc p) t -> p kc t", p=128)), w=[ymt_b[s]], dma=True)
        kb.op("sp", lambda e, s=s, tsl=tsl: e.dma_start(out=h0t[s][:], in_=g.h0f[tsl, :]), w=[h0t_b[s]], dma=True)
        for hf in range(2):
            for kc in range(KC):
                kb.op("pe", lambda e, s=s, hf=hf, kc=kc: e.matmul(ps[hf][:, :], lhsT=ymt[s][:, kc, :], rhs=wo[:, kc, hf * 512:(hf + 1) * 512],
                                                                 start=(kc == 0), stop=(kc == KC - 1)), r=[ymt_b[s], cst], w=[ps_b[hf]])
            kb.op("dve", lambda e, s=s, hf=hf: e.scalar_tensor_tensor(out=sres[s][:, hf * 512:(hf + 1) * 512], in0=h0t[s][:, hf * 512:(hf + 1) * 512],
                                                                     scalar=ALPHA, in1=ps[hf][:, :], op0=ALU.mult, op1=ALU.add),
                  r=[h0t_b[s], ps_b[hf]], w=[sres_b[s]])
        _ln_tile(kb, sres[s], sres_b[s], h1t[s], h1t_b[s], st, mv, rs, st_b, epsc, cst, gam1, bet1)
        kb.op("sp", lambda e, s=s, tsl=tsl: e.dma_start(out=g.h1f[tsl, :], in_=h1t[s][:]), r=[h1t_b[s]], dma=True)
        kb.op("act", lambda e, s=s: e.copy(out=h1bt[s][:], in_=h1t[s][:]), r=[h1t_b[s]], w=[h1bt_b[s]])
        kb.op("sp", lambda e, s=s, tsl=tsl: e.dma_start(out=g.h1b[tsl, :], in_=h1bt[s][:]), r=[h1bt_b[s]], w=[h1b_b], dma=True)
        for hf in range(2):
            p = 2 + hf
            for c in range(4):
                kc = hf * 4 + c
                kb.op("pe", lambda e, s=s, p=p, c=c, kc=kc: e.transpose(out=ps[p][:, c * 128:(c + 1) * 128], in_=h1t[s][:, kc * 128:(kc + 1) * 128], identity=identf[:]),
                      r=[h1t_b[s], cst], w=[ps_b[p]])
            kb.op("act", lambda e, p=p, hf=hf: e.copy(out=h1Tf[:, hf * 4:(hf + 1) * 4, :], in_=ps[p][:].rearrange("p (c t) -> p c t", c=4)), r=[ps_b[p]], w=[h1Tf_b])
        for kc in range(KC):
            kb.op("pe", lambda e, kc=kc: e.matmul(ps[4][:, 0:32], lhsT=h1Tf[:, kc, :], rhs=rwt[:, kc, :], start=(kc == 0), stop=(kc == KC - 1)),
                  r=[h1Tf_b, cst], w=[ps_b[4]])
        kb.op("dve", lambda e: e.tensor_tensor(out=lg[:], in0=ps[4][:, 0:32], in1=rbt[:], op=ALU.add), r=[ps_b[4], cst], w=[lg_b])
        kb.op("dve", lambda e: e.max(out=mx8[:], in_=lg[:]), r=[lg_b], w=[lg_b])
        kb.op("dve", lambda e: e.tensor_scalar(out=msk[:], in0=lg[:], scalar1=mx8[:, 3:4], scalar2=None, op0=ALU.is_ge), r=[lg_b], w=[lg_b])
        kb.op("dve", lambda e: e.tensor_scalar(out=nm0[:], in0=mx8[:, 0:1], scalar1=-1.0, scalar2=None, op0=ALU.mult), r=[lg_b], w=[lg_b])
        kb.op("act", lambda e: e.activation(out=ee[:], in_=lg[:], func=AF.Exp, bias=nm0[:], scale=1.0), r=[lg_b], w=[lg_b])
        kb.op("dve", lambda e: e.tensor_tensor(out=ee[:], in0=ee[:], in1=msk[:], op=ALU.mult), r=[lg_b], w=[lg_b])
        kb.op("dve", lambda e: e.reduce_sum(out=ssum[:], in_=ee[:], axis=AX.X), r=[lg_b], w=[lg_b])
        kb.op("dve", lambda e: e.reciprocal(out=ssum[:], in_=ssum[:]), r=[lg_b], w=[lg_b])
        kb.op("dve", lambda e: e.tensor_scalar(out=gt[:], in0=ee[:], scalar1=ssum[:, 0:1], scalar2=None, op0=ALU.mult), r=[lg_b], w=[lg_b])
        kb.op("dve", lambda e, i=i: e.tensor_copy(out=mskb[:, i, :], in_=msk[:]), r=[lg_b], w=[mskb_b[i]])
        kb.op("pe", lambda e, i=i: e.matmul(ps[5][:, 0:32], lhsT=sub[:], rhs=mskb[:, i, :], start=True, stop=(i == 0)), r=[mskb_b[i], cst], w=[ps_b[5]])
        for j in range(i):
            kb.op("pe", lambda e, j=j, i=i: e.matmul(ps[5][:, 0:32], lhsT=onesb[:], rhs=mskb[:, j, :], start=False, stop=(j == i - 1)),
                  r=[mskb_b[j], cst], w=[ps_b[5]])
        kb.op("dve", lambda e: e.tensor_tensor(out=ridx[:], in0=ps[5][:, 0:32], in1=ecio[:], op=ALU.add), r=[ps_b[5], cst], w=[rt_b])
        kb.op("dve", lambda e: e.tensor_scalar(out=okm[:], in0=ps[5][:, 0:32], scalar1=float(C), scalar2=None, op0=ALU.is_lt), r=[ps_b[5]], w=[rt_b])
        kb.op("dve", lambda e: e.tensor_tensor(out=okm[:], in0=okm[:], in1=msk[:], op=ALU.mult), r=[rt_b, lg_b], w=[rt_b])
        kb.op("dve", lambda e: e.tensor_scalar(out=ridx[:], in0=ridx[:], scalar1=-float(TRASH), scalar2=None, op0=ALU.add), r=[rt_b], w=[rt_b])
        kb.op("dve", lambda e: e.tensor_tensor(out=ridx[:], in0=ridx[:], in1=okm[:], op=ALU.mult), r=[rt_b], w=[rt_b])
        kb.op("dve", lambda e: e.tensor_scalar(out=ridx[:], in0=ridx[:], scalar1=float(TRASH), scalar2=None, op0=ALU.add), r=[rt_b], w=[rt_b])
        for k in range(4):
            kb.op("dve", lambda e, k=k: e.tensor_scalar(out=selk[:], in0=lg[:], scalar1=mx8[:, k:k + 1], scalar2=None, op0=ALU.is_equal), r=[lg_b], w=[rt_b])
            kb.op("dve", lambda e: e.tensor_tensor(out=tmpk[:], in0=selk[:], in1=ridx[:], op=ALU.mult), r=[rt_b], w=[rt_b])
            kb.op("dve", lambda e, i=i, k=k: e.reduce_sum(out=rowf[:, i, k:k + 1], in_=tmpk[:], axis=AX.X), r=[rt_b], w=[rt_b])
            kb.op("dve", lambda e: e.tensor_tensor(out=tmpk[:], in0=selk[:], in1=gt[:], op=ALU.mult), r=[rt_b, lg_b], w=[rt_b])
            kb.op("dve", lambda e, i=i, k=k: e.reduce_sum(out=gsel[:, i, k:k + 1], in_=tmpk[:], axis=AX.X), r=[rt_b], w=[sel_b])
        kb.op("dve", lambda e, i=i: e.tensor_copy(out=rowi[:, i, :], in_=rowf[:, i, :]), r=[rt_b], w=[sel_b])
        kb.op("dve", lambda e, i=i, s=s: e.tensor_scalar(out=tokt[s][:], in0=tok0[:], scalar1=float(i * 128), scalar2=None, op0=ALU.add), r=[cst], w=[tokt_b[s]])
        for k in range(4):
            kb.op("pool", lambda e, i=i, k=k, s=s: e.indirect_dma_start(
                out=g.table[:, :], out_offset=bass.IndirectOffsetOnAxis(ap=rowi[:, i, k:k + 1], axis=0), in_=tokt[s][:, :], in_offset=None),
                r=[sel_b, tokt_b[s]], w=[table_b], dma=True)

    kb.end_phase(); kb.begin_phase()
    A = lambda n, s, d=F32: kb.sbuf("m4b_" + n, s, d)
    cst = kb.buf("cst4b")
    b1t = A("b1t", [128, 32, 8, 2]); identb = A("identb", [128, 128], BF16)
    kb.op("sp", lambda e: e.dma_start(out=identb[:], in_=g.ident_b), w=[cst], dma=True)
    for ex in range(32):
        kb.op("sp", lambda e, ex=ex: e.dma_start(out=b1t[:, ex, :, :], in_=g.exp_b1[ex:ex + 1, :].rearrange("o (c p two) -> p (o c) two", p=128, two=2)),
              w=[cst], dma=True)
    kb.op("dve", lambda e: e.tensor_scalar(out=b1t[:, :, :, 1], in0=b1t[:, :, :, 1], scalar1=1.0, scalar2=None, op0=ALU.add), r=[cst], w=[cst])
    ps = [kb.psum(f"m4bps{i}", [128, 512], F32) for i in range(7)]; ps_b = kb.bufs(7, "m4bps")
    psTb = kb.psum("m4bpsT", [128, 1024], BF16); psTb_b = kb.buf("psTb")
    W1 = [A(f"W1_{i}", [128, KC, 2048], BF16) for i in range(2)]; W2 = [A(f"W2_{i}", [128, KC, D], BF16) for i in range(2)]
    W_b = kb.bufs(2, "W")
    b2t = [A(f"b2t{i}", [128, D]) for i in range(2)]
    idxf = [A(f"idxf{i}", [128, 16]) for i in range(2)]; idxi = [A(f"idxi{i}", [128, 1], I32) for i in range(2)]; idx_b = kb.bufs(2, "idx")
    Xg = [A(f"Xg{i}", [128, D], BF16) for i in range(2)]; Xg_b = kb.bufs(2, "Xg")
    xT = [A(f"xT{i}", [128, KC, C], BF16) for i in range(2)]; xT_b = kb.bufs(2, "xT")
    actT = [A(f"actT{i}", [128, 8, 512], BF16) for i in range(2)]; actT_b = kb.bufs(2, "actT")
    xg = [A(f"xg{i}", [128, 512]) for i in range(2)]; sg = [A(f"sgm{i}", [128, 512]) for i in range(2)]; xl = [A(f"xl{i}", [128, 512]) for i in range(2)]
    xg_b = kb.bufs(2, "xg"); sg_b = kb.bufs(2, "sgm"); xl_b = kb.bufs(2, "xl")
    ot = [A(f"ot{i}", [128, 512]) for i in range(3)]; ot_b = kb.bufs(3, "ot")
    blocks = []
    c0 = 0
    while c0 < C:
        w = min(512, C - c0)
        blocks.append((c0, w))
        c0 += w

    def load_w(ex):
        z = ex % 2
        for kc in range(KC):
            kb.op("pool", lambda e, z=z, kc=kc, ex=ex: e.dma_start(out=W1[z][:, kc, :], in_=g.exp_w1[ex, kc * 128:(kc + 1) * 128, :]), w=[W_b[z]], dma=True)
        for kc in range(KC):
            kb.op("pool", lambda e, z=z, kc=kc, ex=ex: e.dma_start(out=W2[z][:, kc, :], in_=g.exp_w2[ex, kc * 128:(kc + 1) * 128, :]), w=[W_b[z]], dma=True)
        kb.op("sp", lambda e, z=z, ex=ex: e.dma_start(out=b2t[z][:], in_=g.exp_b2[ex:ex + 1, :].broadcast_to([128, D])), w=[W_b[z]], dma=True)

    def gather_x(ex):
        xz = ex % 2
        for s_ in range(NS):
            q = (ex * NS + s_) % 2
            r0 = ex * C + s_ * 128
            kb.op("sp", lambda e, q=q, r0=r0: e.dma_start(out=idxf[q][:], in_=g.table[r0:r0 + 128, :]), r=[table_b], w=[idx_b[q]], dma=True)
            kb.op("dve", lambda e, q=q: e.tensor_copy(out=idxi[q][:], in_=idxf[q][:, 0:1]), r=[idx_b[q]], w=[idx_b[q]])
            kb.op("pool", lambda e, q=q: e.indirect_dma_start(
                out=Xg[q][:, :], out_offset=None, in_=g.h1b[:, :], in_offset=bass.IndirectOffsetOnAxis(ap=idxi[q][:, 0:1], axis=0)),
                r=[idx_b[q], h1b_b], w=[Xg_b[q]], dma=True)
            for kc in range(KC):
                kb.op("pe", lambda e, q=q, kc=kc: e.transpose(out=psTb[:, kc * 128:(kc + 1) * 128], in_=Xg[q][:, kc * 128:(kc + 1) * 128], identity=identb[:]),
                      r=[Xg_b[q], cst], w=[psTb_b])
            kb.op("act", lambda e, xz=xz, s_=s_: e.copy(out=xT[xz][:, :, s_ * 128:(s_ + 1) * 128], in_=psTb[:].rearrange("p (c t) -> p c t", c=8)),
                  r=[psTb_b], w=[xT_b[xz]])

    load_w(0)
    gather_x(0)
    on = 0
    an = 0
    for ex in range(32):
        z = ex % 2
        xz = ex % 2
        if ex + 1 < 32:
            load_w(ex + 1)
            gather_x(ex + 1)
        for (c0, w) in blocks:
            xs = an % 2
            an += 1
            pend = None
            for fc in range(8):
                q = fc % 2
                pg, pl = ps[2 * q], ps[2 * q + 1]
                for which, pp, ppb in ((0, pg, ps_b[2 * q]), (1, pl, ps_b[2 * q + 1])):
                    for kc in range(KC):
                        kb.op("pe", lambda e, z=z, kc=kc, fc=fc, which=which, pp=pp, xz=xz, c0=c0, w=w: e.matmul(
                            pp[:, 0:w], lhsT=W1[z][:, kc, fc * 256 + which:fc * 256 + 256:2], rhs=xT[xz][:, kc, c0:c0 + w], start=(kc == 0), stop=(kc == KC - 1)),
                            r=[W_b[z], xT_b[xz]], w=[ppb])
                kb.op("dve", lambda e, q=q, pg=pg, ex=ex, fc=fc, w=w: e.tensor_scalar(out=xg[q][:, 0:w], in0=pg[:, 0:w], scalar1=b1t[:, ex, fc, 0:1], scalar2=7.0, op0=ALU.add, op1=ALU.min),
                      r=[ps_b[2 * q], cst], w=[xg_b[q]])
                kb.op("act", lambda e, q=q, w=w: e.activation(out=sg[q][:, 0:w], in_=xg[q][:, 0:w], func=AF.Gelu_apprx_sigmoid), r=[xg_b[q]], w=[sg_b[q]])
                kb.op("dve", lambda e, q=q, pl=pl, ex=ex, fc=fc, w=w: e.tensor_scalar(out=xl[q][:, 0:w], in0=pl[:, 0:w], scalar1=b1t[:, ex, fc, 1:2], scalar2=8.0, op0=ALU.add, op1=ALU.min),
                      r=[ps_b[2 * q + 1], cst], w=[xl_b[q]])

                def fin(q=q, xs=xs, fc=fc, w=w):
                    kb.op("dve", lambda e: e.scalar_tensor_tensor(out=actT[xs][:, fc, 0:w], in0=xl[q][:, 0:w], scalar=-6.0, in1=sg[q][:, 0:w], op0=ALU.max, op1=ALU.mult),
                          r=[xl_b[q], sg_b[q]], w=[actT_b[xs]])
                if pend is not None:
                    pend()
                pend = fin
            pend()
            pend = None
            for tt in range(w // 128):
                r0 = ex * C + c0 + tt * 128
                for hf in range(2):
                    pp = 4 + (on % 3)
                    oi = on % 3
                    on += 1
                    for fc in range(8):
                        kb.op("pe", lambda e, pp=pp, xs=xs, fc=fc, tt=tt, z=z, hf=hf: e.matmul(
                            ps[pp][:, :], lhsT=actT[xs][:, fc, tt * 128:(tt + 1) * 128], rhs=W2[z][:, fc, hf * 512:(hf + 1) * 512], start=(fc == 0), stop=(fc == 7)),
                            r=[actT_b[xs], W_b[z]], w=[ps_b[pp]])
                    kb.op("dve", lambda e, pp=pp, oi=oi, z=z, hf=hf: e.tensor_tensor(out=ot[oi][:], in0=ps[pp][:, :], in1=b2t[z][:, hf * 512:(hf + 1) * 512], op=ALU.add),
                          r=[ps_b[pp], W_b[z]], w=[ot_b[oi]])
                    kb.op("sp", lambda e, oi=oi, r0=r0, hf=hf: e.dma_start(out=g.out_all[r0:r0 + 128, hf * 512:(hf + 1) * 512], in_=ot[oi][:]),
                          r=[ot_b[oi]], w=[outall_b], dma=True)

    kb.end_phase(); kb.begin_phase()
    A = lambda n, s, d=F32: kb.sbuf("m4c_" + n, s, d)
    cst = kb.buf("cst4c")
    gam2 = A("gam2", [128, D]); bet2 = A("bet2", [128, D]); epsc = A("epsc", [128, 1])
    kb.op("dve", lambda e: e.memset(epsc[:], LN_EPS), w=[cst])
    kb.op("sp", lambda e: e.dma_start(out=gam2[:], in_=g.ln2_w.broadcast_to([128, D])), w=[cst], dma=True)
    kb.op("sp", lambda e: e.dma_start(out=bet2[:], in_=g.ln2_b.broadcast_to([128, D])), w=[cst], dma=True)
    h0t = [A(f"h0t{i}", [128, D]) for i in range(2)]; h0t_b = kb.bufs(2, "h0tc")
    sres = [A(f"sres{i}", [128, D]) for i in range(2)]; sres_b = kb.bufs(2, "sresc")
    h1t = [A(f"h1t{i}", [128, D]) for i in range(2)]; h1t_b = kb.bufs(2, "h1tc")
    Rk = [A(f"Rk{i}", [128, D]) for i in range(4)]; Rk_b = kb.bufs(4, "Rk")
    st = A("st", [128, 2, 6]); mv = A("mv", [128, 2]); rs = A("rs", [128, 1]); st_b = kb.buf("stc")
    for i in range(NT):
        s = i % 2
        tsl = slice(i * 128, (i + 1) * 128)
        kb.op("sp", lambda e, s=s, tsl=tsl: e.dma_start(out=h0t[s][:], in_=g.h1f[tsl, :]), w=[h0t_b[s]], dma=True)
        for k in range(4):
            kb.op("pool", lambda e, i=i, k=k: e.indirect_dma_start(
                out=Rk[k][:, :], out_offset=None, in_=g.out_all[:, :], in_offset=bass.IndirectOffsetOnAxis(ap=rowi[:, i, k:k + 1], axis=0)),
                r=[sel_b, outall_b], w=[Rk_b[k]], dma=True)
        kb.op("dve", lambda e, s=s: e.tensor_scalar(out=sres[s][:], in0=h0t[s][:], scalar1=ALPHA, scalar2=None, op0=ALU.mult), r=[h0t_b[s]], w=[sres_b[s]])
        for k in range(4):
            kb.op("dve", lambda e, s=s, i=i, k=k: e.scalar_tensor_tensor(out=sres[s][:], in0=Rk[k][:], scalar=gsel[:, i, k:k + 1], in1=sres[s][:], op0=ALU.mult, op1=ALU.add),
                  r=[Rk_b[k], sel_b, sres_b[s]], w=[sres_b[s]])
        _ln_tile(kb, sres[s], sres_b[s], h1t[s], h1t_b[s], st, mv, rs, st_b, epsc, cst, gam2, bet2)
        kb.op("sp", lambda e, s=s, tsl=tsl: e.dma_start(out=g.out[tsl, :], in_=h1t[s][:]), r=[h1t_b[s]], dma=True)


_T = 4096


def _build(T):
    nc = bass.Bass("TRN2", target_bir_lowering=False)
    g = setup(nc, T)
    setup2(nc, g); setup3(nc, g); setup4(nc, g); setup4g(nc, g)
    kb = KB(nc)
    kb.begin_phase(); phase01(kb, g); kb.end_phase()
    kb.begin_phase(); phase2(kb, g); kb.end_phase()
    kb.begin_phase(); phase3(kb, g); kb.end_phase()
    kb.begin_phase(); phase4g(kb, g); kb.end_phase()
    kb.finish(); kb.close()
    return nc


def kernel(**inputs):
    d = {k: np.asarray(v) for k, v in inputs.items()}
    B = d["x"].shape[0]
    T = d["x"].shape[1]
    nc = _build(T)
    maps = [make_inputs(d, b % B, T) for b in range(8)]
    res = run_bass_kernel_spmd(nc, maps, core_ids=list(range(8)))
    out = np.stack([np.asarray(res.results[b]["out"]) for b in range(B)], axis=0)
    return out.astype(np.float32)
```
